# Optimizing a Trainium2 kernel written in Bass

```python
import jax, jax.numpy as jnp
from jax import lax
import numpy as np

D_MODEL = 1024
BATCH = 32
SEQ = 2048
DEPTH = 4

GRID_W = 64
D_ATTN = D_MODEL // 2
HEAD_DIM = 64
N_HEADS = D_ATTN // HEAD_DIM
WIN_H_MAX = 8
WIN_W = 16
Q_COLS = 16
K_COLS = Q_COLS + WIN_W
NEG_INF = -1e30
D_POOL = D_MODEL - D_ATTN
POOL_WINDOWS = (2, 4, 8, 16)
N_POOL_GROUPS = len(POOL_WINDOWS)
POOL_GROUP_DIM = D_POOL // N_POOL_GROUPS
D_MIX = D_ATTN + D_POOL
D_IN = 3 * D_ATTN + D_POOL
N_GROUPS = 4
EXPERTS_PER_GROUP = 4
N_EXPERTS = N_GROUPS * EXPERTS_PER_GROUP
TOP_K_IN_GROUP = 2
D_EXPERT = 512
EPS = 1e-6

kernel_name = "hymba_natten_poolformer_hiermoe_encoder"


def rmsnorm(x, g):
    xf = x.astype(jnp.float32)
    y = xf * lax.rsqrt(jnp.mean(xf * xf, axis=-1, keepdims=True) + EPS)
    return (y * g.astype(jnp.float32)).astype(x.dtype)


def neighbourhood_attention(q, k, v, rpb):
    B, S, H, hd = q.shape
    rows = S // GRID_W
    kh = min(WIN_H_MAX, rows)
    n_cb = GRID_W // Q_COLS
    qc = np.arange(GRID_W).reshape(n_cb, Q_COLS)
    kc_start = np.clip(np.arange(n_cb) * Q_COLS - WIN_W // 2, 0, GRID_W - K_COLS)
    kc = kc_start[:, None] + np.arange(K_COLS)[None, :]
    qc_start = np.clip(qc - WIN_W // 2, 0, GRID_W - WIN_W)
    col_valid = (kc[:, None, :] >= qc_start[:, :, None]) & (kc[:, None, :] < qc_start[:, :, None] + WIN_W)
    dc = np.clip(kc[:, None, :] - qc[:, :, None], -(WIN_W - 1), WIN_W - 1) + WIN_W - 1
    r = np.arange(rows)
    r_start = np.clip(r - kh // 2, 0, rows - kh)
    kr = r_start[:, None] + np.arange(kh)[None, :]
    dr = kr - r[:, None] + WIN_H_MAX - 1
    bias = jnp.take(rpb[:, dr, :], dc, axis=-1)
    bias = bias.transpose(1, 0, 3, 4, 2, 5).astype(jnp.float32)
    valid = col_valid[:, :, None, :]

    scale = hd ** -0.5
    q_rows = (q * scale).reshape(B, rows, n_cb, Q_COLS, H, hd).transpose(1, 0, 4, 2, 3, 5)
    k_grid = k.reshape(B, rows, GRID_W, H, hd)
    v_grid = v.reshape(B, rows, GRID_W, H, hd)

    def row_block(args):
        rs, q_r, b_r = args
        k_r = jnp.take(lax.dynamic_slice_in_dim(k_grid, rs, kh, axis=1), kc, axis=2)
        v_r = jnp.take(lax.dynamic_slice_in_dim(v_grid, rs, kh, axis=1), kc, axis=2)
        s = jnp.einsum('bhnqd,binkhd->bhnqik', q_r, k_r).astype(jnp.float32) + b_r
        s = jnp.where(valid, s, NEG_INF)
        p = jax.nn.softmax(s.reshape(s.shape[:4] + (kh * K_COLS,)), axis=-1)
        p = p.reshape(s.shape).astype(v.dtype)
        return jnp.einsum('bhnqik,binkhd->bhnqd', p, v_r)

    out = lax.map(row_block, (jnp.asarray(r_start, jnp.int32), q_rows, bias))
    return out.transpose(1, 0, 3, 4, 2, 5).reshape(B, S, H * hd)


def pooling_mixer(u, pool_w, pool_scale):
    B, S, _ = u.shape
    uf = u.astype(jnp.float32)
    cs = jnp.concatenate([jnp.zeros((B, 1, D_POOL), jnp.float32), jnp.cumsum(uf, axis=1)], axis=1)
    t = np.arange(S)
    outs = []
    for g, w in enumerate(POOL_WINDOWS):
        lo = np.clip(t - w // 2, 0, S)
        hi = np.clip(t - w // 2 + w, 0, S)
        cnt = (hi - lo).astype(np.float32)[None, :, None]
        sl = slice(g * POOL_GROUP_DIM, (g + 1) * POOL_GROUP_DIM)
        outs.append((cs[:, hi, sl] - cs[:, lo, sl]) / cnt - uf[:, :, sl])
    pooled = jnp.stack(outs, axis=2).astype(u.dtype)
    mixed = jnp.einsum('bsgc,gcd->bsgd', pooled, pool_w).reshape(B, S, D_POOL)
    return mixed * pool_scale


def hierarchical_moe(h, w_rg, w_re, w_gate, w_up, w_down):
    B, S, D = h.shape
    T = B * S
    tok = h.reshape(T, D)
    g_logits = (tok @ w_rg).astype(jnp.float32)
    g_prob = jax.nn.softmax(g_logits, axis=-1)
    g_idx = jnp.argmax(g_logits, axis=-1)
    g_w = jnp.take_along_axis(g_prob, g_idx[:, None], axis=-1)
    e_logits = (tok @ w_re).astype(jnp.float32).reshape(T, N_GROUPS, EXPERTS_PER_GROUP)
    e_logits = jnp.take_along_axis(e_logits, g_idx[:, None, None], axis=1)[:, 0]
    top_v, top_i = lax.top_k(e_logits, TOP_K_IN_GROUP)
    top_w = jax.nn.softmax(top_v, axis=-1) * g_w
    expert_id = g_idx[:, None] * EXPERTS_PER_GROUP + top_i
    combine = jnp.sum(jax.nn.one_hot(expert_id, N_EXPERTS, dtype=jnp.float32) * top_w[..., None], axis=1)
    y = jnp.zeros((T, D), jnp.float32)
    for e in range(N_EXPERTS):
        a = jax.nn.silu(tok @ w_gate[e]) * (tok @ w_up[e])
        y = y + combine[:, e:e + 1] * (a @ w_down[e]).astype(jnp.float32)
    return y.astype(h.dtype).reshape(B, S, D)


def setup_inputs(seed: int = 0) -> dict:
    key = jax.random.key(seed)
    ks = jax.random.split(key, 16)
    f32 = jnp.float32
    res_scale = (2 * DEPTH) ** -0.5
    x = jax.random.normal(ks[0], (BATCH, SEQ, D_MODEL), f32)
    norm_mix_g = 1.0 + 0.05 * jax.random.normal(ks[1], (DEPTH, D_MODEL), f32)
    w_in = jax.random.normal(ks[2], (DEPTH, D_MODEL, D_IN), f32) * D_MODEL ** -0.5
    rpb = 0.2 * jax.random.normal(ks[3], (DEPTH, N_HEADS, 2 * WIN_H_MAX - 1, 2 * WIN_W - 1), f32)
    pool_w = jax.random.normal(ks[4], (DEPTH, N_POOL_GROUPS, POOL_GROUP_DIM, POOL_GROUP_DIM), f32) * POOL_GROUP_DIM ** -0.5
    pool_scale = 1.0 + 0.1 * jax.random.normal(ks[5], (DEPTH, D_POOL), f32)
    w_out = jax.random.normal(ks[6], (DEPTH, D_MIX, D_MODEL), f32) * (D_MIX ** -0.5) * res_scale
    norm_ffn_g = 1.0 + 0.05 * jax.random.normal(ks[7], (DEPTH, D_MODEL), f32)
    w_router_group = jax.random.normal(ks[8], (DEPTH, D_MODEL, N_GROUPS), f32) * D_MODEL ** -0.5
    w_router_expert = jax.random.normal(ks[9], (DEPTH, D_MODEL, N_EXPERTS), f32) * D_MODEL ** -0.5
    w_gate = jax.random.normal(ks[10], (DEPTH, N_EXPERTS, D_MODEL, D_EXPERT), f32) * D_MODEL ** -0.5
    w_up = jax.random.normal(ks[11], (DEPTH, N_EXPERTS, D_MODEL, D_EXPERT), f32) * D_MODEL ** -0.5
    w_down = jax.random.normal(ks[12], (DEPTH, N_EXPERTS, D_EXPERT, D_MODEL), f32) * (D_EXPERT ** -0.5) * res_scale
    final_g = 1.0 + 0.05 * jax.random.normal(ks[13], (D_MODEL,), f32)
    return {"x": x, "norm_mix_g": norm_mix_g, "w_in": w_in, "rpb": rpb, "pool_w": pool_w,
            "pool_scale": pool_scale, "w_out": w_out, "norm_ffn_g": norm_ffn_g,
            "w_router_group": w_router_group, "w_router_expert": w_router_expert,
            "w_gate": w_gate, "w_up": w_up, "w_down": w_down, "final_g": final_g}


def reference(x, norm_mix_g, w_in, rpb, pool_w, pool_scale, w_out, norm_ffn_g,
              w_router_group, w_router_expert, w_gate, w_up, w_down, final_g):
    B, S, _ = x.shape
    for l in range(DEPTH):
        h = rmsnorm(x, norm_mix_g[l])
        proj = h @ w_in[l]
        q = proj[..., :D_ATTN].reshape(B, S, N_HEADS, HEAD_DIM)
        k = proj[..., D_ATTN:2 * D_ATTN].reshape(B, S, N_HEADS, HEAD_DIM)
        v = proj[..., 2 * D_ATTN:3 * D_ATTN].reshape(B, S, N_HEADS, HEAD_DIM)
        u = proj[..., 3 * D_ATTN:]
        a = neighbourhood_attention(q, k, v, rpb[l])
        p = pooling_mixer(u, pool_w[l], pool_scale[l])
        x = x + jnp.concatenate([a, p], axis=-1) @ w_out[l]
        x = x + hierarchical_moe(rmsnorm(x, norm_ffn_g[l]), w_router_group[l], w_router_expert[l],
                                 w_gate[l], w_up[l], w_down[l])
    return rmsnorm(x, final_g)
```

```python
import numpy as np
from contextlib import ExitStack
import concourse.bass as bass
import concourse.mybir as mybir
from concourse.bass_utils import run_bass_kernel_spmd

F32 = mybir.dt.float32
BF16 = mybir.dt.bfloat16
AF = mybir.ActivationFunctionType
ALU = mybir.AluOpType
AX = mybir.AxisListType

D = 1024
S = 2048
L_FULL = 4
NSEQ_FULL = 4
NCORES = 8
NE = 16
DE = 512
EPS = 1e-6
NEG = -30000.0
POOL_W = (2, 4, 8, 16)


class Buf:
    __slots__ = ("name", "w", "r")

    def __init__(self, name):
        self.name = name
        self.w = None
        self.r = {}


class _Eng:
    def __init__(self, name):
        self.name = name
        self.q = []
        self.known = {}
        self.count = 0
        self.psem = None


class Tracker:
    ENGS = ("pe", "act", "dve", "pool", "sp")

    def __init__(self, nc, stack, n_dma_sems=24):
        self.nc = nc
        self.sems = []
        self.eng = {n: _Eng(n) for n in self.ENGS}
        for n in ("pe", "act", "dve", "pool"):
            self.eng[n].psem = self._new_sem(stack, "p_" + n)
        self.dpool = [self._new_sem(stack, f"dq{i}") for i in range(n_dma_sems)]
        self.dcnt = [0] * n_dma_sems
        self.dnext = 0
        self.stack = stack
        self.extra = []

    def _new_sem(self, stack, name):
        h = stack.enter_context(self.nc.semaphore(name))
        self.sems.append(h)
        return len(self.sems) - 1

    def _deps(self, E, reads, writes, extra=()):
        need = {}

        def req(ev):
            if ev is None:
                return
            s, v = ev
            if s == E.psem and E.name == "pe":
                return
            if E.known.get(s, 0) >= v:
                return
            if need.get(s, 0) < v:
                need[s] = v

        for ev in extra:
            req(ev)
        for b in reads:
            req(b.w)
        for b in writes:
            req(b.w)
            for s, v in b.r.items():
                req((s, v))
        for s, v in need.items():
            E.q.append(("wait", s, v))
            E.known[s] = v

    def _mark(self, ev, reads, writes):
        for b in reads:
            if b.r.get(ev[0], 0) < ev[1]:
                b.r[ev[0]] = ev[1]
        for b in writes:
            b.w = ev
            b.r = {}

    def emit(self, eng, fn, reads=(), writes=(), sig=True):
        E = self.eng[eng]
        self._deps(E, reads, writes)
        if sig:
            E.count += 1
            ev = (E.psem, E.count)
        else:
            ev = (E.psem, E.count + 1)
        E.q.append(("op", fn, sig))
        self._mark(ev, reads, writes)
        return ev

    def dma(self, q, out, in_, reads=(), writes=(), own_sem=False):
        E = self.eng[q]
        if own_sem:
            s = self._new_sem(self.stack, f"ds{len(self.sems)}")
            self.extra.append(s)
            prev = 0
            self._deps(E, reads, writes)
            ev = (s, 16)
        else:
            i = self.dnext
            self.dnext = (i + 1) % len(self.dpool)
            s = self.dpool[i]
            prev = self.dcnt[i]
            self._deps(E, reads, writes, extra=[(s, prev)] if prev else ())
            self.dcnt[i] += 16
            ev = (s, self.dcnt[i])
        E.q.append(("dma", out, in_, s))
        self._mark(ev, reads, writes)
        return ev

    def idma(self, out, out_idx, in_, in_idx, reads=(), writes=()):
        E = self.eng["pool"]
        i = self.dnext
        self.dnext = (i + 1) % len(self.dpool)
        s = self.dpool[i]
        prev = self.dcnt[i]
        self._deps(E, reads, writes, extra=[(s, prev)] if prev else ())
        self.dcnt[i] += 16
        ev = (s, self.dcnt[i])
        E.q.append(("idma", out, out_idx, in_, in_idx, s))
        self._mark(ev, reads, writes)
        return ev

    def barrier(self):
        evs = []
        for n in ("pe", "act", "dve", "pool"):
            e = self.eng[n]
            if e.count:
                evs.append((e.psem, e.count))
        for i, s in enumerate(self.dpool):
            if self.dcnt[i]:
                evs.append((s, self.dcnt[i]))
        for s in self.extra:
            evs.append((s, 16))
        for fn in getattr(self, "extra_ev_fns", []):
            evs.extend(fn())
        for n in self.ENGS:
            E = self.eng[n]
            for s, v in evs:
                if s == E.psem:
                    continue
                if E.known.get(s, 0) < v:
                    E.q.append(("wait", s, v))
                    E.known[s] = v

    def flush(self):
        nc = self.nc
        sems = self.sems

        def run(E, h):
            psem = sems[E.psem] if E.psem is not None else None
            for it in E.q:
                if it[0] == "wait":
                    h.wait_ge(sems[it[1]], it[2])
                elif it[0] == "op":
                    ins = it[1](h)
                    if it[2]:
                        ins.then_inc(psem, 1)
                elif it[0] == "idma":
                    oo = bass.IndirectOffsetOnAxis(ap=it[2], axis=0) if it[2] is not None else None
                    io = bass.IndirectOffsetOnAxis(ap=it[4], axis=0) if it[4] is not None else None
                    h.indirect_dma_start(out=it[1], out_offset=oo, in_=it[3], in_offset=io).then_inc(sems[it[5]], 16)
                else:
                    h.dma_start(out=it[1], in_=it[2]).then_inc(sems[it[3]], 16)

        with nc.Block() as block:
            @block.tensor
            def _(h):
                run(self.eng["pe"], h)

            @block.scalar
            def _(h):
                run(self.eng["act"], h)

            @block.vector
            def _(h):
                run(self.eng["dve"], h)

            @block.gpsimd
            def _(h):
                run(self.eng["pool"], h)

            @block.sync
            def _(h):
                run(self.eng["sp"], h)


SPARSE = True
TS = 256
NTILE = 32


def build_program(nseq=NSEQ_FULL, depth=L_FULL, stop_after=None, debug_out=False):
    nc = bass.Bass("TRN2", target_bir_lowering=False)
    NT = nseq * S
    Lw = L_FULL

    def din(name, shape, dt=F32):
        return nc.dram_tensor(name, list(shape), dt, kind="ExternalInput").ap()

    x_c = din("x", [NT, D])
    w_in = din("w_in", [Lw, D, 2048])
    w_out = din("w_out", [Lw, D, D])
    pool_w = din("pool_w", [Lw, 4, 128, 128])
    w_gate = din("w_gate", [Lw, NE, D, DE])
    w_up = din("w_up", [Lw, NE, D, DE])
    w_down = din("w_down", [Lw, NE, DE, D])
    gm_d = din("gm", [128, Lw * 8])
    gf_d = din("gf", [128, Lw * 8])
    ps_d = din("ps", [128, Lw * 4])
    gF_d = din("gF", [128, D])
    wr_d = din("wr", [128, Lw, 8, 20])
    bias_d = din("biasT", [Lw, 4, 128, 2 * 14 * 64])
    poolA_d = din("poolA", [128, 28, 128])
    ident_d = din("ident", [128, 128])
    ustrict_d = din("ustrict", [128, 128])
    iotas_d = din("iotas", [128, 33])
    if stop_after is None:
        out_c = nc.dram_tensor("out", [NT, D], F32, kind="ExternalOutput").ap()
        xT_d = nc.dram_tensor("xT_d", [8, 128, NT], F32, kind="Internal").ap()
    else:
        xT_d = nc.dram_tensor("xT_out", [8, 128, NT], F32, kind="ExternalOutput").ap()
        out_c = None
    w_in_b = nc.dram_tensor("w_in_b", [Lw, D, 2048], BF16, kind="Internal").ap()
    w_out_b = nc.dram_tensor("w_out_b", [Lw, D, D], BF16, kind="Internal").ap()
    pool_w_b = nc.dram_tensor("pool_w_b", [Lw, 4, 128, 128], BF16, kind="Internal").ap()
    w_gate_b = nc.dram_tensor("w_gate_b", [Lw, NE, D, DE], BF16, kind="Internal").ap()
    w_up_b = nc.dram_tensor("w_up_b", [Lw, NE, D, DE], BF16, kind="Internal").ap()
    w_down_b = nc.dram_tensor("w_down_b", [Lw, NE, DE, D], BF16, kind="Internal").ap()
    w_gate_p = [nc.dram_tensor(f"w_gate_p{i}", [NE * 128, 4096], BF16, kind="Internal").ap() for i in range(Lw)]
    w_up_p = [nc.dram_tensor(f"w_up_p{i}", [NE * 128, 4096], BF16, kind="Internal").ap() for i in range(Lw)]
    w_down_p = [nc.dram_tensor(f"w_down_p{i}", [NE * 128, 4096], BF16, kind="Internal").ap() for i in range(Lw)]
    HS_d = nc.dram_tensor("HS_d", [NTILE * TS, D], BF16, kind="Internal").ap()
    YS_d = nc.dram_tensor("YS_d", [NTILE * TS, D], F32, kind="Internal").ap()

    top = ExitStack()
    with top:
        T = Tracker(nc, top)

        uid = [0]

        def sb(stack, name, shape, dt):
            uid[0] += 1
            return stack.enter_context(nc.sbuf_tensor(f"{name}_s{uid[0]}", list(shape), dt))

        banks = [top.enter_context(nc.psum_tensor(f"bank{i}", [128, 512], F32)) for i in range(8)]
        BK = [Buf(f"bank{i}") for i in range(8)]

        onesb = sb(top, "onesb", [128, 128], BF16)
        identf = sb(top, "identf", [128, 128], F32)
        identb = sb(top, "identb", [128, 128], BF16)
        A_bf = sb(top, "A_bf", [128, 28, 128], BF16)
        gm = sb(top, "gm", [128, Lw * 8], F32)
        gf = sb(top, "gf", [128, Lw * 8], F32)
        psc = sb(top, "psc", [128, Lw * 4], F32)
        epsc = sb(top, "epsc", [128, 1], F32)
        ustr_f = sb(top, "ustr_f", [128, 128], F32)
        ustr = sb(top, "ustr", [128, 128], BF16)
        iotas = sb(top, "iotas", [128, 33], F32)
        CONST = Buf("const")
        with nc.sbuf_tensor("A_stage", [128, 28, 128], F32) as A_st:
            ASB = Buf("A_stage")
            T.emit("dve", lambda e: e.memset(onesb[:], 1.0), writes=[CONST])
            T.emit("dve", lambda e: e.memset(epsc[:], EPS), writes=[CONST])
            T.dma("sp", identf[:], ident_d, writes=[CONST])
            T.dma("sp", gm[:], gm_d, writes=[CONST])
            T.dma("sp", gf[:], gf_d, writes=[CONST])
            T.dma("sp", psc[:], ps_d, writes=[CONST])
            T.dma("sp", A_st[:], poolA_d, writes=[ASB])
            T.dma("sp", ustr_f[:], ustrict_d, writes=[CONST])
            T.dma("sp", iotas[:], iotas_d, writes=[CONST])
            T.emit("dve", lambda e: e.tensor_copy(out=ustr[:], in_=ustr_f[:]), reads=[CONST], writes=[CONST])
            T.emit("dve", lambda e: e.tensor_copy(out=identb[:], in_=identf[:]), reads=[CONST], writes=[CONST])
            T.emit("dve", lambda e: e.tensor_copy(out=A_bf[:], in_=A_st[:]), reads=[ASB], writes=[CONST])
            T.barrier()

        WB = {}

        cast_sems = [T._new_sem(top, f"cs{i}") for i in range(8)]
        cast_cnt = [0] * 8
        cast_i = [0]

        def cast(key, dst, src, n=0):
            WB[key] = Buf(str(key))
            E = T.eng["pool"]
            i = cast_i[0] % 8
            cast_i[0] += 1
            sm = cast_sems[i]
            if cast_cnt[i] and E.known.get(sm, 0) < cast_cnt[i]:
                E.q.append(("wait", sm, cast_cnt[i]))
                E.known[sm] = cast_cnt[i]
            cast_cnt[i] += 16
            E.q.append(("dma", dst, src, sm))
            WB[key].w = (sm, cast_cnt[i])

        for l in range(depth):
            cast(("in", l), w_in_b[l], w_in[l])
            cast(("pw", l), pool_w_b[l].rearrange("g (a b) d -> (g a) (b d)", b=16),
                 pool_w[l].rearrange("g (a b) d -> (g a) (b d)", b=16))
            cast(("out", l), w_out_b[l].rearrange("(r q) d -> r (q d)", q=2),
                 w_out[l].rearrange("(r q) d -> r (q d)", q=2))
            for e_ in range(NE):
                cast(("g", l, e_), w_gate_p[l][e_ * 128:(e_ + 1) * 128, :].rearrange("p (k f) -> p k f", f=512),
                     w_gate[l, e_].rearrange("(k p) f -> p k f", p=128))
                cast(("u", l, e_), w_up_p[l][e_ * 128:(e_ + 1) * 128, :].rearrange("p (k f) -> p k f", f=512),
                     w_up[l, e_].rearrange("(k p) f -> p k f", p=128))
                cast(("d", l, e_), w_down_p[l][e_ * 128:(e_ + 1) * 128, :].rearrange("p (k f) -> p k f", f=1024),
                     w_down[l, e_].rearrange("(k p) f -> p k f", p=128))

        XD = [[Buf(f"xd{s}_{t}") for t in range(4)] for s in range(nseq)]
        flip = [0]

        def evac_copy(out, in_, reads, writes, scale=None):
            flip[0] ^= 1
            if scale is not None or flip[0]:
                sc = 1.0 if scale is None else scale
                T.emit("act", lambda e: e.activation(out=out, in_=in_, func=AF.Copy, scale=sc), reads=reads, writes=writes)
            else:
                T.emit("dve", lambda e: e.tensor_copy(out=out, in_=in_), reads=reads, writes=writes)

        def mm(out, lhsT, rhs, start, stop, reads, writes, sig):
            T.emit("pe", lambda e: e.matmul(out, lhsT, rhs, start=start, stop=stop), reads=reads, writes=writes, sig=sig)

        def rstd_from_ss(out, ss_ap, reads, writes):
            T.emit("act", lambda e: e.activation(out=out, in_=ss_ap, func=AF.Sqrt, bias=epsc[:, 0:1], scale=1.0 / D),
                   reads=list(reads) + [CONST], writes=writes)
            T.emit("dve", lambda e: e.reciprocal(out=out, in_=out), reads=writes, writes=writes)

        with ExitStack() as st:
            xin = sb(st, "xin", [128, 2, 4, 1024], F32)
            xTs = sb(st, "xTs", [128, 2, 8, 512], F32)
            XIN = [Buf("xin0"), Buf("xin1")]
            XTS = [Buf("xts0"), Buf("xts1")]
            bi = 0
            for s in range(nseq):
                for t in range(4):
                    b = (s * 4 + t) % 2
                    t0 = s * S + t * 512
                    T.dma("sp", xin[:, b], x_c[t0:t0 + 512, :].rearrange("(j p) d -> p j d", p=128), writes=[XIN[b]])
                    for k in range(8):
                        bk = bi % 4
                        bi += 1
                        for j in range(4):
                            T.emit("pe", lambda e, bk=bk, j=j, k=k, b=b: e.transpose(
                                out=banks[bk][:, j * 128:(j + 1) * 128], in_=xin[:, b, j, k * 128:(k + 1) * 128],
                                identity=identf[:]), reads=[XIN[b], CONST], writes=[BK[bk]], sig=(j == 3))
                        evac_copy(xTs[:, b, k, :], banks[bk][:, :], [BK[bk]], [XTS[b]])
                    T.dma("sp", xT_d[:, :, t0:t0 + 512].rearrange("k p t -> p k t"), xTs[:, b], reads=[XTS[b]],
                          writes=[XD[s][t]])
            T.barrier()

        def m_phase(l, s):
            tok0 = s * S
            with ExitStack() as pm:
                H = sb(pm, "H", [128, 8, S], BF16)
                qT = sb(pm, "qT", [128, 4, S], BF16)
                kT = sb(pm, "kT", [128, 4, S], BF16)
                V2 = sb(pm, "V2", [128, 31, 4, 192], BF16)
                pT = sb(pm, "pT", [128, 4, S], BF16)
                pw_sb = sb(pm, "pw_sb", [128, 4, 128], BF16)
                HT = [Buf(f"H{i}") for i in range(8)]
                QB = [[Buf(f"q{m}_{t}") for t in range(4)] for m in range(4)]
                KB = [[Buf(f"k{m}_{t}") for t in range(4)] for m in range(4)]
                VB = [Buf(f"v{i}") for i in range(31)]
                VONES = Buf("vones")
                PB = [[Buf(f"p{g}_{t}") for t in range(4)] for g in range(4)]
                PW = Buf("pw")
                with ExitStack() as pb:
                    w_in_sb = sb(pb, "w_in_sb", [128, 8, 2048], BF16)
                    U2 = sb(pb, "U2", [128, 16, 512], BF16)
                    pooledT = sb(pb, "pooledT", [128, 2, 512], BF16)
                    sq = sb(pb, "sq", [128, 8, 256], BF16)
                    xt = sb(pb, "xt", [128, 1, 8, 256], F32)
                    rstd = sb(pb, "rstd", [128, 1, 256], F32)
                    WI = [Buf(f"wi{c}") for c in range(4)]
                    UB = [Buf(f"u{j}") for j in range(16)]
                    PLB = [Buf("pl0"), Buf("pl1")]
                    SQ = Buf("sq")
                    XT = [Buf("xt0"), Buf("xt1")]
                    RS = [Buf("rs0"), Buf("rs1")]
                    for c in range(4):
                        T.dma("sp", w_in_sb[:, :, c * 512:(c + 1) * 512],
                              w_in_b[l][:, c * 512:(c + 1) * 512].rearrange("(k p) f -> p k f", p=128),
                              reads=[WB[("in", l)]], writes=[WI[c]])
                    T.dma("sp", pw_sb[:], pool_w_b[l].rearrange("g c d -> c g d"), reads=[WB[("pw", l)]], writes=[PW])
                    T.emit("dve", lambda e: e.memset(V2[:, :, :, 64:128], 1.0), writes=[VONES])
                    for i in range(8):
                        b = 0
                        c0 = i * 256
                        T.dma("sp", xt[:, b], xT_d[:, :, tok0 + c0:tok0 + c0 + 256].rearrange("k p t -> p k t"),
                              reads=[XD[s][i // 2]], writes=[XT[b]])
                        T.emit("act", lambda e, b=b: e.activation(out=sq[:], in_=xt[:, b], func=AF.Square),
                               reads=[XT[b]], writes=[SQ])
                        bk = 6 + b
                        for k in range(8):
                            mm(banks[bk][:, 0:256], onesb[:], sq[:, k, :], k == 0, k == 7, [SQ, CONST], [BK[bk]], k == 7)
                        rstd_from_ss(rstd[:, b, :], banks[bk][:, 0:256], [BK[bk]], [RS[b]])
                        for k in range(8):
                            T.emit("dve", lambda e, b=b, k=k, c0=c0: e.scalar_tensor_tensor(
                                out=H[:, k, c0:c0 + 256], in0=xt[:, b, k, :], scalar=gm[:, l * 8 + k:l * 8 + k + 1],
                                in1=rstd[:, b, :], op0=ALU.mult, op1=ALU.mult),
                                reads=[XT[b], RS[b], CONST], writes=[HT[i]])
                    bi = 0
                    for t in range(4):
                        for m in range(8):
                            bk = bi % 4
                            bi += 1
                            for k in range(8):
                                mm(banks[bk][:, :], w_in_sb[:, k, m * 128:(m + 1) * 128], H[:, k, t * 512:(t + 1) * 512],
                                   k == 0, k == 7, [WI[m // 4], HT[2 * t], HT[2 * t + 1]], [BK[bk]], k == 7)
                            if m < 4:
                                evac_copy(qT[:, m, t * 512:(t + 1) * 512], banks[bk][:, :], [BK[bk]], [QB[m][t]], scale=0.125)
                            else:
                                T.emit("dve", lambda e, bk=bk, m=m, t=t: e.tensor_copy(
                                    out=kT[:, m - 4, t * 512:(t + 1) * 512], in_=banks[bk][:, :]),
                                    reads=[BK[bk]], writes=[KB[m - 4][t]])
                    for sidx in range(31):
                        bk = bi % 4
                        bi += 1
                        a0 = 64 * sidx
                        hts = sorted({a0 // 256, (a0 + 127) // 256})
                        for k in range(8):
                            mm(banks[bk][:, :], H[:, k, a0:a0 + 128], w_in_sb[:, k, 1024:1536], k == 0, k == 7,
                               [WI[2]] + [HT[i] for i in hts], [BK[bk]], k == 7)
                        evac_copy(V2[:, sidx, :, :].rearrange("p a (b d) -> p a b d", d=64)[:, :, 0:3:2, :],
                                  banks[bk][:, :].rearrange("p (a b d) -> p a b d", b=2, d=64), [BK[bk]], [VB[sidx]])
                    for j in range(16):
                        bk = bi % 4
                        bi += 1
                        for k in range(8):
                            mm(banks[bk][:, :], H[:, k, j * 128:(j + 1) * 128], w_in_sb[:, k, 1536:2048], k == 0, k == 7,
                               [WI[3], HT[j // 2]], [BK[bk]], k == 7)
                        evac_copy(U2[:, j, :], banks[bk][:, :], [BK[bk]], [UB[j]])
                    for t in range(4):
                        for g in range(4):
                            pbuf = g % 2
                            bk = bi % 4
                            bi += 1
                            for Tq in range(4):
                                Tt = 4 * t + Tq
                                terms = []
                                if Tt > 0:
                                    terms.append((Tt - 1, 0))
                                if Tt == 0:
                                    terms += [(Tt, 3), (Tt, 4)]
                                elif Tt == 15:
                                    terms += [(Tt, 5), (Tt, 6)]
                                else:
                                    terms.append((Tt, 1))
                                if Tt < 15:
                                    terms.append((Tt + 1, 2))
                                for ti, (tp, var) in enumerate(terms):
                                    mm(banks[bk][:, Tq * 128:(Tq + 1) * 128], U2[:, tp, g * 128:(g + 1) * 128],
                                       A_bf[:, g * 7 + var, :], ti == 0, ti == len(terms) - 1,
                                       [UB[tp], CONST], [BK[bk]], (Tq == 3 and ti == len(terms) - 1))
                            T.emit("act", lambda e, bk=bk, pbuf=pbuf, g=g: e.activation(
                                out=pooledT[:, pbuf, :], in_=banks[bk][:, :], func=AF.Copy),
                                reads=[BK[bk]], writes=[PLB[pbuf]])
                            bk2 = 4 + (g % 2)
                            mm(banks[bk2][:, :], pw_sb[:, g, :], pooledT[:, pbuf, :], True, True, [PW, PLB[pbuf]], [BK[bk2]], True)
                            T.emit("dve", lambda e, bk2=bk2, g=g, t=t: e.tensor_scalar(
                                out=pT[:, g, t * 512:(t + 1) * 512], in0=banks[bk2][:, :],
                                scalar1=psc[:, l * 4 + g:l * 4 + g + 1], scalar2=None, op0=ALU.mult),
                                reads=[BK[bk2], CONST], writes=[PB[g][t]])
                    T.barrier()
                with ExitStack() as pc:
                    bias = sb(pc, "bias", [128, 4, 2, 14, 64], F32)
                    Sb = sb(pc, "Sb", [128, 2, 2, 4, 64], F32)
                    Pm = sb(pc, "Pm", [128, 2, 2, 4, 64], BF16)
                    Rr = sb(pc, "Rr", [128, 2, 4, 64], F32)
                    w_out_sb = sb(pc, "w_out_sb", [128, 8, D], BF16)
                    xt2 = sb(pc, "xt2", [128, 2, 8, 256], F32)
                    BIAS = [Buf(f"bias{hp}") for hp in range(4)]
                    SBB = [Buf("sb0"), Buf("sb1")]
                    PMB = [Buf("pm0"), Buf("pm1")]
                    RRB = [Buf("rr0"), Buf("rr1")]
                    WO = [Buf("wo0"), Buf("wo1")]
                    XT2 = [Buf("xt20"), Buf("xt21")]
                    for hp in range(4):
                        T.dma("sp", bias[:, hp].rearrange("p a b c -> p (a b c)"), bias_d[l, hp], writes=[BIAS[hp]])
                    for c in range(2):
                        T.dma("sp", w_out_sb[:, :, c * 512:(c + 1) * 512],
                              w_out_b[l][:, c * 512:(c + 1) * 512].rearrange("(k p) f -> p k f", p=128),
                              reads=[WB[("out", l)]], writes=[WO[c]])
                    units = [(r, hp) for r in range(32) for hp in range(4)]

                    def rstart(r):
                        return min(max(r - 4, 0), 24)

                    def emit_qk(ui):
                        r, hp = units[ui]
                        u2 = ui % 2
                        rs_ = rstart(r)
                        i0 = rs_ - r + 7
                        kts = sorted({(rs_ * 64) // 512, (rs_ * 64 + 511) // 512})
                        for half, bkb in ((0, 0), (1, 2)):
                            bk = bkb + u2
                            p0 = 64 * half
                            for c in range(4):
                                ks = (rs_ + 2 * c) * 64
                                mm(banks[bk][:, c * 64:(c + 1) * 64], kT[p0:p0 + 64, hp, ks:ks + 128],
                                   qT[p0:p0 + 64, hp, r * 64:(r + 1) * 64], True, True,
                                   [KB[hp][t] for t in kts] + [QB[hp][r // 8]], [BK[bk]], c == 3)
                            T.emit("dve", lambda e, bk=bk, u2=u2, half=half, hp=hp, i0=i0: e.scalar_tensor_tensor(
                                out=Sb[:, u2, half], in0=banks[bk][:, 0:256].rearrange("p (c q) -> p c q", q=64),
                                scalar=70.0, in1=bias[:, hp, half, i0:i0 + 7:2, :], op0=ALU.min, op1=ALU.add),
                                reads=[BK[bk], BIAS[hp]], writes=[SBB[u2]])
                        T.emit("act", lambda e, u2=u2: e.activation(out=Pm[:, u2], in_=Sb[:, u2], func=AF.Exp),
                               reads=[SBB[u2]], writes=[PMB[u2]])

                    def emit_pv(ui):
                        r, hp = units[ui]
                        u2 = ui % 2
                        rs_ = rstart(r)
                        ob = 4 + (r % 2)
                        for half in (0, 1):
                            h = 2 * hp + half
                            for c in range(4):
                                sidx = rs_ + 2 * c
                                lhsT = V2[:, sidx, hp, 64 * half:64 * half + 128]
                                col = (hp * 2 + half) * 64
                                mm(banks[ob][:, col:col + 64], lhsT, Pm[:, u2, half, c, :], c == 0, c == 3,
                                   [VB[sidx], VONES, PMB[u2]], [BK[ob]], c == 3)
                        if hp == 3:
                            rb = r % 2
                            Ov = banks[ob][:, :].rearrange("p (a b q) -> p a b q", b=2, q=64)
                            T.emit("dve", lambda e, rb=rb, ob=ob: e.reciprocal(
                                out=Rr[0:64, rb], in_=banks[ob][64:128, :].rearrange("p (a b q) -> p a b q", b=2, q=64)[:, :, 0, :]),
                                reads=[BK[ob]], writes=[RRB[rb]])
                            T.emit("dve", lambda e, rb=rb, ob=ob: e.reciprocal(
                                out=Rr[64:128, rb], in_=banks[ob][0:64, :].rearrange("p (a b q) -> p a b q", b=2, q=64)[:, :, 1, :]),
                                reads=[BK[ob]], writes=[RRB[rb]])
                            T.emit("dve", lambda e, rb=rb, ob=ob, r=r: e.tensor_tensor(
                                out=H[0:64, 0:4, r * 64:(r + 1) * 64],
                                in0=banks[ob][0:64, :].rearrange("p (a b q) -> p a b q", b=2, q=64)[:, :, 0, :],
                                in1=Rr[0:64, rb], op=ALU.mult), reads=[BK[ob], RRB[rb]], writes=[HT[r // 4]])
                            T.emit("dve", lambda e, rb=rb, ob=ob, r=r: e.tensor_tensor(
                                out=H[64:128, 0:4, r * 64:(r + 1) * 64],
                                in0=banks[ob][64:128, :].rearrange("p (a b q) -> p a b q", b=2, q=64)[:, :, 1, :],
                                in1=Rr[64:128, rb], op=ALU.mult), reads=[BK[ob], RRB[rb]], writes=[HT[r // 4]])

                    for ui in range(len(units)):
                        emit_qk(ui)
                        if ui > 0:
                            emit_pv(ui - 1)
                    emit_pv(len(units) - 1)
                    for i in range(8):
                        b = i % 2
                        c0 = i * 256
                        T.dma("sp", xt2[:, b], xT_d[:, :, tok0 + c0:tok0 + c0 + 256].rearrange("k p t -> p k t"),
                              reads=[XD[s][i // 2]], writes=[XT2[b]])
                        for m in range(8):
                            bk = 6 + (m % 2)
                            for k in range(8):
                                rhs = H[:, k, c0:c0 + 256] if k < 4 else pT[:, k - 4, c0:c0 + 256]
                                rd = [HT[i]] if k < 4 else [PB[k - 4][i // 2]]
                                mm(banks[bk][:, 0:256], w_out_sb[:, k, m * 128:(m + 1) * 128], rhs, k == 0, k == 7,
                                   [WO[m // 4]] + rd, [BK[bk]], k == 7)
                            T.emit("dve", lambda e, b=b, m=m, bk=bk: e.tensor_tensor(
                                out=xt2[:, b, m, :], in0=banks[bk][:, 0:256], in1=xt2[:, b, m, :], op=ALU.add),
                                reads=[BK[bk], XT2[b]], writes=[XT2[b]])
                        T.dma("sp", xT_d[:, :, tok0 + c0:tok0 + c0 + 256].rearrange("k p t -> p k t"), xt2[:, b],
                              reads=[XT2[b]], writes=[XD[s][i // 2]])
                    T.barrier()

        def final_out(xT, XB, s):
            tok0 = s * S
            if True:
                if True:
                    with ExitStack() as po:
                        gF = sb(po, "gF", [128, D], F32)
                        ost = sb(po, "ost", [128, 1, D], F32)
                        junk = sb(po, "junk", [128, 512], BF16)
                        ssA = sb(po, "ssA", [128, 2], F32)
                        rsF = sb(po, "rsF", [128, 1], F32)
                        GF, OST, JK, SSA, RSF = Buf("gF"), [Buf("ost0"), Buf("ost1")], Buf("junk"), Buf("ssA"), Buf("rsF")
                        T.dma("sp", gF[:], gF_d, writes=[GF])
                        for j in range(16):
                            ob = 0
                            t = j // 4
                            for hb in range(2):
                                bk = 2 * (j % 2) + hb
                                for kk in range(4):
                                    k = hb * 4 + kk
                                    T.emit("pe", lambda e, bk=bk, kk=kk, k=k, j=j: e.transpose(
                                        out=banks[bk][:, kk * 128:(kk + 1) * 128], in_=xT[:, k, j * 128:(j + 1) * 128],
                                        identity=identf[:]), reads=[XB[k][t], CONST], writes=[BK[bk]], sig=(kk == 3))
                                T.emit("act", lambda e, bk=bk, hb=hb: e.activation(
                                    out=junk[:], in_=banks[bk][:, :], func=AF.Square, accum_out=ssA[:, hb:hb + 1]),
                                    reads=[BK[bk]], writes=[JK, SSA])
                            T.emit("dve", lambda e: e.tensor_tensor(out=rsF[:], in0=ssA[:, 0:1], in1=ssA[:, 1:2], op=ALU.add),
                                   reads=[SSA], writes=[RSF])
                            rstd_from_ss(rsF[:], rsF[:], [RSF], [RSF])
                            for hb in range(2):
                                bk = 2 * (j % 2) + hb
                                T.emit("dve", lambda e, bk=bk, hb=hb, ob=ob: e.scalar_tensor_tensor(
                                    out=ost[:, ob, hb * 512:(hb + 1) * 512], in0=banks[bk][:, :], scalar=rsF[:, 0:1],
                                    in1=gF[:, hb * 512:(hb + 1) * 512], op0=ALU.mult, op1=ALU.mult),
                                    reads=[BK[bk], RSF, GF], writes=[OST[ob]])
                            T.dma("sp", out_c[tok0 + j * 128:tok0 + (j + 1) * 128, :], ost[:, ob, :], reads=[OST[ob]],
                                  writes=[XD[s][t]])

        def f_phase_sparse(l, s, last):
            U32 = mybir.dt.uint32
            tok0 = s * S
            with ExitStack() as pf:
                xT = sb(pf, "xT", [128, 8, S], F32)
                ring = sb(pf, "ring", [128, 6, 4096], BF16)
                wr_sb = sb(pf, "wr_sb", [128, 8, 20], F32)
                wrp = sb(pf, "wrp", [128, 8, 20], F32)
                rt = sb(pf, "rt", [128, 16], F32)
                LGs = sb(pf, "LGs", [128, 16, 20], F32)
                r1 = sb(pf, "r1", [128, 16, 16], F32)
                gmax = sb(pf, "gmax", [128, 16], F32)
                goh = sb(pf, "goh", [128, 16, 4], F32)
                gex = sb(pf, "gex", [128, 16, 4], F32)
                gw = sb(pf, "gw", [128, 16], F32)
                esel = sb(pf, "esel", [128, 16, 4], F32)
                em = sb(pf, "em", [128, 16, 4], F32)
                m1 = sb(pf, "m1", [128, 16], F32)
                m2 = sb(pf, "m2", [128, 16], F32)
                oh1 = sb(pf, "oh1", [128, 16, 4], F32)
                oh2 = sb(pf, "oh2", [128, 16, 4], F32)
                w1 = sb(pf, "w1", [128, 16], F32)
                w2 = sb(pf, "w2", [128, 16], F32)
                M1 = sb(pf, "M1", [128, 16, 16], F32)
                M2 = sb(pf, "M2", [128, 16, 16], F32)
                Mb = sb(pf, "Mb", [128, 256], BF16)
                TOT = sb(pf, "TOT", [128, 16, 16], F32)
                JP = sb(pf, "JP", [128, 16, 16], F32)
                SL = sb(pf, "SL", [128, 16, 16], F32)
                ne = sb(pf, "ne", [128, 16], F32)
                ntl = sb(pf, "ntl", [128, 16], F32)
                base = sb(pf, "base", [128, 16], F32)
                bend = sb(pf, "bend", [128, 16], F32)
                b256 = sb(pf, "b256", [128, 16], F32)
                sl1 = sb(pf, "sl1", [128, 16], F32)
                sl2 = sb(pf, "sl2", [128, 16], F32)
                s1u = sb(pf, "s1u", [128, 16], U32)
                s2u = sb(pf, "s2u", [128, 16], U32)
                eidx = sb(pf, "eidx", [128, NTILE], F32)
                widf = sb(pf, "widf", [128, NTILE], F32)
                widx = sb(pf, "widx", [128, NTILE], U32)
                XB = [[Buf(f"X{k}_{t}") for t in range(4)] for k in range(8)]
                RSLOT = [Buf(f"ring{i}") for i in range(6)]
                WR, WRP = Buf("wr"), Buf("wrp")
                ROUT = Buf("router")
                HSB = [Buf(f"hs{i}") for i in range(32)]
                YSB = [Buf(f"ys{i}") for i in range(NTILE)]

                def load_tile_w(i):
                    for j, wp in enumerate((w_gate_p, w_up_p, w_down_p)):
                        slot = (3 * i + j) % 6
                        T.idma(ring[:, slot, :], None, wp[l], widx[:, i:i + 1],
                               reads=[ROUT] + [WB[(("g", "u", "d")[j], l, e_)] for e_ in range(NE)], writes=[RSLOT[slot]])

                for k in range(8):
                    T.dma("sp", xT[:, k, :], xT_d[k, :, tok0:tok0 + S], reads=[XD[s][t] for t in range(4)],
                          writes=[XB[k][t] for t in range(4)])
                T.dma("sp", wr_sb[:], wr_d[:, l], writes=[WR])
                for k in range(8):
                    T.emit("dve", lambda e, k=k: e.tensor_scalar(out=wrp[:, k, :], in0=wr_sb[:, k, :],
                                                                 scalar1=gf[:, l * 8 + k:l * 8 + k + 1], scalar2=None,
                                                                 op0=ALU.mult), reads=[WR, CONST], writes=[WRP])

                def dv(fn, extra_reads=()):
                    T.emit("dve", fn, reads=[ROUT] + list(extra_reads), writes=[ROUT])

                def b3(ap2, n):
                    return ap2.unsqueeze(2).broadcast_to([128, 16, n])

                with ExitStack() as pa:
                    H = sb(pa, "Hf", [128, 8, S], BF16)
                    sq = sb(pa, "sqf", [128, 8, 512], BF16)
                    rstd = sb(pa, "rstdf", [128, 512], F32)
                    hrow = sb(pa, "hrow", [128, 2, D], BF16)
                    HB = [Buf(f"Hf{t}") for t in range(4)]
                    SQ, RS = Buf("sq"), Buf("rs")
                    HROW = [Buf("hrow0"), Buf("hrow1")]
                    for t in range(4):
                        c0 = t * 512
                        T.emit("act", lambda e, c0=c0: e.activation(out=sq[:], in_=xT[:, :, c0:c0 + 512], func=AF.Square),
                               reads=[XB[k][t] for k in range(8)], writes=[SQ])
                        for k in range(8):
                            mm(banks[6][:, :], onesb[:], sq[:, k, :], k == 0, k == 7, [SQ, CONST], [BK[6]], k == 7)
                        rstd_from_ss(rstd[:], banks[6][:, :], [BK[6]], [RS])
                        for k in range(8):
                            T.emit("dve", lambda e, k=k, c0=c0: e.scalar_tensor_tensor(
                                out=H[:, k, c0:c0 + 512], in0=xT[:, k, c0:c0 + 512], scalar=gf[:, l * 8 + k:l * 8 + k + 1],
                                in1=rstd[:], op0=ALU.mult, op1=ALU.mult), reads=[XB[k][t], RS, CONST], writes=[HB[t]])
                        for j in range(4):
                            jj = 4 * t + j
                            for k in range(8):
                                mm(banks[7][:, 320 + jj:320 + jj + 1], sq[:, k, j * 128:(j + 1) * 128], onesb[:, 0:1],
                                   k == 0, k == 7, [SQ, CONST], [BK[7]], False)
                            for k in range(8):
                                mm(banks[7][:, jj * 20:(jj + 1) * 20], xT[:, k, c0 + j * 128:c0 + (j + 1) * 128], wrp[:, k, :],
                                   k == 0, k == 7, [XB[k][t], WRP], [BK[7]], (k == 7))
                    rstd_from_ss(rt[:], banks[7][:, 320:336], [BK[7], ROUT], [ROUT])
                    dv(lambda e: e.tensor_tensor(out=LGs[:], in0=banks[7][:, 0:320].rearrange("p (j e) -> p j e", e=20),
                                                 in1=b3(rt[:], 20), op=ALU.mult), [BK[7]])
                    dv(lambda e: e.tensor_reduce(out=gmax[:], in_=LGs[:, :, 0:4], axis=AX.X, op=ALU.max))
                    dv(lambda e: e.tensor_tensor(out=goh[:], in0=LGs[:, :, 0:4], in1=b3(gmax[:], 4), op=ALU.is_equal))
                    dv(lambda e: e.tensor_tensor(out=gex[:], in0=LGs[:, :, 0:4], in1=b3(gmax[:], 4), op=ALU.subtract))
                    T.emit("act", lambda e: e.activation(out=gex[:], in_=gex[:], func=AF.Exp), reads=[ROUT], writes=[ROUT])
                    dv(lambda e: e.tensor_reduce(out=gw[:], in_=gex[:], axis=AX.X, op=ALU.add))
                    dv(lambda e: e.reciprocal(out=gw[:], in_=gw[:]))
                    dv(lambda e: e.tensor_tensor(out=r1[:].rearrange("p j (g i) -> p j g i", i=4),
                                                 in0=LGs[:, :, 4:20].rearrange("p j (g i) -> p j g i", i=4),
                                                 in1=goh[:].unsqueeze(3).broadcast_to([128, 16, 4, 4]), op=ALU.mult))
                    dv(lambda e: e.tensor_reduce(out=esel[:], in_=r1[:].rearrange("p j (g i) -> p j i g", i=4),
                                                 axis=AX.X, op=ALU.add))
                    dv(lambda e: e.tensor_reduce(out=m1[:], in_=esel[:], axis=AX.X, op=ALU.max))
                    dv(lambda e: e.tensor_tensor(out=oh1[:], in0=esel[:], in1=b3(m1[:], 4), op=ALU.is_equal))
                    dv(lambda e: e.scalar_tensor_tensor(out=em[:], in0=oh1[:], scalar=-1.0e30, in1=esel[:],
                                                        op0=ALU.mult, op1=ALU.add))
                    dv(lambda e: e.tensor_reduce(out=m2[:], in_=em[:], axis=AX.X, op=ALU.max))
                    dv(lambda e: e.tensor_tensor(out=oh2[:], in0=em[:], in1=b3(m2[:], 4), op=ALU.is_equal))
                    dv(lambda e: e.tensor_tensor(out=w2[:], in0=m2[:], in1=m1[:], op=ALU.subtract))
                    T.emit("act", lambda e: e.activation(out=w2[:], in_=w2[:], func=AF.Exp), reads=[ROUT], writes=[ROUT])
                    dv(lambda e: e.tensor_scalar(out=w2[:], in0=w2[:], scalar1=1.0, scalar2=None, op0=ALU.add))
                    dv(lambda e: e.reciprocal(out=w1[:], in_=w2[:]))
                    dv(lambda e: e.tensor_tensor(out=w1[:], in0=w1[:], in1=gw[:], op=ALU.mult))
                    dv(lambda e: e.tensor_tensor(out=w2[:], in0=gw[:], in1=w1[:], op=ALU.subtract))
                    g44 = lambda ap: ap.rearrange("p j (g i) -> p j g i", i=4)
                    dv(lambda e: e.tensor_tensor(out=g44(M1[:]), in0=goh[:].unsqueeze(3).broadcast_to([128, 16, 4, 4]),
                                                 in1=oh1[:].unsqueeze(2).broadcast_to([128, 16, 4, 4]), op=ALU.mult))
                    dv(lambda e: e.tensor_tensor(out=g44(M2[:]), in0=goh[:].unsqueeze(3).broadcast_to([128, 16, 4, 4]),
                                                 in1=oh2[:].unsqueeze(2).broadcast_to([128, 16, 4, 4]), op=ALU.mult))
                    dv(lambda e: e.tensor_tensor(out=Mb[:].rearrange("p (j e) -> p j e", e=16), in0=M1[:], in1=M2[:], op=ALU.add))
                    mm(banks[6][:, 0:256], ustr[:], Mb[:], True, True, [ROUT, CONST], [BK[6]], True)
                    mm(banks[7][:, 0:256], onesb[:], Mb[:], True, True, [ROUT, CONST], [BK[7]], True)
                    dv(lambda e: e.tensor_copy(out=TOT[:].rearrange("p j e -> p (j e)"), in_=banks[7][:, 0:256]), [BK[7]])
                    dv(lambda e: e.memset(JP[:, 0, :], 0.0))
                    for j in range(1, 16):
                        dv(lambda e, j=j: e.tensor_tensor(out=JP[:, j, :], in0=JP[:, j - 1, :], in1=TOT[:, j - 1, :], op=ALU.add))
                    dv(lambda e: e.tensor_tensor(out=ne[:], in0=JP[:, 15, :], in1=TOT[:, 15, :], op=ALU.add))
                    dv(lambda e: e.tensor_scalar(out=ntl[:], in0=ne[:], scalar1=0.0, scalar2=None, op0=ALU.is_gt))
                    for q in range(1, S // TS):
                        dv(lambda e, q=q: e.scalar_tensor_tensor(out=ntl[:], in0=ne[:], scalar=float(TS * q), in1=ntl[:],
                                                                 op0=ALU.is_gt, op1=ALU.add))
                    dv(lambda e: e.memset(base[:, 0:1], 0.0))
                    for e_ in range(1, 16):
                        dv(lambda e, e_=e_: e.tensor_tensor(out=base[:, e_:e_ + 1], in0=base[:, e_ - 1:e_],
                                                            in1=ntl[:, e_ - 1:e_], op=ALU.add))
                    dv(lambda e: e.tensor_tensor(out=bend[:], in0=base[:], in1=ntl[:], op=ALU.add))
                    dv(lambda e: e.tensor_scalar(out=b256[:], in0=base[:], scalar1=float(TS), scalar2=None, op0=ALU.mult))
                    dv(lambda e: e.tensor_tensor(out=SL[:], in0=banks[6][:, 0:256].rearrange("p (j e) -> p j e", e=16),
                                                 in1=JP[:], op=ALU.add), [BK[6]])
                    dv(lambda e: e.tensor_tensor(out=SL[:], in0=SL[:], in1=b256[:].unsqueeze(1).broadcast_to([128, 16, 16]),
                                                 op=ALU.add))
                    dv(lambda e: e.tensor_tensor(out=M1[:], in0=M1[:], in1=SL[:], op=ALU.mult))
                    dv(lambda e: e.tensor_tensor(out=M2[:], in0=M2[:], in1=SL[:], op=ALU.mult))
                    dv(lambda e: e.tensor_reduce(out=sl1[:], in_=M1[:], axis=AX.X, op=ALU.add))
                    dv(lambda e: e.tensor_reduce(out=sl2[:], in_=M2[:], axis=AX.X, op=ALU.add))
                    dv(lambda e: e.tensor_copy(out=s1u[:], in_=sl1[:]))
                    dv(lambda e: e.tensor_copy(out=s2u[:], in_=sl2[:]))
                    dv(lambda e: e.memset(eidx[:], 0.0))
                    for e_ in range(16):
                        dv(lambda e, e_=e_: e.scalar_tensor_tensor(out=eidx[:], in0=iotas[:, 1:1 + NTILE], scalar=bend[:, e_:e_ + 1],
                                                                   in1=eidx[:], op0=ALU.is_ge, op1=ALU.add), [CONST])
                    dv(lambda e: e.tensor_scalar(out=eidx[:], in0=eidx[:], scalar1=15.0, scalar2=None, op0=ALU.min))
                    dv(lambda e: e.tensor_scalar(out=widf[:], in0=eidx[:], scalar1=128.0, scalar2=iotas[:, 0:1],
                                                 op0=ALU.mult, op1=ALU.add), [CONST])
                    dv(lambda e: e.tensor_copy(out=widx[:], in_=widf[:]))
                    load_tile_w(0)
                    for j in range(16):
                        hb = j % 2
                        bk = 4 + hb
                        pv = banks[bk][:, :].bitcast(BF16)
                        for k in range(8):
                            T.emit("pe", lambda e, pv=pv, k=k, j=j: e.transpose(
                                out=pv[:, k * 128:(k + 1) * 128], in_=H[:, k, j * 128:(j + 1) * 128], identity=identb[:]),
                                reads=[HB[j // 4], CONST], writes=[BK[bk]], sig=(k == 7))
                        evac_copy(hrow[:, hb, :], pv, [BK[bk]], [HROW[hb]])
                        T.idma(HS_d, s1u[:, j:j + 1], hrow[:, hb, :], None, reads=[HROW[hb], ROUT], writes=[HSB[2 * j]])
                        T.idma(HS_d, s2u[:, j:j + 1], hrow[:, hb, :], None, reads=[HROW[hb], ROUT], writes=[HSB[2 * j + 1]])
                    T.barrier()

                with ExitStack() as pbx:
                    hs_sb = sb(pbx, "hs_sb", [128, 2, 2, D], BF16)
                    hcT = sb(pbx, "hcT", [128, 2, 8, TS], BF16)
                    a_e = sb(pbx, "a_e", [128, 2, 4, TS], BF16)
                    sg = sb(pbx, "sg", [128, 2, TS], F32)
                    ys = sb(pbx, "ys", [128, 2, 2, D], F32)
                    HSS = [Buf("hss0"), Buf("hss1")]
                    HCT = [Buf("hct0"), Buf("hct1")]
                    AE = [[Buf(f"ae{b}_{f}") for f in range(4)] for b in range(2)]
                    SG = [Buf("sg0"), Buf("sg1")]
                    YS = [Buf("ysb0"), Buf("ysb1")]

                    def emit_gu(i):
                        ab = i % 2
                        sg_, su_ = (3 * i) % 6, (3 * i + 1) % 6
                        T.dma("sp", hs_sb[:, ab], HS_d[i * TS:(i + 1) * TS, :].rearrange("(a p) d -> p a d", p=128),
                              reads=HSB, writes=[HSS[ab]])
                        for hb in range(2):
                            bk = 6 + hb
                            pv = banks[bk][:, :].bitcast(BF16)
                            for kk in range(4):
                                k = hb * 4 + kk
                                for a in range(2):
                                    T.emit("pe", lambda e, pv=pv, kk=kk, a=a, k=k, ab=ab: e.transpose(
                                        out=pv[:, kk * TS + a * 128:kk * TS + (a + 1) * 128],
                                        in_=hs_sb[:, ab, a, k * 128:(k + 1) * 128], identity=identb[:]),
                                        reads=[HSS[ab], CONST], writes=[BK[bk]], sig=(kk == 3 and a == 1))
                            evac_copy(hcT[:, ab, hb * 4:(hb + 1) * 4, :].rearrange("p k t -> p (k t)"), pv, [BK[bk]], [HCT[ab]])
                        for f in range(4):
                            bg, bu = f % 2, 2 + f % 2
                            for k in range(8):
                                mm(banks[bg][:, 0:TS], ring[:, sg_, k * 512 + f * 128:k * 512 + (f + 1) * 128], hcT[:, ab, k, :],
                                   k == 0, k == 7, [RSLOT[sg_], HCT[ab]], [BK[bg]], k == 7)
                            for k in range(8):
                                mm(banks[bu][:, 0:TS], ring[:, su_, k * 512 + f * 128:k * 512 + (f + 1) * 128], hcT[:, ab, k, :],
                                   k == 0, k == 7, [RSLOT[su_], HCT[ab]], [BK[bu]], k == 7)
                            i2 = f % 2
                            T.emit("act", lambda e, bg=bg, i2=i2: e.activation(out=sg[:, i2, :], in_=banks[bg][:, 0:TS], func=AF.Silu),
                                   reads=[BK[bg]], writes=[SG[i2]])
                            T.emit("dve", lambda e, bu=bu, i2=i2, ab=ab, f=f: e.tensor_tensor(
                                out=a_e[:, ab, f, :], in0=banks[bu][:, 0:TS], in1=sg[:, i2, :], op=ALU.mult),
                                reads=[BK[bu], SG[i2]], writes=[AE[ab][f]])

                    def emit_d(i):
                        ab = i % 2
                        sd_ = (3 * i + 2) % 6
                        for a in range(2):
                            for dh in range(2):
                                bk = 4 + dh
                                for f in range(4):
                                    mm(banks[bk][:, :], a_e[:, ab, f, a * 128:(a + 1) * 128],
                                       ring[:, sd_, f * 1024 + dh * 512:f * 1024 + (dh + 1) * 512],
                                       f == 0, f == 3, [RSLOT[sd_], AE[ab][f]], [BK[bk]], f == 3)
                                evac_copy(ys[:, ab, a, dh * 512:(dh + 1) * 512], banks[bk][:, :], [BK[bk]], [YS[ab]])
                        T.dma("sp", YS_d[i * TS:(i + 1) * TS, :].rearrange("(a p) d -> p a d", p=128), ys[:, ab],
                              reads=[YS[ab]], writes=[YSB[i]])

                    for i in range(NTILE):
                        emit_gu(i)
                        if i > 0:
                            emit_d(i - 1)
                        if i + 1 < NTILE:
                            load_tile_w(i + 1)
                    emit_d(NTILE - 1)
                    T.barrier()

                with ExitStack() as pcx:
                    g0 = sb(pcx, "g0", [128, 2, D], F32)
                    g1 = sb(pcx, "g1", [128, 2, D], F32)
                    G0 = [Buf("g00"), Buf("g01")]
                    G1 = [Buf("g10"), Buf("g11")]
                    for j in range(16):
                        b = j % 2
                        T.idma(g0[:, b, :], None, YS_d, s1u[:, j:j + 1], reads=YSB + [ROUT], writes=[G0[b]])
                        T.idma(g1[:, b, :], None, YS_d, s2u[:, j:j + 1], reads=YSB + [ROUT], writes=[G1[b]])
                        T.emit("dve", lambda e, b=b, j=j: e.tensor_scalar(out=g0[:, b, :], in0=g0[:, b, :], scalar1=w1[:, j:j + 1],
                                                                          scalar2=None, op0=ALU.mult), reads=[G0[b], ROUT], writes=[G0[b]])
                        T.emit("dve", lambda e, b=b, j=j: e.scalar_tensor_tensor(out=g0[:, b, :], in0=g1[:, b, :], scalar=w2[:, j:j + 1],
                                                                                 in1=g0[:, b, :], op0=ALU.mult, op1=ALU.add),
                               reads=[G0[b], G1[b], ROUT], writes=[G0[b]])
                        for hb in range(2):
                            bk = 2 * b + hb
                            for kk in range(4):
                                k = hb * 4 + kk
                                T.emit("pe", lambda e, bk=bk, kk=kk, k=k, b=b: e.transpose(
                                    out=banks[bk][:, kk * 128:(kk + 1) * 128], in_=g0[:, b, k * 128:(k + 1) * 128],
                                    identity=identf[:]), reads=[G0[b], CONST], writes=[BK[bk]], sig=(kk == 3))
                            T.emit("dve", lambda e, bk=bk, hb=hb, j=j: e.tensor_tensor(
                                out=xT[:, hb * 4:(hb + 1) * 4, j * 128:(j + 1) * 128],
                                in0=banks[bk][:, :].rearrange("p (k t) -> p k t", t=128),
                                in1=xT[:, hb * 4:(hb + 1) * 4, j * 128:(j + 1) * 128], op=ALU.add),
                                reads=[BK[bk]] + [XB[hb * 4 + kk][j // 4] for kk in range(4)],
                                writes=[XB[hb * 4 + kk][j // 4] for kk in range(4)])
                    T.barrier()

                if not last:
                    for k in range(8):
                        T.dma("sp", xT_d[k, :, tok0:tok0 + S], xT[:, k, :], reads=[XB[k][t] for t in range(4)],
                              writes=[XD[s][t] for t in range(4)])
                else:
                    final_out(xT, XB, s)
                T.barrier()

        def f_phase(l, s, last):
            tok0 = s * S
            with ExitStack() as pf:
                xT = sb(pf, "xT", [128, 8, S], F32)
                H = sb(pf, "Hf", [128, 8, S], BF16)
                ring = sb(pf, "ring", [128, 6, 4096], BF16)
                sq = sb(pf, "sqf", [128, 8, 512], BF16)
                rstd = sb(pf, "rstdf", [128, 512], F32)
                wr_sb = sb(pf, "wr_sb", [128, 8, 20], F32)
                wrp = sb(pf, "wrp", [128, 8, 20], F32)
                a_e = sb(pf, "a_e", [128, 2, 4, 512], BF16)
                sg = sb(pf, "sg", [128, 2, 512], F32)
                tt = sb(pf, "tt", [128, 2, 512], F32)
                Cb = sb(pf, "Cb", [128, 2, 512], F32)
                rt = sb(pf, "rt", [128, 16], F32)
                LGs = sb(pf, "LGs", [128, 16, 20], F32)
                r1 = sb(pf, "r1", [128, 16, 16], F32)
                r2 = sb(pf, "r2", [128, 16, 16], F32)
                gmax = sb(pf, "gmax", [128, 16], F32)
                goh = sb(pf, "goh", [128, 16, 4], F32)
                gex = sb(pf, "gex", [128, 16, 4], F32)
                gw = sb(pf, "gw", [128, 16], F32)
                esel = sb(pf, "esel", [128, 16, 4], F32)
                em = sb(pf, "em", [128, 16, 4], F32)
                m1 = sb(pf, "m1", [128, 16], F32)
                m2 = sb(pf, "m2", [128, 16], F32)
                oh1 = sb(pf, "oh1", [128, 16, 4], F32)
                oh2 = sb(pf, "oh2", [128, 16, 4], F32)
                w1 = sb(pf, "w1", [128, 16], F32)
                w2 = sb(pf, "w2", [128, 16], F32)
                c4 = sb(pf, "c4", [128, 16, 4], F32)
                Cm = sb(pf, "Cm", [128, 16, 16], F32)
                Chi = sb(pf, "Chi", [128, 16, 16], BF16)
                Clo = sb(pf, "Clo", [128, 16, 16], BF16)
                XB = [[Buf(f"X{k}_{t}") for t in range(4)] for k in range(8)]
                HB = [Buf(f"Hf{t}") for t in range(4)]
                RSLOT = [Buf(f"ring{i}") for i in range(6)]
                SQ, RS, WR, WRP, RT = Buf("sq"), Buf("rs"), Buf("wr"), Buf("wrp"), Buf("rt")
                ROUT = Buf("router")
                AE = [[Buf(f"ae{b}_{f}") for f in range(4)] for b in range(2)]
                SG = [Buf(f"sg{i}") for i in range(2)]
                TT = [Buf(f"tt{i}") for i in range(2)]
                CB = [Buf("cb0"), Buf("cb1")]

                def load_expert(e):
                    e4 = e // 4
                    for j, (nm, wb) in enumerate((("g", w_gate_b), ("u", w_up_b))):
                        slot = (3 * e + j) % 6
                        T.dma("sp", ring[:, slot, :].rearrange("p (k f) -> p k f", f=512),
                              wb[l, e].rearrange("(k p) f -> p k f", p=128), reads=[WB[(nm, l, e4)]], writes=[RSLOT[slot]])
                    slot = (3 * e + 2) % 6
                    T.dma("sp", ring[:, slot, :].rearrange("p (k f) -> p k f", f=1024),
                          w_down_b[l, e].rearrange("(k p) f -> p k f", p=128), reads=[WB[("d", l, e4)]], writes=[RSLOT[slot]])

                for k in range(8):
                    T.dma("sp", xT[:, k, :], xT_d[k, :, tok0:tok0 + S], reads=[XD[s][t] for t in range(4)],
                          writes=[XB[k][t] for t in range(4)])
                T.dma("sp", wr_sb[:], wr_d[:, l], writes=[WR])
                load_expert(0)
                for k in range(8):
                    T.emit("dve", lambda e, k=k: e.tensor_scalar(out=wrp[:, k, :], in0=wr_sb[:, k, :],
                                                                 scalar1=gf[:, l * 8 + k:l * 8 + k + 1], scalar2=None,
                                                                 op0=ALU.mult), reads=[WR, CONST], writes=[WRP])
                for t in range(4):
                    c0 = t * 512
                    T.emit("act", lambda e, c0=c0: e.activation(out=sq[:], in_=xT[:, :, c0:c0 + 512], func=AF.Square),
                           reads=[XB[k][t] for k in range(8)], writes=[SQ])
                    for k in range(8):
                        mm(banks[6][:, :], onesb[:], sq[:, k, :], k == 0, k == 7, [SQ, CONST], [BK[6]], k == 7)
                    rstd_from_ss(rstd[:], banks[6][:, :], [BK[6]], [RS])
                    for k in range(8):
                        T.emit("dve", lambda e, k=k, c0=c0: e.scalar_tensor_tensor(
                            out=H[:, k, c0:c0 + 512], in0=xT[:, k, c0:c0 + 512], scalar=gf[:, l * 8 + k:l * 8 + k + 1],
                            in1=rstd[:], op0=ALU.mult, op1=ALU.mult), reads=[XB[k][t], RS, CONST], writes=[HB[t]])
                    for j in range(4):
                        jj = 4 * t + j
                        for k in range(8):
                            mm(banks[7][:, 320 + jj:320 + jj + 1], sq[:, k, j * 128:(j + 1) * 128], onesb[:, 0:1],
                               k == 0, k == 7, [SQ, CONST], [BK[7]], False)
                        for k in range(8):
                            mm(banks[7][:, jj * 20:(jj + 1) * 20], xT[:, k, c0 + j * 128:c0 + (j + 1) * 128], wrp[:, k, :],
                               k == 0, k == 7, [XB[k][t], WRP], [BK[7]], (k == 7))
                R_ = [ROUT]

                def dv(fn, extra_reads=()):
                    T.emit("dve", fn, reads=[ROUT] + list(extra_reads), writes=[ROUT])

                def b3(ap2, n):
                    return ap2.unsqueeze(2).broadcast_to([128, 16, n])

                rstd_from_ss(rt[:], banks[7][:, 320:336], [BK[7], ROUT], [ROUT])
                dv(lambda e: e.tensor_tensor(out=LGs[:], in0=banks[7][:, 0:320].rearrange("p (j e) -> p j e", e=20),
                                             in1=b3(rt[:], 20), op=ALU.mult), [BK[7]])
                dv(lambda e: e.tensor_reduce(out=gmax[:], in_=LGs[:, :, 0:4], axis=AX.X, op=ALU.max))
                dv(lambda e: e.tensor_tensor(out=goh[:], in0=LGs[:, :, 0:4], in1=b3(gmax[:], 4), op=ALU.is_equal))
                dv(lambda e: e.tensor_tensor(out=gex[:], in0=LGs[:, :, 0:4], in1=b3(gmax[:], 4), op=ALU.subtract))
                T.emit("act", lambda e: e.activation(out=gex[:], in_=gex[:], func=AF.Exp), reads=[ROUT], writes=[ROUT])
                dv(lambda e: e.tensor_reduce(out=gw[:], in_=gex[:], axis=AX.X, op=ALU.add))
                dv(lambda e: e.reciprocal(out=gw[:], in_=gw[:]))
                dv(lambda e: e.tensor_tensor(out=r1[:].rearrange("p j (g i) -> p j g i", i=4),
                                             in0=LGs[:, :, 4:20].rearrange("p j (g i) -> p j g i", i=4),
                                             in1=goh[:].unsqueeze(3).broadcast_to([128, 16, 4, 4]), op=ALU.mult))
                dv(lambda e: e.tensor_reduce(out=esel[:], in_=r1[:].rearrange("p j (g i) -> p j i g", i=4),
                                             axis=AX.X, op=ALU.add))
                dv(lambda e: e.tensor_reduce(out=m1[:], in_=esel[:], axis=AX.X, op=ALU.max))
                dv(lambda e: e.tensor_tensor(out=oh1[:], in0=esel[:], in1=b3(m1[:], 4), op=ALU.is_equal))
                dv(lambda e: e.scalar_tensor_tensor(out=em[:], in0=oh1[:], scalar=-1.0e30, in1=esel[:],
                                                    op0=ALU.mult, op1=ALU.add))
                dv(lambda e: e.tensor_reduce(out=m2[:], in_=em[:], axis=AX.X, op=ALU.max))
                dv(lambda e: e.tensor_tensor(out=oh2[:], in0=em[:], in1=b3(m2[:], 4), op=ALU.is_equal))
                dv(lambda e: e.tensor_tensor(out=w2[:], in0=m2[:], in1=m1[:], op=ALU.subtract))
                T.emit("act", lambda e: e.activation(out=w2[:], in_=w2[:], func=AF.Exp), reads=[ROUT], writes=[ROUT])
                dv(lambda e: e.tensor_scalar(out=w2[:], in0=w2[:], scalar1=1.0, scalar2=None, op0=ALU.add))
                dv(lambda e: e.reciprocal(out=w1[:], in_=w2[:]))
                dv(lambda e: e.tensor_tensor(out=w1[:], in0=w1[:], in1=gw[:], op=ALU.mult))
                dv(lambda e: e.tensor_tensor(out=w2[:], in0=gw[:], in1=w1[:], op=ALU.subtract))
                dv(lambda e: e.tensor_tensor(out=c4[:], in0=oh1[:], in1=b3(w1[:], 4), op=ALU.mult))
                dv(lambda e: e.tensor_tensor(out=oh2[:], in0=oh2[:], in1=b3(w2[:], 4), op=ALU.mult))
                dv(lambda e: e.tensor_tensor(out=c4[:], in0=c4[:], in1=oh2[:], op=ALU.add))
                dv(lambda e: e.tensor_tensor(out=Cm[:].rearrange("p j (g i) -> p j g i", i=4),
                                             in0=goh[:].unsqueeze(3).broadcast_to([128, 16, 4, 4]),
                                             in1=c4[:].unsqueeze(2).broadcast_to([128, 16, 4, 4]), op=ALU.mult))
                dv(lambda e: e.tensor_copy(out=Chi[:], in_=Cm[:]))
                dv(lambda e: e.tensor_tensor(out=r2[:], in0=Cm[:], in1=Chi[:], op=ALU.subtract))
                dv(lambda e: e.tensor_copy(out=Clo[:], in_=r2[:]))

                steps = [(e_, t_) for e_ in range(NE) for t_ in range(4)]

                def emit_gu(idx):
                    e_, t = steps[idx]
                    ab = idx % 2
                    c0 = t * 512
                    sg_, su_, sd_ = (3 * e_) % 6, (3 * e_ + 1) % 6, (3 * e_ + 2) % 6
                    n = 0
                    for j in range(4):
                        for Cx in (Chi, Clo):
                            mm(banks[6][:, j * 128:(j + 1) * 128], Cx[:, 4 * t + j, e_:e_ + 1].broadcast_to([128, 128]),
                               identb[:], Cx is Chi, Cx is Clo, [ROUT, CONST], [BK[6]], (j == 3 and Cx is Clo))
                    T.emit("act", lambda e, ab=ab: e.activation(out=Cb[:, ab, :], in_=banks[6][:, :], func=AF.Copy),
                           reads=[BK[6]], writes=[CB[ab]])
                    for f in range(4):
                        bg, bu = f % 2, 2 + f % 2
                        for k in range(8):
                            mm(banks[bg][:, :], ring[:, sg_, k * 512 + f * 128:k * 512 + (f + 1) * 128], H[:, k, c0:c0 + 512],
                               k == 0, k == 7, [RSLOT[sg_], HB[t]], [BK[bg]], k == 7)
                        for k in range(8):
                            mm(banks[bu][:, :], ring[:, su_, k * 512 + f * 128:k * 512 + (f + 1) * 128], H[:, k, c0:c0 + 512],
                               k == 0, k == 7, [RSLOT[su_], HB[t]], [BK[bu]], k == 7)
                        i3 = f % 2
                        T.emit("act", lambda e, bg=bg, i3=i3: e.activation(out=sg[:, i3, :], in_=banks[bg][:, :], func=AF.Silu),
                               reads=[BK[bg]], writes=[SG[i3]])
                        T.emit("dve", lambda e, bu=bu, i3=i3: e.tensor_tensor(out=tt[:, i3, :], in0=banks[bu][:, :],
                                                                            in1=sg[:, i3, :], op=ALU.mult),
                               reads=[BK[bu], SG[i3]], writes=[TT[i3]])
                        T.emit("dve", lambda e, ab=ab, f=f, i3=i3: e.tensor_tensor(out=a_e[:, ab, f, :], in0=tt[:, i3, :],
                                                                                   in1=Cb[:, ab, :], op=ALU.mult),
                               reads=[TT[i3], CB[ab]], writes=[AE[ab][f]])

                def emit_d(idx):
                    e_, t = steps[idx]
                    ab = idx % 2
                    c0 = t * 512
                    sd_ = (3 * e_ + 2) % 6
                    for m in range(8):
                        bk = 4 + m % 2
                        for f in range(4):
                            mm(banks[bk][:, :], ring[:, sd_, f * 1024 + m * 128:f * 1024 + (m + 1) * 128], a_e[:, ab, f, :],
                               f == 0, f == 3, [RSLOT[sd_], AE[ab][f]], [BK[bk]], f == 3)
                        T.emit("dve", lambda e, m=m, bk=bk, c0=c0: e.tensor_tensor(
                            out=xT[:, m, c0:c0 + 512], in0=banks[bk][:, :], in1=xT[:, m, c0:c0 + 512], op=ALU.add),
                            reads=[BK[bk], XB[m][t]], writes=[XB[m][t]])

                for idx in range(len(steps)):
                    e_, t = steps[idx]
                    emit_gu(idx)
                    if idx > 0:
                        emit_d(idx - 1)
                    if t == 0 and e_ + 1 < NE:
                        load_expert(e_ + 1)
                emit_d(len(steps) - 1)

                if not last:
                    for k in range(8):
                        T.dma("sp", xT_d[k, :, tok0:tok0 + S], xT[:, k, :], reads=[XB[k][t] for t in range(4)],
                              writes=[XD[s][t] for t in range(4)])
                else:
                    final_out(xT, XB, s)
                T.barrier()

        done = False
        for l in range(depth):
            for s in range(nseq):
                m_phase(l, s)
            if stop_after == ("M", l):
                done = True
                break
            for s in range(nseq):
                (f_phase_sparse if SPARSE else f_phase)(l, s, last=(l == depth - 1 and stop_after is None))
            if stop_after == ("F", l):
                done = True
                break
        T.barrier()
        T.flush()
    return nc


def _bias_index_map():
    hp = np.arange(4)[:, None, None, None, None]
    p = np.arange(128)[None, :, None, None, None]
    eo = np.arange(2)[None, None, :, None, None]
    i = np.arange(14)[None, None, None, :, None]
    cq = np.arange(64)[None, None, None, None, :]
    kc = p % 64
    half = p // 64
    h = 2 * hp + eo
    dr = i + half
    dc = np.clip(kc - cq, -15, 15) + 15
    qs = np.clip(cq - 8, 0, 48)
    valid = (kc >= qs) & (kc < qs + 16)
    flat = h * (15 * 31 + 1) + np.where(valid, dr * 31 + dc, 15 * 31)
    return np.broadcast_to(flat, (4, 128, 2, 14, 64)).copy()


def _pool_consts():
    import ml_dtypes
    out = np.zeros((4, 7, 128, 128), np.float32)
    Sr = 512
    t = np.arange(Sr)
    for g, w in enumerate(POOL_W):
        lo = np.clip(t - w // 2, 0, Sr)
        hi = np.clip(t - w // 2 + w, 0, Sr)
        cnt = (hi - lo).astype(np.float64)
        A = np.zeros((Sr, Sr), np.float64)
        for tt_ in range(Sr):
            A[lo[tt_]:hi[tt_], tt_] = 1.0 / cnt[tt_]
        A -= np.eye(Sr)

        def blk(a, b):
            return A[a * 128:(a + 1) * 128, b * 128:(b + 1) * 128]

        def hilo(M):
            hi_ = M.astype(np.float32).astype(ml_dtypes.bfloat16).astype(np.float64)
            lo_ = (M - hi_).astype(np.float32).astype(ml_dtypes.bfloat16).astype(np.float64)
            return hi_, lo_

        out[g, 0] = blk(1, 2)
        out[g, 1] = blk(2, 2)
        out[g, 2] = blk(3, 2)
        out[g, 3], out[g, 4] = hilo(blk(0, 0))
        out[g, 5], out[g, 6] = hilo(blk(3, 3))
    return np.ascontiguousarray(out.reshape(28, 128, 128).transpose(1, 0, 2))


_BIAS_MAP = None
_PROG = {}


def prep_inputs(inp, nseq=NSEQ_FULL, ncores=NCORES):
    global _BIAS_MAP
    f = lambda a: np.ascontiguousarray(np.asarray(a, dtype=np.float32))
    Lw = L_FULL
    if _BIAS_MAP is None:
        _BIAS_MAP = _bias_index_map()
    rpb = f(inp["rpb"])
    pad = np.concatenate([rpb.reshape(Lw, 8, 15 * 31), np.full((Lw, 8, 1), NEG, np.float32)], axis=2).reshape(Lw, -1)
    biasT = pad[:, _BIAS_MAP]
    biasT = np.ascontiguousarray(biasT.reshape(Lw, 4, 128, 2 * 14 * 64))
    shared = {
        "w_in": f(inp["w_in"]), "w_out": f(inp["w_out"]), "pool_w": f(inp["pool_w"]),
        "w_gate": f(inp["w_gate"]), "w_up": f(inp["w_up"]), "w_down": f(inp["w_down"]),
        "gm": np.ascontiguousarray(f(inp["norm_mix_g"]).reshape(Lw, 8, 128).transpose(2, 0, 1).reshape(128, Lw * 8)),
        "gf": np.ascontiguousarray(f(inp["norm_ffn_g"]).reshape(Lw, 8, 128).transpose(2, 0, 1).reshape(128, Lw * 8)),
        "ps": np.ascontiguousarray(f(inp["pool_scale"]).reshape(Lw, 4, 128).transpose(2, 0, 1).reshape(128, Lw * 4)),
        "gF": np.ascontiguousarray(np.broadcast_to(f(inp["final_g"])[None, :], (128, D))),
        "wr": np.ascontiguousarray(np.concatenate([f(inp["w_router_group"]), f(inp["w_router_expert"])], axis=-1)
                                   .reshape(Lw, 8, 128, 20).transpose(2, 0, 1, 3)),
        "biasT": biasT,
        "poolA": _pool_consts(),
        "ident": np.eye(128, dtype=np.float32),
        "ustrict": np.triu(np.ones((128, 128), np.float32), k=1),
        "iotas": np.ascontiguousarray(np.concatenate([np.arange(128, dtype=np.float32)[:, None],
                                                      np.broadcast_to(np.arange(32, dtype=np.float32)[None, :], (128, 32))], axis=1)),
    }
    x = f(inp["x"]).reshape(-1, S, D)
    maps = []
    for c in range(ncores):
        m = dict(shared)
        m["x"] = np.ascontiguousarray(x[c * nseq:(c + 1) * nseq].reshape(nseq * S, D))
        maps.append(m)
    return maps


def kernel(x, norm_mix_g, w_in, rpb, pool_w, pool_scale, w_out, norm_ffn_g, w_router_group, w_router_expert,
           w_gate, w_up, w_down, final_g):
    inp = dict(x=x, norm_mix_g=norm_mix_g, w_in=w_in, rpb=rpb, pool_w=pool_w, pool_scale=pool_scale, w_out=w_out,
               norm_ffn_g=norm_ffn_g, w_router_group=w_router_group, w_router_expert=w_router_expert,
               w_gate=w_gate, w_up=w_up, w_down=w_down, final_g=final_g)
    maps = prep_inputs(inp)
    if "full" not in _PROG:
        _PROG["full"] = build_program()
    nc = _PROG["full"]
    res = run_bass_kernel_spmd(nc, maps, core_ids=list(range(NCORES)))
    outs = [np.asarray(r["out"], dtype=np.float32).reshape(NSEQ_FULL, S, D) for r in res.results]
    return np.concatenate(outs, axis=0)
```

```python
import numpy as np
from contextlib import ExitStack
import concourse.bass as bass
import concourse.mybir as mybir
from concourse.bass_utils import run_bass_kernel_spmd

F32 = mybir.dt.float32
BF16 = mybir.dt.bfloat16
AF = mybir.ActivationFunctionType
ALU = mybir.AluOpType
AX = mybir.AxisListType

D = 1024
S = 2048
L_FULL = 4
NSEQ_FULL = 4
NCORES = 8
NE = 16
DE = 512
EPS = 1e-6
NEG = -30000.0
POOL_W = (2, 4, 8, 16)


class Buf:
    __slots__ = ("name", "w", "r")

    def __init__(self, name):
        self.name = name
        self.w = None
        self.r = {}


class _Eng:
    def __init__(self, name):
        self.name = name
        self.q = []
        self.known = {}
        self.count = 0
        self.psem = None


class Tracker:
    ENGS = ("pe", "act", "dve", "pool", "sp")

    def __init__(self, nc, stack, n_dma_sems=24):
        self.nc = nc
        self.sems = []
        self.eng = {n: _Eng(n) for n in self.ENGS}
        for n in ("pe", "act", "dve", "pool"):
            self.eng[n].psem = self._new_sem(stack, "p_" + n)
        self.dpool = [self._new_sem(stack, f"dq{i}") for i in range(n_dma_sems)]
        self.dcnt = [0] * n_dma_sems
        self.dnext = 0
        self.stack = stack
        self.extra = []

    def _new_sem(self, stack, name):
        h = stack.enter_context(self.nc.semaphore(name))
        self.sems.append(h)
        return len(self.sems) - 1

    def _deps(self, E, reads, writes, extra=()):
        need = {}

        def req(ev):
            if ev is None:
                return
            s, v = ev
            if s == E.psem and E.name == "pe":
                return
            if E.known.get(s, 0) >= v:
                return
            if need.get(s, 0) < v:
                need[s] = v

        for ev in extra:
            req(ev)
        for b in reads:
            req(b.w)
        for b in writes:
            req(b.w)
            for s, v in b.r.items():
                req((s, v))
        for s, v in need.items():
            E.q.append(("wait", s, v))
            E.known[s] = v

    def _mark(self, ev, reads, writes):
        for b in reads:
            if b.r.get(ev[0], 0) < ev[1]:
                b.r[ev[0]] = ev[1]
        for b in writes:
            b.w = ev
            b.r = {}

    def emit(self, eng, fn, reads=(), writes=(), sig=True):
        E = self.eng[eng]
        self._deps(E, reads, writes)
        if sig:
            E.count += 1
            ev = (E.psem, E.count)
        else:
            ev = (E.psem, E.count + 1)
        E.q.append(("op", fn, sig))
        self._mark(ev, reads, writes)
        return ev

    def dma(self, q, out, in_, reads=(), writes=(), own_sem=False):
        E = self.eng[q]
        if own_sem:
            s = self._new_sem(self.stack, f"ds{len(self.sems)}")
            self.extra.append(s)
            prev = 0
            self._deps(E, reads, writes)
            ev = (s, 16)
        else:
            i = self.dnext
            self.dnext = (i + 1) % len(self.dpool)
            s = self.dpool[i]
            prev = self.dcnt[i]
            self._deps(E, reads, writes, extra=[(s, prev)] if prev else ())
            self.dcnt[i] += 16
            ev = (s, self.dcnt[i])
        E.q.append(("dma", out, in_, s))
        self._mark(ev, reads, writes)
        return ev

    def idma(self, out, out_idx, in_, in_idx, reads=(), writes=()):
        E = self.eng["pool"]
        i = self.dnext
        self.dnext = (i + 1) % len(self.dpool)
        s = self.dpool[i]
        prev = self.dcnt[i]
        self._deps(E, reads, writes, extra=[(s, prev)] if prev else ())
        self.dcnt[i] += 16
        ev = (s, self.dcnt[i])
        E.q.append(("idma", out, out_idx, in_, in_idx, s))
        self._mark(ev, reads, writes)
        return ev

    def barrier(self):
        evs = []
        for n in ("pe", "act", "dve", "pool"):
            e = self.eng[n]
            if e.count:
                evs.append((e.psem, e.count))
        for i, s in enumerate(self.dpool):
            if self.dcnt[i]:
                evs.append((s, self.dcnt[i]))
        for s in self.extra:
            evs.append((s, 16))
        for fn in getattr(self, "extra_ev_fns", []):
            evs.extend(fn())
        for n in self.ENGS:
            E = self.eng[n]
            for s, v in evs:
                if s == E.psem:
                    continue
                if E.known.get(s, 0) < v:
                    E.q.append(("wait", s, v))
                    E.known[s] = v

    def flush(self):
        nc = self.nc
        sems = self.sems

        def run(E, h):
            psem = sems[E.psem] if E.psem is not None else None
            for it in E.q:
                if it[0] == "wait":
                    h.wait_ge(sems[it[1]], it[2])
                elif it[0] == "op":
                    ins = it[1](h)
                    if it[2]:
                        ins.then_inc(psem, 1)
                elif it[0] == "idma":
                    oo = bass.IndirectOffsetOnAxis(ap=it[2], axis=0) if it[2] is not None else None
                    io = bass.IndirectOffsetOnAxis(ap=it[4], axis=0) if it[4] is not None else None
                    h.indirect_dma_start(out=it[1], out_offset=oo, in_=it[3], in_offset=io).then_inc(sems[it[5]], 16)
                else:
                    h.dma_start(out=it[1], in_=it[2]).then_inc(sems[it[3]], 16)

        with nc.Block() as block:
            @block.tensor
            def _(h):
                run(self.eng["pe"], h)

            @block.scalar
            def _(h):
                run(self.eng["act"], h)

            @block.vector
            def _(h):
                run(self.eng["dve"], h)

            @block.gpsimd
            def _(h):
                run(self.eng["pool"], h)

            @block.sync
            def _(h):
                run(self.eng["sp"], h)


SPARSE = True
TS = 256
NTILE = 32


def build_program(nseq=NSEQ_FULL, depth=L_FULL, stop_after=None, debug_out=False):
    nc = bass.Bass("TRN2", target_bir_lowering=False)
    NT = nseq * S
    Lw = L_FULL

    def din(name, shape, dt=F32):
        return nc.dram_tensor(name, list(shape), dt, kind="ExternalInput").ap()

    x_c = din("x", [NT, D])
    w_in = din("w_in", [Lw, D, 2048])
    w_out = din("w_out", [Lw, D, D])
    pool_w = din("pool_w", [Lw, 4, 128, 128])
    w_gate = din("w_gate", [Lw, NE, D, DE])
    w_up = din("w_up", [Lw, NE, D, DE])
    w_down = din("w_down", [Lw, NE, DE, D])
    gm_d = din("gm", [128, Lw * 8])
    gf_d = din("gf", [128, Lw * 8])
    ps_d = din("ps", [128, Lw * 4])
    gF_d = din("gF", [128, D])
    wr_d = din("wr", [128, Lw, 8, 20])
    bias_d = din("biasT", [Lw, 4, 128, 2 * 14 * 64])
    poolA_d = din("poolA", [128, 28, 128])
    ident_d = din("ident", [128, 128])
    ustrict_d = din("ustrict", [128, 128])
    iotas_d = din("iotas", [128, 33])
    if stop_after is None:
        out_c = nc.dram_tensor("out", [NT, D], F32, kind="ExternalOutput").ap()
        xT_d = nc.dram_tensor("xT_d", [8, 128, NT], F32, kind="Internal").ap()
    else:
        xT_d = nc.dram_tensor("xT_out", [8, 128, NT], F32, kind="ExternalOutput").ap()
        out_c = None
    w_in_b = nc.dram_tensor("w_in_b", [Lw, D, 2048], BF16, kind="Internal").ap()
    w_out_b = nc.dram_tensor("w_out_b", [Lw, D, D], BF16, kind="Internal").ap()
    pool_w_b = nc.dram_tensor("pool_w_b", [Lw, 4, 128, 128], BF16, kind="Internal").ap()
    w_gate_b = nc.dram_tensor("w_gate_b", [Lw, NE, D, DE], BF16, kind="Internal").ap()
    w_up_b = nc.dram_tensor("w_up_b", [Lw, NE, D, DE], BF16, kind="Internal").ap()
    w_down_b = nc.dram_tensor("w_down_b", [Lw, NE, DE, D], BF16, kind="Internal").ap()
    w_gate_p = [nc.dram_tensor(f"w_gate_p{i}", [NE * 128, 4096], BF16, kind="Internal").ap() for i in range(Lw)]
    w_up_p = [nc.dram_tensor(f"w_up_p{i}", [NE * 128, 4096], BF16, kind="Internal").ap() for i in range(Lw)]
    w_down_p = [nc.dram_tensor(f"w_down_p{i}", [NE * 128, 4096], BF16, kind="Internal").ap() for i in range(Lw)]
    HS_d = nc.dram_tensor("HS_d", [NTILE * TS, D], BF16, kind="Internal").ap()
    YS_d = nc.dram_tensor("YS_d", [NTILE * TS, D], F32, kind="Internal").ap()

    top = ExitStack()
    with top:
        T = Tracker(nc, top)

        uid = [0]

        def sb(stack, name, shape, dt):
            uid[0] += 1
            return stack.enter_context(nc.sbuf_tensor(f"{name}_s{uid[0]}", list(shape), dt))

        banks = [top.enter_context(nc.psum_tensor(f"bank{i}", [128, 512], F32)) for i in range(8)]
        BK = [Buf(f"bank{i}") for i in range(8)]

        onesb = sb(top, "onesb", [128, 128], BF16)
        identf = sb(top, "identf", [128, 128], F32)
        identb = sb(top, "identb", [128, 128], BF16)
        A_bf = sb(top, "A_bf", [128, 28, 128], BF16)
        gm = sb(top, "gm", [128, Lw * 8], F32)
        gf = sb(top, "gf", [128, Lw * 8], F32)
        psc = sb(top, "psc", [128, Lw * 4], F32)
        epsc = sb(top, "epsc", [128, 1], F32)
        ustr_f = sb(top, "ustr_f", [128, 128], F32)
        ustr = sb(top, "ustr", [128, 128], BF16)
        iotas = sb(top, "iotas", [128, 33], F32)
        CONST = Buf("const")
        with nc.sbuf_tensor("A_stage", [128, 28, 128], F32) as A_st:
            ASB = Buf("A_stage")
            T.emit("dve", lambda e: e.memset(onesb[:], 1.0), writes=[CONST])
            T.emit("dve", lambda e: e.memset(epsc[:], EPS), writes=[CONST])
            T.dma("sp", identf[:], ident_d, writes=[CONST])
            T.dma("sp", gm[:], gm_d, writes=[CONST])
            T.dma("sp", gf[:], gf_d, writes=[CONST])
            T.dma("sp", psc[:], ps_d, writes=[CONST])
            T.dma("sp", A_st[:], poolA_d, writes=[ASB])
            T.dma("sp", ustr_f[:], ustrict_d, writes=[CONST])
            T.dma("sp", iotas[:], iotas_d, writes=[CONST])
            T.emit("dve", lambda e: e.tensor_copy(out=ustr[:], in_=ustr_f[:]), reads=[CONST], writes=[CONST])
            T.emit("dve", lambda e: e.tensor_copy(out=identb[:], in_=identf[:]), reads=[CONST], writes=[CONST])
            T.emit("dve", lambda e: e.tensor_copy(out=A_bf[:], in_=A_st[:]), reads=[ASB], writes=[CONST])
            T.barrier()

        WB = {}

        cast_sems = [T._new_sem(top, f"cs{i}") for i in range(8)]
        cast_cnt = [0] * 8
        cast_i = [0]

        def cast(key, dst, src, n=0):
            WB[key] = Buf(str(key))
            E = T.eng["pool"]
            i = cast_i[0] % 8
            cast_i[0] += 1
            sm = cast_sems[i]
            if cast_cnt[i] and E.known.get(sm, 0) < cast_cnt[i]:
                E.q.append(("wait", sm, cast_cnt[i]))
                E.known[sm] = cast_cnt[i]
            cast_cnt[i] += 16
            E.q.append(("dma", dst, src, sm))
            WB[key].w = (sm, cast_cnt[i])

        for l in range(depth):
            cast(("in", l), w_in_b[l], w_in[l])
            cast(("pw", l), pool_w_b[l].rearrange("g (a b) d -> (g a) (b d)", b=16),
                 pool_w[l].rearrange("g (a b) d -> (g a) (b d)", b=16))
            cast(("out", l), w_out_b[l].rearrange("(r q) d -> r (q d)", q=2),
                 w_out[l].rearrange("(r q) d -> r (q d)", q=2))
            for e_ in range(NE):
                cast(("g", l, e_), w_gate_p[l][e_ * 128:(e_ + 1) * 128, :].rearrange("p (k f) -> p k f", f=512),
                     w_gate[l, e_].rearrange("(k p) f -> p k f", p=128))
                cast(("u", l, e_), w_up_p[l][e_ * 128:(e_ + 1) * 128, :].rearrange("p (k f) -> p k f", f=512),
                     w_up[l, e_].rearrange("(k p) f -> p k f", p=128))
                cast(("d", l, e_), w_down_p[l][e_ * 128:(e_ + 1) * 128, :].rearrange("p (k f) -> p k f", f=1024),
                     w_down[l, e_].rearrange("(k p) f -> p k f", p=128))

        XD = [[Buf(f"xd{s}_{t}") for t in range(4)] for s in range(nseq)]
        flip = [0]

        def evac_copy(out, in_, reads, writes, scale=None):
            flip[0] ^= 1
            if scale is not None or flip[0]:
                sc = 1.0 if scale is None else scale
                T.emit("act", lambda e: e.activation(out=out, in_=in_, func=AF.Copy, scale=sc), reads=reads, writes=writes)
            else:
                T.emit("dve", lambda e: e.tensor_copy(out=out, in_=in_), reads=reads, writes=writes)

        def mm(out, lhsT, rhs, start, stop, reads, writes, sig):
            T.emit("pe", lambda e: e.matmul(out, lhsT, rhs, start=start, stop=stop), reads=reads, writes=writes, sig=sig)

        def rstd_from_ss(out, ss_ap, reads, writes):
            T.emit("act", lambda e: e.activation(out=out, in_=ss_ap, func=AF.Sqrt, bias=epsc[:, 0:1], scale=1.0 / D),
                   reads=list(reads) + [CONST], writes=writes)
            T.emit("dve", lambda e: e.reciprocal(out=out, in_=out), reads=writes, writes=writes)

        with ExitStack() as st:
            xin = sb(st, "xin", [128, 2, 4, 1024], F32)
            xTs = sb(st, "xTs", [128, 2, 8, 512], F32)
            XIN = [Buf("xin0"), Buf("xin1")]
            XTS = [Buf("xts0"), Buf("xts1")]
            bi = 0
            for s in range(nseq):
                for t in range(4):
                    b = (s * 4 + t) % 2
                    t0 = s * S + t * 512
                    T.dma("sp", xin[:, b], x_c[t0:t0 + 512, :].rearrange("(j p) d -> p j d", p=128), writes=[XIN[b]])
                    for k in range(8):
                        bk = bi % 4
                        bi += 1
                        for j in range(4):
                            T.emit("pe", lambda e, bk=bk, j=j, k=k, b=b: e.transpose(
                                out=banks[bk][:, j * 128:(j + 1) * 128], in_=xin[:, b, j, k * 128:(k + 1) * 128],
                                identity=identf[:]), reads=[XIN[b], CONST], writes=[BK[bk]], sig=(j == 3))
                        evac_copy(xTs[:, b, k, :], banks[bk][:, :], [BK[bk]], [XTS[b]])
                    T.dma("sp", xT_d[:, :, t0:t0 + 512].rearrange("k p t -> p k t"), xTs[:, b], reads=[XTS[b]],
                          writes=[XD[s][t]])
            T.barrier()

        def m_phase(l, s):
            tok0 = s * S
            with ExitStack() as pm:
                H = sb(pm, "H", [128, 8, S], BF16)
                qT = sb(pm, "qT", [128, 4, S], BF16)
                kT = sb(pm, "kT", [128, 4, S], BF16)
                V2 = sb(pm, "V2", [128, 31, 4, 192], BF16)
                pT = sb(pm, "pT", [128, 4, S], BF16)
                pw_sb = sb(pm, "pw_sb", [128, 4, 128], BF16)
                HT = [Buf(f"H{i}") for i in range(8)]
                QB = [[Buf(f"q{m}_{t}") for t in range(4)] for m in range(4)]
                KB = [[Buf(f"k{m}_{t}") for t in range(4)] for m in range(4)]
                VB = [Buf(f"v{i}") for i in range(31)]
                VONES = Buf("vones")
                PB = [[Buf(f"p{g}_{t}") for t in range(4)] for g in range(4)]
                PW = Buf("pw")
                with ExitStack() as pb:
                    w_in_sb = sb(pb, "w_in_sb", [128, 8, 2048], BF16)
                    U2 = sb(pb, "U2", [128, 16, 512], BF16)
                    pooledT = sb(pb, "pooledT", [128, 2, 512], BF16)
                    sq = sb(pb, "sq", [128, 8, 256], BF16)
                    xt = sb(pb, "xt", [128, 1, 8, 256], F32)
                    rstd = sb(pb, "rstd", [128, 1, 256], F32)
                    WI = [Buf(f"wi{c}") for c in range(4)]
                    UB = [Buf(f"u{j}") for j in range(16)]
                    PLB = [Buf("pl0"), Buf("pl1")]
                    SQ = Buf("sq")
                    XT = [Buf("xt0"), Buf("xt1")]
                    RS = [Buf("rs0"), Buf("rs1")]
                    for c in range(4):
                        T.dma("sp", w_in_sb[:, :, c * 512:(c + 1) * 512],
                              w_in_b[l][:, c * 512:(c + 1) * 512].rearrange("(k p) f -> p k f", p=128),
                              reads=[WB[("in", l)]], writes=[WI[c]])
                    T.dma("sp", pw_sb[:], pool_w_b[l].rearrange("g c d -> c g d"), reads=[WB[("pw", l)]], writes=[PW])
                    T.emit("dve", lambda e: e.memset(V2[:, :, :, 64:128], 1.0), writes=[VONES])
                    def emit_norm(i):
                        b = 0
                        c0 = i * 256
                        T.dma("sp", xt[:, b], xT_d[:, :, tok0 + c0:tok0 + c0 + 256].rearrange("k p t -> p k t"),
                              reads=[XD[s][i // 2]], writes=[XT[b]])
                        T.emit("act", lambda e, b=b: e.activation(out=sq[:], in_=xt[:, b], func=AF.Square),
                               reads=[XT[b]], writes=[SQ])
                        bk = 6 + b
                        for k in range(8):
                            mm(banks[bk][:, 0:256], onesb[:], sq[:, k, :], k == 0, k == 7, [SQ, CONST], [BK[bk]], k == 7)
                        rstd_from_ss(rstd[:, b, :], banks[bk][:, 0:256], [BK[bk]], [RS[b]])
                        for k in range(8):
                            T.emit("dve", lambda e, b=b, k=k, c0=c0: e.scalar_tensor_tensor(
                                out=H[:, k, c0:c0 + 256], in0=xt[:, b, k, :], scalar=gm[:, l * 8 + k:l * 8 + k + 1],
                                in1=rstd[:, b, :], op0=ALU.mult, op1=ALU.mult),
                                reads=[XT[b], RS[b], CONST], writes=[HT[i]])
                    bi_ = [0]

                    def emit_qkproj(t):
                        bi = bi_[0]
                        for m in range(8):
                            bk = bi % 4
                            bi += 1
                            for k in range(8):
                                mm(banks[bk][:, :], w_in_sb[:, k, m * 128:(m + 1) * 128], H[:, k, t * 512:(t + 1) * 512],
                                   k == 0, k == 7, [WI[m // 4], HT[2 * t], HT[2 * t + 1]], [BK[bk]], k == 7)
                            if m < 4:
                                evac_copy(qT[:, m, t * 512:(t + 1) * 512], banks[bk][:, :], [BK[bk]], [QB[m][t]], scale=0.125)
                            else:
                                T.emit("dve", lambda e, bk=bk, m=m, t=t: e.tensor_copy(
                                    out=kT[:, m - 4, t * 512:(t + 1) * 512], in_=banks[bk][:, :]),
                                    reads=[BK[bk]], writes=[KB[m - 4][t]])
                        bi_[0] = bi

                    for t in range(4):
                        emit_norm(2 * t)
                        emit_norm(2 * t + 1)
                        if t > 0:
                            emit_qkproj(t - 1)
                    emit_qkproj(3)
                    bi = bi_[0]
                    for sidx in range(31):
                        bk = bi % 4
                        bi += 1
                        a0 = 64 * sidx
                        hts = sorted({a0 // 256, (a0 + 127) // 256})
                        for k in range(8):
                            mm(banks[bk][:, :], H[:, k, a0:a0 + 128], w_in_sb[:, k, 1024:1536], k == 0, k == 7,
                               [WI[2]] + [HT[i] for i in hts], [BK[bk]], k == 7)
                        evac_copy(V2[:, sidx, :, :].rearrange("p a (b d) -> p a b d", d=64)[:, :, 0:3:2, :],
                                  banks[bk][:, :].rearrange("p (a b d) -> p a b d", b=2, d=64), [BK[bk]], [VB[sidx]])
                    for j in range(16):
                        bk = bi % 4
                        bi += 1
                        for k in range(8):
                            mm(banks[bk][:, :], H[:, k, j * 128:(j + 1) * 128], w_in_sb[:, k, 1536:2048], k == 0, k == 7,
                               [WI[3], HT[j // 2]], [BK[bk]], k == 7)
                        evac_copy(U2[:, j, :], banks[bk][:, :], [BK[bk]], [UB[j]])
                    for t in range(4):
                        for g in range(4):
                            pbuf = g % 2
                            bk = bi % 4
                            bi += 1
                            for Tq in range(4):
                                Tt = 4 * t + Tq
                                terms = []
                                if Tt > 0:
                                    terms.append((Tt - 1, 0))
                                if Tt == 0:
                                    terms += [(Tt, 3), (Tt, 4)]
                                elif Tt == 15:
                                    terms += [(Tt, 5), (Tt, 6)]
                                else:
                                    terms.append((Tt, 1))
                                if Tt < 15:
                                    terms.append((Tt + 1, 2))
                                for ti, (tp, var) in enumerate(terms):
                                    mm(banks[bk][:, Tq * 128:(Tq + 1) * 128], U2[:, tp, g * 128:(g + 1) * 128],
                                       A_bf[:, g * 7 + var, :], ti == 0, ti == len(terms) - 1,
                                       [UB[tp], CONST], [BK[bk]], (Tq == 3 and ti == len(terms) - 1))
                            T.emit("act", lambda e, bk=bk, pbuf=pbuf, g=g: e.activation(
                                out=pooledT[:, pbuf, :], in_=banks[bk][:, :], func=AF.Copy),
                                reads=[BK[bk]], writes=[PLB[pbuf]])
                            bk2 = 4 + (g % 2)
                            mm(banks[bk2][:, :], pw_sb[:, g, :], pooledT[:, pbuf, :], True, True, [PW, PLB[pbuf]], [BK[bk2]], True)
                            T.emit("dve", lambda e, bk2=bk2, g=g, t=t: e.tensor_scalar(
                                out=pT[:, g, t * 512:(t + 1) * 512], in0=banks[bk2][:, :],
                                scalar1=psc[:, l * 4 + g:l * 4 + g + 1], scalar2=None, op0=ALU.mult),
                                reads=[BK[bk2], CONST], writes=[PB[g][t]])
                    T.barrier()
                with ExitStack() as pc:
                    bias = sb(pc, "bias", [128, 4, 2, 14, 64], F32)
                    Sb = sb(pc, "Sb", [128, 4, 2, 4, 64], F32)
                    Pm = sb(pc, "Pm", [128, 4, 2, 4, 64], BF16)
                    Rr = sb(pc, "Rr", [128, 2, 4, 64], F32)
                    w_out_sb = sb(pc, "w_out_sb", [128, 8, D], BF16)
                    xt2 = sb(pc, "xt2", [128, 2, 8, 128], F32)
                    BIAS = [Buf(f"bias{hp}") for hp in range(4)]
                    SBB = [Buf(f"sb{i}") for i in range(4)]
                    PMB = [Buf(f"pm{i}") for i in range(4)]
                    RRB = [Buf("rr0"), Buf("rr1")]
                    WO = [Buf("wo0"), Buf("wo1")]
                    XT2 = [Buf("xt20"), Buf("xt21")]
                    for hp in range(4):
                        T.dma("sp", bias[:, hp].rearrange("p a b c -> p (a b c)"), bias_d[l, hp], writes=[BIAS[hp]])
                        T.emit("act", lambda e, hp=hp: e.activation(out=bias[:, hp].rearrange("p a b c -> p (a b c)"),
                                                                    in_=bias[:, hp].rearrange("p a b c -> p (a b c)"), func=AF.Exp),
                               reads=[BIAS[hp]], writes=[BIAS[hp]])
                    for c in range(2):
                        T.dma("sp", w_out_sb[:, :, c * 512:(c + 1) * 512],
                              w_out_b[l][:, c * 512:(c + 1) * 512].rearrange("(k p) f -> p k f", p=128),
                              reads=[WB[("out", l)]], writes=[WO[c]])
                    units = [(r, hp) for r in range(32) for hp in range(4)]

                    def rstart(r):
                        return min(max(r - 4, 0), 24)

                    def emit_qk(ui):
                        r, hp = units[ui]
                        u4 = ui % 4
                        rs_ = rstart(r)
                        i0 = rs_ - r + 7
                        kts = sorted({(rs_ * 64) // 512, (rs_ * 64 + 511) // 512})
                        for half in (0, 1):
                            bk = (u4 // 2) * 2 + half
                            cb = (u4 % 2) * 256
                            p0 = 64 * half
                            for c in range(4):
                                ks = (rs_ + 2 * c) * 64
                                mm(banks[bk][:, cb + c * 64:cb + (c + 1) * 64], kT[p0:p0 + 64, hp, ks:ks + 128],
                                   qT[p0:p0 + 64, hp, r * 64:(r + 1) * 64], True, True,
                                   [KB[hp][t] for t in kts] + [QB[hp][r // 8]], [BK[bk]], c == 3)
                            T.emit("act", lambda e, bk=bk, cb=cb, u4=u4, half=half: e.activation(
                                out=Sb[:, u4, half], in_=banks[bk][:, cb:cb + 256].rearrange("p (c q) -> p c q", q=64),
                                func=AF.Exp), reads=[BK[bk]], writes=[SBB[u4]])
                        T.emit("dve", lambda e, u4=u4, hp=hp, i0=i0: e.tensor_tensor(
                            out=Pm[:, u4], in0=Sb[:, u4], in1=bias[:, hp, :, i0:i0 + 7:2, :], op=ALU.mult),
                            reads=[SBB[u4], BIAS[hp]], writes=[PMB[u4]])

                    def emit_pv(ui):
                        r, hp = units[ui]
                        u2 = ui % 4
                        rs_ = rstart(r)
                        ob = 4 + (r % 2)
                        for half in (0, 1):
                            h = 2 * hp + half
                            for c in range(4):
                                sidx = rs_ + 2 * c
                                lhsT = V2[:, sidx, hp, 64 * half:64 * half + 128]
                                col = (hp * 2 + half) * 64
                                mm(banks[ob][:, col:col + 64], lhsT, Pm[:, u2, half, c, :], c == 0, c == 3,
                                   [VB[sidx], VONES, PMB[u2]], [BK[ob]], c == 3)
                        if hp == 3:
                            rb = r % 2
                            Ov = banks[ob][:, :].rearrange("p (a b q) -> p a b q", b=2, q=64)
                            T.emit("dve", lambda e, rb=rb, ob=ob: e.reciprocal(
                                out=Rr[0:64, rb], in_=banks[ob][64:128, :].rearrange("p (a b q) -> p a b q", b=2, q=64)[:, :, 0, :]),
                                reads=[BK[ob]], writes=[RRB[rb]])
                            T.emit("dve", lambda e, rb=rb, ob=ob: e.reciprocal(
                                out=Rr[64:128, rb], in_=banks[ob][0:64, :].rearrange("p (a b q) -> p a b q", b=2, q=64)[:, :, 1, :]),
                                reads=[BK[ob]], writes=[RRB[rb]])
                            T.emit("dve", lambda e, rb=rb, ob=ob, r=r: e.tensor_tensor(
                                out=H[0:64, 0:4, r * 64:(r + 1) * 64],
                                in0=banks[ob][0:64, :].rearrange("p (a b q) -> p a b q", b=2, q=64)[:, :, 0, :],
                                in1=Rr[0:64, rb], op=ALU.mult), reads=[BK[ob], RRB[rb]], writes=[HT[r // 4]])
                            T.emit("dve", lambda e, rb=rb, ob=ob, r=r: e.tensor_tensor(
                                out=H[64:128, 0:4, r * 64:(r + 1) * 64],
                                in0=banks[ob][64:128, :].rearrange("p (a b q) -> p a b q", b=2, q=64)[:, :, 1, :],
                                in1=Rr[64:128, rb], op=ALU.mult), reads=[BK[ob], RRB[rb]], writes=[HT[r // 4]])

                    for ui in range(len(units)):
                        emit_qk(ui)
                        if ui > 1:
                            emit_pv(ui - 2)
                    emit_pv(len(units) - 2)
                    emit_pv(len(units) - 1)
                    for i in range(16):
                        b = i % 2
                        c0 = i * 128
                        T.dma("sp", xt2[:, b], xT_d[:, :, tok0 + c0:tok0 + c0 + 128].rearrange("k p t -> p k t"),
                              reads=[XD[s][i // 4]], writes=[XT2[b]])
                        for m in range(8):
                            bk = 6 + (m % 2)
                            for k in range(8):
                                rhs = H[:, k, c0:c0 + 128] if k < 4 else pT[:, k - 4, c0:c0 + 128]
                                rd = [HT[i // 2]] if k < 4 else [PB[k - 4][i // 4]]
                                mm(banks[bk][:, 0:128], w_out_sb[:, k, m * 128:(m + 1) * 128], rhs, k == 0, k == 7,
                                   [WO[m // 4]] + rd, [BK[bk]], k == 7)
                            T.emit("dve", lambda e, b=b, m=m, bk=bk: e.tensor_tensor(
                                out=xt2[:, b, m, :], in0=banks[bk][:, 0:128], in1=xt2[:, b, m, :], op=ALU.add),
                                reads=[BK[bk], XT2[b]], writes=[XT2[b]])
                        T.dma("sp", xT_d[:, :, tok0 + c0:tok0 + c0 + 128].rearrange("k p t -> p k t"), xt2[:, b],
                              reads=[XT2[b]], writes=[XD[s][i // 4]])
                    T.barrier()

        def final_out(xT, XB, s):
            tok0 = s * S
            if True:
                if True:
                    with ExitStack() as po:
                        gF = sb(po, "gF", [128, D], F32)
                        ost = sb(po, "ost", [128, 1, D], F32)
                        junk = sb(po, "junk", [128, 512], BF16)
                        ssA = sb(po, "ssA", [128, 2], F32)
                        rsF = sb(po, "rsF", [128, 1], F32)
                        GF, OST, JK, SSA, RSF = Buf("gF"), [Buf("ost0"), Buf("ost1")], Buf("junk"), Buf("ssA"), Buf("rsF")
                        T.dma("sp", gF[:], gF_d, writes=[GF])
                        for j in range(16):
                            ob = 0
                            t = j // 4
                            for hb in range(2):
                                bk = 2 * (j % 2) + hb
                                for kk in range(4):
                                    k = hb * 4 + kk
                                    T.emit("pe", lambda e, bk=bk, kk=kk, k=k, j=j: e.transpose(
                                        out=banks[bk][:, kk * 128:(kk + 1) * 128], in_=xT[:, k, j * 128:(j + 1) * 128],
                                        identity=identf[:]), reads=[XB[k][t], CONST], writes=[BK[bk]], sig=(kk == 3))
                                T.emit("act", lambda e, bk=bk, hb=hb: e.activation(
                                    out=junk[:], in_=banks[bk][:, :], func=AF.Square, accum_out=ssA[:, hb:hb + 1]),
                                    reads=[BK[bk]], writes=[JK, SSA])
                            T.emit("dve", lambda e: e.tensor_tensor(out=rsF[:], in0=ssA[:, 0:1], in1=ssA[:, 1:2], op=ALU.add),
                                   reads=[SSA], writes=[RSF])
                            rstd_from_ss(rsF[:], rsF[:], [RSF], [RSF])
                            for hb in range(2):
                                bk = 2 * (j % 2) + hb
                                T.emit("dve", lambda e, bk=bk, hb=hb, ob=ob: e.scalar_tensor_tensor(
                                    out=ost[:, ob, hb * 512:(hb + 1) * 512], in0=banks[bk][:, :], scalar=rsF[:, 0:1],
                                    in1=gF[:, hb * 512:(hb + 1) * 512], op0=ALU.mult, op1=ALU.mult),
                                    reads=[BK[bk], RSF, GF], writes=[OST[ob]])
                            T.dma("sp", out_c[tok0 + j * 128:tok0 + (j + 1) * 128, :], ost[:, ob, :], reads=[OST[ob]],
                                  writes=[XD[s][t]])

        def f_phase_sparse(l, s, last):
            U32 = mybir.dt.uint32
            tok0 = s * S
            with ExitStack() as pf:
                xT = sb(pf, "xT", [128, 8, S], F32)
                ring = sb(pf, "ring", [128, 6, 4096], BF16)
                wr_sb = sb(pf, "wr_sb", [128, 8, 20], F32)
                wrp = sb(pf, "wrp", [128, 8, 20], F32)
                rt = sb(pf, "rt", [128, 16], F32)
                LGs = sb(pf, "LGs", [128, 16, 20], F32)
                r1 = sb(pf, "r1", [128, 16, 16], F32)
                gmax = sb(pf, "gmax", [128, 16], F32)
                goh = sb(pf, "goh", [128, 16, 4], F32)
                gex = sb(pf, "gex", [128, 16, 4], F32)
                gw = sb(pf, "gw", [128, 16], F32)
                esel = sb(pf, "esel", [128, 16, 4], F32)
                em = sb(pf, "em", [128, 16, 4], F32)
                m1 = sb(pf, "m1", [128, 16], F32)
                m2 = sb(pf, "m2", [128, 16], F32)
                oh1 = sb(pf, "oh1", [128, 16, 4], F32)
                oh2 = sb(pf, "oh2", [128, 16, 4], F32)
                w1 = sb(pf, "w1", [128, 16], F32)
                w2 = sb(pf, "w2", [128, 16], F32)
                M1 = sb(pf, "M1", [128, 16, 16], F32)
                M2 = sb(pf, "M2", [128, 16, 16], F32)
                Mb = sb(pf, "Mb", [128, 256], BF16)
                TOT = sb(pf, "TOT", [128, 16, 16], F32)
                JP = sb(pf, "JP", [128, 16, 16], F32)
                SL = sb(pf, "SL", [128, 16, 16], F32)
                ne = sb(pf, "ne", [128, 16], F32)
                ntl = sb(pf, "ntl", [128, 16], F32)
                base = sb(pf, "base", [128, 16], F32)
                bend = sb(pf, "bend", [128, 16], F32)
                b256 = sb(pf, "b256", [128, 16], F32)
                sl1 = sb(pf, "sl1", [128, 16], F32)
                sl2 = sb(pf, "sl2", [128, 16], F32)
                s1u = sb(pf, "s1u", [128, 16], U32)
                s2u = sb(pf, "s2u", [128, 16], U32)
                eidx = sb(pf, "eidx", [128, NTILE], F32)
                widf = sb(pf, "widf", [128, NTILE], F32)
                widx = sb(pf, "widx", [128, NTILE], U32)
                XB = [[Buf(f"X{k}_{t}") for t in range(4)] for k in range(8)]
                RSLOT = [Buf(f"ring{i}") for i in range(6)]
                WR, WRP = Buf("wr"), Buf("wrp")
                ROUT = Buf("router")
                HSB = [Buf(f"hs{i}") for i in range(32)]
                YSB = [Buf(f"ys{i}") for i in range(NTILE)]

                def load_tile_w(i, which=(0, 1, 2)):
                    for j, wp in enumerate((w_gate_p, w_up_p, w_down_p)):
                        if j not in which:
                            continue
                        slot = (3 * i + j) % 6
                        T.idma(ring[:, slot, :], None, wp[l], widx[:, i:i + 1],
                               reads=[ROUT] + [WB[(("g", "u", "d")[j], l, e_)] for e_ in range(NE)], writes=[RSLOT[slot]])

                for k in range(8):
                    T.dma("sp", xT[:, k, :], xT_d[k, :, tok0:tok0 + S], reads=[XD[s][t] for t in range(4)],
                          writes=[XB[k][t] for t in range(4)])
                T.dma("sp", wr_sb[:], wr_d[:, l], writes=[WR])
                for k in range(8):
                    T.emit("dve", lambda e, k=k: e.tensor_scalar(out=wrp[:, k, :], in0=wr_sb[:, k, :],
                                                                 scalar1=gf[:, l * 8 + k:l * 8 + k + 1], scalar2=None,
                                                                 op0=ALU.mult), reads=[WR, CONST], writes=[WRP])

                def dv(fn, extra_reads=()):
                    T.emit("dve", fn, reads=[ROUT] + list(extra_reads), writes=[ROUT])

                def b3(ap2, n):
                    return ap2.unsqueeze(2).broadcast_to([128, 16, n])

                with ExitStack() as pa:
                    H = sb(pa, "Hf", [128, 8, S], BF16)
                    sq = sb(pa, "sqf", [128, 8, 512], BF16)
                    rstd = sb(pa, "rstdf", [128, 512], F32)
                    hrow = sb(pa, "hrow", [128, 2, D], BF16)
                    HB = [Buf(f"Hf{t}") for t in range(4)]
                    SQ, RS = Buf("sq"), Buf("rs")
                    HROW = [Buf("hrow0"), Buf("hrow1")]
                    for t in range(4):
                        c0 = t * 512
                        T.emit("act", lambda e, c0=c0: e.activation(out=sq[:], in_=xT[:, :, c0:c0 + 512], func=AF.Square),
                               reads=[XB[k][t] for k in range(8)], writes=[SQ])
                        for k in range(8):
                            mm(banks[6][:, :], onesb[:], sq[:, k, :], k == 0, k == 7, [SQ, CONST], [BK[6]], k == 7)
                        rstd_from_ss(rstd[:], banks[6][:, :], [BK[6]], [RS])
                        for k in range(8):
                            T.emit("dve", lambda e, k=k, c0=c0: e.scalar_tensor_tensor(
                                out=H[:, k, c0:c0 + 512], in0=xT[:, k, c0:c0 + 512], scalar=gf[:, l * 8 + k:l * 8 + k + 1],
                                in1=rstd[:], op0=ALU.mult, op1=ALU.mult), reads=[XB[k][t], RS, CONST], writes=[HB[t]])
                        for j in range(4):
                            jj = 4 * t + j
                            for k in range(8):
                                mm(banks[7][:, 320 + jj:320 + jj + 1], sq[:, k, j * 128:(j + 1) * 128], onesb[:, 0:1],
                                   k == 0, k == 7, [SQ, CONST], [BK[7]], False)
                            for k in range(8):
                                mm(banks[7][:, jj * 20:(jj + 1) * 20], xT[:, k, c0 + j * 128:c0 + (j + 1) * 128], wrp[:, k, :],
                                   k == 0, k == 7, [XB[k][t], WRP], [BK[7]], (k == 7))
                    rstd_from_ss(rt[:], banks[7][:, 320:336], [BK[7], ROUT], [ROUT])
                    dv(lambda e: e.tensor_tensor(out=LGs[:], in0=banks[7][:, 0:320].rearrange("p (j e) -> p j e", e=20),
                                                 in1=b3(rt[:], 20), op=ALU.mult), [BK[7]])
                    dv(lambda e: e.tensor_reduce(out=gmax[:], in_=LGs[:, :, 0:4], axis=AX.X, op=ALU.max))
                    dv(lambda e: e.tensor_tensor(out=goh[:], in0=LGs[:, :, 0:4], in1=b3(gmax[:], 4), op=ALU.is_equal))
                    dv(lambda e: e.tensor_tensor(out=gex[:], in0=LGs[:, :, 0:4], in1=b3(gmax[:], 4), op=ALU.subtract))
                    T.emit("act", lambda e: e.activation(out=gex[:], in_=gex[:], func=AF.Exp), reads=[ROUT], writes=[ROUT])
                    dv(lambda e: e.tensor_reduce(out=gw[:], in_=gex[:], axis=AX.X, op=ALU.add))
                    dv(lambda e: e.reciprocal(out=gw[:], in_=gw[:]))
                    dv(lambda e: e.tensor_tensor(out=r1[:].rearrange("p j (g i) -> p j g i", i=4),
                                                 in0=LGs[:, :, 4:20].rearrange("p j (g i) -> p j g i", i=4),
                                                 in1=goh[:].unsqueeze(3).broadcast_to([128, 16, 4, 4]), op=ALU.mult))
                    dv(lambda e: e.tensor_reduce(out=esel[:], in_=r1[:].rearrange("p j (g i) -> p j i g", i=4),
                                                 axis=AX.X, op=ALU.add))
                    dv(lambda e: e.tensor_reduce(out=m1[:], in_=esel[:], axis=AX.X, op=ALU.max))
                    dv(lambda e: e.tensor_tensor(out=oh1[:], in0=esel[:], in1=b3(m1[:], 4), op=ALU.is_equal))
                    dv(lambda e: e.scalar_tensor_tensor(out=em[:], in0=oh1[:], scalar=-1.0e30, in1=esel[:],
                                                        op0=ALU.mult, op1=ALU.add))
                    dv(lambda e: e.tensor_reduce(out=m2[:], in_=em[:], axis=AX.X, op=ALU.max))
                    dv(lambda e: e.tensor_tensor(out=oh2[:], in0=em[:], in1=b3(m2[:], 4), op=ALU.is_equal))
                    dv(lambda e: e.tensor_tensor(out=w2[:], in0=m2[:], in1=m1[:], op=ALU.subtract))
                    T.emit("act", lambda e: e.activation(out=w2[:], in_=w2[:], func=AF.Exp), reads=[ROUT], writes=[ROUT])
                    dv(lambda e: e.tensor_scalar(out=w2[:], in0=w2[:], scalar1=1.0, scalar2=None, op0=ALU.add))
                    dv(lambda e: e.reciprocal(out=w1[:], in_=w2[:]))
                    dv(lambda e: e.tensor_tensor(out=w1[:], in0=w1[:], in1=gw[:], op=ALU.mult))
                    dv(lambda e: e.tensor_tensor(out=w2[:], in0=gw[:], in1=w1[:], op=ALU.subtract))
                    g44 = lambda ap: ap.rearrange("p j (g i) -> p j g i", i=4)
                    dv(lambda e: e.tensor_tensor(out=g44(M1[:]), in0=goh[:].unsqueeze(3).broadcast_to([128, 16, 4, 4]),
                                                 in1=oh1[:].unsqueeze(2).broadcast_to([128, 16, 4, 4]), op=ALU.mult))
                    dv(lambda e: e.tensor_tensor(out=g44(M2[:]), in0=goh[:].unsqueeze(3).broadcast_to([128, 16, 4, 4]),
                                                 in1=oh2[:].unsqueeze(2).broadcast_to([128, 16, 4, 4]), op=ALU.mult))
                    dv(lambda e: e.tensor_tensor(out=Mb[:].rearrange("p (j e) -> p j e", e=16), in0=M1[:], in1=M2[:], op=ALU.add))
                    mm(banks[6][:, 0:256], ustr[:], Mb[:], True, True, [ROUT, CONST], [BK[6]], True)
                    mm(banks[7][:, 0:256], onesb[:], Mb[:], True, True, [ROUT, CONST], [BK[7]], True)
                    dv(lambda e: e.tensor_copy(out=TOT[:].rearrange("p j e -> p (j e)"), in_=banks[7][:, 0:256]), [BK[7]])
                    dv(lambda e: e.memset(JP[:, 0, :], 0.0))
                    for j in range(1, 16):
                        dv(lambda e, j=j: e.tensor_tensor(out=JP[:, j, :], in0=JP[:, j - 1, :], in1=TOT[:, j - 1, :], op=ALU.add))
                    dv(lambda e: e.tensor_tensor(out=ne[:], in0=JP[:, 15, :], in1=TOT[:, 15, :], op=ALU.add))
                    dv(lambda e: e.tensor_scalar(out=ntl[:], in0=ne[:], scalar1=0.0, scalar2=None, op0=ALU.is_gt))
                    for q in range(1, S // TS):
                        dv(lambda e, q=q: e.scalar_tensor_tensor(out=ntl[:], in0=ne[:], scalar=float(TS * q), in1=ntl[:],
                                                                 op0=ALU.is_gt, op1=ALU.add))
                    dv(lambda e: e.memset(base[:, 0:1], 0.0))
                    for e_ in range(1, 16):
                        dv(lambda e, e_=e_: e.tensor_tensor(out=base[:, e_:e_ + 1], in0=base[:, e_ - 1:e_],
                                                            in1=ntl[:, e_ - 1:e_], op=ALU.add))
                    dv(lambda e: e.tensor_tensor(out=bend[:], in0=base[:], in1=ntl[:], op=ALU.add))
                    dv(lambda e: e.tensor_scalar(out=b256[:], in0=base[:], scalar1=float(TS), scalar2=None, op0=ALU.mult))
                    dv(lambda e: e.tensor_tensor(out=SL[:], in0=banks[6][:, 0:256].rearrange("p (j e) -> p j e", e=16),
                                                 in1=JP[:], op=ALU.add), [BK[6]])
                    dv(lambda e: e.tensor_tensor(out=SL[:], in0=SL[:], in1=b256[:].unsqueeze(1).broadcast_to([128, 16, 16]),
                                                 op=ALU.add))
                    dv(lambda e: e.tensor_tensor(out=M1[:], in0=M1[:], in1=SL[:], op=ALU.mult))
                    dv(lambda e: e.tensor_tensor(out=M2[:], in0=M2[:], in1=SL[:], op=ALU.mult))
                    dv(lambda e: e.tensor_reduce(out=sl1[:], in_=M1[:], axis=AX.X, op=ALU.add))
                    dv(lambda e: e.tensor_reduce(out=sl2[:], in_=M2[:], axis=AX.X, op=ALU.add))
                    dv(lambda e: e.tensor_copy(out=s1u[:], in_=sl1[:]))
                    dv(lambda e: e.tensor_copy(out=s2u[:], in_=sl2[:]))
                    dv(lambda e: e.memset(eidx[:], 0.0))
                    for e_ in range(16):
                        dv(lambda e, e_=e_: e.scalar_tensor_tensor(out=eidx[:], in0=iotas[:, 1:1 + NTILE], scalar=bend[:, e_:e_ + 1],
                                                                   in1=eidx[:], op0=ALU.is_ge, op1=ALU.add), [CONST])
                    dv(lambda e: e.tensor_scalar(out=eidx[:], in0=eidx[:], scalar1=15.0, scalar2=None, op0=ALU.min))
                    dv(lambda e: e.tensor_scalar(out=widf[:], in0=eidx[:], scalar1=128.0, scalar2=iotas[:, 0:1],
                                                 op0=ALU.mult, op1=ALU.add), [CONST])
                    dv(lambda e: e.tensor_copy(out=widx[:], in_=widf[:]))
                    load_tile_w(0)
                    load_tile_w(1, which=(0, 1))
                    for j in range(16):
                        hb = j % 2
                        bk = 4 + hb
                        pv = banks[bk][:, :].bitcast(BF16)
                        for k in range(8):
                            T.emit("pe", lambda e, pv=pv, k=k, j=j: e.transpose(
                                out=pv[:, k * 128:(k + 1) * 128], in_=H[:, k, j * 128:(j + 1) * 128], identity=identb[:]),
                                reads=[HB[j // 4], CONST], writes=[BK[bk]], sig=(k == 7))
                        evac_copy(hrow[:, hb, :], pv, [BK[bk]], [HROW[hb]])
                        T.idma(HS_d, s1u[:, j:j + 1], hrow[:, hb, :], None, reads=[HROW[hb], ROUT], writes=[HSB[2 * j]])
                        T.idma(HS_d, s2u[:, j:j + 1], hrow[:, hb, :], None, reads=[HROW[hb], ROUT], writes=[HSB[2 * j + 1]])
                    T.barrier()

                with ExitStack() as pbx:
                    hs_sb = sb(pbx, "hs_sb", [128, 3, 2, D], BF16)
                    hcT = sb(pbx, "hcT", [128, 2, 8, TS], BF16)
                    a_e = sb(pbx, "a_e", [128, 2, 4, TS], BF16)
                    sg = sb(pbx, "sg", [128, 2, TS], F32)
                    ys = sb(pbx, "ys", [128, 2, 2, D], F32)
                    HSS = [Buf("hss0"), Buf("hss1"), Buf("hss2")]
                    HCT = [Buf("hct0"), Buf("hct1")]
                    AE = [[Buf(f"ae{b}_{f}") for f in range(4)] for b in range(2)]
                    SG = [Buf("sg0"), Buf("sg1")]
                    YS = [Buf("ysb0"), Buf("ysb1")]

                    def emit_hs(i):
                        hb3 = i % 3
                        T.dma("sp", hs_sb[:, hb3], HS_d[i * TS:(i + 1) * TS, :].rearrange("(a p) d -> p a d", p=128),
                              reads=HSB, writes=[HSS[hb3]])

                    def emit_tr(i):
                        ab = i % 2
                        hb3 = i % 3
                        for hb in range(2):
                            bk = 6 + hb
                            pv = banks[bk][:, :].bitcast(BF16)
                            for kk in range(4):
                                k = hb * 4 + kk
                                for a in range(2):
                                    T.emit("pe", lambda e, pv=pv, kk=kk, a=a, k=k, hb3=hb3: e.transpose(
                                        out=pv[:, kk * TS + a * 128:kk * TS + (a + 1) * 128],
                                        in_=hs_sb[:, hb3, a, k * 128:(k + 1) * 128], identity=identb[:]),
                                        reads=[HSS[hb3], CONST], writes=[BK[bk]], sig=(kk == 3 and a == 1))
                            evac_copy(hcT[:, ab, hb * 4:(hb + 1) * 4, :].rearrange("p k t -> p (k t)"), pv, [BK[bk]], [HCT[ab]])

                    def emit_gu(i):
                        ab = i % 2
                        sg_, su_ = (3 * i) % 6, (3 * i + 1) % 6
                        for f in range(4):
                            bg, bu = f % 2, 2 + f % 2
                            for k in range(8):
                                mm(banks[bg][:, 0:TS], ring[:, sg_, k * 512 + f * 128:k * 512 + (f + 1) * 128], hcT[:, ab, k, :],
                                   k == 0, k == 7, [RSLOT[sg_], HCT[ab]], [BK[bg]], k == 7)
                            for k in range(8):
                                mm(banks[bu][:, 0:TS], ring[:, su_, k * 512 + f * 128:k * 512 + (f + 1) * 128], hcT[:, ab, k, :],
                                   k == 0, k == 7, [RSLOT[su_], HCT[ab]], [BK[bu]], k == 7)
                            i2 = f % 2
                            T.emit("act", lambda e, bg=bg, i2=i2: e.activation(out=sg[:, i2, :], in_=banks[bg][:, 0:TS], func=AF.Silu),
                                   reads=[BK[bg]], writes=[SG[i2]])
                            T.emit("dve", lambda e, bu=bu, i2=i2, ab=ab, f=f: e.tensor_tensor(
                                out=a_e[:, ab, f, :], in0=banks[bu][:, 0:TS], in1=sg[:, i2, :], op=ALU.mult),
                                reads=[BK[bu], SG[i2]], writes=[AE[ab][f]])

                    def emit_d(i):
                        ab = i % 2
                        sd_ = (3 * i + 2) % 6
                        for a in range(2):
                            for dh in range(2):
                                bk = 4 + dh
                                for f in range(4):
                                    mm(banks[bk][:, :], a_e[:, ab, f, a * 128:(a + 1) * 128],
                                       ring[:, sd_, f * 1024 + dh * 512:f * 1024 + (dh + 1) * 512],
                                       f == 0, f == 3, [RSLOT[sd_], AE[ab][f]], [BK[bk]], f == 3)
                                evac_copy(ys[:, ab, a, dh * 512:(dh + 1) * 512], banks[bk][:, :], [BK[bk]], [YS[ab]])
                        T.dma("sp", YS_d[i * TS:(i + 1) * TS, :].rearrange("(a p) d -> p a d", p=128), ys[:, ab],
                              reads=[YS[ab]], writes=[YSB[i]])

                    emit_hs(0)
                    emit_hs(1)
                    emit_tr(0)
                    for i in range(NTILE):
                        if i + 2 < NTILE:
                            emit_hs(i + 2)
                        if i + 1 < NTILE:
                            emit_tr(i + 1)
                        emit_gu(i)
                        if i + 2 < NTILE:
                            load_tile_w(i + 2, which=(0, 1))
                        if i > 0:
                            emit_d(i - 1)
                        if i + 1 < NTILE:
                            load_tile_w(i + 1, which=(2,))
                    emit_d(NTILE - 1)
                    T.barrier()

                with ExitStack() as pcx:
                    g0 = sb(pcx, "g0", [128, 4, D], F32)
                    g1 = sb(pcx, "g1", [128, 4, D], F32)
                    G0 = [Buf(f"g0{i}") for i in range(4)]
                    G1 = [Buf(f"g1{i}") for i in range(4)]
                    for j in range(16):
                        b = j % 4
                        T.idma(g0[:, b, :], None, YS_d, s1u[:, j:j + 1], reads=YSB + [ROUT], writes=[G0[b]])
                        T.idma(g1[:, b, :], None, YS_d, s2u[:, j:j + 1], reads=YSB + [ROUT], writes=[G1[b]])
                        T.emit("dve", lambda e, b=b, j=j: e.tensor_scalar(out=g0[:, b, :], in0=g0[:, b, :], scalar1=w1[:, j:j + 1],
                                                                          scalar2=None, op0=ALU.mult), reads=[G0[b], ROUT], writes=[G0[b]])
                        T.emit("dve", lambda e, b=b, j=j: e.scalar_tensor_tensor(out=g0[:, b, :], in0=g1[:, b, :], scalar=w2[:, j:j + 1],
                                                                                 in1=g0[:, b, :], op0=ALU.mult, op1=ALU.add),
                               reads=[G0[b], G1[b], ROUT], writes=[G0[b]])
                        for hb in range(2):
                            bk = 2 * (j % 2) + hb
                            for kk in range(4):
                                k = hb * 4 + kk
                                T.emit("pe", lambda e, bk=bk, kk=kk, k=k, b=b: e.transpose(
                                    out=banks[bk][:, kk * 128:(kk + 1) * 128], in_=g0[:, b, k * 128:(k + 1) * 128],
                                    identity=identf[:]), reads=[G0[b], CONST], writes=[BK[bk]], sig=(kk == 3))
                            T.emit("dve", lambda e, bk=bk, hb=hb, j=j: e.tensor_tensor(
                                out=xT[:, hb * 4:(hb + 1) * 4, j * 128:(j + 1) * 128],
                                in0=banks[bk][:, :].rearrange("p (k t) -> p k t", t=128),
                                in1=xT[:, hb * 4:(hb + 1) * 4, j * 128:(j + 1) * 128], op=ALU.add),
                                reads=[BK[bk]] + [XB[hb * 4 + kk][j // 4] for kk in range(4)],
                                writes=[XB[hb * 4 + kk][j // 4] for kk in range(4)])
                    T.barrier()

                if not last:
                    for k in range(8):
                        T.dma("sp", xT_d[k, :, tok0:tok0 + S], xT[:, k, :], reads=[XB[k][t] for t in range(4)],
                              writes=[XD[s][t] for t in range(4)])
                else:
                    final_out(xT, XB, s)
                T.barrier()

        def f_phase(l, s, last):
            tok0 = s * S
            with ExitStack() as pf:
                xT = sb(pf, "xT", [128, 8, S], F32)
                H = sb(pf, "Hf", [128, 8, S], BF16)
                ring = sb(pf, "ring", [128, 6, 4096], BF16)
                sq = sb(pf, "sqf", [128, 8, 512], BF16)
                rstd = sb(pf, "rstdf", [128, 512], F32)
                wr_sb = sb(pf, "wr_sb", [128, 8, 20], F32)
                wrp = sb(pf, "wrp", [128, 8, 20], F32)
                a_e = sb(pf, "a_e", [128, 2, 4, 512], BF16)
                sg = sb(pf, "sg", [128, 2, 512], F32)
                tt = sb(pf, "tt", [128, 2, 512], F32)
                Cb = sb(pf, "Cb", [128, 2, 512], F32)
                rt = sb(pf, "rt", [128, 16], F32)
                LGs = sb(pf, "LGs", [128, 16, 20], F32)
                r1 = sb(pf, "r1", [128, 16, 16], F32)
                r2 = sb(pf, "r2", [128, 16, 16], F32)
                gmax = sb(pf, "gmax", [128, 16], F32)
                goh = sb(pf, "goh", [128, 16, 4], F32)
                gex = sb(pf, "gex", [128, 16, 4], F32)
                gw = sb(pf, "gw", [128, 16], F32)
                esel = sb(pf, "esel", [128, 16, 4], F32)
                em = sb(pf, "em", [128, 16, 4], F32)
                m1 = sb(pf, "m1", [128, 16], F32)
                m2 = sb(pf, "m2", [128, 16], F32)
                oh1 = sb(pf, "oh1", [128, 16, 4], F32)
                oh2 = sb(pf, "oh2", [128, 16, 4], F32)
                w1 = sb(pf, "w1", [128, 16], F32)
                w2 = sb(pf, "w2", [128, 16], F32)
                c4 = sb(pf, "c4", [128, 16, 4], F32)
                Cm = sb(pf, "Cm", [128, 16, 16], F32)
                Chi = sb(pf, "Chi", [128, 16, 16], BF16)
                Clo = sb(pf, "Clo", [128, 16, 16], BF16)
                XB = [[Buf(f"X{k}_{t}") for t in range(4)] for k in range(8)]
                HB = [Buf(f"Hf{t}") for t in range(4)]
                RSLOT = [Buf(f"ring{i}") for i in range(6)]
                SQ, RS, WR, WRP, RT = Buf("sq"), Buf("rs"), Buf("wr"), Buf("wrp"), Buf("rt")
                ROUT = Buf("router")
                AE = [[Buf(f"ae{b}_{f}") for f in range(4)] for b in range(2)]
                SG = [Buf(f"sg{i}") for i in range(2)]
                TT = [Buf(f"tt{i}") for i in range(2)]
                CB = [Buf("cb0"), Buf("cb1")]

                def load_expert(e):
                    e4 = e // 4
                    for j, (nm, wb) in enumerate((("g", w_gate_b), ("u", w_up_b))):
                        slot = (3 * e + j) % 6
                        T.dma("sp", ring[:, slot, :].rearrange("p (k f) -> p k f", f=512),
                              wb[l, e].rearrange("(k p) f -> p k f", p=128), reads=[WB[(nm, l, e4)]], writes=[RSLOT[slot]])
                    slot = (3 * e + 2) % 6
                    T.dma("sp", ring[:, slot, :].rearrange("p (k f) -> p k f", f=1024),
                          w_down_b[l, e].rearrange("(k p) f -> p k f", p=128), reads=[WB[("d", l, e4)]], writes=[RSLOT[slot]])

                for k in range(8):
                    T.dma("sp", xT[:, k, :], xT_d[k, :, tok0:tok0 + S], reads=[XD[s][t] for t in range(4)],
                          writes=[XB[k][t] for t in range(4)])
                T.dma("sp", wr_sb[:], wr_d[:, l], writes=[WR])
                load_expert(0)
                for k in range(8):
                    T.emit("dve", lambda e, k=k: e.tensor_scalar(out=wrp[:, k, :], in0=wr_sb[:, k, :],
                                                                 scalar1=gf[:, l * 8 + k:l * 8 + k + 1], scalar2=None,
                                                                 op0=ALU.mult), reads=[WR, CONST], writes=[WRP])
                for t in range(4):
                    c0 = t * 512
                    T.emit("act", lambda e, c0=c0: e.activation(out=sq[:], in_=xT[:, :, c0:c0 + 512], func=AF.Square),
                           reads=[XB[k][t] for k in range(8)], writes=[SQ])
                    for k in range(8):
                        mm(banks[6][:, :], onesb[:], sq[:, k, :], k == 0, k == 7, [SQ, CONST], [BK[6]], k == 7)
                    rstd_from_ss(rstd[:], banks[6][:, :], [BK[6]], [RS])
                    for k in range(8):
                        T.emit("dve", lambda e, k=k, c0=c0: e.scalar_tensor_tensor(
                            out=H[:, k, c0:c0 + 512], in0=xT[:, k, c0:c0 + 512], scalar=gf[:, l * 8 + k:l * 8 + k + 1],
                            in1=rstd[:], op0=ALU.mult, op1=ALU.mult), reads=[XB[k][t], RS, CONST], writes=[HB[t]])
                    for j in range(4):
                        jj = 4 * t + j
                        for k in range(8):
                            mm(banks[7][:, 320 + jj:320 + jj + 1], sq[:, k, j * 128:(j + 1) * 128], onesb[:, 0:1],
                               k == 0, k == 7, [SQ, CONST], [BK[7]], False)
                        for k in range(8):
                            mm(banks[7][:, jj * 20:(jj + 1) * 20], xT[:, k, c0 + j * 128:c0 + (j + 1) * 128], wrp[:, k, :],
                               k == 0, k == 7, [XB[k][t], WRP], [BK[7]], (k == 7))
                R_ = [ROUT]

                def dv(fn, extra_reads=()):
                    T.emit("dve", fn, reads=[ROUT] + list(extra_reads), writes=[ROUT])

                def b3(ap2, n):
                    return ap2.unsqueeze(2).broadcast_to([128, 16, n])

                rstd_from_ss(rt[:], banks[7][:, 320:336], [BK[7], ROUT], [ROUT])
                dv(lambda e: e.tensor_tensor(out=LGs[:], in0=banks[7][:, 0:320].rearrange("p (j e) -> p j e", e=20),
                                             in1=b3(rt[:], 20), op=ALU.mult), [BK[7]])
                dv(lambda e: e.tensor_reduce(out=gmax[:], in_=LGs[:, :, 0:4], axis=AX.X, op=ALU.max))
                dv(lambda e: e.tensor_tensor(out=goh[:], in0=LGs[:, :, 0:4], in1=b3(gmax[:], 4), op=ALU.is_equal))
                dv(lambda e: e.tensor_tensor(out=gex[:], in0=LGs[:, :, 0:4], in1=b3(gmax[:], 4), op=ALU.subtract))
                T.emit("act", lambda e: e.activation(out=gex[:], in_=gex[:], func=AF.Exp), reads=[ROUT], writes=[ROUT])
                dv(lambda e: e.tensor_reduce(out=gw[:], in_=gex[:], axis=AX.X, op=ALU.add))
                dv(lambda e: e.reciprocal(out=gw[:], in_=gw[:]))
                dv(lambda e: e.tensor_tensor(out=r1[:].rearrange("p j (g i) -> p j g i", i=4),
                                             in0=LGs[:, :, 4:20].rearrange("p j (g i) -> p j g i", i=4),
                                             in1=goh[:].unsqueeze(3).broadcast_to([128, 16, 4, 4]), op=ALU.mult))
                dv(lambda e: e.tensor_reduce(out=esel[:], in_=r1[:].rearrange("p j (g i) -> p j i g", i=4),
                                             axis=AX.X, op=ALU.add))
                dv(lambda e: e.tensor_reduce(out=m1[:], in_=esel[:], axis=AX.X, op=ALU.max))
                dv(lambda e: e.tensor_tensor(out=oh1[:], in0=esel[:], in1=b3(m1[:], 4), op=ALU.is_equal))
                dv(lambda e: e.scalar_tensor_tensor(out=em[:], in0=oh1[:], scalar=-1.0e30, in1=esel[:],
                                                    op0=ALU.mult, op1=ALU.add))
                dv(lambda e: e.tensor_reduce(out=m2[:], in_=em[:], axis=AX.X, op=ALU.max))
                dv(lambda e: e.tensor_tensor(out=oh2[:], in0=em[:], in1=b3(m2[:], 4), op=ALU.is_equal))
                dv(lambda e: e.tensor_tensor(out=w2[:], in0=m2[:], in1=m1[:], op=ALU.subtract))
                T.emit("act", lambda e: e.activation(out=w2[:], in_=w2[:], func=AF.Exp), reads=[ROUT], writes=[ROUT])
                dv(lambda e: e.tensor_scalar(out=w2[:], in0=w2[:], scalar1=1.0, scalar2=None, op0=ALU.add))
                dv(lambda e: e.reciprocal(out=w1[:], in_=w2[:]))
                dv(lambda e: e.tensor_tensor(out=w1[:], in0=w1[:], in1=gw[:], op=ALU.mult))
                dv(lambda e: e.tensor_tensor(out=w2[:], in0=gw[:], in1=w1[:], op=ALU.subtract))
                dv(lambda e: e.tensor_tensor(out=c4[:], in0=oh1[:], in1=b3(w1[:], 4), op=ALU.mult))
                dv(lambda e: e.tensor_tensor(out=oh2[:], in0=oh2[:], in1=b3(w2[:], 4), op=ALU.mult))
                dv(lambda e: e.tensor_tensor(out=c4[:], in0=c4[:], in1=oh2[:], op=ALU.add))
                dv(lambda e: e.tensor_tensor(out=Cm[:].rearrange("p j (g i) -> p j g i", i=4),
                                             in0=goh[:].unsqueeze(3).broadcast_to([128, 16, 4, 4]),
                                             in1=c4[:].unsqueeze(2).broadcast_to([128, 16, 4, 4]), op=ALU.mult))
                dv(lambda e: e.tensor_copy(out=Chi[:], in_=Cm[:]))
                dv(lambda e: e.tensor_tensor(out=r2[:], in0=Cm[:], in1=Chi[:], op=ALU.subtract))
                dv(lambda e: e.tensor_copy(out=Clo[:], in_=r2[:]))

                steps = [(e_, t_) for e_ in range(NE) for t_ in range(4)]

                def emit_gu(idx):
                    e_, t = steps[idx]
                    ab = idx % 2
                    c0 = t * 512
                    sg_, su_, sd_ = (3 * e_) % 6, (3 * e_ + 1) % 6, (3 * e_ + 2) % 6
                    n = 0
                    for j in range(4):
                        for Cx in (Chi, Clo):
                            mm(banks[6][:, j * 128:(j + 1) * 128], Cx[:, 4 * t + j, e_:e_ + 1].broadcast_to([128, 128]),
                               identb[:], Cx is Chi, Cx is Clo, [ROUT, CONST], [BK[6]], (j == 3 and Cx is Clo))
                    T.emit("act", lambda e, ab=ab: e.activation(out=Cb[:, ab, :], in_=banks[6][:, :], func=AF.Copy),
                           reads=[BK[6]], writes=[CB[ab]])
                    for f in range(4):
                        bg, bu = f % 2, 2 + f % 2
                        for k in range(8):
                            mm(banks[bg][:, :], ring[:, sg_, k * 512 + f * 128:k * 512 + (f + 1) * 128], H[:, k, c0:c0 + 512],
                               k == 0, k == 7, [RSLOT[sg_], HB[t]], [BK[bg]], k == 7)
                        for k in range(8):
                            mm(banks[bu][:, :], ring[:, su_, k * 512 + f * 128:k * 512 + (f + 1) * 128], H[:, k, c0:c0 + 512],
                               k == 0, k == 7, [RSLOT[su_], HB[t]], [BK[bu]], k == 7)
                        i3 = f % 2
                        T.emit("act", lambda e, bg=bg, i3=i3: e.activation(out=sg[:, i3, :], in_=banks[bg][:, :], func=AF.Silu),
                               reads=[BK[bg]], writes=[SG[i3]])
                        T.emit("dve", lambda e, bu=bu, i3=i3: e.tensor_tensor(out=tt[:, i3, :], in0=banks[bu][:, :],
                                                                            in1=sg[:, i3, :], op=ALU.mult),
                               reads=[BK[bu], SG[i3]], writes=[TT[i3]])
                        T.emit("dve", lambda e, ab=ab, f=f, i3=i3: e.tensor_tensor(out=a_e[:, ab, f, :], in0=tt[:, i3, :],
                                                                                   in1=Cb[:, ab, :], op=ALU.mult),
                               reads=[TT[i3], CB[ab]], writes=[AE[ab][f]])

                def emit_d(idx):
                    e_, t = steps[idx]
                    ab = idx % 2
                    c0 = t * 512
                    sd_ = (3 * e_ + 2) % 6
                    for m in range(8):
                        bk = 4 + m % 2
                        for f in range(4):
                            mm(banks[bk][:, :], ring[:, sd_, f * 1024 + m * 128:f * 1024 + (m + 1) * 128], a_e[:, ab, f, :],
                               f == 0, f == 3, [RSLOT[sd_], AE[ab][f]], [BK[bk]], f == 3)
                        T.emit("dve", lambda e, m=m, bk=bk, c0=c0: e.tensor_tensor(
                            out=xT[:, m, c0:c0 + 512], in0=banks[bk][:, :], in1=xT[:, m, c0:c0 + 512], op=ALU.add),
                            reads=[BK[bk], XB[m][t]], writes=[XB[m][t]])

                for idx in range(len(steps)):
                    e_, t = steps[idx]
                    emit_gu(idx)
                    if idx > 0:
                        emit_d(idx - 1)
                    if t == 0 and e_ + 1 < NE:
                        load_expert(e_ + 1)
                emit_d(len(steps) - 1)

                if not last:
                    for k in range(8):
                        T.dma("sp", xT_d[k, :, tok0:tok0 + S], xT[:, k, :], reads=[XB[k][t] for t in range(4)],
                              writes=[XD[s][t] for t in range(4)])
                else:
                    final_out(xT, XB, s)
                T.barrier()

        done = False
        for l in range(depth):
            for s in range(nseq):
                m_phase(l, s)
            if stop_after == ("M", l):
                done = True
                break
            for s in range(nseq):
                (f_phase_sparse if SPARSE else f_phase)(l, s, last=(l == depth - 1 and stop_after is None))
            if stop_after == ("F", l):
                done = True
                break
        T.barrier()
        T.flush()
    return nc


def _bias_index_map():
    hp = np.arange(4)[:, None, None, None, None]
    p = np.arange(128)[None, :, None, None, None]
    eo = np.arange(2)[None, None, :, None, None]
    i = np.arange(14)[None, None, None, :, None]
    cq = np.arange(64)[None, None, None, None, :]
    kc = p % 64
    half = p // 64
    h = 2 * hp + eo
    dr = i + half
    dc = np.clip(kc - cq, -15, 15) + 15
    qs = np.clip(cq - 8, 0, 48)
    valid = (kc >= qs) & (kc < qs + 16)
    flat = h * (15 * 31 + 1) + np.where(valid, dr * 31 + dc, 15 * 31)
    return np.broadcast_to(flat, (4, 128, 2, 14, 64)).copy()


def _pool_consts():
    import ml_dtypes
    out = np.zeros((4, 7, 128, 128), np.float32)
    Sr = 512
    t = np.arange(Sr)
    for g, w in enumerate(POOL_W):
        lo = np.clip(t - w // 2, 0, Sr)
        hi = np.clip(t - w // 2 + w, 0, Sr)
        cnt = (hi - lo).astype(np.float64)
        A = np.zeros((Sr, Sr), np.float64)
        for tt_ in range(Sr):
            A[lo[tt_]:hi[tt_], tt_] = 1.0 / cnt[tt_]
        A -= np.eye(Sr)

        def blk(a, b):
            return A[a * 128:(a + 1) * 128, b * 128:(b + 1) * 128]

        def hilo(M):
            hi_ = M.astype(np.float32).astype(ml_dtypes.bfloat16).astype(np.float64)
            lo_ = (M - hi_).astype(np.float32).astype(ml_dtypes.bfloat16).astype(np.float64)
            return hi_, lo_

        out[g, 0] = blk(1, 2)
        out[g, 1] = blk(2, 2)
        out[g, 2] = blk(3, 2)
        out[g, 3], out[g, 4] = hilo(blk(0, 0))
        out[g, 5], out[g, 6] = hilo(blk(3, 3))
    return np.ascontiguousarray(out.reshape(28, 128, 128).transpose(1, 0, 2))


_BIAS_MAP = None
_PROG = {}


def prep_inputs(inp, nseq=NSEQ_FULL, ncores=NCORES):
    global _BIAS_MAP
    f = lambda a: np.ascontiguousarray(np.asarray(a, dtype=np.float32))
    Lw = L_FULL
    if _BIAS_MAP is None:
        _BIAS_MAP = _bias_index_map()
    rpb = f(inp["rpb"])
    pad = np.concatenate([rpb.reshape(Lw, 8, 15 * 31), np.full((Lw, 8, 1), NEG, np.float32)], axis=2).reshape(Lw, -1)
    biasT = pad[:, _BIAS_MAP]
    biasT = np.ascontiguousarray(biasT.reshape(Lw, 4, 128, 2 * 14 * 64))
    shared = {
        "w_in": f(inp["w_in"]), "w_out": f(inp["w_out"]), "pool_w": f(inp["pool_w"]),
        "w_gate": f(inp["w_gate"]), "w_up": f(inp["w_up"]), "w_down": f(inp["w_down"]),
        "gm": np.ascontiguousarray(f(inp["norm_mix_g"]).reshape(Lw, 8, 128).transpose(2, 0, 1).reshape(128, Lw * 8)),
        "gf": np.ascontiguousarray(f(inp["norm_ffn_g"]).reshape(Lw, 8, 128).transpose(2, 0, 1).reshape(128, Lw * 8)),
        "ps": np.ascontiguousarray(f(inp["pool_scale"]).reshape(Lw, 4, 128).transpose(2, 0, 1).reshape(128, Lw * 4)),
        "gF": np.ascontiguousarray(np.broadcast_to(f(inp["final_g"])[None, :], (128, D))),
        "wr": np.ascontiguousarray(np.concatenate([f(inp["w_router_group"]), f(inp["w_router_expert"])], axis=-1)
                                   .reshape(Lw, 8, 128, 20).transpose(2, 0, 1, 3)),
        "biasT": biasT,
        "poolA": _pool_consts(),
        "ident": np.eye(128, dtype=np.float32),
        "ustrict": np.triu(np.ones((128, 128), np.float32), k=1),
        "iotas": np.ascontiguousarray(np.concatenate([np.arange(128, dtype=np.float32)[:, None],
                                                      np.broadcast_to(np.arange(32, dtype=np.float32)[None, :], (128, 32))], axis=1)),
    }
    x = f(inp["x"]).reshape(-1, S, D)
    maps = []
    for c in range(ncores):
        m = dict(shared)
        m["x"] = np.ascontiguousarray(x[c * nseq:(c + 1) * nseq].reshape(nseq * S, D))
        maps.append(m)
    return maps


def kernel(x, norm_mix_g, w_in, rpb, pool_w, pool_scale, w_out, norm_ffn_g, w_router_group, w_router_expert,
           w_gate, w_up, w_down, final_g):
    inp = dict(x=x, norm_mix_g=norm_mix_g, w_in=w_in, rpb=rpb, pool_w=pool_w, pool_scale=pool_scale, w_out=w_out,
               norm_ffn_g=norm_ffn_g, w_router_group=w_router_group, w_router_expert=w_router_expert,
               w_gate=w_gate, w_up=w_up, w_down=w_down, final_g=final_g)
    maps = prep_inputs(inp)
    if "full" not in _PROG:
        _PROG["full"] = build_program()
    nc = _PROG["full"]
    res = run_bass_kernel_spmd(nc, maps, core_ids=list(range(NCORES)))
    outs = [np.asarray(r["out"], dtype=np.float32).reshape(NSEQ_FULL, S, D) for r in res.results]
    return np.concatenate(outs, axis=0)
```

```python
import numpy as np
from contextlib import ExitStack
import concourse.bass as bass
import concourse.mybir as mybir
from concourse.bass_utils import run_bass_kernel_spmd

F32 = mybir.dt.float32
BF16 = mybir.dt.bfloat16
AF = mybir.ActivationFunctionType
ALU = mybir.AluOpType
AX = mybir.AxisListType

D = 1024
S = 2048
L_FULL = 4
NSEQ_FULL = 4
NCORES = 8
NE = 16
DE = 512
EPS = 1e-6
NEG = -30000.0
POOL_W = (2, 4, 8, 16)


class Buf:
    __slots__ = ("name", "w", "r")

    def __init__(self, name):
        self.name = name
        self.w = None
        self.r = {}


class _Eng:
    def __init__(self, name):
        self.name = name
        self.q = []
        self.known = {}
        self.count = 0
        self.psem = None


class Tracker:
    ENGS = ("pe", "act", "dve", "pool", "sp")

    def __init__(self, nc, stack, n_dma_sems=24):
        self.nc = nc
        self.sems = []
        self.eng = {n: _Eng(n) for n in self.ENGS}
        for n in ("pe", "act", "dve", "pool"):
            self.eng[n].psem = self._new_sem(stack, "p_" + n)
        self.dpool = [self._new_sem(stack, f"dq{i}") for i in range(n_dma_sems)]
        self.dcnt = [0] * n_dma_sems
        self.dnext = 0
        self.stack = stack
        self.extra = []

    def _new_sem(self, stack, name):
        h = stack.enter_context(self.nc.semaphore(name))
        self.sems.append(h)
        return len(self.sems) - 1

    def _deps(self, E, reads, writes, extra=()):
        need = {}

        def req(ev):
            if ev is None:
                return
            s, v = ev
            if s == E.psem and E.name == "pe":
                return
            if E.known.get(s, 0) >= v:
                return
            if need.get(s, 0) < v:
                need[s] = v

        for ev in extra:
            req(ev)
        for b in reads:
            req(b.w)
        for b in writes:
            req(b.w)
            for s, v in b.r.items():
                req((s, v))
        for s, v in need.items():
            E.q.append(("wait", s, v))
            E.known[s] = v

    def _mark(self, ev, reads, writes):
        for b in reads:
            if b.r.get(ev[0], 0) < ev[1]:
                b.r[ev[0]] = ev[1]
        for b in writes:
            b.w = ev
            b.r = {}

    def emit(self, eng, fn, reads=(), writes=(), sig=True):
        E = self.eng[eng]
        self._deps(E, reads, writes)
        if sig:
            E.count += 1
            ev = (E.psem, E.count)
        else:
            ev = (E.psem, E.count + 1)
        E.q.append(("op", fn, sig))
        self._mark(ev, reads, writes)
        return ev

    def dma(self, q, out, in_, reads=(), writes=(), own_sem=False):
        E = self.eng[q]
        if own_sem:
            s = self._new_sem(self.stack, f"ds{len(self.sems)}")
            self.extra.append(s)
            prev = 0
            self._deps(E, reads, writes)
            ev = (s, 16)
        else:
            i = self.dnext
            self.dnext = (i + 1) % len(self.dpool)
            s = self.dpool[i]
            prev = self.dcnt[i]
            self._deps(E, reads, writes, extra=[(s, prev)] if prev else ())
            self.dcnt[i] += 16
            ev = (s, self.dcnt[i])
        E.q.append(("dma", out, in_, s))
        self._mark(ev, reads, writes)
        return ev

    def idma(self, out, out_idx, in_, in_idx, reads=(), writes=(), bounds=None):
        E = self.eng["pool"]
        i = self.dnext
        self.dnext = (i + 1) % len(self.dpool)
        s = self.dpool[i]
        prev = self.dcnt[i]
        self._deps(E, reads, writes, extra=[(s, prev)] if prev else ())
        self.dcnt[i] += 16
        ev = (s, self.dcnt[i])
        E.q.append(("idma", out, out_idx, in_, in_idx, s, bounds))
        self._mark(ev, reads, writes)
        return ev

    def barrier(self):
        evs = []
        for n in ("pe", "act", "dve", "pool"):
            e = self.eng[n]
            if e.count:
                evs.append((e.psem, e.count))
        for i, s in enumerate(self.dpool):
            if self.dcnt[i]:
                evs.append((s, self.dcnt[i]))
        for s in self.extra:
            evs.append((s, 16))
        for fn in getattr(self, "extra_ev_fns", []):
            evs.extend(fn())
        for n in self.ENGS:
            E = self.eng[n]
            for s, v in evs:
                if s == E.psem:
                    continue
                if E.known.get(s, 0) < v:
                    E.q.append(("wait", s, v))
                    E.known[s] = v

    def flush(self):
        nc = self.nc
        sems = self.sems

        def run(E, h):
            psem = sems[E.psem] if E.psem is not None else None
            for it in E.q:
                if it[0] == "wait":
                    h.wait_ge(sems[it[1]], it[2])
                elif it[0] == "op":
                    ins = it[1](h)
                    if it[2]:
                        ins.then_inc(psem, 1)
                elif it[0] == "idma":
                    oo = bass.IndirectOffsetOnAxis(ap=it[2], axis=0) if it[2] is not None else None
                    io = bass.IndirectOffsetOnAxis(ap=it[4], axis=0) if it[4] is not None else None
                    if it[6] is None:
                        h.indirect_dma_start(out=it[1], out_offset=oo, in_=it[3], in_offset=io).then_inc(sems[it[5]], 16)
                    else:
                        h.indirect_dma_start(out=it[1], out_offset=oo, in_=it[3], in_offset=io, bounds_check=it[6],
                                             oob_is_err=False).then_inc(sems[it[5]], 16)
                else:
                    h.dma_start(out=it[1], in_=it[2]).then_inc(sems[it[3]], 16)

        with nc.Block() as block:
            @block.tensor
            def _(h):
                run(self.eng["pe"], h)

            @block.scalar
            def _(h):
                run(self.eng["act"], h)

            @block.vector
            def _(h):
                run(self.eng["dve"], h)

            @block.gpsimd
            def _(h):
                run(self.eng["pool"], h)

            @block.sync
            def _(h):
                run(self.eng["sp"], h)


SPARSE = True
POOL_FROM_LAYER = 1
TS = 256
NTILE = 32


def build_program(nseq=NSEQ_FULL, depth=L_FULL, stop_after=None, debug_out=False):
    nc = bass.Bass("TRN2", target_bir_lowering=False)
    NT = nseq * S
    Lw = L_FULL

    def din(name, shape, dt=F32):
        return nc.dram_tensor(name, list(shape), dt, kind="ExternalInput").ap()

    x_c = din("x", [NT, D])
    w_in = din("w_in", [Lw, D, 2048])
    w_out = din("w_out", [Lw, D, D])
    pool_w = din("pool_w", [Lw, 4, 128, 128])
    w_gate = din("w_gate", [Lw, NE, D, DE])
    w_up = din("w_up", [Lw, NE, D, DE])
    w_down = din("w_down", [Lw, NE, DE, D])
    gm_d = din("gm", [128, Lw * 8])
    gf_d = din("gf", [128, Lw * 8])
    ps_d = din("ps", [128, Lw * 4])
    gF_d = din("gF", [128, D])
    wr_d = din("wr", [128, Lw, 8, 20])
    bias_d = din("biasT", [Lw, 4, 128, 2 * 14 * 64])
    poolA_d = din("poolA", [128, 28, 128])
    ident_d = din("ident", [128, 128])
    ustrict_d = din("ustrict", [128, 128])
    iotas_d = din("iotas", [128, 33])
    if stop_after is None:
        out_c = nc.dram_tensor("out", [NT, D], F32, kind="ExternalOutput").ap()
        xT_d = nc.dram_tensor("xT_d", [8, 128, NT], F32, kind="Internal").ap()
    else:
        xT_d = nc.dram_tensor("xT_out", [8, 128, NT], F32, kind="ExternalOutput").ap()
        out_c = None
    w_in_b = nc.dram_tensor("w_in_b", [Lw, D, 2048], BF16, kind="Internal").ap()
    w_out_b = nc.dram_tensor("w_out_b", [Lw, D, D], BF16, kind="Internal").ap()
    pool_w_b = nc.dram_tensor("pool_w_b", [Lw, 4, 128, 128], BF16, kind="Internal").ap()
    w_gate_b = nc.dram_tensor("w_gate_b", [Lw, NE, D, DE], BF16, kind="Internal").ap()
    w_up_b = nc.dram_tensor("w_up_b", [Lw, NE, D, DE], BF16, kind="Internal").ap()
    w_down_b = nc.dram_tensor("w_down_b", [Lw, NE, DE, D], BF16, kind="Internal").ap()
    w_gate_p = [nc.dram_tensor(f"w_gate_p{i}", [NE * 128, 4096], BF16, kind="Internal").ap() for i in range(Lw)]
    w_up_p = [nc.dram_tensor(f"w_up_p{i}", [NE * 128, 4096], BF16, kind="Internal").ap() for i in range(Lw)]
    w_down_p = [nc.dram_tensor(f"w_down_p{i}", [NE * 128, 4096], BF16, kind="Internal").ap() for i in range(Lw)]
    HS_d = nc.dram_tensor("HS_d", [NTILE * TS, D], BF16, kind="Internal").ap()
    YS_d = nc.dram_tensor("YS_d", [NTILE * TS, D], F32, kind="Internal").ap()

    top = ExitStack()
    with top:
        T = Tracker(nc, top)

        uid = [0]

        def sb(stack, name, shape, dt):
            uid[0] += 1
            return stack.enter_context(nc.sbuf_tensor(f"{name}_s{uid[0]}", list(shape), dt))

        banks = [top.enter_context(nc.psum_tensor(f"bank{i}", [128, 512], F32)) for i in range(8)]
        BK = [Buf(f"bank{i}") for i in range(8)]

        onesb = sb(top, "onesb", [128, 128], BF16)
        identf = sb(top, "identf", [128, 128], F32)
        identb = sb(top, "identb", [128, 128], BF16)
        A_bf = sb(top, "A_bf", [128, 28, 128], BF16)
        gm = sb(top, "gm", [128, Lw * 8], F32)
        gf = sb(top, "gf", [128, Lw * 8], F32)
        psc = sb(top, "psc", [128, Lw * 4], F32)
        epsc = sb(top, "epsc", [128, 1], F32)
        ustr_f = sb(top, "ustr_f", [128, 128], F32)
        ustr = sb(top, "ustr", [128, 128], BF16)
        iotas = sb(top, "iotas", [128, 33], F32)
        CONST = Buf("const")
        with nc.sbuf_tensor("A_stage", [128, 28, 128], F32) as A_st:
            ASB = Buf("A_stage")
            T.emit("dve", lambda e: e.memset(onesb[:], 1.0), writes=[CONST])
            T.emit("dve", lambda e: e.memset(epsc[:], EPS), writes=[CONST])
            T.dma("sp", identf[:], ident_d, writes=[CONST])
            T.dma("sp", gm[:], gm_d, writes=[CONST])
            T.dma("sp", gf[:], gf_d, writes=[CONST])
            T.dma("sp", psc[:], ps_d, writes=[CONST])
            T.dma("sp", A_st[:], poolA_d, writes=[ASB])
            T.dma("sp", ustr_f[:], ustrict_d, writes=[CONST])
            T.dma("sp", iotas[:], iotas_d, writes=[CONST])
            T.emit("dve", lambda e: e.tensor_copy(out=ustr[:], in_=ustr_f[:]), reads=[CONST], writes=[CONST])
            T.emit("dve", lambda e: e.tensor_copy(out=identb[:], in_=identf[:]), reads=[CONST], writes=[CONST])
            T.emit("dve", lambda e: e.tensor_copy(out=A_bf[:], in_=A_st[:]), reads=[ASB], writes=[CONST])
            T.barrier()

        WB = {}

        cast_sems = [T._new_sem(top, f"cs{i}") for i in range(8)]
        cast_cnt = [0] * 8
        cast_i = [0]

        def cast(key, dst, src, n=0):
            WB[key] = Buf(str(key))
            E = T.eng["pool"]
            i = cast_i[0] % 8
            cast_i[0] += 1
            sm = cast_sems[i]
            if cast_cnt[i] and E.known.get(sm, 0) < cast_cnt[i]:
                E.q.append(("wait", sm, cast_cnt[i]))
                E.known[sm] = cast_cnt[i]
            cast_cnt[i] += 16
            E.q.append(("dma", dst, src, sm))
            WB[key].w = (sm, cast_cnt[i])

        XD = [[Buf(f"xd{s}_{t}") for t in range(4)] for s in range(nseq)]
        flip = [0]

        def evac_copy(out, in_, reads, writes, scale=None):
            flip[0] ^= 1
            if scale is not None or flip[0]:
                sc = 1.0 if scale is None else scale
                T.emit("act", lambda e: e.activation(out=out, in_=in_, func=AF.Copy, scale=sc), reads=reads, writes=writes)
            else:
                T.emit("dve", lambda e: e.tensor_copy(out=out, in_=in_), reads=reads, writes=writes)

        def mm(out, lhsT, rhs, start, stop, reads, writes, sig):
            T.emit("pe", lambda e: e.matmul(out, lhsT, rhs, start=start, stop=stop), reads=reads, writes=writes, sig=sig)

        def rstd_from_ss(out, ss_ap, reads, writes):
            T.emit("act", lambda e: e.activation(out=out, in_=ss_ap, func=AF.Sqrt, bias=epsc[:, 0:1], scale=1.0 / D),
                   reads=list(reads) + [CONST], writes=writes)
            T.emit("dve", lambda e: e.reciprocal(out=out, in_=out), reads=writes, writes=writes)

        with ExitStack() as st:
            xin = sb(st, "xin", [128, 2, 4, 1024], F32)
            xTs = sb(st, "xTs", [128, 2, 8, 512], F32)
            XIN = [Buf("xin0"), Buf("xin1")]
            XTS = [Buf("xts0"), Buf("xts1")]
            bi = 0
            for s in range(nseq):
                for t in range(4):
                    b = (s * 4 + t) % 2
                    t0 = s * S + t * 512
                    T.dma("sp", xin[:, b], x_c[t0:t0 + 512, :].rearrange("(j p) d -> p j d", p=128), writes=[XIN[b]])
                    for k in range(8):
                        bk = bi % 4
                        bi += 1
                        for j in range(4):
                            T.emit("pe", lambda e, bk=bk, j=j, k=k, b=b: e.transpose(
                                out=banks[bk][:, j * 128:(j + 1) * 128], in_=xin[:, b, j, k * 128:(k + 1) * 128],
                                identity=identf[:]), reads=[XIN[b], CONST], writes=[BK[bk]], sig=(j == 3))
                        evac_copy(xTs[:, b, k, :], banks[bk][:, :], [BK[bk]], [XTS[b]])
                    T.dma("sp", xT_d[:, :, t0:t0 + 512].rearrange("k p t -> p k t"), xTs[:, b], reads=[XTS[b]],
                          writes=[XD[s][t]])
            T.barrier()

        for l in range(depth):
            cast(("in", l), w_in_b[l], w_in[l])
            cast(("pw", l), pool_w_b[l].rearrange("g (a b) d -> (g a) (b d)", b=16),
                 pool_w[l].rearrange("g (a b) d -> (g a) (b d)", b=16))
            cast(("out", l), w_out_b[l].rearrange("(r q) d -> r (q d)", q=2),
                 w_out[l].rearrange("(r q) d -> r (q d)", q=2))
            for e_ in range(NE):
                cast(("g", l, e_), w_gate_p[l][e_ * 128:(e_ + 1) * 128, :].rearrange("p (k f) -> p k f", f=512),
                     w_gate[l, e_].rearrange("(k p) f -> p k f", p=128))
                cast(("u", l, e_), w_up_p[l][e_ * 128:(e_ + 1) * 128, :].rearrange("p (k f) -> p k f", f=512),
                     w_up[l, e_].rearrange("(k p) f -> p k f", p=128))
                cast(("d", l, e_), w_down_p[l][e_ * 128:(e_ + 1) * 128, :].rearrange("p (k f) -> p k f", f=1024),
                     w_down[l, e_].rearrange("(k p) f -> p k f", p=128))

        def m_phase(l, s):
            tok0 = s * S
            with ExitStack() as pm:
                H = sb(pm, "H", [128, 8, S], BF16)
                qT = sb(pm, "qT", [128, 4, S], BF16)
                kT = sb(pm, "kT", [128, 4, S], BF16)
                V2 = sb(pm, "V2", [128, 31, 4, 192], BF16)
                pT = sb(pm, "pT", [128, 4, S], BF16)
                pw_sb = sb(pm, "pw_sb", [128, 4, 128], BF16)
                HT = [Buf(f"H{i}") for i in range(8)]
                QB = [[Buf(f"q{m}_{t}") for t in range(4)] for m in range(4)]
                KB = [[Buf(f"k{m}_{t}") for t in range(4)] for m in range(4)]
                VB = [Buf(f"v{i}") for i in range(31)]
                VONES = Buf("vones")
                PB = [[Buf(f"p{g}_{t}") for t in range(4)] for g in range(4)]
                PW = Buf("pw")
                with ExitStack() as pb:
                    w_in_sb = sb(pb, "w_in_sb", [128, 8, 2048], BF16)
                    U2 = sb(pb, "U2", [128, 16, 512], BF16)
                    pooledT = sb(pb, "pooledT", [128, 2, 512], BF16)
                    sq = sb(pb, "sq", [128, 8, 256], BF16)
                    xt = sb(pb, "xt", [128, 1, 8, 256], F32)
                    rstd = sb(pb, "rstd", [128, 1, 256], F32)
                    WI = [Buf(f"wi{c}") for c in range(4)]
                    UB = [Buf(f"u{j}") for j in range(16)]
                    PLB = [Buf("pl0"), Buf("pl1")]
                    SQ = Buf("sq")
                    XT = [Buf("xt0"), Buf("xt1")]
                    RS = [Buf("rs0"), Buf("rs1")]
                    for c in range(4):
                        T.dma("sp", w_in_sb[:, :, c * 512:(c + 1) * 512],
                              w_in_b[l][:, c * 512:(c + 1) * 512].rearrange("(k p) f -> p k f", p=128),
                              reads=[WB[("in", l)]], writes=[WI[c]])
                    T.dma("sp", pw_sb[:], pool_w_b[l].rearrange("g c d -> c g d"), reads=[WB[("pw", l)]], writes=[PW])
                    T.emit("dve", lambda e: e.memset(V2[:, :, :, 64:128], 1.0), writes=[VONES])
                    def emit_norm(i):
                        b = 0
                        c0 = i * 256
                        T.dma("sp", xt[:, b], xT_d[:, :, tok0 + c0:tok0 + c0 + 256].rearrange("k p t -> p k t"),
                              reads=[XD[s][i // 2]], writes=[XT[b]])
                        T.emit("act", lambda e, b=b: e.activation(out=sq[:], in_=xt[:, b], func=AF.Square),
                               reads=[XT[b]], writes=[SQ])
                        bk = 6 + b
                        for k in range(8):
                            mm(banks[bk][:, 0:256], onesb[:], sq[:, k, :], k == 0, k == 7, [SQ, CONST], [BK[bk]], k == 7)
                        rstd_from_ss(rstd[:, b, :], banks[bk][:, 0:256], [BK[bk]], [RS[b]])
                        for k in range(8):
                            T.emit("dve", lambda e, b=b, k=k, c0=c0: e.scalar_tensor_tensor(
                                out=H[:, k, c0:c0 + 256], in0=xt[:, b, k, :], scalar=gm[:, l * 8 + k:l * 8 + k + 1],
                                in1=rstd[:, b, :], op0=ALU.mult, op1=ALU.mult),
                                reads=[XT[b], RS[b], CONST], writes=[HT[i]])
                    bi_ = [0]

                    def emit_qkproj(t):
                        bi = bi_[0]
                        for m in range(8):
                            bk = bi % 4
                            bi += 1
                            for k in range(8):
                                mm(banks[bk][:, :], w_in_sb[:, k, m * 128:(m + 1) * 128], H[:, k, t * 512:(t + 1) * 512],
                                   k == 0, k == 7, [WI[m // 4], HT[2 * t], HT[2 * t + 1]], [BK[bk]], k == 7)
                            if m < 4:
                                evac_copy(qT[:, m, t * 512:(t + 1) * 512], banks[bk][:, :], [BK[bk]], [QB[m][t]], scale=0.125)
                            else:
                                T.emit("dve", lambda e, bk=bk, m=m, t=t: e.tensor_copy(
                                    out=kT[:, m - 4, t * 512:(t + 1) * 512], in_=banks[bk][:, :]),
                                    reads=[BK[bk]], writes=[KB[m - 4][t]])
                        bi_[0] = bi

                    for t in range(4):
                        emit_norm(2 * t)
                        emit_norm(2 * t + 1)
                        if t > 0:
                            emit_qkproj(t - 1)
                    emit_qkproj(3)
                    bi = bi_[0]
                    for sidx in range(31):
                        bk = bi % 4
                        bi += 1
                        a0 = 64 * sidx
                        hts = sorted({a0 // 256, (a0 + 127) // 256})
                        for k in range(8):
                            mm(banks[bk][:, :], H[:, k, a0:a0 + 128], w_in_sb[:, k, 1024:1536], k == 0, k == 7,
                               [WI[2]] + [HT[i] for i in hts], [BK[bk]], k == 7)
                        evac_copy(V2[:, sidx, :, :].rearrange("p a (b d) -> p a b d", d=64)[:, :, 0:3:2, :],
                                  banks[bk][:, :].rearrange("p (a b d) -> p a b d", b=2, d=64), [BK[bk]], [VB[sidx]])
                    for j in range(16):
                        bk = bi % 4
                        bi += 1
                        for k in range(8):
                            mm(banks[bk][:, :], H[:, k, j * 128:(j + 1) * 128], w_in_sb[:, k, 1536:2048], k == 0, k == 7,
                               [WI[3], HT[j // 2]], [BK[bk]], k == 7)
                        evac_copy(U2[:, j, :], banks[bk][:, :], [BK[bk]], [UB[j]])
                    for t in range(4):
                        for g in range(4):
                            pbuf = g % 2
                            bk = bi % 4
                            bi += 1
                            for Tq in range(4):
                                Tt = 4 * t + Tq
                                terms = []
                                if Tt > 0:
                                    terms.append((Tt - 1, 0))
                                if Tt == 0:
                                    terms += [(Tt, 3), (Tt, 4)]
                                elif Tt == 15:
                                    terms += [(Tt, 5), (Tt, 6)]
                                else:
                                    terms.append((Tt, 1))
                                if Tt < 15:
                                    terms.append((Tt + 1, 2))
                                for ti, (tp, var) in enumerate(terms):
                                    mm(banks[bk][:, Tq * 128:(Tq + 1) * 128], U2[:, tp, g * 128:(g + 1) * 128],
                                       A_bf[:, g * 7 + var, :], ti == 0, ti == len(terms) - 1,
                                       [UB[tp], CONST], [BK[bk]], (Tq == 3 and ti == len(terms) - 1))
                            T.emit("act", lambda e, bk=bk, pbuf=pbuf, g=g: e.activation(
                                out=pooledT[:, pbuf, :], in_=banks[bk][:, :], func=AF.Copy),
                                reads=[BK[bk]], writes=[PLB[pbuf]])
                            bk2 = 4 + (g % 2)
                            mm(banks[bk2][:, :], pw_sb[:, g, :], pooledT[:, pbuf, :], True, True, [PW, PLB[pbuf]], [BK[bk2]], True)
                            T.emit("dve", lambda e, bk2=bk2, g=g, t=t: e.tensor_scalar(
                                out=pT[:, g, t * 512:(t + 1) * 512], in0=banks[bk2][:, :],
                                scalar1=psc[:, l * 4 + g:l * 4 + g + 1], scalar2=None, op0=ALU.mult),
                                reads=[BK[bk2], CONST], writes=[PB[g][t]])
                    T.barrier()
                with ExitStack() as pc:
                    bias = sb(pc, "bias", [128, 4, 2, 14, 64], F32)
                    Sb = sb(pc, "Sb", [128, 4, 2, 4, 64], F32)
                    Pm = sb(pc, "Pm", [128, 4, 2, 4, 64], BF16)
                    Rr = sb(pc, "Rr", [128, 2, 4, 64], F32)
                    w_out_sb = sb(pc, "w_out_sb", [128, 8, D], BF16)
                    xt2 = sb(pc, "xt2", [128, 2, 8, 128], F32)
                    BIAS = [Buf(f"bias{hp}") for hp in range(4)]
                    SBB = [Buf(f"sb{i}") for i in range(4)]
                    PMB = [Buf(f"pm{i}") for i in range(4)]
                    RRB = [Buf("rr0"), Buf("rr1")]
                    WO = [Buf("wo0"), Buf("wo1")]
                    XT2 = [Buf("xt20"), Buf("xt21")]
                    for hp in range(4):
                        T.dma("sp", bias[:, hp].rearrange("p a b c -> p (a b c)"), bias_d[l, hp], writes=[BIAS[hp]])
                        T.emit("act", lambda e, hp=hp: e.activation(out=bias[:, hp].rearrange("p a b c -> p (a b c)"),
                                                                    in_=bias[:, hp].rearrange("p a b c -> p (a b c)"), func=AF.Exp),
                               reads=[BIAS[hp]], writes=[BIAS[hp]])
                    for c in range(2):
                        T.dma("sp", w_out_sb[:, :, c * 512:(c + 1) * 512],
                              w_out_b[l][:, c * 512:(c + 1) * 512].rearrange("(k p) f -> p k f", p=128),
                              reads=[WB[("out", l)]], writes=[WO[c]])
                    units = [(r, hp) for r in range(32) for hp in range(4)]

                    def rstart(r):
                        return min(max(r - 4, 0), 24)

                    def emit_qk(ui):
                        r, hp = units[ui]
                        u4 = ui % 4
                        rs_ = rstart(r)
                        i0 = rs_ - r + 7
                        kts = sorted({(rs_ * 64) // 512, (rs_ * 64 + 511) // 512})
                        for half in (0, 1):
                            bk = (u4 // 2) * 2 + half
                            cb = (u4 % 2) * 256
                            p0 = 64 * half
                            for c in range(4):
                                ks = (rs_ + 2 * c) * 64
                                mm(banks[bk][:, cb + c * 64:cb + (c + 1) * 64], kT[p0:p0 + 64, hp, ks:ks + 128],
                                   qT[p0:p0 + 64, hp, r * 64:(r + 1) * 64], True, True,
                                   [KB[hp][t] for t in kts] + [QB[hp][r // 8]], [BK[bk]], c == 3)
                            T.emit("act", lambda e, bk=bk, cb=cb, u4=u4, half=half: e.activation(
                                out=Sb[:, u4, half], in_=banks[bk][:, cb:cb + 256].rearrange("p (c q) -> p c q", q=64),
                                func=AF.Exp), reads=[BK[bk]], writes=[SBB[u4]])
                        T.emit("pool" if (l >= POOL_FROM_LAYER and ui % 2 == 1) else "dve", lambda e, u4=u4, hp=hp, i0=i0: e.tensor_tensor(
                            out=Pm[:, u4], in0=Sb[:, u4], in1=bias[:, hp, :, i0:i0 + 7:2, :], op=ALU.mult),
                            reads=[SBB[u4], BIAS[hp]], writes=[PMB[u4]])

                    def emit_pv(ui):
                        r, hp = units[ui]
                        u2 = ui % 4
                        rs_ = rstart(r)
                        ob = 4 + (r % 2)
                        for half in (0, 1):
                            h = 2 * hp + half
                            for c in range(4):
                                sidx = rs_ + 2 * c
                                lhsT = V2[:, sidx, hp, 64 * half:64 * half + 128]
                                col = (hp * 2 + half) * 64
                                mm(banks[ob][:, col:col + 64], lhsT, Pm[:, u2, half, c, :], c == 0, c == 3,
                                   [VB[sidx], VONES, PMB[u2]], [BK[ob]], c == 3)
                        if hp == 3:
                            rb = r % 2
                            Ov = banks[ob][:, :].rearrange("p (a b q) -> p a b q", b=2, q=64)
                            T.emit("dve", lambda e, rb=rb, ob=ob: e.reciprocal(
                                out=Rr[0:64, rb], in_=banks[ob][64:128, :].rearrange("p (a b q) -> p a b q", b=2, q=64)[:, :, 0, :]),
                                reads=[BK[ob]], writes=[RRB[rb]])
                            T.emit("dve", lambda e, rb=rb, ob=ob: e.reciprocal(
                                out=Rr[64:128, rb], in_=banks[ob][0:64, :].rearrange("p (a b q) -> p a b q", b=2, q=64)[:, :, 1, :]),
                                reads=[BK[ob]], writes=[RRB[rb]])
                            T.emit("dve", lambda e, rb=rb, ob=ob, r=r: e.tensor_tensor(
                                out=H[0:64, 0:4, r * 64:(r + 1) * 64],
                                in0=banks[ob][0:64, :].rearrange("p (a b q) -> p a b q", b=2, q=64)[:, :, 0, :],
                                in1=Rr[0:64, rb], op=ALU.mult), reads=[BK[ob], RRB[rb]], writes=[HT[r // 4]])
                            T.emit("dve", lambda e, rb=rb, ob=ob, r=r: e.tensor_tensor(
                                out=H[64:128, 0:4, r * 64:(r + 1) * 64],
                                in0=banks[ob][64:128, :].rearrange("p (a b q) -> p a b q", b=2, q=64)[:, :, 1, :],
                                in1=Rr[64:128, rb], op=ALU.mult), reads=[BK[ob], RRB[rb]], writes=[HT[r // 4]])

                    for ui in range(len(units)):
                        emit_qk(ui)
                        if ui > 1:
                            emit_pv(ui - 2)
                    emit_pv(len(units) - 2)
                    emit_pv(len(units) - 1)
                    for i in range(16):
                        b = i % 2
                        c0 = i * 128
                        T.dma("sp", xt2[:, b], xT_d[:, :, tok0 + c0:tok0 + c0 + 128].rearrange("k p t -> p k t"),
                              reads=[XD[s][i // 4]], writes=[XT2[b]])
                        for m in range(8):
                            bk = 6 + (m % 2)
                            for k in range(8):
                                rhs = H[:, k, c0:c0 + 128] if k < 4 else pT[:, k - 4, c0:c0 + 128]
                                rd = [HT[i // 2]] if k < 4 else [PB[k - 4][i // 4]]
                                mm(banks[bk][:, 0:128], w_out_sb[:, k, m * 128:(m + 1) * 128], rhs, k == 0, k == 7,
                                   [WO[m // 4]] + rd, [BK[bk]], k == 7)
                            T.emit("dve", lambda e, b=b, m=m, bk=bk: e.tensor_tensor(
                                out=xt2[:, b, m, :], in0=banks[bk][:, 0:128], in1=xt2[:, b, m, :], op=ALU.add),
                                reads=[BK[bk], XT2[b]], writes=[XT2[b]])
                        T.dma("sp", xT_d[:, :, tok0 + c0:tok0 + c0 + 128].rearrange("k p t -> p k t"), xt2[:, b],
                              reads=[XT2[b]], writes=[XD[s][i // 4]])
                    T.barrier()

        def final_out(xT, XB, s):
            tok0 = s * S
            if True:
                if True:
                    with ExitStack() as po:
                        gF = sb(po, "gF", [128, D], F32)
                        ost = sb(po, "ost", [128, 1, D], F32)
                        junk = sb(po, "junk", [128, 512], BF16)
                        ssA = sb(po, "ssA", [128, 2], F32)
                        rsF = sb(po, "rsF", [128, 1], F32)
                        GF, OST, JK, SSA, RSF = Buf("gF"), [Buf("ost0"), Buf("ost1")], Buf("junk"), Buf("ssA"), Buf("rsF")
                        T.dma("sp", gF[:], gF_d, writes=[GF])
                        for j in range(16):
                            ob = 0
                            t = j // 4
                            for hb in range(2):
                                bk = 2 * (j % 2) + hb
                                for kk in range(4):
                                    k = hb * 4 + kk
                                    T.emit("pe", lambda e, bk=bk, kk=kk, k=k, j=j: e.transpose(
                                        out=banks[bk][:, kk * 128:(kk + 1) * 128], in_=xT[:, k, j * 128:(j + 1) * 128],
                                        identity=identf[:]), reads=[XB[k][t], CONST], writes=[BK[bk]], sig=(kk == 3))
                                T.emit("act", lambda e, bk=bk, hb=hb: e.activation(
                                    out=junk[:], in_=banks[bk][:, :], func=AF.Square, accum_out=ssA[:, hb:hb + 1]),
                                    reads=[BK[bk]], writes=[JK, SSA])
                            T.emit("dve", lambda e: e.tensor_tensor(out=rsF[:], in0=ssA[:, 0:1], in1=ssA[:, 1:2], op=ALU.add),
                                   reads=[SSA], writes=[RSF])
                            rstd_from_ss(rsF[:], rsF[:], [RSF], [RSF])
                            for hb in range(2):
                                bk = 2 * (j % 2) + hb
                                T.emit("dve", lambda e, bk=bk, hb=hb, ob=ob: e.scalar_tensor_tensor(
                                    out=ost[:, ob, hb * 512:(hb + 1) * 512], in0=banks[bk][:, :], scalar=rsF[:, 0:1],
                                    in1=gF[:, hb * 512:(hb + 1) * 512], op0=ALU.mult, op1=ALU.mult),
                                    reads=[BK[bk], RSF, GF], writes=[OST[ob]])
                            T.dma("sp", out_c[tok0 + j * 128:tok0 + (j + 1) * 128, :], ost[:, ob, :], reads=[OST[ob]],
                                  writes=[XD[s][t]])

        def f_phase_sparse(l, s, last):
            U32 = mybir.dt.uint32
            tok0 = s * S
            with ExitStack() as pf:
                xT = sb(pf, "xT", [128, 8, S], F32)
                ring = sb(pf, "ring", [128, 6, 4096], BF16)
                wr_sb = sb(pf, "wr_sb", [128, 8, 20], F32)
                wrp = sb(pf, "wrp", [128, 8, 20], F32)
                rt = sb(pf, "rt", [128, 16], F32)
                LGs = sb(pf, "LGs", [128, 16, 20], F32)
                r1 = sb(pf, "r1", [128, 16, 16], F32)
                gmax = sb(pf, "gmax", [128, 16], F32)
                goh = sb(pf, "goh", [128, 16, 4], F32)
                gex = sb(pf, "gex", [128, 16, 4], F32)
                gw = sb(pf, "gw", [128, 16], F32)
                esel = sb(pf, "esel", [128, 16, 4], F32)
                em = sb(pf, "em", [128, 16, 4], F32)
                m1 = sb(pf, "m1", [128, 16], F32)
                m2 = sb(pf, "m2", [128, 16], F32)
                oh1 = sb(pf, "oh1", [128, 16, 4], F32)
                oh2 = sb(pf, "oh2", [128, 16, 4], F32)
                w1 = sb(pf, "w1", [128, 16], F32)
                w2 = sb(pf, "w2", [128, 16], F32)
                M1 = sb(pf, "M1", [128, 16, 16], F32)
                M2 = sb(pf, "M2", [128, 16, 16], F32)
                Mb = sb(pf, "Mb", [128, 256], BF16)
                TOT = sb(pf, "TOT", [128, 16, 16], F32)
                JP = sb(pf, "JP", [128, 16, 16], F32)
                SL = sb(pf, "SL", [128, 16, 16], F32)
                ne = sb(pf, "ne", [128, 16], F32)
                ntl = sb(pf, "ntl", [128, 16], F32)
                base = sb(pf, "base", [128, 16], F32)
                bend = sb(pf, "bend", [128, 16], F32)
                b256 = sb(pf, "b256", [128, 16], F32)
                sl1 = sb(pf, "sl1", [128, 16], F32)
                sl2 = sb(pf, "sl2", [128, 16], F32)
                s1u = sb(pf, "s1u", [128, 16], U32)
                s2u = sb(pf, "s2u", [128, 16], U32)
                eidx = sb(pf, "eidx", [128, NTILE], F32)
                widf = sb(pf, "widf", [128, NTILE], F32)
                widx = sb(pf, "widx", [128, NTILE], U32)
                XB = [[Buf(f"X{k}_{t}") for t in range(4)] for k in range(8)]
                RSLOT = [Buf(f"ring{i}") for i in range(6)]
                WR, WRP = Buf("wr"), Buf("wrp")
                ROUT = Buf("router")
                HSB = [Buf(f"hs{i}") for i in range(32)]
                YSB = [Buf(f"ys{i}") for i in range(NTILE)]

                def load_tile_w(i, which=(0, 1, 2)):
                    for j, wp in enumerate((w_gate_p, w_up_p, w_down_p)):
                        if j not in which:
                            continue
                        slot = (3 * i + j) % 6
                        T.idma(ring[:, slot, :], None, wp[l], widx[:, i:i + 1],
                               reads=[ROUT] + [WB[(("g", "u", "d")[j], l, e_)] for e_ in range(NE)], writes=[RSLOT[slot]])

                for k in range(8):
                    T.dma("sp", xT[:, k, :], xT_d[k, :, tok0:tok0 + S], reads=[XD[s][t] for t in range(4)],
                          writes=[XB[k][t] for t in range(4)])
                T.dma("sp", wr_sb[:], wr_d[:, l], writes=[WR])
                for k in range(8):
                    T.emit("dve", lambda e, k=k: e.tensor_scalar(out=wrp[:, k, :], in0=wr_sb[:, k, :],
                                                                 scalar1=gf[:, l * 8 + k:l * 8 + k + 1], scalar2=None,
                                                                 op0=ALU.mult), reads=[WR, CONST], writes=[WRP])

                def dv(fn, extra_reads=()):
                    T.emit("dve", fn, reads=[ROUT] + list(extra_reads), writes=[ROUT])

                def b3(ap2, n):
                    return ap2.unsqueeze(2).broadcast_to([128, 16, n])

                with ExitStack() as pa:
                    H = sb(pa, "Hf", [128, 8, S], BF16)
                    sq = sb(pa, "sqf", [128, 8, 512], BF16)
                    rstd = sb(pa, "rstdf", [128, 512], F32)
                    hrow = sb(pa, "hrow", [128, 2, D], BF16)
                    HB = [Buf(f"Hf{t}") for t in range(4)]
                    SQ, RS = Buf("sq"), Buf("rs")
                    HROW = [Buf("hrow0"), Buf("hrow1")]
                    for t in range(4):
                        c0 = t * 512
                        T.emit("act", lambda e, c0=c0: e.activation(out=sq[:], in_=xT[:, :, c0:c0 + 512], func=AF.Square),
                               reads=[XB[k][t] for k in range(8)], writes=[SQ])
                        for k in range(8):
                            mm(banks[6][:, :], onesb[:], sq[:, k, :], k == 0, k == 7, [SQ, CONST], [BK[6]], k == 7)
                        rstd_from_ss(rstd[:], banks[6][:, :], [BK[6]], [RS])
                        for k in range(8):
                            T.emit("dve", lambda e, k=k, c0=c0: e.scalar_tensor_tensor(
                                out=H[:, k, c0:c0 + 512], in0=xT[:, k, c0:c0 + 512], scalar=gf[:, l * 8 + k:l * 8 + k + 1],
                                in1=rstd[:], op0=ALU.mult, op1=ALU.mult), reads=[XB[k][t], RS, CONST], writes=[HB[t]])
                        for j in range(4):
                            jj = 4 * t + j
                            for k in range(8):
                                mm(banks[7][:, 320 + jj:320 + jj + 1], sq[:, k, j * 128:(j + 1) * 128], onesb[:, 0:1],
                                   k == 0, k == 7, [SQ, CONST], [BK[7]], False)
                            for k in range(8):
                                mm(banks[7][:, jj * 20:(jj + 1) * 20], xT[:, k, c0 + j * 128:c0 + (j + 1) * 128], wrp[:, k, :],
                                   k == 0, k == 7, [XB[k][t], WRP], [BK[7]], (k == 7))
                    rstd_from_ss(rt[:], banks[7][:, 320:336], [BK[7], ROUT], [ROUT])
                    dv(lambda e: e.tensor_tensor(out=LGs[:], in0=banks[7][:, 0:320].rearrange("p (j e) -> p j e", e=20),
                                                 in1=b3(rt[:], 20), op=ALU.mult), [BK[7]])
                    dv(lambda e: e.tensor_reduce(out=gmax[:], in_=LGs[:, :, 0:4], axis=AX.X, op=ALU.max))
                    dv(lambda e: e.tensor_tensor(out=goh[:], in0=LGs[:, :, 0:4], in1=b3(gmax[:], 4), op=ALU.is_equal))
                    dv(lambda e: e.tensor_tensor(out=gex[:], in0=LGs[:, :, 0:4], in1=b3(gmax[:], 4), op=ALU.subtract))
                    T.emit("act", lambda e: e.activation(out=gex[:], in_=gex[:], func=AF.Exp), reads=[ROUT], writes=[ROUT])
                    dv(lambda e: e.tensor_reduce(out=gw[:], in_=gex[:], axis=AX.X, op=ALU.add))
                    dv(lambda e: e.reciprocal(out=gw[:], in_=gw[:]))
                    dv(lambda e: e.tensor_tensor(out=r1[:].rearrange("p j (g i) -> p j g i", i=4),
                                                 in0=LGs[:, :, 4:20].rearrange("p j (g i) -> p j g i", i=4),
                                                 in1=goh[:].unsqueeze(3).broadcast_to([128, 16, 4, 4]), op=ALU.mult))
                    dv(lambda e: e.tensor_reduce(out=esel[:], in_=r1[:].rearrange("p j (g i) -> p j i g", i=4),
                                                 axis=AX.X, op=ALU.add))
                    dv(lambda e: e.tensor_reduce(out=m1[:], in_=esel[:], axis=AX.X, op=ALU.max))
                    dv(lambda e: e.tensor_tensor(out=oh1[:], in0=esel[:], in1=b3(m1[:], 4), op=ALU.is_equal))
                    dv(lambda e: e.scalar_tensor_tensor(out=em[:], in0=oh1[:], scalar=-1.0e30, in1=esel[:],
                                                        op0=ALU.mult, op1=ALU.add))
                    dv(lambda e: e.tensor_reduce(out=m2[:], in_=em[:], axis=AX.X, op=ALU.max))
                    dv(lambda e: e.tensor_tensor(out=oh2[:], in0=em[:], in1=b3(m2[:], 4), op=ALU.is_equal))
                    dv(lambda e: e.tensor_tensor(out=w2[:], in0=m2[:], in1=m1[:], op=ALU.subtract))
                    T.emit("act", lambda e: e.activation(out=w2[:], in_=w2[:], func=AF.Exp), reads=[ROUT], writes=[ROUT])
                    dv(lambda e: e.tensor_scalar(out=w2[:], in0=w2[:], scalar1=1.0, scalar2=None, op0=ALU.add))
                    dv(lambda e: e.reciprocal(out=w1[:], in_=w2[:]))
                    dv(lambda e: e.tensor_tensor(out=w1[:], in0=w1[:], in1=gw[:], op=ALU.mult))
                    dv(lambda e: e.tensor_tensor(out=w2[:], in0=gw[:], in1=w1[:], op=ALU.subtract))
                    g44 = lambda ap: ap.rearrange("p j (g i) -> p j g i", i=4)
                    dv(lambda e: e.tensor_tensor(out=g44(M1[:]), in0=goh[:].unsqueeze(3).broadcast_to([128, 16, 4, 4]),
                                                 in1=oh1[:].unsqueeze(2).broadcast_to([128, 16, 4, 4]), op=ALU.mult))
                    dv(lambda e: e.tensor_tensor(out=g44(M2[:]), in0=goh[:].unsqueeze(3).broadcast_to([128, 16, 4, 4]),
                                                 in1=oh2[:].unsqueeze(2).broadcast_to([128, 16, 4, 4]), op=ALU.mult))
                    dv(lambda e: e.tensor_tensor(out=Mb[:].rearrange("p (j e) -> p j e", e=16), in0=M1[:], in1=M2[:], op=ALU.add))
                    mm(banks[6][:, 0:256], ustr[:], Mb[:], True, True, [ROUT, CONST], [BK[6]], True)
                    mm(banks[7][:, 0:256], onesb[:], Mb[:], True, True, [ROUT, CONST], [BK[7]], True)
                    dv(lambda e: e.tensor_copy(out=TOT[:].rearrange("p j e -> p (j e)"), in_=banks[7][:, 0:256]), [BK[7]])
                    dv(lambda e: e.memset(JP[:, 0, :], 0.0))
                    for j in range(1, 16):
                        dv(lambda e, j=j: e.tensor_tensor(out=JP[:, j, :], in0=JP[:, j - 1, :], in1=TOT[:, j - 1, :], op=ALU.add))
                    dv(lambda e: e.tensor_tensor(out=ne[:], in0=JP[:, 15, :], in1=TOT[:, 15, :], op=ALU.add))
                    dv(lambda e: e.tensor_scalar(out=ntl[:], in0=ne[:], scalar1=0.0, scalar2=None, op0=ALU.is_gt))
                    for q in range(1, S // TS):
                        dv(lambda e, q=q: e.scalar_tensor_tensor(out=ntl[:], in0=ne[:], scalar=float(TS * q), in1=ntl[:],
                                                                 op0=ALU.is_gt, op1=ALU.add))
                    dv(lambda e: e.memset(base[:, 0:1], 0.0))
                    for e_ in range(1, 16):
                        dv(lambda e, e_=e_: e.tensor_tensor(out=base[:, e_:e_ + 1], in0=base[:, e_ - 1:e_],
                                                            in1=ntl[:, e_ - 1:e_], op=ALU.add))
                    dv(lambda e: e.tensor_tensor(out=bend[:], in0=base[:], in1=ntl[:], op=ALU.add))
                    dv(lambda e: e.tensor_scalar(out=b256[:], in0=base[:], scalar1=float(TS), scalar2=None, op0=ALU.mult))
                    dv(lambda e: e.tensor_tensor(out=SL[:], in0=banks[6][:, 0:256].rearrange("p (j e) -> p j e", e=16),
                                                 in1=JP[:], op=ALU.add), [BK[6]])
                    dv(lambda e: e.tensor_tensor(out=SL[:], in0=SL[:], in1=b256[:].unsqueeze(1).broadcast_to([128, 16, 16]),
                                                 op=ALU.add))
                    dv(lambda e: e.tensor_tensor(out=M1[:], in0=M1[:], in1=SL[:], op=ALU.mult))
                    dv(lambda e: e.tensor_tensor(out=M2[:], in0=M2[:], in1=SL[:], op=ALU.mult))
                    dv(lambda e: e.tensor_reduce(out=sl1[:], in_=M1[:], axis=AX.X, op=ALU.add))
                    dv(lambda e: e.tensor_reduce(out=sl2[:], in_=M2[:], axis=AX.X, op=ALU.add))
                    dv(lambda e: e.tensor_copy(out=s1u[:], in_=sl1[:]))
                    dv(lambda e: e.tensor_copy(out=s2u[:], in_=sl2[:]))
                    dv(lambda e: e.memset(eidx[:], 0.0))
                    for e_ in range(16):
                        dv(lambda e, e_=e_: e.scalar_tensor_tensor(out=eidx[:], in0=iotas[:, 1:1 + NTILE], scalar=bend[:, e_:e_ + 1],
                                                                   in1=eidx[:], op0=ALU.is_ge, op1=ALU.add), [CONST])
                    dv(lambda e: e.tensor_scalar(out=eidx[:], in0=eidx[:], scalar1=15.0, scalar2=None, op0=ALU.min))
                    dv(lambda e: e.tensor_scalar(out=widf[:], in0=eidx[:], scalar1=128.0, scalar2=iotas[:, 0:1],
                                                 op0=ALU.mult, op1=ALU.add), [CONST])
                    dv(lambda e: e.tensor_copy(out=widx[:], in_=widf[:]))
                    load_tile_w(0)
                    load_tile_w(1, which=(0, 1))
                    for j in range(16):
                        hb = j % 2
                        bk = 4 + hb
                        pv = banks[bk][:, :].bitcast(BF16)
                        for k in range(8):
                            T.emit("pe", lambda e, pv=pv, k=k, j=j: e.transpose(
                                out=pv[:, k * 128:(k + 1) * 128], in_=H[:, k, j * 128:(j + 1) * 128], identity=identb[:]),
                                reads=[HB[j // 4], CONST], writes=[BK[bk]], sig=(k == 7))
                        evac_copy(hrow[:, hb, :], pv, [BK[bk]], [HROW[hb]])
                        T.idma(HS_d, s1u[:, j:j + 1], hrow[:, hb, :], None, reads=[HROW[hb], ROUT], writes=[HSB[2 * j]])
                        T.idma(HS_d, s2u[:, j:j + 1], hrow[:, hb, :], None, reads=[HROW[hb], ROUT], writes=[HSB[2 * j + 1]])
                    T.barrier()

                with ExitStack() as pbx:
                    hs_sb = sb(pbx, "hs_sb", [128, 3, 2, D], BF16)
                    hcT = sb(pbx, "hcT", [128, 2, 8, TS], BF16)
                    a_e = sb(pbx, "a_e", [128, 2, 4, TS], BF16)
                    sg = sb(pbx, "sg", [128, 2, TS], F32)
                    ys = sb(pbx, "ys", [128, 2, 2, D], F32)
                    HSS = [Buf("hss0"), Buf("hss1"), Buf("hss2")]
                    HCT = [Buf("hct0"), Buf("hct1")]
                    AE = [[Buf(f"ae{b}_{f}") for f in range(4)] for b in range(2)]
                    SG = [Buf("sg0"), Buf("sg1")]
                    YS = [Buf("ysb0"), Buf("ysb1")]

                    def emit_hs(i):
                        hb3 = i % 3
                        T.dma("sp", hs_sb[:, hb3], HS_d[i * TS:(i + 1) * TS, :].rearrange("(a p) d -> p a d", p=128),
                              reads=HSB, writes=[HSS[hb3]])

                    def emit_tr(i):
                        ab = i % 2
                        hb3 = i % 3
                        for hb in range(2):
                            bk = 6 + hb
                            pv = banks[bk][:, :].bitcast(BF16)
                            for kk in range(4):
                                k = hb * 4 + kk
                                for a in range(2):
                                    T.emit("pe", lambda e, pv=pv, kk=kk, a=a, k=k, hb3=hb3: e.transpose(
                                        out=pv[:, kk * TS + a * 128:kk * TS + (a + 1) * 128],
                                        in_=hs_sb[:, hb3, a, k * 128:(k + 1) * 128], identity=identb[:]),
                                        reads=[HSS[hb3], CONST], writes=[BK[bk]], sig=(kk == 3 and a == 1))
                            evac_copy(hcT[:, ab, hb * 4:(hb + 1) * 4, :].rearrange("p k t -> p (k t)"), pv, [BK[bk]], [HCT[ab]])

                    def emit_gu(i):
                        ab = i % 2
                        sg_, su_ = (3 * i) % 6, (3 * i + 1) % 6
                        for f in range(4):
                            bg, bu = f % 2, 2 + f % 2
                            for k in range(8):
                                mm(banks[bg][:, 0:TS], ring[:, sg_, k * 512 + f * 128:k * 512 + (f + 1) * 128], hcT[:, ab, k, :],
                                   k == 0, k == 7, [RSLOT[sg_], HCT[ab]], [BK[bg]], k == 7)
                            for k in range(8):
                                mm(banks[bu][:, 0:TS], ring[:, su_, k * 512 + f * 128:k * 512 + (f + 1) * 128], hcT[:, ab, k, :],
                                   k == 0, k == 7, [RSLOT[su_], HCT[ab]], [BK[bu]], k == 7)
                            i2 = f % 2
                            T.emit("act", lambda e, bg=bg, i2=i2: e.activation(out=sg[:, i2, :], in_=banks[bg][:, 0:TS], func=AF.Silu),
                                   reads=[BK[bg]], writes=[SG[i2]])
                            T.emit("dve", lambda e, bu=bu, i2=i2, ab=ab, f=f: e.tensor_tensor(
                                out=a_e[:, ab, f, :], in0=banks[bu][:, 0:TS], in1=sg[:, i2, :], op=ALU.mult),
                                reads=[BK[bu], SG[i2]], writes=[AE[ab][f]])

                    def emit_d(i):
                        ab = i % 2
                        sd_ = (3 * i + 2) % 6
                        for a in range(2):
                            for dh in range(2):
                                bk = 4 + dh
                                for f in range(4):
                                    mm(banks[bk][:, :], a_e[:, ab, f, a * 128:(a + 1) * 128],
                                       ring[:, sd_, f * 1024 + dh * 512:f * 1024 + (dh + 1) * 512],
                                       f == 0, f == 3, [RSLOT[sd_], AE[ab][f]], [BK[bk]], f == 3)
                                evac_copy(ys[:, ab, a, dh * 512:(dh + 1) * 512], banks[bk][:, :], [BK[bk]], [YS[ab]])
                        T.dma("sp", YS_d[i * TS:(i + 1) * TS, :].rearrange("(a p) d -> p a d", p=128), ys[:, ab],
                              reads=[YS[ab]], writes=[YSB[i]])

                    emit_hs(0)
                    emit_hs(1)
                    emit_tr(0)
                    for i in range(NTILE):
                        if i + 2 < NTILE:
                            emit_hs(i + 2)
                        if i + 1 < NTILE:
                            emit_tr(i + 1)
                        emit_gu(i)
                        if i + 2 < NTILE:
                            load_tile_w(i + 2, which=(0, 1))
                        if i > 0:
                            emit_d(i - 1)
                        if i + 1 < NTILE:
                            load_tile_w(i + 1, which=(2,))
                    emit_d(NTILE - 1)
                    T.barrier()

                with ExitStack() as pcx:
                    g0 = sb(pcx, "g0", [128, 4, D], F32)
                    g1 = sb(pcx, "g1", [128, 4, D], F32)
                    G0 = [Buf(f"g0{i}") for i in range(4)]
                    G1 = [Buf(f"g1{i}") for i in range(4)]
                    for j in range(16):
                        b = j % 4
                        T.idma(g0[:, b, :], None, YS_d, s1u[:, j:j + 1], reads=YSB + [ROUT], writes=[G0[b]])
                        T.idma(g1[:, b, :], None, YS_d, s2u[:, j:j + 1], reads=YSB + [ROUT], writes=[G1[b]])
                        T.emit("dve", lambda e, b=b, j=j: e.tensor_scalar(out=g0[:, b, :], in0=g0[:, b, :], scalar1=w1[:, j:j + 1],
                                                                          scalar2=None, op0=ALU.mult), reads=[G0[b], ROUT], writes=[G0[b]])
                        T.emit("dve", lambda e, b=b, j=j: e.scalar_tensor_tensor(out=g0[:, b, :], in0=g1[:, b, :], scalar=w2[:, j:j + 1],
                                                                                 in1=g0[:, b, :], op0=ALU.mult, op1=ALU.add),
                               reads=[G0[b], G1[b], ROUT], writes=[G0[b]])
                        for hb in range(2):
                            bk = 2 * (j % 2) + hb
                            for kk in range(4):
                                k = hb * 4 + kk
                                T.emit("pe", lambda e, bk=bk, kk=kk, k=k, b=b: e.transpose(
                                    out=banks[bk][:, kk * 128:(kk + 1) * 128], in_=g0[:, b, k * 128:(k + 1) * 128],
                                    identity=identf[:]), reads=[G0[b], CONST], writes=[BK[bk]], sig=(kk == 3))
                            T.emit("dve", lambda e, bk=bk, hb=hb, j=j: e.tensor_tensor(
                                out=xT[:, hb * 4:(hb + 1) * 4, j * 128:(j + 1) * 128],
                                in0=banks[bk][:, :].rearrange("p (k t) -> p k t", t=128),
                                in1=xT[:, hb * 4:(hb + 1) * 4, j * 128:(j + 1) * 128], op=ALU.add),
                                reads=[BK[bk]] + [XB[hb * 4 + kk][j // 4] for kk in range(4)],
                                writes=[XB[hb * 4 + kk][j // 4] for kk in range(4)])

                if not last:
                    for k in range(8):
                        T.dma("sp", xT_d[k, :, tok0:tok0 + S], xT[:, k, :], reads=[XB[k][t] for t in range(4)],
                              writes=[XD[s][t] for t in range(4)])
                else:
                    T.barrier()
                    final_out(xT, XB, s)
                T.barrier()

        def f_phase(l, s, last):
            tok0 = s * S
            with ExitStack() as pf:
                xT = sb(pf, "xT", [128, 8, S], F32)
                H = sb(pf, "Hf", [128, 8, S], BF16)
                ring = sb(pf, "ring", [128, 6, 4096], BF16)
                sq = sb(pf, "sqf", [128, 8, 512], BF16)
                rstd = sb(pf, "rstdf", [128, 512], F32)
                wr_sb = sb(pf, "wr_sb", [128, 8, 20], F32)
                wrp = sb(pf, "wrp", [128, 8, 20], F32)
                a_e = sb(pf, "a_e", [128, 2, 4, 512], BF16)
                sg = sb(pf, "sg", [128, 2, 512], F32)
                tt = sb(pf, "tt", [128, 2, 512], F32)
                Cb = sb(pf, "Cb", [128, 2, 512], F32)
                rt = sb(pf, "rt", [128, 16], F32)
                LGs = sb(pf, "LGs", [128, 16, 20], F32)
                r1 = sb(pf, "r1", [128, 16, 16], F32)
                r2 = sb(pf, "r2", [128, 16, 16], F32)
                gmax = sb(pf, "gmax", [128, 16], F32)
                goh = sb(pf, "goh", [128, 16, 4], F32)
                gex = sb(pf, "gex", [128, 16, 4], F32)
                gw = sb(pf, "gw", [128, 16], F32)
                esel = sb(pf, "esel", [128, 16, 4], F32)
                em = sb(pf, "em", [128, 16, 4], F32)
                m1 = sb(pf, "m1", [128, 16], F32)
                m2 = sb(pf, "m2", [128, 16], F32)
                oh1 = sb(pf, "oh1", [128, 16, 4], F32)
                oh2 = sb(pf, "oh2", [128, 16, 4], F32)
                w1 = sb(pf, "w1", [128, 16], F32)
                w2 = sb(pf, "w2", [128, 16], F32)
                c4 = sb(pf, "c4", [128, 16, 4], F32)
                Cm = sb(pf, "Cm", [128, 16, 16], F32)
                Chi = sb(pf, "Chi", [128, 16, 16], BF16)
                Clo = sb(pf, "Clo", [128, 16, 16], BF16)
                XB = [[Buf(f"X{k}_{t}") for t in range(4)] for k in range(8)]
                HB = [Buf(f"Hf{t}") for t in range(4)]
                RSLOT = [Buf(f"ring{i}") for i in range(6)]
                SQ, RS, WR, WRP, RT = Buf("sq"), Buf("rs"), Buf("wr"), Buf("wrp"), Buf("rt")
                ROUT = Buf("router")
                AE = [[Buf(f"ae{b}_{f}") for f in range(4)] for b in range(2)]
                SG = [Buf(f"sg{i}") for i in range(2)]
                TT = [Buf(f"tt{i}") for i in range(2)]
                CB = [Buf("cb0"), Buf("cb1")]

                def load_expert(e):
                    e4 = e // 4
                    for j, (nm, wb) in enumerate((("g", w_gate_b), ("u", w_up_b))):
                        slot = (3 * e + j) % 6
                        T.dma("sp", ring[:, slot, :].rearrange("p (k f) -> p k f", f=512),
                              wb[l, e].rearrange("(k p) f -> p k f", p=128), reads=[WB[(nm, l, e4)]], writes=[RSLOT[slot]])
                    slot = (3 * e + 2) % 6
                    T.dma("sp", ring[:, slot, :].rearrange("p (k f) -> p k f", f=1024),
                          w_down_b[l, e].rearrange("(k p) f -> p k f", p=128), reads=[WB[("d", l, e4)]], writes=[RSLOT[slot]])

                for k in range(8):
                    T.dma("sp", xT[:, k, :], xT_d[k, :, tok0:tok0 + S], reads=[XD[s][t] for t in range(4)],
                          writes=[XB[k][t] for t in range(4)])
                T.dma("sp", wr_sb[:], wr_d[:, l], writes=[WR])
                load_expert(0)
                for k in range(8):
                    T.emit("dve", lambda e, k=k: e.tensor_scalar(out=wrp[:, k, :], in0=wr_sb[:, k, :],
                                                                 scalar1=gf[:, l * 8 + k:l * 8 + k + 1], scalar2=None,
                                                                 op0=ALU.mult), reads=[WR, CONST], writes=[WRP])
                for t in range(4):
                    c0 = t * 512
                    T.emit("act", lambda e, c0=c0: e.activation(out=sq[:], in_=xT[:, :, c0:c0 + 512], func=AF.Square),
                           reads=[XB[k][t] for k in range(8)], writes=[SQ])
                    for k in range(8):
                        mm(banks[6][:, :], onesb[:], sq[:, k, :], k == 0, k == 7, [SQ, CONST], [BK[6]], k == 7)
                    rstd_from_ss(rstd[:], banks[6][:, :], [BK[6]], [RS])
                    for k in range(8):
                        T.emit("dve", lambda e, k=k, c0=c0: e.scalar_tensor_tensor(
                            out=H[:, k, c0:c0 + 512], in0=xT[:, k, c0:c0 + 512], scalar=gf[:, l * 8 + k:l * 8 + k + 1],
                            in1=rstd[:], op0=ALU.mult, op1=ALU.mult), reads=[XB[k][t], RS, CONST], writes=[HB[t]])
                    for j in range(4):
                        jj = 4 * t + j
                        for k in range(8):
                            mm(banks[7][:, 320 + jj:320 + jj + 1], sq[:, k, j * 128:(j + 1) * 128], onesb[:, 0:1],
                               k == 0, k == 7, [SQ, CONST], [BK[7]], False)
                        for k in range(8):
                            mm(banks[7][:, jj * 20:(jj + 1) * 20], xT[:, k, c0 + j * 128:c0 + (j + 1) * 128], wrp[:, k, :],
                               k == 0, k == 7, [XB[k][t], WRP], [BK[7]], (k == 7))
                R_ = [ROUT]

                def dv(fn, extra_reads=()):
                    T.emit("dve", fn, reads=[ROUT] + list(extra_reads), writes=[ROUT])

                def b3(ap2, n):
                    return ap2.unsqueeze(2).broadcast_to([128, 16, n])

                rstd_from_ss(rt[:], banks[7][:, 320:336], [BK[7], ROUT], [ROUT])
                dv(lambda e: e.tensor_tensor(out=LGs[:], in0=banks[7][:, 0:320].rearrange("p (j e) -> p j e", e=20),
                                             in1=b3(rt[:], 20), op=ALU.mult), [BK[7]])
                dv(lambda e: e.tensor_reduce(out=gmax[:], in_=LGs[:, :, 0:4], axis=AX.X, op=ALU.max))
                dv(lambda e: e.tensor_tensor(out=goh[:], in0=LGs[:, :, 0:4], in1=b3(gmax[:], 4), op=ALU.is_equal))
                dv(lambda e: e.tensor_tensor(out=gex[:], in0=LGs[:, :, 0:4], in1=b3(gmax[:], 4), op=ALU.subtract))
                T.emit("act", lambda e: e.activation(out=gex[:], in_=gex[:], func=AF.Exp), reads=[ROUT], writes=[ROUT])
                dv(lambda e: e.tensor_reduce(out=gw[:], in_=gex[:], axis=AX.X, op=ALU.add))
                dv(lambda e: e.reciprocal(out=gw[:], in_=gw[:]))
                dv(lambda e: e.tensor_tensor(out=r1[:].rearrange("p j (g i) -> p j g i", i=4),
                                             in0=LGs[:, :, 4:20].rearrange("p j (g i) -> p j g i", i=4),
                                             in1=goh[:].unsqueeze(3).broadcast_to([128, 16, 4, 4]), op=ALU.mult))
                dv(lambda e: e.tensor_reduce(out=esel[:], in_=r1[:].rearrange("p j (g i) -> p j i g", i=4),
                                             axis=AX.X, op=ALU.add))
                dv(lambda e: e.tensor_reduce(out=m1[:], in_=esel[:], axis=AX.X, op=ALU.max))
                dv(lambda e: e.tensor_tensor(out=oh1[:], in0=esel[:], in1=b3(m1[:], 4), op=ALU.is_equal))
                dv(lambda e: e.scalar_tensor_tensor(out=em[:], in0=oh1[:], scalar=-1.0e30, in1=esel[:],
                                                    op0=ALU.mult, op1=ALU.add))
                dv(lambda e: e.tensor_reduce(out=m2[:], in_=em[:], axis=AX.X, op=ALU.max))
                dv(lambda e: e.tensor_tensor(out=oh2[:], in0=em[:], in1=b3(m2[:], 4), op=ALU.is_equal))
                dv(lambda e: e.tensor_tensor(out=w2[:], in0=m2[:], in1=m1[:], op=ALU.subtract))
                T.emit("act", lambda e: e.activation(out=w2[:], in_=w2[:], func=AF.Exp), reads=[ROUT], writes=[ROUT])
                dv(lambda e: e.tensor_scalar(out=w2[:], in0=w2[:], scalar1=1.0, scalar2=None, op0=ALU.add))
                dv(lambda e: e.reciprocal(out=w1[:], in_=w2[:]))
                dv(lambda e: e.tensor_tensor(out=w1[:], in0=w1[:], in1=gw[:], op=ALU.mult))
                dv(lambda e: e.tensor_tensor(out=w2[:], in0=gw[:], in1=w1[:], op=ALU.subtract))
                dv(lambda e: e.tensor_tensor(out=c4[:], in0=oh1[:], in1=b3(w1[:], 4), op=ALU.mult))
                dv(lambda e: e.tensor_tensor(out=oh2[:], in0=oh2[:], in1=b3(w2[:], 4), op=ALU.mult))
                dv(lambda e: e.tensor_tensor(out=c4[:], in0=c4[:], in1=oh2[:], op=ALU.add))
                dv(lambda e: e.tensor_tensor(out=Cm[:].rearrange("p j (g i) -> p j g i", i=4),
                                             in0=goh[:].unsqueeze(3).broadcast_to([128, 16, 4, 4]),
                                             in1=c4[:].unsqueeze(2).broadcast_to([128, 16, 4, 4]), op=ALU.mult))
                dv(lambda e: e.tensor_copy(out=Chi[:], in_=Cm[:]))
                dv(lambda e: e.tensor_tensor(out=r2[:], in0=Cm[:], in1=Chi[:], op=ALU.subtract))
                dv(lambda e: e.tensor_copy(out=Clo[:], in_=r2[:]))

                steps = [(e_, t_) for e_ in range(NE) for t_ in range(4)]

                def emit_gu(idx):
                    e_, t = steps[idx]
                    ab = idx % 2
                    c0 = t * 512
                    sg_, su_, sd_ = (3 * e_) % 6, (3 * e_ + 1) % 6, (3 * e_ + 2) % 6
                    n = 0
                    for j in range(4):
                        for Cx in (Chi, Clo):
                            mm(banks[6][:, j * 128:(j + 1) * 128], Cx[:, 4 * t + j, e_:e_ + 1].broadcast_to([128, 128]),
                               identb[:], Cx is Chi, Cx is Clo, [ROUT, CONST], [BK[6]], (j == 3 and Cx is Clo))
                    T.emit("act", lambda e, ab=ab: e.activation(out=Cb[:, ab, :], in_=banks[6][:, :], func=AF.Copy),
                           reads=[BK[6]], writes=[CB[ab]])
                    for f in range(4):
                        bg, bu = f % 2, 2 + f % 2
                        for k in range(8):
                            mm(banks[bg][:, :], ring[:, sg_, k * 512 + f * 128:k * 512 + (f + 1) * 128], H[:, k, c0:c0 + 512],
                               k == 0, k == 7, [RSLOT[sg_], HB[t]], [BK[bg]], k == 7)
                        for k in range(8):
                            mm(banks[bu][:, :], ring[:, su_, k * 512 + f * 128:k * 512 + (f + 1) * 128], H[:, k, c0:c0 + 512],
                               k == 0, k == 7, [RSLOT[su_], HB[t]], [BK[bu]], k == 7)
                        i3 = f % 2
                        T.emit("act", lambda e, bg=bg, i3=i3: e.activation(out=sg[:, i3, :], in_=banks[bg][:, :], func=AF.Silu),
                               reads=[BK[bg]], writes=[SG[i3]])
                        T.emit("dve", lambda e, bu=bu, i3=i3: e.tensor_tensor(out=tt[:, i3, :], in0=banks[bu][:, :],
                                                                            in1=sg[:, i3, :], op=ALU.mult),
                               reads=[BK[bu], SG[i3]], writes=[TT[i3]])
                        T.emit("dve", lambda e, ab=ab, f=f, i3=i3: e.tensor_tensor(out=a_e[:, ab, f, :], in0=tt[:, i3, :],
                                                                                   in1=Cb[:, ab, :], op=ALU.mult),
                               reads=[TT[i3], CB[ab]], writes=[AE[ab][f]])

                def emit_d(idx):
                    e_, t = steps[idx]
                    ab = idx % 2
                    c0 = t * 512
                    sd_ = (3 * e_ + 2) % 6
                    for m in range(8):
                        bk = 4 + m % 2
                        for f in range(4):
                            mm(banks[bk][:, :], ring[:, sd_, f * 1024 + m * 128:f * 1024 + (m + 1) * 128], a_e[:, ab, f, :],
                               f == 0, f == 3, [RSLOT[sd_], AE[ab][f]], [BK[bk]], f == 3)
                        T.emit("dve", lambda e, m=m, bk=bk, c0=c0: e.tensor_tensor(
                            out=xT[:, m, c0:c0 + 512], in0=banks[bk][:, :], in1=xT[:, m, c0:c0 + 512], op=ALU.add),
                            reads=[BK[bk], XB[m][t]], writes=[XB[m][t]])

                for idx in range(len(steps)):
                    e_, t = steps[idx]
                    emit_gu(idx)
                    if idx > 0:
                        emit_d(idx - 1)
                    if t == 0 and e_ + 1 < NE:
                        load_expert(e_ + 1)
                emit_d(len(steps) - 1)

                if not last:
                    for k in range(8):
                        T.dma("sp", xT_d[k, :, tok0:tok0 + S], xT[:, k, :], reads=[XB[k][t] for t in range(4)],
                              writes=[XD[s][t] for t in range(4)])
                else:
                    final_out(xT, XB, s)
                T.barrier()

        done = False
        for l in range(depth):
            for s in range(nseq):
                m_phase(l, s)
            if stop_after == ("M", l):
                done = True
                break
            for s in range(nseq):
                (f_phase_sparse if SPARSE else f_phase)(l, s, last=(l == depth - 1 and stop_after is None))
            if stop_after == ("F", l):
                done = True
                break
        T.barrier()
        T.flush()
    return nc


def _bias_index_map():
    hp = np.arange(4)[:, None, None, None, None]
    p = np.arange(128)[None, :, None, None, None]
    eo = np.arange(2)[None, None, :, None, None]
    i = np.arange(14)[None, None, None, :, None]
    cq = np.arange(64)[None, None, None, None, :]
    kc = p % 64
    half = p // 64
    h = 2 * hp + eo
    dr = i + half
    dc = np.clip(kc - cq, -15, 15) + 15
    qs = np.clip(cq - 8, 0, 48)
    valid = (kc >= qs) & (kc < qs + 16)
    flat = h * (15 * 31 + 1) + np.where(valid, dr * 31 + dc, 15 * 31)
    return np.broadcast_to(flat, (4, 128, 2, 14, 64)).copy()


def _pool_consts():
    import ml_dtypes
    out = np.zeros((4, 7, 128, 128), np.float32)
    Sr = 512
    t = np.arange(Sr)
    for g, w in enumerate(POOL_W):
        lo = np.clip(t - w // 2, 0, Sr)
        hi = np.clip(t - w // 2 + w, 0, Sr)
        cnt = (hi - lo).astype(np.float64)
        A = np.zeros((Sr, Sr), np.float64)
        for tt_ in range(Sr):
            A[lo[tt_]:hi[tt_], tt_] = 1.0 / cnt[tt_]
        A -= np.eye(Sr)

        def blk(a, b):
            return A[a * 128:(a + 1) * 128, b * 128:(b + 1) * 128]

        def hilo(M):
            hi_ = M.astype(np.float32).astype(ml_dtypes.bfloat16).astype(np.float64)
            lo_ = (M - hi_).astype(np.float32).astype(ml_dtypes.bfloat16).astype(np.float64)
            return hi_, lo_

        out[g, 0] = blk(1, 2)
        out[g, 1] = blk(2, 2)
        out[g, 2] = blk(3, 2)
        out[g, 3], out[g, 4] = hilo(blk(0, 0))
        out[g, 5], out[g, 6] = hilo(blk(3, 3))
    return np.ascontiguousarray(out.reshape(28, 128, 128).transpose(1, 0, 2))


_BIAS_MAP = None
_PROG = {}


def prep_inputs(inp, nseq=NSEQ_FULL, ncores=NCORES):
    global _BIAS_MAP
    f = lambda a: np.ascontiguousarray(np.asarray(a, dtype=np.float32))
    Lw = L_FULL
    if _BIAS_MAP is None:
        _BIAS_MAP = _bias_index_map()
    rpb = f(inp["rpb"])
    pad = np.concatenate([rpb.reshape(Lw, 8, 15 * 31), np.full((Lw, 8, 1), NEG, np.float32)], axis=2).reshape(Lw, -1)
    biasT = pad[:, _BIAS_MAP]
    biasT = np.ascontiguousarray(biasT.reshape(Lw, 4, 128, 2 * 14 * 64))
    shared = {
        "w_in": f(inp["w_in"]), "w_out": f(inp["w_out"]), "pool_w": f(inp["pool_w"]),
        "w_gate": f(inp["w_gate"]), "w_up": f(inp["w_up"]), "w_down": f(inp["w_down"]),
        "gm": np.ascontiguousarray(f(inp["norm_mix_g"]).reshape(Lw, 8, 128).transpose(2, 0, 1).reshape(128, Lw * 8)),
        "gf": np.ascontiguousarray(f(inp["norm_ffn_g"]).reshape(Lw, 8, 128).transpose(2, 0, 1).reshape(128, Lw * 8)),
        "ps": np.ascontiguousarray(f(inp["pool_scale"]).reshape(Lw, 4, 128).transpose(2, 0, 1).reshape(128, Lw * 4)),
        "gF": np.ascontiguousarray(np.broadcast_to(f(inp["final_g"])[None, :], (128, D))),
        "wr": np.ascontiguousarray(np.concatenate([f(inp["w_router_group"]), f(inp["w_router_expert"])], axis=-1)
                                   .reshape(Lw, 8, 128, 20).transpose(2, 0, 1, 3)),
        "biasT": biasT,
        "poolA": _pool_consts(),
        "ident": np.eye(128, dtype=np.float32),
        "ustrict": np.triu(np.ones((128, 128), np.float32), k=1),
        "iotas": np.ascontiguousarray(np.concatenate([np.arange(128, dtype=np.float32)[:, None],
                                                      np.broadcast_to(np.arange(32, dtype=np.float32)[None, :], (128, 32))], axis=1)),
    }
    x = f(inp["x"]).reshape(-1, S, D)
    maps = []
    for c in range(ncores):
        m = dict(shared)
        m["x"] = np.ascontiguousarray(x[c * nseq:(c + 1) * nseq].reshape(nseq * S, D))
        maps.append(m)
    return maps


def kernel(x, norm_mix_g, w_in, rpb, pool_w, pool_scale, w_out, norm_ffn_g, w_router_group, w_router_expert,
           w_gate, w_up, w_down, final_g):
    inp = dict(x=x, norm_mix_g=norm_mix_g, w_in=w_in, rpb=rpb, pool_w=pool_w, pool_scale=pool_scale, w_out=w_out,
               norm_ffn_g=norm_ffn_g, w_router_group=w_router_group, w_router_expert=w_router_expert,
               w_gate=w_gate, w_up=w_up, w_down=w_down, final_g=final_g)
    maps = prep_inputs(inp)
    if "full" not in _PROG:
        _PROG["full"] = build_program()
    nc = _PROG["full"]
    res = run_bass_kernel_spmd(nc, maps, core_ids=list(range(NCORES)))
    outs = [np.asarray(r["out"], dtype=np.float32).reshape(NSEQ_FULL, S, D) for r in res.results]
    return np.concatenate(outs, axis=0)
```

```python
import numpy as np
from contextlib import ExitStack
import concourse.bass as bass
import concourse.mybir as mybir
from concourse.bass_utils import run_bass_kernel_spmd

F32 = mybir.dt.float32
BF16 = mybir.dt.bfloat16
AF = mybir.ActivationFunctionType
ALU = mybir.AluOpType
AX = mybir.AxisListType

D = 1024
S = 2048
L_FULL = 4
NSEQ_FULL = 4
NCORES = 8
NE = 16
DE = 512
EPS = 1e-6
NEG = -30000.0
POOL_W = (2, 4, 8, 16)


class Buf:
    __slots__ = ("name", "w", "r")

    def __init__(self, name):
        self.name = name
        self.w = None
        self.r = {}


class _Eng:
    def __init__(self, name):
        self.name = name
        self.q = []
        self.known = {}
        self.count = 0
        self.psem = None


class Tracker:
    ENGS = ("pe", "act", "dve", "pool", "sp")

    def __init__(self, nc, stack, n_dma_sems=24):
        self.nc = nc
        self.sems = []
        self.eng = {n: _Eng(n) for n in self.ENGS}
        for n in ("pe", "act", "dve", "pool"):
            self.eng[n].psem = self._new_sem(stack, "p_" + n)
        self.dpool = [self._new_sem(stack, f"dq{i}") for i in range(n_dma_sems)]
        self.dcnt = [0] * n_dma_sems
        self.dnext = 0
        self.stack = stack
        self.extra = []

    def _new_sem(self, stack, name):
        h = stack.enter_context(self.nc.semaphore(name))
        self.sems.append(h)
        return len(self.sems) - 1

    def _deps(self, E, reads, writes, extra=()):
        need = {}

        def req(ev):
            if ev is None:
                return
            s, v = ev
            if s == E.psem and E.name == "pe":
                return
            if E.known.get(s, 0) >= v:
                return
            if need.get(s, 0) < v:
                need[s] = v

        for ev in extra:
            req(ev)
        for b in reads:
            req(b.w)
        for b in writes:
            req(b.w)
            for s, v in b.r.items():
                req((s, v))
        for s, v in need.items():
            E.q.append(("wait", s, v))
            E.known[s] = v

    def _mark(self, ev, reads, writes):
        for b in reads:
            if b.r.get(ev[0], 0) < ev[1]:
                b.r[ev[0]] = ev[1]
        for b in writes:
            b.w = ev
            b.r = {}

    def emit(self, eng, fn, reads=(), writes=(), sig=True):
        E = self.eng[eng]
        self._deps(E, reads, writes)
        if sig:
            E.count += 1
            ev = (E.psem, E.count)
        else:
            ev = (E.psem, E.count + 1)
        E.q.append(("op", fn, sig))
        self._mark(ev, reads, writes)
        return ev

    def dma(self, q, out, in_, reads=(), writes=(), own_sem=False):
        E = self.eng[q]
        if own_sem:
            s = self._new_sem(self.stack, f"ds{len(self.sems)}")
            self.extra.append(s)
            prev = 0
            self._deps(E, reads, writes)
            ev = (s, 16)
        else:
            i = self.dnext
            self.dnext = (i + 1) % len(self.dpool)
            s = self.dpool[i]
            prev = self.dcnt[i]
            self._deps(E, reads, writes, extra=[(s, prev)] if prev else ())
            self.dcnt[i] += 16
            ev = (s, self.dcnt[i])
        E.q.append(("dma", out, in_, s))
        self._mark(ev, reads, writes)
        return ev

    def idma(self, out, out_idx, in_, in_idx, reads=(), writes=(), bounds=None):
        E = self.eng["pool"]
        i = self.dnext
        self.dnext = (i + 1) % len(self.dpool)
        s = self.dpool[i]
        prev = self.dcnt[i]
        self._deps(E, reads, writes, extra=[(s, prev)] if prev else ())
        self.dcnt[i] += 16
        ev = (s, self.dcnt[i])
        E.q.append(("idma", out, out_idx, in_, in_idx, s, bounds))
        self._mark(ev, reads, writes)
        return ev

    def barrier(self):
        evs = []
        for n in ("pe", "act", "dve", "pool"):
            e = self.eng[n]
            if e.count:
                evs.append((e.psem, e.count))
        for i, s in enumerate(self.dpool):
            if self.dcnt[i]:
                evs.append((s, self.dcnt[i]))
        for s in self.extra:
            evs.append((s, 16))
        for fn in getattr(self, "extra_ev_fns", []):
            evs.extend(fn())
        for n in self.ENGS:
            E = self.eng[n]
            for s, v in evs:
                if s == E.psem:
                    continue
                if E.known.get(s, 0) < v:
                    E.q.append(("wait", s, v))
                    E.known[s] = v

    def flush(self):
        nc = self.nc
        sems = self.sems

        def run(E, h):
            psem = sems[E.psem] if E.psem is not None else None
            for it in E.q:
                if it[0] == "wait":
                    h.wait_ge(sems[it[1]], it[2])
                elif it[0] == "op":
                    ins = it[1](h)
                    if it[2]:
                        ins.then_inc(psem, 1)
                elif it[0] == "idma":
                    oo = bass.IndirectOffsetOnAxis(ap=it[2], axis=0) if it[2] is not None else None
                    io = bass.IndirectOffsetOnAxis(ap=it[4], axis=0) if it[4] is not None else None
                    if it[6] is None:
                        h.indirect_dma_start(out=it[1], out_offset=oo, in_=it[3], in_offset=io).then_inc(sems[it[5]], 16)
                    else:
                        h.indirect_dma_start(out=it[1], out_offset=oo, in_=it[3], in_offset=io, bounds_check=it[6],
                                             oob_is_err=False).then_inc(sems[it[5]], 16)
                else:
                    h.dma_start(out=it[1], in_=it[2]).then_inc(sems[it[3]], 16)

        with nc.Block() as block:
            @block.tensor
            def _(h):
                run(self.eng["pe"], h)

            @block.scalar
            def _(h):
                run(self.eng["act"], h)

            @block.vector
            def _(h):
                run(self.eng["dve"], h)

            @block.gpsimd
            def _(h):
                run(self.eng["pool"], h)

            @block.sync
            def _(h):
                run(self.eng["sp"], h)


SPARSE = True
POOL_FROM_LAYER = 99
TS = 256
NTILE = 32


def build_program(nseq=NSEQ_FULL, depth=L_FULL, stop_after=None, debug_out=False):
    nc = bass.Bass("TRN2", target_bir_lowering=False)
    NT = nseq * S
    Lw = L_FULL

    def din(name, shape, dt=F32):
        return nc.dram_tensor(name, list(shape), dt, kind="ExternalInput").ap()

    x_c = din("x", [NT, D])
    w_in = din("w_in", [Lw, D, 2048])
    w_out = din("w_out", [Lw, D, D])
    pool_w = din("pool_w", [Lw, 4, 128, 128])
    w_gate = din("w_gate", [Lw, NE, D, DE])
    w_up = din("w_up", [Lw, NE, D, DE])
    w_down = din("w_down", [Lw, NE, DE, D])
    gm_d = din("gm", [128, Lw * 8])
    gf_d = din("gf", [128, Lw * 8])
    ps_d = din("ps", [128, Lw * 4])
    gF_d = din("gF", [128, D])
    wr_d = din("wr", [128, Lw, 8, 20])
    bias_d = din("biasT", [Lw, 4, 128, 2 * 14 * 64])
    poolA_d = din("poolA", [128, 28, 128])
    ident_d = din("ident", [128, 128])
    ustrict_d = din("ustrict", [128, 128])
    iotas_d = din("iotas", [128, 33])
    if stop_after is None:
        out_c = nc.dram_tensor("out", [NT, D], F32, kind="ExternalOutput").ap()
        xT_d = nc.dram_tensor("xT_d", [NT // 128, 128, 8, 128], F32, kind="Internal").ap()
    else:
        xT_d = nc.dram_tensor("xT_out", [NT // 128, 128, 8, 128], F32, kind="ExternalOutput").ap()
        out_c = None
    w_in_b = nc.dram_tensor("w_in_b", [Lw, D, 2048], BF16, kind="Internal").ap()
    w_out_b = nc.dram_tensor("w_out_b", [Lw, D, D], BF16, kind="Internal").ap()
    pool_w_b = nc.dram_tensor("pool_w_b", [Lw, 4, 128, 128], BF16, kind="Internal").ap()
    w_gate_b = nc.dram_tensor("w_gate_b", [Lw, NE, D, DE], BF16, kind="Internal").ap()
    w_up_b = nc.dram_tensor("w_up_b", [Lw, NE, D, DE], BF16, kind="Internal").ap()
    w_down_b = nc.dram_tensor("w_down_b", [Lw, NE, DE, D], BF16, kind="Internal").ap()
    w_gate_p = [nc.dram_tensor(f"w_gate_p{i}", [NE * 128, 4096], BF16, kind="Internal").ap() for i in range(Lw)]
    w_up_p = [nc.dram_tensor(f"w_up_p{i}", [NE * 128, 4096], BF16, kind="Internal").ap() for i in range(Lw)]
    w_down_p = [nc.dram_tensor(f"w_down_p{i}", [NE * 128, 4096], BF16, kind="Internal").ap() for i in range(Lw)]
    HS_d = nc.dram_tensor("HS_d", [NTILE * TS, D], BF16, kind="Internal").ap()
    YS_d = nc.dram_tensor("YS_d", [NTILE * TS, D], F32, kind="Internal").ap()

    top = ExitStack()
    with top:
        T = Tracker(nc, top)

        uid = [0]

        def sb(stack, name, shape, dt):
            uid[0] += 1
            return stack.enter_context(nc.sbuf_tensor(f"{name}_s{uid[0]}", list(shape), dt))

        banks = [top.enter_context(nc.psum_tensor(f"bank{i}", [128, 512], F32)) for i in range(8)]
        BK = [Buf(f"bank{i}") for i in range(8)]

        onesb = sb(top, "onesb", [128, 128], BF16)
        identf = sb(top, "identf", [128, 128], F32)
        identb = sb(top, "identb", [128, 128], BF16)
        A_bf = sb(top, "A_bf", [128, 28, 128], BF16)
        gm = sb(top, "gm", [128, Lw * 8], F32)
        gf = sb(top, "gf", [128, Lw * 8], F32)
        psc = sb(top, "psc", [128, Lw * 4], F32)
        epsc = sb(top, "epsc", [128, 1], F32)
        ustr_f = sb(top, "ustr_f", [128, 128], F32)
        ustr = sb(top, "ustr", [128, 128], BF16)
        iotas = sb(top, "iotas", [128, 33], F32)
        CONST = Buf("const")
        with nc.sbuf_tensor("A_stage", [128, 28, 128], F32) as A_st:
            ASB = Buf("A_stage")
            T.emit("dve", lambda e: e.memset(onesb[:], 1.0), writes=[CONST])
            T.emit("dve", lambda e: e.memset(epsc[:], EPS), writes=[CONST])
            T.dma("sp", identf[:], ident_d, writes=[CONST])
            T.dma("sp", gm[:], gm_d, writes=[CONST])
            T.dma("sp", gf[:], gf_d, writes=[CONST])
            T.dma("sp", psc[:], ps_d, writes=[CONST])
            T.dma("sp", A_st[:], poolA_d, writes=[ASB])
            T.dma("sp", ustr_f[:], ustrict_d, writes=[CONST])
            T.dma("sp", iotas[:], iotas_d, writes=[CONST])
            T.emit("dve", lambda e: e.tensor_copy(out=ustr[:], in_=ustr_f[:]), reads=[CONST], writes=[CONST])
            T.emit("dve", lambda e: e.tensor_copy(out=identb[:], in_=identf[:]), reads=[CONST], writes=[CONST])
            T.emit("dve", lambda e: e.tensor_copy(out=A_bf[:], in_=A_st[:]), reads=[ASB], writes=[CONST])
            T.barrier()

        WB = {}

        cast_sems = [T._new_sem(top, f"cs{i}") for i in range(8)]
        cast_cnt = [0] * 8
        cast_i = [0]

        def cast(key, dst, src, n=0):
            WB[key] = Buf(str(key))
            E = T.eng["pool"]
            i = cast_i[0] % 8
            cast_i[0] += 1
            sm = cast_sems[i]
            if cast_cnt[i] and E.known.get(sm, 0) < cast_cnt[i]:
                E.q.append(("wait", sm, cast_cnt[i]))
                E.known[sm] = cast_cnt[i]
            cast_cnt[i] += 16
            E.q.append(("dma", dst, src, sm))
            WB[key].w = (sm, cast_cnt[i])

        for l in range(depth):
            cast(("in", l), w_in_b[l], w_in[l])
            cast(("pw", l), pool_w_b[l].rearrange("g (a b) d -> (g a) (b d)", b=16),
                 pool_w[l].rearrange("g (a b) d -> (g a) (b d)", b=16))
            cast(("out", l), w_out_b[l].rearrange("(r q) d -> r (q d)", q=2),
                 w_out[l].rearrange("(r q) d -> r (q d)", q=2))
            for e_ in range(NE):
                cast(("g", l, e_), w_gate_p[l][e_ * 128:(e_ + 1) * 128, :].rearrange("p (k f) -> p k f", f=512),
                     w_gate[l, e_].rearrange("(k p) f -> p k f", p=128))
                cast(("u", l, e_), w_up_p[l][e_ * 128:(e_ + 1) * 128, :].rearrange("p (k f) -> p k f", f=512),
                     w_up[l, e_].rearrange("(k p) f -> p k f", p=128))
                cast(("d", l, e_), w_down_p[l][e_ * 128:(e_ + 1) * 128, :].rearrange("p (k f) -> p k f", f=1024),
                     w_down[l, e_].rearrange("(k p) f -> p k f", p=128))

        XD = [[Buf(f"xd{s}_{t}") for t in range(4)] for s in range(nseq)]
        flip = [0]

        def evac_copy(out, in_, reads, writes, scale=None):
            flip[0] ^= 1
            if scale is not None or flip[0]:
                sc = 1.0 if scale is None else scale
                T.emit("act", lambda e: e.activation(out=out, in_=in_, func=AF.Copy, scale=sc), reads=reads, writes=writes)
            else:
                T.emit("dve", lambda e: e.tensor_copy(out=out, in_=in_), reads=reads, writes=writes)

        def mm(out, lhsT, rhs, start, stop, reads, writes, sig):
            T.emit("pe", lambda e: e.matmul(out, lhsT, rhs, start=start, stop=stop), reads=reads, writes=writes, sig=sig)

        def rstd_from_ss(out, ss_ap, reads, writes):
            T.emit("act", lambda e: e.activation(out=out, in_=ss_ap, func=AF.Sqrt, bias=epsc[:, 0:1], scale=1.0 / D),
                   reads=list(reads) + [CONST], writes=writes)
            T.emit("dve", lambda e: e.reciprocal(out=out, in_=out), reads=writes, writes=writes)

        with ExitStack() as st:
            xin = sb(st, "xin", [128, 2, 4, 1024], F32)
            xTs = sb(st, "xTs", [128, 2, 4, 8, 128], F32)
            XIN = [Buf("xin0"), Buf("xin1")]
            XTS = [Buf("xts0"), Buf("xts1")]
            bi = 0
            for s in range(nseq):
                for t in range(4):
                    b = (s * 4 + t) % 2
                    t0 = s * S + t * 512
                    T.dma("sp", xin[:, b], x_c[t0:t0 + 512, :].rearrange("(j p) d -> p j d", p=128), writes=[XIN[b]])
                    for k in range(8):
                        bk = bi % 4
                        bi += 1
                        for j in range(4):
                            T.emit("pe", lambda e, bk=bk, j=j, k=k, b=b: e.transpose(
                                out=banks[bk][:, j * 128:(j + 1) * 128], in_=xin[:, b, j, k * 128:(k + 1) * 128],
                                identity=identf[:]), reads=[XIN[b], CONST], writes=[BK[bk]], sig=(j == 3))
                        evac_copy(xTs[:, b, :, k, :], banks[bk][:, :].rearrange("p (j t) -> p j t", t=128), [BK[bk]], [XTS[b]])
                    T.dma("sp", xT_d[t0 // 128:t0 // 128 + 4].rearrange("j p k t -> p j k t"), xTs[:, b], reads=[XTS[b]],
                          writes=[XD[s][t]])
            T.barrier()

        def m_phase(l, s):
            tok0 = s * S
            with ExitStack() as pm:
                H = sb(pm, "H", [128, 8, S], BF16)
                qT = sb(pm, "qT", [128, 4, S], BF16)
                kT = sb(pm, "kT", [128, 4, S], BF16)
                V2 = sb(pm, "V2", [128, 31, 4, 192], BF16)
                pT = sb(pm, "pT", [128, 4, S], BF16)
                pw_sb = sb(pm, "pw_sb", [128, 4, 128], BF16)
                HT = [Buf(f"H{i}") for i in range(8)]
                QB = [[Buf(f"q{m}_{t}") for t in range(4)] for m in range(4)]
                KB = [[Buf(f"k{m}_{t}") for t in range(4)] for m in range(4)]
                VB = [Buf(f"v{i}") for i in range(31)]
                VONES = Buf("vones")
                PB = [[Buf(f"p{g}_{t}") for t in range(4)] for g in range(4)]
                PW = Buf("pw")
                with ExitStack() as pb:
                    w_in_sb = sb(pb, "w_in_sb", [128, 8, 2048], BF16)
                    U2 = sb(pb, "U2", [128, 16, 512], BF16)
                    pooledT = sb(pb, "pooledT", [128, 2, 512], BF16)
                    sq = sb(pb, "sq", [128, 2, 8, 128], BF16)
                    xt = sb(pb, "xt", [128, 2, 8, 128], F32)
                    rstd = sb(pb, "rstd", [128, 1, 256], F32)
                    WI = [Buf(f"wi{c}") for c in range(4)]
                    UB = [Buf(f"u{j}") for j in range(16)]
                    PLB = [Buf("pl0"), Buf("pl1")]
                    SQ = Buf("sq")
                    XT = [Buf("xt0"), Buf("xt1")]
                    RS = [Buf("rs0"), Buf("rs1")]
                    for c in range(4):
                        T.dma("sp", w_in_sb[:, :, c * 512:(c + 1) * 512],
                              w_in_b[l][:, c * 512:(c + 1) * 512].rearrange("(k p) f -> p k f", p=128),
                              reads=[WB[("in", l)]], writes=[WI[c]])
                    T.dma("sp", pw_sb[:], pool_w_b[l].rearrange("g c d -> c g d"), reads=[WB[("pw", l)]], writes=[PW])
                    T.emit("dve", lambda e: e.memset(V2[:, :, :, 64:128], 1.0), writes=[VONES])
                    def emit_norm(i):
                        b = 0
                        c0 = i * 256
                        tl = (tok0 + c0) // 128
                        T.dma("sp", xt[:], xT_d[tl:tl + 2].rearrange("j p k t -> p j k t"),
                              reads=[XD[s][i // 2]], writes=[XT[b]])
                        T.emit("act", lambda e: e.activation(out=sq[:], in_=xt[:], func=AF.Square),
                               reads=[XT[b]], writes=[SQ])
                        bk = 6 + b
                        for j in range(2):
                            for k in range(8):
                                mm(banks[bk][:, j * 128:(j + 1) * 128], onesb[:], sq[:, j, k, :], k == 0, k == 7, [SQ, CONST], [BK[bk]],
                                   (k == 7 and j == 1))
                        rstd_from_ss(rstd[:, b, :], banks[bk][:, 0:256], [BK[bk]], [RS[b]])
                        for k in range(8):
                            T.emit("dve", lambda e, b=b, k=k, c0=c0: e.scalar_tensor_tensor(
                                out=H[:, k, c0:c0 + 256].rearrange("p (j t) -> p j t", t=128), in0=xt[:, :, k, :],
                                scalar=gm[:, l * 8 + k:l * 8 + k + 1],
                                in1=rstd[:, b, :].rearrange("p (j t) -> p j t", t=128), op0=ALU.mult, op1=ALU.mult),
                                reads=[XT[b], RS[b], CONST], writes=[HT[i]])
                    bi_ = [0]

                    def emit_qkproj(t):
                        bi = bi_[0]
                        for m in range(8):
                            bk = bi % 4
                            bi += 1
                            for k in range(8):
                                mm(banks[bk][:, :], w_in_sb[:, k, m * 128:(m + 1) * 128], H[:, k, t * 512:(t + 1) * 512],
                                   k == 0, k == 7, [WI[m // 4], HT[2 * t], HT[2 * t + 1]], [BK[bk]], k == 7)
                            if m < 4:
                                evac_copy(qT[:, m, t * 512:(t + 1) * 512], banks[bk][:, :], [BK[bk]], [QB[m][t]], scale=0.125)
                            else:
                                T.emit("dve", lambda e, bk=bk, m=m, t=t: e.tensor_copy(
                                    out=kT[:, m - 4, t * 512:(t + 1) * 512], in_=banks[bk][:, :]),
                                    reads=[BK[bk]], writes=[KB[m - 4][t]])
                        bi_[0] = bi

                    for t in range(4):
                        emit_norm(2 * t)
                        emit_norm(2 * t + 1)
                        if t > 0:
                            emit_qkproj(t - 1)
                    emit_qkproj(3)
                    bi = bi_[0]
                    for sidx in range(31):
                        bk = bi % 4
                        bi += 1
                        a0 = 64 * sidx
                        hts = sorted({a0 // 256, (a0 + 127) // 256})
                        for k in range(8):
                            mm(banks[bk][:, :], H[:, k, a0:a0 + 128], w_in_sb[:, k, 1024:1536], k == 0, k == 7,
                               [WI[2]] + [HT[i] for i in hts], [BK[bk]], k == 7)
                        evac_copy(V2[:, sidx, :, :].rearrange("p a (b d) -> p a b d", d=64)[:, :, 0:3:2, :],
                                  banks[bk][:, :].rearrange("p (a b d) -> p a b d", b=2, d=64), [BK[bk]], [VB[sidx]])
                    for j in range(16):
                        bk = bi % 4
                        bi += 1
                        for k in range(8):
                            mm(banks[bk][:, :], H[:, k, j * 128:(j + 1) * 128], w_in_sb[:, k, 1536:2048], k == 0, k == 7,
                               [WI[3], HT[j // 2]], [BK[bk]], k == 7)
                        evac_copy(U2[:, j, :], banks[bk][:, :], [BK[bk]], [UB[j]])
                    for t in range(4):
                        for g in range(4):
                            pbuf = g % 2
                            bk = bi % 4
                            bi += 1
                            for Tq in range(4):
                                Tt = 4 * t + Tq
                                terms = []
                                if Tt > 0:
                                    terms.append((Tt - 1, 0))
                                if Tt == 0:
                                    terms += [(Tt, 3), (Tt, 4)]
                                elif Tt == 15:
                                    terms += [(Tt, 5), (Tt, 6)]
                                else:
                                    terms.append((Tt, 1))
                                if Tt < 15:
                                    terms.append((Tt + 1, 2))
                                for ti, (tp, var) in enumerate(terms):
                                    mm(banks[bk][:, Tq * 128:(Tq + 1) * 128], U2[:, tp, g * 128:(g + 1) * 128],
                                       A_bf[:, g * 7 + var, :], ti == 0, ti == len(terms) - 1,
                                       [UB[tp], CONST], [BK[bk]], (Tq == 3 and ti == len(terms) - 1))
                            T.emit("act", lambda e, bk=bk, pbuf=pbuf, g=g: e.activation(
                                out=pooledT[:, pbuf, :], in_=banks[bk][:, :], func=AF.Copy),
                                reads=[BK[bk]], writes=[PLB[pbuf]])
                            bk2 = 4 + (g % 2)
                            mm(banks[bk2][:, :], pw_sb[:, g, :], pooledT[:, pbuf, :], True, True, [PW, PLB[pbuf]], [BK[bk2]], True)
                            T.emit("dve", lambda e, bk2=bk2, g=g, t=t: e.tensor_scalar(
                                out=pT[:, g, t * 512:(t + 1) * 512], in0=banks[bk2][:, :],
                                scalar1=psc[:, l * 4 + g:l * 4 + g + 1], scalar2=None, op0=ALU.mult),
                                reads=[BK[bk2], CONST], writes=[PB[g][t]])
                    T.barrier()
                with ExitStack() as pc:
                    bias = sb(pc, "bias", [128, 4, 2, 14, 64], F32)
                    Sb = sb(pc, "Sb", [128, 4, 2, 4, 64], F32)
                    Pm = sb(pc, "Pm", [128, 4, 2, 4, 64], BF16)
                    Rr = sb(pc, "Rr", [128, 2, 4, 64], F32)
                    w_out_sb = sb(pc, "w_out_sb", [128, 8, D], BF16)
                    xt2 = sb(pc, "xt2", [128, 2, 8, 128], F32)
                    BIAS = [Buf(f"bias{hp}") for hp in range(4)]
                    SBB = [Buf(f"sb{i}") for i in range(4)]
                    PMB = [Buf(f"pm{i}") for i in range(4)]
                    RRB = [Buf("rr0"), Buf("rr1")]
                    WO = [Buf("wo0"), Buf("wo1")]
                    XT2 = [Buf("xt20"), Buf("xt21")]
                    for hp in range(4):
                        T.dma("sp", bias[:, hp].rearrange("p a b c -> p (a b c)"), bias_d[l, hp], writes=[BIAS[hp]])
                        T.emit("act", lambda e, hp=hp: e.activation(out=bias[:, hp].rearrange("p a b c -> p (a b c)"),
                                                                    in_=bias[:, hp].rearrange("p a b c -> p (a b c)"), func=AF.Exp),
                               reads=[BIAS[hp]], writes=[BIAS[hp]])
                    for c in range(2):
                        T.dma("sp", w_out_sb[:, :, c * 512:(c + 1) * 512],
                              w_out_b[l][:, c * 512:(c + 1) * 512].rearrange("(k p) f -> p k f", p=128),
                              reads=[WB[("out", l)]], writes=[WO[c]])
                    units = [(r, hp) for r in range(32) for hp in range(4)]

                    def rstart(r):
                        return min(max(r - 4, 0), 24)

                    def emit_qk(ui):
                        r, hp = units[ui]
                        u4 = ui % 4
                        rs_ = rstart(r)
                        i0 = rs_ - r + 7
                        kts = sorted({(rs_ * 64) // 512, (rs_ * 64 + 511) // 512})
                        for half in (0, 1):
                            bk = (u4 // 2) * 2 + half
                            cb = (u4 % 2) * 256
                            p0 = 64 * half
                            for c in range(4):
                                ks = (rs_ + 2 * c) * 64
                                mm(banks[bk][:, cb + c * 64:cb + (c + 1) * 64], kT[p0:p0 + 64, hp, ks:ks + 128],
                                   qT[p0:p0 + 64, hp, r * 64:(r + 1) * 64], True, True,
                                   [KB[hp][t] for t in kts] + [QB[hp][r // 8]], [BK[bk]], c == 3)
                            T.emit("act", lambda e, bk=bk, cb=cb, u4=u4, half=half: e.activation(
                                out=Sb[:, u4, half], in_=banks[bk][:, cb:cb + 256].rearrange("p (c q) -> p c q", q=64),
                                func=AF.Exp), reads=[BK[bk]], writes=[SBB[u4]])
                        T.emit("pool" if (l >= POOL_FROM_LAYER and ui % 2 == 1) else "dve", lambda e, u4=u4, hp=hp, i0=i0: e.tensor_tensor(
                            out=Pm[:, u4], in0=Sb[:, u4], in1=bias[:, hp, :, i0:i0 + 7:2, :], op=ALU.mult),
                            reads=[SBB[u4], BIAS[hp]], writes=[PMB[u4]])

                    def emit_pv(ui):
                        r, hp = units[ui]
                        u2 = ui % 4
                        rs_ = rstart(r)
                        ob = 4 + (r % 2)
                        for half in (0, 1):
                            h = 2 * hp + half
                            for c in range(4):
                                sidx = rs_ + 2 * c
                                lhsT = V2[:, sidx, hp, 64 * half:64 * half + 128]
                                col = (hp * 2 + half) * 64
                                mm(banks[ob][:, col:col + 64], lhsT, Pm[:, u2, half, c, :], c == 0, c == 3,
                                   [VB[sidx], VONES, PMB[u2]], [BK[ob]], c == 3)
                        if hp == 3:
                            rb = r % 2
                            Ov = banks[ob][:, :].rearrange("p (a b q) -> p a b q", b=2, q=64)
                            T.emit("dve", lambda e, rb=rb, ob=ob: e.reciprocal(
                                out=Rr[0:64, rb], in_=banks[ob][64:128, :].rearrange("p (a b q) -> p a b q", b=2, q=64)[:, :, 0, :]),
                                reads=[BK[ob]], writes=[RRB[rb]])
                            T.emit("dve", lambda e, rb=rb, ob=ob: e.reciprocal(
                                out=Rr[64:128, rb], in_=banks[ob][0:64, :].rearrange("p (a b q) -> p a b q", b=2, q=64)[:, :, 1, :]),
                                reads=[BK[ob]], writes=[RRB[rb]])
                            T.emit("dve", lambda e, rb=rb, ob=ob, r=r: e.tensor_tensor(
                                out=H[0:64, 0:4, r * 64:(r + 1) * 64],
                                in0=banks[ob][0:64, :].rearrange("p (a b q) -> p a b q", b=2, q=64)[:, :, 0, :],
                                in1=Rr[0:64, rb], op=ALU.mult), reads=[BK[ob], RRB[rb]], writes=[HT[r // 4]])
                            T.emit("dve", lambda e, rb=rb, ob=ob, r=r: e.tensor_tensor(
                                out=H[64:128, 0:4, r * 64:(r + 1) * 64],
                                in0=banks[ob][64:128, :].rearrange("p (a b q) -> p a b q", b=2, q=64)[:, :, 1, :],
                                in1=Rr[64:128, rb], op=ALU.mult), reads=[BK[ob], RRB[rb]], writes=[HT[r // 4]])

                    for ui in range(len(units)):
                        emit_qk(ui)
                        if ui > 1:
                            emit_pv(ui - 2)
                    emit_pv(len(units) - 2)
                    emit_pv(len(units) - 1)
                    for i in range(16):
                        b = i % 2
                        c0 = i * 128
                        T.dma("sp", xt2[:, b], xT_d[(tok0 + c0) // 128],
                              reads=[XD[s][i // 4]], writes=[XT2[b]])
                        for m in range(8):
                            bk = 6 + (m % 2)
                            for k in range(8):
                                rhs = H[:, k, c0:c0 + 128] if k < 4 else pT[:, k - 4, c0:c0 + 128]
                                rd = [HT[i // 2]] if k < 4 else [PB[k - 4][i // 4]]
                                mm(banks[bk][:, 0:128], w_out_sb[:, k, m * 128:(m + 1) * 128], rhs, k == 0, k == 7,
                                   [WO[m // 4]] + rd, [BK[bk]], k == 7)
                            T.emit("dve", lambda e, b=b, m=m, bk=bk: e.tensor_tensor(
                                out=xt2[:, b, m, :], in0=banks[bk][:, 0:128], in1=xt2[:, b, m, :], op=ALU.add),
                                reads=[BK[bk], XT2[b]], writes=[XT2[b]])
                        T.dma("sp", xT_d[(tok0 + c0) // 128], xt2[:, b],
                              reads=[XT2[b]], writes=[XD[s][i // 4]])
                    T.barrier()

        def final_out(xT, XB, s):
            tok0 = s * S
            if True:
                if True:
                    with ExitStack() as po:
                        gF = sb(po, "gF", [128, D], F32)
                        ost = sb(po, "ost", [128, 1, D], F32)
                        junk = sb(po, "junk", [128, 512], BF16)
                        ssA = sb(po, "ssA", [128, 2], F32)
                        rsF = sb(po, "rsF", [128, 1], F32)
                        GF, OST, JK, SSA, RSF = Buf("gF"), [Buf("ost0"), Buf("ost1")], Buf("junk"), Buf("ssA"), Buf("rsF")
                        T.dma("sp", gF[:], gF_d, writes=[GF])
                        for j in range(16):
                            ob = 0
                            t = j // 4
                            for hb in range(2):
                                bk = 2 * (j % 2) + hb
                                for kk in range(4):
                                    k = hb * 4 + kk
                                    T.emit("pe", lambda e, bk=bk, kk=kk, k=k, j=j: e.transpose(
                                        out=banks[bk][:, kk * 128:(kk + 1) * 128], in_=xT[:, j, k, :],
                                        identity=identf[:]), reads=[XB[k][t], CONST], writes=[BK[bk]], sig=(kk == 3))
                                T.emit("act", lambda e, bk=bk, hb=hb: e.activation(
                                    out=junk[:], in_=banks[bk][:, :], func=AF.Square, accum_out=ssA[:, hb:hb + 1]),
                                    reads=[BK[bk]], writes=[JK, SSA])
                            T.emit("dve", lambda e: e.tensor_tensor(out=rsF[:], in0=ssA[:, 0:1], in1=ssA[:, 1:2], op=ALU.add),
                                   reads=[SSA], writes=[RSF])
                            rstd_from_ss(rsF[:], rsF[:], [RSF], [RSF])
                            for hb in range(2):
                                bk = 2 * (j % 2) + hb
                                T.emit("dve", lambda e, bk=bk, hb=hb, ob=ob: e.scalar_tensor_tensor(
                                    out=ost[:, ob, hb * 512:(hb + 1) * 512], in0=banks[bk][:, :], scalar=rsF[:, 0:1],
                                    in1=gF[:, hb * 512:(hb + 1) * 512], op0=ALU.mult, op1=ALU.mult),
                                    reads=[BK[bk], RSF, GF], writes=[OST[ob]])
                            T.dma("sp", out_c[tok0 + j * 128:tok0 + (j + 1) * 128, :], ost[:, ob, :], reads=[OST[ob]],
                                  writes=[XD[s][t]])

        def f_phase_sparse(l, s, last):
            U32 = mybir.dt.uint32
            tok0 = s * S
            with ExitStack() as pf:
                xT = sb(pf, "xT", [128, 16, 8, 128], F32)
                ring = sb(pf, "ring", [128, 6, 4096], BF16)
                wr_sb = sb(pf, "wr_sb", [128, 8, 20], F32)
                wrp = sb(pf, "wrp", [128, 8, 20], F32)
                rt = sb(pf, "rt", [128, 16], F32)
                LGs = sb(pf, "LGs", [128, 16, 20], F32)
                r1 = sb(pf, "r1", [128, 16, 16], F32)
                gmax = sb(pf, "gmax", [128, 16], F32)
                goh = sb(pf, "goh", [128, 16, 4], F32)
                gex = sb(pf, "gex", [128, 16, 4], F32)
                gw = sb(pf, "gw", [128, 16], F32)
                esel = sb(pf, "esel", [128, 16, 4], F32)
                em = sb(pf, "em", [128, 16, 4], F32)
                m1 = sb(pf, "m1", [128, 16], F32)
                m2 = sb(pf, "m2", [128, 16], F32)
                oh1 = sb(pf, "oh1", [128, 16, 4], F32)
                oh2 = sb(pf, "oh2", [128, 16, 4], F32)
                w1 = sb(pf, "w1", [128, 16], F32)
                w2 = sb(pf, "w2", [128, 16], F32)
                M1 = sb(pf, "M1", [128, 16, 16], F32)
                M2 = sb(pf, "M2", [128, 16, 16], F32)
                Mb = sb(pf, "Mb", [128, 256], BF16)
                TOT = sb(pf, "TOT", [128, 16, 16], F32)
                JP = sb(pf, "JP", [128, 16, 16], F32)
                SL = sb(pf, "SL", [128, 16, 16], F32)
                ne = sb(pf, "ne", [128, 16], F32)
                ntl = sb(pf, "ntl", [128, 16], F32)
                base = sb(pf, "base", [128, 16], F32)
                bend = sb(pf, "bend", [128, 16], F32)
                b256 = sb(pf, "b256", [128, 16], F32)
                sl1 = sb(pf, "sl1", [128, 16], F32)
                sl2 = sb(pf, "sl2", [128, 16], F32)
                s1u = sb(pf, "s1u", [128, 16], U32)
                s2u = sb(pf, "s2u", [128, 16], U32)
                eidx = sb(pf, "eidx", [128, NTILE], F32)
                widf = sb(pf, "widf", [128, NTILE], F32)
                widx = sb(pf, "widx", [128, NTILE], U32)
                XB = [[Buf(f"X{k}_{t}") for t in range(4)] for k in range(8)]
                RSLOT = [Buf(f"ring{i}") for i in range(6)]
                WR, WRP = Buf("wr"), Buf("wrp")
                ROUT = Buf("router")
                HSB = [Buf(f"hs{i}") for i in range(32)]
                YSB = [Buf(f"ys{i}") for i in range(NTILE)]

                def load_tile_w(i, which=(0, 1, 2)):
                    for j, wp in enumerate((w_gate_p, w_up_p, w_down_p)):
                        if j not in which:
                            continue
                        slot = (3 * i + j) % 6
                        T.idma(ring[:, slot, :], None, wp[l], widx[:, i:i + 1],
                               reads=[ROUT] + [WB[(("g", "u", "d")[j], l, e_)] for e_ in range(NE)], writes=[RSLOT[slot]])

                for t in range(4):
                    tl = tok0 // 128 + 4 * t
                    T.dma("sp", xT[:, 4 * t:4 * t + 4], xT_d[tl:tl + 4].rearrange("j p k t -> p j k t"), reads=[XD[s][t]],
                          writes=[XB[k][t] for k in range(8)])
                T.dma("sp", wr_sb[:], wr_d[:, l], writes=[WR])
                for k in range(8):
                    T.emit("dve", lambda e, k=k: e.tensor_scalar(out=wrp[:, k, :], in0=wr_sb[:, k, :],
                                                                 scalar1=gf[:, l * 8 + k:l * 8 + k + 1], scalar2=None,
                                                                 op0=ALU.mult), reads=[WR, CONST], writes=[WRP])

                def dv(fn, extra_reads=()):
                    T.emit("dve", fn, reads=[ROUT] + list(extra_reads), writes=[ROUT])

                def b3(ap2, n):
                    return ap2.unsqueeze(2).broadcast_to([128, 16, n])

                with ExitStack() as pa:
                    H = sb(pa, "Hf", [128, 8, S], BF16)
                    sq = sb(pa, "sqf", [128, 4, 8, 128], BF16)
                    rstd = sb(pa, "rstdf", [128, 512], F32)
                    hrow = sb(pa, "hrow", [128, 2, D], BF16)
                    HB = [Buf(f"Hf{t}") for t in range(4)]
                    SQ, RS = Buf("sq"), Buf("rs")
                    HROW = [Buf("hrow0"), Buf("hrow1")]
                    for t in range(4):
                        c0 = t * 512
                        T.emit("act", lambda e, t=t: e.activation(out=sq[:], in_=xT[:, 4 * t:4 * t + 4], func=AF.Square),
                               reads=[XB[k][t] for k in range(8)], writes=[SQ])
                        for j in range(4):
                            for k in range(8):
                                mm(banks[6][:, j * 128:(j + 1) * 128], onesb[:], sq[:, j, k, :], k == 0, k == 7, [SQ, CONST], [BK[6]],
                                   (k == 7 and j == 3))
                        rstd_from_ss(rstd[:], banks[6][:, :], [BK[6]], [RS])
                        for k in range(8):
                            T.emit("dve", lambda e, k=k, c0=c0, t=t: e.scalar_tensor_tensor(
                                out=H[:, k, c0:c0 + 512].rearrange("p (j t) -> p j t", t=128), in0=xT[:, 4 * t:4 * t + 4, k, :],
                                scalar=gf[:, l * 8 + k:l * 8 + k + 1],
                                in1=rstd[:].rearrange("p (j t) -> p j t", t=128), op0=ALU.mult, op1=ALU.mult),
                                reads=[XB[k][t], RS, CONST], writes=[HB[t]])
                        for j in range(4):
                            jj = 4 * t + j
                            for k in range(8):
                                mm(banks[7][:, 320 + jj:320 + jj + 1], sq[:, j, k, :], onesb[:, 0:1],
                                   k == 0, k == 7, [SQ, CONST], [BK[7]], False)
                            for k in range(8):
                                mm(banks[7][:, jj * 20:(jj + 1) * 20], xT[:, jj, k, :], wrp[:, k, :],
                                   k == 0, k == 7, [XB[k][t], WRP], [BK[7]], (k == 7))
                    rstd_from_ss(rt[:], banks[7][:, 320:336], [BK[7], ROUT], [ROUT])
                    dv(lambda e: e.tensor_tensor(out=LGs[:], in0=banks[7][:, 0:320].rearrange("p (j e) -> p j e", e=20),
                                                 in1=b3(rt[:], 20), op=ALU.mult), [BK[7]])
                    dv(lambda e: e.tensor_reduce(out=gmax[:], in_=LGs[:, :, 0:4], axis=AX.X, op=ALU.max))
                    dv(lambda e: e.tensor_tensor(out=goh[:], in0=LGs[:, :, 0:4], in1=b3(gmax[:], 4), op=ALU.is_equal))
                    dv(lambda e: e.tensor_tensor(out=gex[:], in0=LGs[:, :, 0:4], in1=b3(gmax[:], 4), op=ALU.subtract))
                    T.emit("act", lambda e: e.activation(out=gex[:], in_=gex[:], func=AF.Exp), reads=[ROUT], writes=[ROUT])
                    dv(lambda e: e.tensor_reduce(out=gw[:], in_=gex[:], axis=AX.X, op=ALU.add))
                    dv(lambda e: e.reciprocal(out=gw[:], in_=gw[:]))
                    dv(lambda e: e.tensor_tensor(out=r1[:].rearrange("p j (g i) -> p j g i", i=4),
                                                 in0=LGs[:, :, 4:20].rearrange("p j (g i) -> p j g i", i=4),
                                                 in1=goh[:].unsqueeze(3).broadcast_to([128, 16, 4, 4]), op=ALU.mult))
                    dv(lambda e: e.tensor_reduce(out=esel[:], in_=r1[:].rearrange("p j (g i) -> p j i g", i=4),
                                                 axis=AX.X, op=ALU.add))
                    dv(lambda e: e.tensor_reduce(out=m1[:], in_=esel[:], axis=AX.X, op=ALU.max))
                    dv(lambda e: e.tensor_tensor(out=oh1[:], in0=esel[:], in1=b3(m1[:], 4), op=ALU.is_equal))
                    dv(lambda e: e.scalar_tensor_tensor(out=em[:], in0=oh1[:], scalar=-1.0e30, in1=esel[:],
                                                        op0=ALU.mult, op1=ALU.add))
                    dv(lambda e: e.tensor_reduce(out=m2[:], in_=em[:], axis=AX.X, op=ALU.max))
                    dv(lambda e: e.tensor_tensor(out=oh2[:], in0=em[:], in1=b3(m2[:], 4), op=ALU.is_equal))
                    dv(lambda e: e.tensor_tensor(out=w2[:], in0=m2[:], in1=m1[:], op=ALU.subtract))
                    T.emit("act", lambda e: e.activation(out=w2[:], in_=w2[:], func=AF.Exp), reads=[ROUT], writes=[ROUT])
                    dv(lambda e: e.tensor_scalar(out=w2[:], in0=w2[:], scalar1=1.0, scalar2=None, op0=ALU.add))
                    dv(lambda e: e.reciprocal(out=w1[:], in_=w2[:]))
                    dv(lambda e: e.tensor_tensor(out=w1[:], in0=w1[:], in1=gw[:], op=ALU.mult))
                    dv(lambda e: e.tensor_tensor(out=w2[:], in0=gw[:], in1=w1[:], op=ALU.subtract))
                    g44 = lambda ap: ap.rearrange("p j (g i) -> p j g i", i=4)
                    dv(lambda e: e.tensor_tensor(out=g44(M1[:]), in0=goh[:].unsqueeze(3).broadcast_to([128, 16, 4, 4]),
                                                 in1=oh1[:].unsqueeze(2).broadcast_to([128, 16, 4, 4]), op=ALU.mult))
                    dv(lambda e: e.tensor_tensor(out=g44(M2[:]), in0=goh[:].unsqueeze(3).broadcast_to([128, 16, 4, 4]),
                                                 in1=oh2[:].unsqueeze(2).broadcast_to([128, 16, 4, 4]), op=ALU.mult))
                    dv(lambda e: e.tensor_tensor(out=Mb[:].rearrange("p (j e) -> p j e", e=16), in0=M1[:], in1=M2[:], op=ALU.add))
                    mm(banks[6][:, 0:256], ustr[:], Mb[:], True, True, [ROUT, CONST], [BK[6]], True)
                    mm(banks[7][:, 0:256], onesb[:], Mb[:], True, True, [ROUT, CONST], [BK[7]], True)
                    dv(lambda e: e.tensor_copy(out=TOT[:].rearrange("p j e -> p (j e)"), in_=banks[7][:, 0:256]), [BK[7]])
                    dv(lambda e: e.memset(JP[:, 0, :], 0.0))
                    for j in range(1, 16):
                        dv(lambda e, j=j: e.tensor_tensor(out=JP[:, j, :], in0=JP[:, j - 1, :], in1=TOT[:, j - 1, :], op=ALU.add))
                    dv(lambda e: e.tensor_tensor(out=ne[:], in0=JP[:, 15, :], in1=TOT[:, 15, :], op=ALU.add))
                    dv(lambda e: e.tensor_scalar(out=ntl[:], in0=ne[:], scalar1=0.0, scalar2=None, op0=ALU.is_gt))
                    for q in range(1, S // TS):
                        dv(lambda e, q=q: e.scalar_tensor_tensor(out=ntl[:], in0=ne[:], scalar=float(TS * q), in1=ntl[:],
                                                                 op0=ALU.is_gt, op1=ALU.add))
                    dv(lambda e: e.memset(base[:, 0:1], 0.0))
                    for e_ in range(1, 16):
                        dv(lambda e, e_=e_: e.tensor_tensor(out=base[:, e_:e_ + 1], in0=base[:, e_ - 1:e_],
                                                            in1=ntl[:, e_ - 1:e_], op=ALU.add))
                    dv(lambda e: e.tensor_tensor(out=bend[:], in0=base[:], in1=ntl[:], op=ALU.add))
                    dv(lambda e: e.tensor_scalar(out=b256[:], in0=base[:], scalar1=float(TS), scalar2=None, op0=ALU.mult))
                    dv(lambda e: e.tensor_tensor(out=SL[:], in0=banks[6][:, 0:256].rearrange("p (j e) -> p j e", e=16),
                                                 in1=JP[:], op=ALU.add), [BK[6]])
                    dv(lambda e: e.tensor_tensor(out=SL[:], in0=SL[:], in1=b256[:].unsqueeze(1).broadcast_to([128, 16, 16]),
                                                 op=ALU.add))
                    dv(lambda e: e.tensor_tensor(out=M1[:], in0=M1[:], in1=SL[:], op=ALU.mult))
                    dv(lambda e: e.tensor_tensor(out=M2[:], in0=M2[:], in1=SL[:], op=ALU.mult))
                    dv(lambda e: e.tensor_reduce(out=sl1[:], in_=M1[:], axis=AX.X, op=ALU.add))
                    dv(lambda e: e.tensor_reduce(out=sl2[:], in_=M2[:], axis=AX.X, op=ALU.add))
                    dv(lambda e: e.tensor_copy(out=s1u[:], in_=sl1[:]))
                    dv(lambda e: e.tensor_copy(out=s2u[:], in_=sl2[:]))
                    dv(lambda e: e.memset(eidx[:], 0.0))
                    for e_ in range(16):
                        dv(lambda e, e_=e_: e.scalar_tensor_tensor(out=eidx[:], in0=iotas[:, 1:1 + NTILE], scalar=bend[:, e_:e_ + 1],
                                                                   in1=eidx[:], op0=ALU.is_ge, op1=ALU.add), [CONST])
                    dv(lambda e: e.tensor_scalar(out=eidx[:], in0=eidx[:], scalar1=15.0, scalar2=None, op0=ALU.min))
                    dv(lambda e: e.tensor_scalar(out=widf[:], in0=eidx[:], scalar1=128.0, scalar2=iotas[:, 0:1],
                                                 op0=ALU.mult, op1=ALU.add), [CONST])
                    dv(lambda e: e.tensor_copy(out=widx[:], in_=widf[:]))
                    load_tile_w(0)
                    load_tile_w(1, which=(0, 1))
                    for j in range(16):
                        hb = j % 2
                        bk = 4 + hb
                        pv = banks[bk][:, :].bitcast(BF16)
                        for k in range(8):
                            T.emit("pe", lambda e, pv=pv, k=k, j=j: e.transpose(
                                out=pv[:, k * 128:(k + 1) * 128], in_=H[:, k, j * 128:(j + 1) * 128], identity=identb[:]),
                                reads=[HB[j // 4], CONST], writes=[BK[bk]], sig=(k == 7))
                        evac_copy(hrow[:, hb, :], pv, [BK[bk]], [HROW[hb]])
                        T.idma(HS_d, s1u[:, j:j + 1], hrow[:, hb, :], None, reads=[HROW[hb], ROUT], writes=[HSB[2 * j]])
                        T.idma(HS_d, s2u[:, j:j + 1], hrow[:, hb, :], None, reads=[HROW[hb], ROUT], writes=[HSB[2 * j + 1]])
                    T.barrier()

                with ExitStack() as pbx:
                    hs_sb = sb(pbx, "hs_sb", [128, 3, 2, D], BF16)
                    hcT = sb(pbx, "hcT", [128, 2, 8, TS], BF16)
                    a_e = sb(pbx, "a_e", [128, 2, 4, TS], BF16)
                    sg = sb(pbx, "sg", [128, 2, TS], F32)
                    ys = sb(pbx, "ys", [128, 2, 2, D], F32)
                    HSS = [Buf("hss0"), Buf("hss1"), Buf("hss2")]
                    HCT = [Buf("hct0"), Buf("hct1")]
                    AE = [[Buf(f"ae{b}_{f}") for f in range(4)] for b in range(2)]
                    SG = [Buf("sg0"), Buf("sg1")]
                    YS = [Buf("ysb0"), Buf("ysb1")]

                    def emit_hs(i):
                        hb3 = i % 3
                        T.dma("sp", hs_sb[:, hb3], HS_d[i * TS:(i + 1) * TS, :].rearrange("(a p) d -> p a d", p=128),
                              reads=HSB, writes=[HSS[hb3]])

                    def emit_tr(i):
                        ab = i % 2
                        hb3 = i % 3
                        for hb in range(2):
                            bk = 6 + hb
                            pv = banks[bk][:, :].bitcast(BF16)
                            for kk in range(4):
                                k = hb * 4 + kk
                                for a in range(2):
                                    T.emit("pe", lambda e, pv=pv, kk=kk, a=a, k=k, hb3=hb3: e.transpose(
                                        out=pv[:, kk * TS + a * 128:kk * TS + (a + 1) * 128],
                                        in_=hs_sb[:, hb3, a, k * 128:(k + 1) * 128], identity=identb[:]),
                                        reads=[HSS[hb3], CONST], writes=[BK[bk]], sig=(kk == 3 and a == 1))
                            evac_copy(hcT[:, ab, hb * 4:(hb + 1) * 4, :].rearrange("p k t -> p (k t)"), pv, [BK[bk]], [HCT[ab]])

                    def emit_gu(i):
                        ab = i % 2
                        sg_, su_ = (3 * i) % 6, (3 * i + 1) % 6
                        for f in range(4):
                            bg, bu = f % 2, 2 + f % 2
                            for k in range(8):
                                mm(banks[bg][:, 0:TS], ring[:, sg_, k * 512 + f * 128:k * 512 + (f + 1) * 128], hcT[:, ab, k, :],
                                   k == 0, k == 7, [RSLOT[sg_], HCT[ab]], [BK[bg]], k == 7)
                            for k in range(8):
                                mm(banks[bu][:, 0:TS], ring[:, su_, k * 512 + f * 128:k * 512 + (f + 1) * 128], hcT[:, ab, k, :],
                                   k == 0, k == 7, [RSLOT[su_], HCT[ab]], [BK[bu]], k == 7)
                            i2 = f % 2
                            T.emit("act", lambda e, bg=bg, i2=i2: e.activation(out=sg[:, i2, :], in_=banks[bg][:, 0:TS], func=AF.Silu),
                                   reads=[BK[bg]], writes=[SG[i2]])
                            T.emit("dve", lambda e, bu=bu, i2=i2, ab=ab, f=f: e.tensor_tensor(
                                out=a_e[:, ab, f, :], in0=banks[bu][:, 0:TS], in1=sg[:, i2, :], op=ALU.mult),
                                reads=[BK[bu], SG[i2]], writes=[AE[ab][f]])

                    def emit_d(i):
                        ab = i % 2
                        sd_ = (3 * i + 2) % 6
                        for a in range(2):
                            for dh in range(2):
                                bk = 4 + dh
                                for f in range(4):
                                    mm(banks[bk][:, :], a_e[:, ab, f, a * 128:(a + 1) * 128],
                                       ring[:, sd_, f * 1024 + dh * 512:f * 1024 + (dh + 1) * 512],
                                       f == 0, f == 3, [RSLOT[sd_], AE[ab][f]], [BK[bk]], f == 3)
                                evac_copy(ys[:, ab, a, dh * 512:(dh + 1) * 512], banks[bk][:, :], [BK[bk]], [YS[ab]])
                        T.dma("sp", YS_d[i * TS:(i + 1) * TS, :].rearrange("(a p) d -> p a d", p=128), ys[:, ab],
                              reads=[YS[ab]], writes=[YSB[i]])

                    emit_hs(0)
                    emit_hs(1)
                    emit_tr(0)
                    for i in range(NTILE):
                        if i + 2 < NTILE:
                            emit_hs(i + 2)
                        if i + 1 < NTILE:
                            emit_tr(i + 1)
                        emit_gu(i)
                        if i + 2 < NTILE:
                            load_tile_w(i + 2, which=(0, 1))
                        if i > 0:
                            emit_d(i - 1)
                        if i + 1 < NTILE:
                            load_tile_w(i + 1, which=(2,))
                    emit_d(NTILE - 1)
                    T.barrier()

                with ExitStack() as pcx:
                    g0 = sb(pcx, "g0", [128, 4, D], F32)
                    g1 = sb(pcx, "g1", [128, 4, D], F32)
                    G0 = [Buf(f"g0{i}") for i in range(4)]
                    G1 = [Buf(f"g1{i}") for i in range(4)]
                    for j in range(16):
                        b = j % 4
                        T.idma(g0[:, b, :], None, YS_d, s1u[:, j:j + 1], reads=YSB + [ROUT], writes=[G0[b]])
                        T.idma(g1[:, b, :], None, YS_d, s2u[:, j:j + 1], reads=YSB + [ROUT], writes=[G1[b]])
                        T.emit("dve", lambda e, b=b, j=j: e.tensor_scalar(out=g0[:, b, :], in0=g0[:, b, :], scalar1=w1[:, j:j + 1],
                                                                          scalar2=None, op0=ALU.mult), reads=[G0[b], ROUT], writes=[G0[b]])
                        T.emit("dve", lambda e, b=b, j=j: e.scalar_tensor_tensor(out=g0[:, b, :], in0=g1[:, b, :], scalar=w2[:, j:j + 1],
                                                                                 in1=g0[:, b, :], op0=ALU.mult, op1=ALU.add),
                               reads=[G0[b], G1[b], ROUT], writes=[G0[b]])
                        for hb in range(2):
                            bk = 2 * (j % 2) + hb
                            for kk in range(4):
                                k = hb * 4 + kk
                                T.emit("pe", lambda e, bk=bk, kk=kk, k=k, b=b: e.transpose(
                                    out=banks[bk][:, kk * 128:(kk + 1) * 128], in_=g0[:, b, k * 128:(k + 1) * 128],
                                    identity=identf[:]), reads=[G0[b], CONST], writes=[BK[bk]], sig=(kk == 3))
                            T.emit("dve", lambda e, bk=bk, hb=hb, j=j: e.tensor_tensor(
                                out=xT[:, j, hb * 4:(hb + 1) * 4, :],
                                in0=banks[bk][:, :].rearrange("p (k t) -> p k t", t=128),
                                in1=xT[:, j, hb * 4:(hb + 1) * 4, :], op=ALU.add),
                                reads=[BK[bk]] + [XB[hb * 4 + kk][j // 4] for kk in range(4)],
                                writes=[XB[hb * 4 + kk][j // 4] for kk in range(4)])

                if not last:
                    for t in range(4):
                        tl = tok0 // 128 + 4 * t
                        T.dma("sp", xT_d[tl:tl + 4].rearrange("j p k t -> p j k t"), xT[:, 4 * t:4 * t + 4],
                              reads=[XB[k][t] for k in range(8)], writes=[XD[s][t]])
                else:
                    T.barrier()
                    final_out(xT, XB, s)
                T.barrier()

        def f_phase(l, s, last):
            tok0 = s * S
            with ExitStack() as pf:
                xT = sb(pf, "xT", [128, 8, S], F32)
                H = sb(pf, "Hf", [128, 8, S], BF16)
                ring = sb(pf, "ring", [128, 6, 4096], BF16)
                sq = sb(pf, "sqf", [128, 8, 512], BF16)
                rstd = sb(pf, "rstdf", [128, 512], F32)
                wr_sb = sb(pf, "wr_sb", [128, 8, 20], F32)
                wrp = sb(pf, "wrp", [128, 8, 20], F32)
                a_e = sb(pf, "a_e", [128, 2, 4, 512], BF16)
                sg = sb(pf, "sg", [128, 2, 512], F32)
                tt = sb(pf, "tt", [128, 2, 512], F32)
                Cb = sb(pf, "Cb", [128, 2, 512], F32)
                rt = sb(pf, "rt", [128, 16], F32)
                LGs = sb(pf, "LGs", [128, 16, 20], F32)
                r1 = sb(pf, "r1", [128, 16, 16], F32)
                r2 = sb(pf, "r2", [128, 16, 16], F32)
                gmax = sb(pf, "gmax", [128, 16], F32)
                goh = sb(pf, "goh", [128, 16, 4], F32)
                gex = sb(pf, "gex", [128, 16, 4], F32)
                gw = sb(pf, "gw", [128, 16], F32)
                esel = sb(pf, "esel", [128, 16, 4], F32)
                em = sb(pf, "em", [128, 16, 4], F32)
                m1 = sb(pf, "m1", [128, 16], F32)
                m2 = sb(pf, "m2", [128, 16], F32)
                oh1 = sb(pf, "oh1", [128, 16, 4], F32)
                oh2 = sb(pf, "oh2", [128, 16, 4], F32)
                w1 = sb(pf, "w1", [128, 16], F32)
                w2 = sb(pf, "w2", [128, 16], F32)
                c4 = sb(pf, "c4", [128, 16, 4], F32)
                Cm = sb(pf, "Cm", [128, 16, 16], F32)
                Chi = sb(pf, "Chi", [128, 16, 16], BF16)
                Clo = sb(pf, "Clo", [128, 16, 16], BF16)
                XB = [[Buf(f"X{k}_{t}") for t in range(4)] for k in range(8)]
                HB = [Buf(f"Hf{t}") for t in range(4)]
                RSLOT = [Buf(f"ring{i}") for i in range(6)]
                SQ, RS, WR, WRP, RT = Buf("sq"), Buf("rs"), Buf("wr"), Buf("wrp"), Buf("rt")
                ROUT = Buf("router")
                AE = [[Buf(f"ae{b}_{f}") for f in range(4)] for b in range(2)]
                SG = [Buf(f"sg{i}") for i in range(2)]
                TT = [Buf(f"tt{i}") for i in range(2)]
                CB = [Buf("cb0"), Buf("cb1")]

                def load_expert(e):
                    e4 = e // 4
                    for j, (nm, wb) in enumerate((("g", w_gate_b), ("u", w_up_b))):
                        slot = (3 * e + j) % 6
                        T.dma("sp", ring[:, slot, :].rearrange("p (k f) -> p k f", f=512),
                              wb[l, e].rearrange("(k p) f -> p k f", p=128), reads=[WB[(nm, l, e4)]], writes=[RSLOT[slot]])
                    slot = (3 * e + 2) % 6
                    T.dma("sp", ring[:, slot, :].rearrange("p (k f) -> p k f", f=1024),
                          w_down_b[l, e].rearrange("(k p) f -> p k f", p=128), reads=[WB[("d", l, e4)]], writes=[RSLOT[slot]])

                for k in range(8):
                    T.dma("sp", xT[:, k, :], xT_d[k, :, tok0:tok0 + S], reads=[XD[s][t] for t in range(4)],
                          writes=[XB[k][t] for t in range(4)])
                T.dma("sp", wr_sb[:], wr_d[:, l], writes=[WR])
                load_expert(0)
                for k in range(8):
                    T.emit("dve", lambda e, k=k: e.tensor_scalar(out=wrp[:, k, :], in0=wr_sb[:, k, :],
                                                                 scalar1=gf[:, l * 8 + k:l * 8 + k + 1], scalar2=None,
                                                                 op0=ALU.mult), reads=[WR, CONST], writes=[WRP])
                for t in range(4):
                    c0 = t * 512
                    T.emit("act", lambda e, c0=c0: e.activation(out=sq[:], in_=xT[:, :, c0:c0 + 512], func=AF.Square),
                           reads=[XB[k][t] for k in range(8)], writes=[SQ])
                    for k in range(8):
                        mm(banks[6][:, :], onesb[:], sq[:, k, :], k == 0, k == 7, [SQ, CONST], [BK[6]], k == 7)
                    rstd_from_ss(rstd[:], banks[6][:, :], [BK[6]], [RS])
                    for k in range(8):
                        T.emit("dve", lambda e, k=k, c0=c0: e.scalar_tensor_tensor(
                            out=H[:, k, c0:c0 + 512], in0=xT[:, k, c0:c0 + 512], scalar=gf[:, l * 8 + k:l * 8 + k + 1],
                            in1=rstd[:], op0=ALU.mult, op1=ALU.mult), reads=[XB[k][t], RS, CONST], writes=[HB[t]])
                    for j in range(4):
                        jj = 4 * t + j
                        for k in range(8):
                            mm(banks[7][:, 320 + jj:320 + jj + 1], sq[:, k, j * 128:(j + 1) * 128], onesb[:, 0:1],
                               k == 0, k == 7, [SQ, CONST], [BK[7]], False)
                        for k in range(8):
                            mm(banks[7][:, jj * 20:(jj + 1) * 20], xT[:, k, c0 + j * 128:c0 + (j + 1) * 128], wrp[:, k, :],
                               k == 0, k == 7, [XB[k][t], WRP], [BK[7]], (k == 7))
                R_ = [ROUT]

                def dv(fn, extra_reads=()):
                    T.emit("dve", fn, reads=[ROUT] + list(extra_reads), writes=[ROUT])

                def b3(ap2, n):
                    return ap2.unsqueeze(2).broadcast_to([128, 16, n])

                rstd_from_ss(rt[:], banks[7][:, 320:336], [BK[7], ROUT], [ROUT])
                dv(lambda e: e.tensor_tensor(out=LGs[:], in0=banks[7][:, 0:320].rearrange("p (j e) -> p j e", e=20),
                                             in1=b3(rt[:], 20), op=ALU.mult), [BK[7]])
                dv(lambda e: e.tensor_reduce(out=gmax[:], in_=LGs[:, :, 0:4], axis=AX.X, op=ALU.max))
                dv(lambda e: e.tensor_tensor(out=goh[:], in0=LGs[:, :, 0:4], in1=b3(gmax[:], 4), op=ALU.is_equal))
                dv(lambda e: e.tensor_tensor(out=gex[:], in0=LGs[:, :, 0:4], in1=b3(gmax[:], 4), op=ALU.subtract))
                T.emit("act", lambda e: e.activation(out=gex[:], in_=gex[:], func=AF.Exp), reads=[ROUT], writes=[ROUT])
                dv(lambda e: e.tensor_reduce(out=gw[:], in_=gex[:], axis=AX.X, op=ALU.add))
                dv(lambda e: e.reciprocal(out=gw[:], in_=gw[:]))
                dv(lambda e: e.tensor_tensor(out=r1[:].rearrange("p j (g i) -> p j g i", i=4),
                                             in0=LGs[:, :, 4:20].rearrange("p j (g i) -> p j g i", i=4),
                                             in1=goh[:].unsqueeze(3).broadcast_to([128, 16, 4, 4]), op=ALU.mult))
                dv(lambda e: e.tensor_reduce(out=esel[:], in_=r1[:].rearrange("p j (g i) -> p j i g", i=4),
                                             axis=AX.X, op=ALU.add))
                dv(lambda e: e.tensor_reduce(out=m1[:], in_=esel[:], axis=AX.X, op=ALU.max))
                dv(lambda e: e.tensor_tensor(out=oh1[:], in0=esel[:], in1=b3(m1[:], 4), op=ALU.is_equal))
                dv(lambda e: e.scalar_tensor_tensor(out=em[:], in0=oh1[:], scalar=-1.0e30, in1=esel[:],
                                                    op0=ALU.mult, op1=ALU.add))
                dv(lambda e: e.tensor_reduce(out=m2[:], in_=em[:], axis=AX.X, op=ALU.max))
                dv(lambda e: e.tensor_tensor(out=oh2[:], in0=em[:], in1=b3(m2[:], 4), op=ALU.is_equal))
                dv(lambda e: e.tensor_tensor(out=w2[:], in0=m2[:], in1=m1[:], op=ALU.subtract))
                T.emit("act", lambda e: e.activation(out=w2[:], in_=w2[:], func=AF.Exp), reads=[ROUT], writes=[ROUT])
                dv(lambda e: e.tensor_scalar(out=w2[:], in0=w2[:], scalar1=1.0, scalar2=None, op0=ALU.add))
                dv(lambda e: e.reciprocal(out=w1[:], in_=w2[:]))
                dv(lambda e: e.tensor_tensor(out=w1[:], in0=w1[:], in1=gw[:], op=ALU.mult))
                dv(lambda e: e.tensor_tensor(out=w2[:], in0=gw[:], in1=w1[:], op=ALU.subtract))
                dv(lambda e: e.tensor_tensor(out=c4[:], in0=oh1[:], in1=b3(w1[:], 4), op=ALU.mult))
                dv(lambda e: e.tensor_tensor(out=oh2[:], in0=oh2[:], in1=b3(w2[:], 4), op=ALU.mult))
                dv(lambda e: e.tensor_tensor(out=c4[:], in0=c4[:], in1=oh2[:], op=ALU.add))
                dv(lambda e: e.tensor_tensor(out=Cm[:].rearrange("p j (g i) -> p j g i", i=4),
                                             in0=goh[:].unsqueeze(3).broadcast_to([128, 16, 4, 4]),
                                             in1=c4[:].unsqueeze(2).broadcast_to([128, 16, 4, 4]), op=ALU.mult))
                dv(lambda e: e.tensor_copy(out=Chi[:], in_=Cm[:]))
                dv(lambda e: e.tensor_tensor(out=r2[:], in0=Cm[:], in1=Chi[:], op=ALU.subtract))
                dv(lambda e: e.tensor_copy(out=Clo[:], in_=r2[:]))

                steps = [(e_, t_) for e_ in range(NE) for t_ in range(4)]

                def emit_gu(idx):
                    e_, t = steps[idx]
                    ab = idx % 2
                    c0 = t * 512
                    sg_, su_, sd_ = (3 * e_) % 6, (3 * e_ + 1) % 6, (3 * e_ + 2) % 6
                    n = 0
                    for j in range(4):
                        for Cx in (Chi, Clo):
                            mm(banks[6][:, j * 128:(j + 1) * 128], Cx[:, 4 * t + j, e_:e_ + 1].broadcast_to([128, 128]),
                               identb[:], Cx is Chi, Cx is Clo, [ROUT, CONST], [BK[6]], (j == 3 and Cx is Clo))
                    T.emit("act", lambda e, ab=ab: e.activation(out=Cb[:, ab, :], in_=banks[6][:, :], func=AF.Copy),
                           reads=[BK[6]], writes=[CB[ab]])
                    for f in range(4):
                        bg, bu = f % 2, 2 + f % 2
                        for k in range(8):
                            mm(banks[bg][:, :], ring[:, sg_, k * 512 + f * 128:k * 512 + (f + 1) * 128], H[:, k, c0:c0 + 512],
                               k == 0, k == 7, [RSLOT[sg_], HB[t]], [BK[bg]], k == 7)
                        for k in range(8):
                            mm(banks[bu][:, :], ring[:, su_, k * 512 + f * 128:k * 512 + (f + 1) * 128], H[:, k, c0:c0 + 512],
                               k == 0, k == 7, [RSLOT[su_], HB[t]], [BK[bu]], k == 7)
                        i3 = f % 2
                        T.emit("act", lambda e, bg=bg, i3=i3: e.activation(out=sg[:, i3, :], in_=banks[bg][:, :], func=AF.Silu),
                               reads=[BK[bg]], writes=[SG[i3]])
                        T.emit("dve", lambda e, bu=bu, i3=i3: e.tensor_tensor(out=tt[:, i3, :], in0=banks[bu][:, :],
                                                                            in1=sg[:, i3, :], op=ALU.mult),
                               reads=[BK[bu], SG[i3]], writes=[TT[i3]])
                        T.emit("dve", lambda e, ab=ab, f=f, i3=i3: e.tensor_tensor(out=a_e[:, ab, f, :], in0=tt[:, i3, :],
                                                                                   in1=Cb[:, ab, :], op=ALU.mult),
                               reads=[TT[i3], CB[ab]], writes=[AE[ab][f]])

                def emit_d(idx):
                    e_, t = steps[idx]
                    ab = idx % 2
                    c0 = t * 512
                    sd_ = (3 * e_ + 2) % 6
                    for m in range(8):
                        bk = 4 + m % 2
                        for f in range(4):
                            mm(banks[bk][:, :], ring[:, sd_, f * 1024 + m * 128:f * 1024 + (m + 1) * 128], a_e[:, ab, f, :],
                               f == 0, f == 3, [RSLOT[sd_], AE[ab][f]], [BK[bk]], f == 3)
                        T.emit("dve", lambda e, m=m, bk=bk, c0=c0: e.tensor_tensor(
                            out=xT[:, m, c0:c0 + 512], in0=banks[bk][:, :], in1=xT[:, m, c0:c0 + 512], op=ALU.add),
                            reads=[BK[bk], XB[m][t]], writes=[XB[m][t]])

                for idx in range(len(steps)):
                    e_, t = steps[idx]
                    emit_gu(idx)
                    if idx > 0:
                        emit_d(idx - 1)
                    if t == 0 and e_ + 1 < NE:
                        load_expert(e_ + 1)
                emit_d(len(steps) - 1)

                if not last:
                    for k in range(8):
                        T.dma("sp", xT_d[k, :, tok0:tok0 + S], xT[:, k, :], reads=[XB[k][t] for t in range(4)],
                              writes=[XD[s][t] for t in range(4)])
                else:
                    final_out(xT, XB, s)
                T.barrier()

        done = False
        for l in range(depth):
            for s in range(nseq):
                m_phase(l, s)
            if stop_after == ("M", l):
                done = True
                break
            for s in range(nseq):
                (f_phase_sparse if SPARSE else f_phase)(l, s, last=(l == depth - 1 and stop_after is None))
            if stop_after == ("F", l):
                done = True
                break
        T.barrier()
        T.flush()
    return nc


def _bias_index_map():
    hp = np.arange(4)[:, None, None, None, None]
    p = np.arange(128)[None, :, None, None, None]
    eo = np.arange(2)[None, None, :, None, None]
    i = np.arange(14)[None, None, None, :, None]
    cq = np.arange(64)[None, None, None, None, :]
    kc = p % 64
    half = p // 64
    h = 2 * hp + eo
    dr = i + half
    dc = np.clip(kc - cq, -15, 15) + 15
    qs = np.clip(cq - 8, 0, 48)
    valid = (kc >= qs) & (kc < qs + 16)
    flat = h * (15 * 31 + 1) + np.where(valid, dr * 31 + dc, 15 * 31)
    return np.broadcast_to(flat, (4, 128, 2, 14, 64)).copy()


def _pool_consts():
    import ml_dtypes
    out = np.zeros((4, 7, 128, 128), np.float32)
    Sr = 512
    t = np.arange(Sr)
    for g, w in enumerate(POOL_W):
        lo = np.clip(t - w // 2, 0, Sr)
        hi = np.clip(t - w // 2 + w, 0, Sr)
        cnt = (hi - lo).astype(np.float64)
        A = np.zeros((Sr, Sr), np.float64)
        for tt_ in range(Sr):
            A[lo[tt_]:hi[tt_], tt_] = 1.0 / cnt[tt_]
        A -= np.eye(Sr)

        def blk(a, b):
            return A[a * 128:(a + 1) * 128, b * 128:(b + 1) * 128]

        def hilo(M):
            hi_ = M.astype(np.float32).astype(ml_dtypes.bfloat16).astype(np.float64)
            lo_ = (M - hi_).astype(np.float32).astype(ml_dtypes.bfloat16).astype(np.float64)
            return hi_, lo_

        out[g, 0] = blk(1, 2)
        out[g, 1] = blk(2, 2)
        out[g, 2] = blk(3, 2)
        out[g, 3], out[g, 4] = hilo(blk(0, 0))
        out[g, 5], out[g, 6] = hilo(blk(3, 3))
    return np.ascontiguousarray(out.reshape(28, 128, 128).transpose(1, 0, 2))


_BIAS_MAP = None
_PROG = {}


def prep_inputs(inp, nseq=NSEQ_FULL, ncores=NCORES):
    global _BIAS_MAP
    f = lambda a: np.ascontiguousarray(np.asarray(a, dtype=np.float32))
    Lw = L_FULL
    if _BIAS_MAP is None:
        _BIAS_MAP = _bias_index_map()
    rpb = f(inp["rpb"])
    pad = np.concatenate([rpb.reshape(Lw, 8, 15 * 31), np.full((Lw, 8, 1), NEG, np.float32)], axis=2).reshape(Lw, -1)
    biasT = pad[:, _BIAS_MAP]
    biasT = np.ascontiguousarray(biasT.reshape(Lw, 4, 128, 2 * 14 * 64))
    shared = {
        "w_in": f(inp["w_in"]), "w_out": f(inp["w_out"]), "pool_w": f(inp["pool_w"]),
        "w_gate": f(inp["w_gate"]), "w_up": f(inp["w_up"]), "w_down": f(inp["w_down"]),
        "gm": np.ascontiguousarray(f(inp["norm_mix_g"]).reshape(Lw, 8, 128).transpose(2, 0, 1).reshape(128, Lw * 8)),
        "gf": np.ascontiguousarray(f(inp["norm_ffn_g"]).reshape(Lw, 8, 128).transpose(2, 0, 1).reshape(128, Lw * 8)),
        "ps": np.ascontiguousarray(f(inp["pool_scale"]).reshape(Lw, 4, 128).transpose(2, 0, 1).reshape(128, Lw * 4)),
        "gF": np.ascontiguousarray(np.broadcast_to(f(inp["final_g"])[None, :], (128, D))),
        "wr": np.ascontiguousarray(np.concatenate([f(inp["w_router_group"]), f(inp["w_router_expert"])], axis=-1)
                                   .reshape(Lw, 8, 128, 20).transpose(2, 0, 1, 3)),
        "biasT": biasT,
        "poolA": _pool_consts(),
        "ident": np.eye(128, dtype=np.float32),
        "ustrict": np.triu(np.ones((128, 128), np.float32), k=1),
        "iotas": np.ascontiguousarray(np.concatenate([np.arange(128, dtype=np.float32)[:, None],
                                                      np.broadcast_to(np.arange(32, dtype=np.float32)[None, :], (128, 32))], axis=1)),
    }
    x = f(inp["x"]).reshape(-1, S, D)
    maps = []
    for c in range(ncores):
        m = dict(shared)
        m["x"] = np.ascontiguousarray(x[c * nseq:(c + 1) * nseq].reshape(nseq * S, D))
        maps.append(m)
    return maps


def kernel(x, norm_mix_g, w_in, rpb, pool_w, pool_scale, w_out, norm_ffn_g, w_router_group, w_router_expert,
           w_gate, w_up, w_down, final_g):
    inp = dict(x=x, norm_mix_g=norm_mix_g, w_in=w_in, rpb=rpb, pool_w=pool_w, pool_scale=pool_scale, w_out=w_out,
               norm_ffn_g=norm_ffn_g, w_router_group=w_router_group, w_router_expert=w_router_expert,
               w_gate=w_gate, w_up=w_up, w_down=w_down, final_g=final_g)
    maps = prep_inputs(inp)
    if "full" not in _PROG:
        _PROG["full"] = build_program()
    nc = _PROG["full"]
    res = run_bass_kernel_spmd(nc, maps, core_ids=list(range(NCORES)))
    outs = [np.asarray(r["out"], dtype=np.float32).reshape(NSEQ_FULL, S, D) for r in res.results]
    return np.concatenate(outs, axis=0)
```

```python
import numpy as np
from contextlib import ExitStack
import concourse.bass as bass
import concourse.mybir as mybir
from concourse.bass_utils import run_bass_kernel_spmd

F32 = mybir.dt.float32
BF16 = mybir.dt.bfloat16
AF = mybir.ActivationFunctionType
ALU = mybir.AluOpType
AX = mybir.AxisListType

D = 1024
S = 2048
L_FULL = 4
NSEQ_FULL = 4
NCORES = 8
NE = 16
DE = 512
EPS = 1e-6
NEG = -30000.0
POOL_W = (2, 4, 8, 16)


class Buf:
    __slots__ = ("name", "w", "r")

    def __init__(self, name):
        self.name = name
        self.w = None
        self.r = {}


class _Eng:
    def __init__(self, name):
        self.name = name
        self.q = []
        self.known = {}
        self.count = 0
        self.psem = None


class Tracker:
    ENGS = ("pe", "act", "dve", "pool", "sp")

    def __init__(self, nc, stack, n_dma_sems=24):
        self.nc = nc
        self.sems = []
        self.eng = {n: _Eng(n) for n in self.ENGS}
        for n in ("pe", "act", "dve", "pool"):
            self.eng[n].psem = self._new_sem(stack, "p_" + n)
        self.dpool = [self._new_sem(stack, f"dq{i}") for i in range(n_dma_sems)]
        self.dcnt = [0] * n_dma_sems
        self.dnext = 0
        self.stack = stack
        self.extra = []

    def _new_sem(self, stack, name):
        h = stack.enter_context(self.nc.semaphore(name))
        self.sems.append(h)
        return len(self.sems) - 1

    def _deps(self, E, reads, writes, extra=()):
        need = {}

        def req(ev):
            if ev is None:
                return
            s, v = ev
            if s == E.psem and E.name == "pe":
                return
            if E.known.get(s, 0) >= v:
                return
            if need.get(s, 0) < v:
                need[s] = v

        for ev in extra:
            req(ev)
        for b in reads:
            req(b.w)
        for b in writes:
            req(b.w)
            for s, v in b.r.items():
                req((s, v))
        for s, v in need.items():
            E.q.append(("wait", s, v))
            E.known[s] = v

    def _mark(self, ev, reads, writes):
        for b in reads:
            if b.r.get(ev[0], 0) < ev[1]:
                b.r[ev[0]] = ev[1]
        for b in writes:
            b.w = ev
            b.r = {}

    def emit(self, eng, fn, reads=(), writes=(), sig=True):
        E = self.eng[eng]
        self._deps(E, reads, writes)
        if sig:
            E.count += 1
            ev = (E.psem, E.count)
        else:
            ev = (E.psem, E.count + 1)
        E.q.append(("op", fn, sig))
        self._mark(ev, reads, writes)
        return ev

    def dma(self, q, out, in_, reads=(), writes=(), own_sem=False):
        E = self.eng[q]
        if own_sem:
            s = self._new_sem(self.stack, f"ds{len(self.sems)}")
            self.extra.append(s)
            prev = 0
            self._deps(E, reads, writes)
            ev = (s, 16)
        else:
            i = self.dnext
            self.dnext = (i + 1) % len(self.dpool)
            s = self.dpool[i]
            prev = self.dcnt[i]
            self._deps(E, reads, writes, extra=[(s, prev)] if prev else ())
            self.dcnt[i] += 16
            ev = (s, self.dcnt[i])
        E.q.append(("dma", out, in_, s))
        self._mark(ev, reads, writes)
        return ev

    def idma(self, out, out_idx, in_, in_idx, reads=(), writes=(), bounds=None):
        E = self.eng["pool"]
        i = self.dnext
        self.dnext = (i + 1) % len(self.dpool)
        s = self.dpool[i]
        prev = self.dcnt[i]
        self._deps(E, reads, writes, extra=[(s, prev)] if prev else ())
        self.dcnt[i] += 16
        ev = (s, self.dcnt[i])
        E.q.append(("idma", out, out_idx, in_, in_idx, s, bounds))
        self._mark(ev, reads, writes)
        return ev

    def barrier(self):
        evs = []
        for n in ("pe", "act", "dve", "pool"):
            e = self.eng[n]
            if e.count:
                evs.append((e.psem, e.count))
        for i, s in enumerate(self.dpool):
            if self.dcnt[i]:
                evs.append((s, self.dcnt[i]))
        for s in self.extra:
            evs.append((s, 16))
        for fn in getattr(self, "extra_ev_fns", []):
            evs.extend(fn())
        for n in self.ENGS:
            E = self.eng[n]
            for s, v in evs:
                if s == E.psem:
                    continue
                if E.known.get(s, 0) < v:
                    E.q.append(("wait", s, v))
                    E.known[s] = v

    def flush(self):
        nc = self.nc
        sems = self.sems

        def run(E, h):
            psem = sems[E.psem] if E.psem is not None else None
            for it in E.q:
                if it[0] == "wait":
                    h.wait_ge(sems[it[1]], it[2])
                elif it[0] == "op":
                    ins = it[1](h)
                    if it[2]:
                        ins.then_inc(psem, 1)
                elif it[0] == "idma":
                    oo = bass.IndirectOffsetOnAxis(ap=it[2], axis=0) if it[2] is not None else None
                    io = bass.IndirectOffsetOnAxis(ap=it[4], axis=0) if it[4] is not None else None
                    if it[6] is None:
                        h.indirect_dma_start(out=it[1], out_offset=oo, in_=it[3], in_offset=io).then_inc(sems[it[5]], 16)
                    else:
                        h.indirect_dma_start(out=it[1], out_offset=oo, in_=it[3], in_offset=io, bounds_check=it[6],
                                             oob_is_err=False).then_inc(sems[it[5]], 16)
                else:
                    h.dma_start(out=it[1], in_=it[2]).then_inc(sems[it[3]], 16)

        with nc.Block() as block:
            @block.tensor
            def _(h):
                run(self.eng["pe"], h)

            @block.scalar
            def _(h):
                run(self.eng["act"], h)

            @block.vector
            def _(h):
                run(self.eng["dve"], h)

            @block.gpsimd
            def _(h):
                run(self.eng["pool"], h)

            @block.sync
            def _(h):
                run(self.eng["sp"], h)


SPARSE = True
INTERLEAVE_OUT = False
POOL_FROM_LAYER = 99
TS = 256
NTILE = 32


def build_program(nseq=NSEQ_FULL, depth=L_FULL, stop_after=None, debug_out=False):
    nc = bass.Bass("TRN2", target_bir_lowering=False)
    NT = nseq * S
    Lw = L_FULL

    def din(name, shape, dt=F32):
        return nc.dram_tensor(name, list(shape), dt, kind="ExternalInput").ap()

    x_c = din("x", [NT, D])
    w_in = din("w_in", [Lw, D, 2048])
    w_out = din("w_out", [Lw, D, D])
    pool_w = din("pool_w", [Lw, 4, 128, 128])
    w_gate = din("w_gate", [Lw, NE, D, DE])
    w_up = din("w_up", [Lw, NE, D, DE])
    w_down = din("w_down", [Lw, NE, DE, D])
    gm_d = din("gm", [128, Lw * 8])
    gf_d = din("gf", [128, Lw * 8])
    ps_d = din("ps", [128, Lw * 4])
    gF_d = din("gF", [128, D])
    wr_d = din("wr", [128, Lw, 8, 20])
    bias_d = din("biasT", [Lw, 4, 128, 2 * 14 * 64])
    poolA_d = din("poolA", [128, 28, 128])
    ident_d = din("ident", [128, 128])
    ustrict_d = din("ustrict", [128, 128])
    iotas_d = din("iotas", [128, 33])
    if stop_after is None:
        out_c = nc.dram_tensor("out", [NT, D], F32, kind="ExternalOutput").ap()
        xT_d = nc.dram_tensor("xT_d", [NT // 128, 128, 8, 128], F32, kind="Internal").ap()
    else:
        xT_d = nc.dram_tensor("xT_out", [NT // 128, 128, 8, 128], F32, kind="ExternalOutput").ap()
        out_c = None
    w_in_b = nc.dram_tensor("w_in_b", [Lw, D, 2048], BF16, kind="Internal").ap()
    w_out_b = nc.dram_tensor("w_out_b", [Lw, D, D], BF16, kind="Internal").ap()
    pool_w_b = nc.dram_tensor("pool_w_b", [Lw, 4, 128, 128], BF16, kind="Internal").ap()
    w_gate_b = nc.dram_tensor("w_gate_b", [Lw, NE, D, DE], BF16, kind="Internal").ap()
    w_up_b = nc.dram_tensor("w_up_b", [Lw, NE, D, DE], BF16, kind="Internal").ap()
    w_down_b = nc.dram_tensor("w_down_b", [Lw, NE, DE, D], BF16, kind="Internal").ap()
    w_gate_p = [nc.dram_tensor(f"w_gate_p{i}", [NE * 128, 4096], BF16, kind="Internal").ap() for i in range(Lw)]
    w_up_p = [nc.dram_tensor(f"w_up_p{i}", [NE * 128, 4096], BF16, kind="Internal").ap() for i in range(Lw)]
    w_down_p = [nc.dram_tensor(f"w_down_p{i}", [NE * 128, 4096], BF16, kind="Internal").ap() for i in range(Lw)]
    HS_d = nc.dram_tensor("HS_d", [NTILE * TS, D], BF16, kind="Internal").ap()
    YS_d = nc.dram_tensor("YS_d", [NTILE * TS, D], F32, kind="Internal").ap()

    top = ExitStack()
    with top:
        T = Tracker(nc, top)

        uid = [0]

        def sb(stack, name, shape, dt):
            uid[0] += 1
            return stack.enter_context(nc.sbuf_tensor(f"{name}_s{uid[0]}", list(shape), dt))

        banks = [top.enter_context(nc.psum_tensor(f"bank{i}", [128, 512], F32)) for i in range(8)]
        BK = [Buf(f"bank{i}") for i in range(8)]

        onesb = sb(top, "onesb", [128, 128], BF16)
        identf = sb(top, "identf", [128, 128], F32)
        identb = sb(top, "identb", [128, 128], BF16)
        A_bf = sb(top, "A_bf", [128, 28, 128], BF16)
        gm = sb(top, "gm", [128, Lw * 8], F32)
        gf = sb(top, "gf", [128, Lw * 8], F32)
        psc = sb(top, "psc", [128, Lw * 4], F32)
        epsc = sb(top, "epsc", [128, 1], F32)
        ustr_f = sb(top, "ustr_f", [128, 128], F32)
        ustr = sb(top, "ustr", [128, 128], BF16)
        iotas = sb(top, "iotas", [128, 33], F32)
        CONST = Buf("const")
        with nc.sbuf_tensor("A_stage", [128, 28, 128], F32) as A_st:
            ASB = Buf("A_stage")
            T.emit("dve", lambda e: e.memset(onesb[:], 1.0), writes=[CONST])
            T.emit("dve", lambda e: e.memset(epsc[:], EPS), writes=[CONST])
            T.dma("sp", identf[:], ident_d, writes=[CONST])
            T.dma("sp", gm[:], gm_d, writes=[CONST])
            T.dma("sp", gf[:], gf_d, writes=[CONST])
            T.dma("sp", psc[:], ps_d, writes=[CONST])
            T.dma("sp", A_st[:], poolA_d, writes=[ASB])
            T.dma("sp", ustr_f[:], ustrict_d, writes=[CONST])
            T.dma("sp", iotas[:], iotas_d, writes=[CONST])
            T.emit("dve", lambda e: e.tensor_copy(out=ustr[:], in_=ustr_f[:]), reads=[CONST], writes=[CONST])
            T.emit("dve", lambda e: e.tensor_copy(out=identb[:], in_=identf[:]), reads=[CONST], writes=[CONST])
            T.emit("dve", lambda e: e.tensor_copy(out=A_bf[:], in_=A_st[:]), reads=[ASB], writes=[CONST])
            T.barrier()

        WB = {}

        cast_sems = [T._new_sem(top, f"cs{i}") for i in range(8)]
        cast_cnt = [0] * 8
        cast_i = [0]

        def cast(key, dst, src, n=0):
            WB[key] = Buf(str(key))
            E = T.eng["pool"]
            i = cast_i[0] % 8
            cast_i[0] += 1
            sm = cast_sems[i]
            if cast_cnt[i] and E.known.get(sm, 0) < cast_cnt[i]:
                E.q.append(("wait", sm, cast_cnt[i]))
                E.known[sm] = cast_cnt[i]
            cast_cnt[i] += 16
            E.q.append(("dma", dst, src, sm))
            WB[key].w = (sm, cast_cnt[i])

        for l in range(depth):
            cast(("in", l), w_in_b[l], w_in[l])
            cast(("pw", l), pool_w_b[l].rearrange("g (a b) d -> (g a) (b d)", b=16),
                 pool_w[l].rearrange("g (a b) d -> (g a) (b d)", b=16))
            cast(("out", l), w_out_b[l].rearrange("(r q) d -> r (q d)", q=2),
                 w_out[l].rearrange("(r q) d -> r (q d)", q=2))
            for e_ in range(NE):
                cast(("g", l, e_), w_gate_p[l][e_ * 128:(e_ + 1) * 128, :].rearrange("p (k f) -> p k f", f=512),
                     w_gate[l, e_].rearrange("(k p) f -> p k f", p=128))
                cast(("u", l, e_), w_up_p[l][e_ * 128:(e_ + 1) * 128, :].rearrange("p (k f) -> p k f", f=512),
                     w_up[l, e_].rearrange("(k p) f -> p k f", p=128))
                cast(("d", l, e_), w_down_p[l][e_ * 128:(e_ + 1) * 128, :].rearrange("p (k f) -> p k f", f=1024),
                     w_down[l, e_].rearrange("(k p) f -> p k f", p=128))

        XD = [[Buf(f"xd{s}_{t}") for t in range(4)] for s in range(nseq)]
        flip = [0]

        def evac_copy(out, in_, reads, writes, scale=None):
            flip[0] ^= 1
            if scale is not None or flip[0]:
                sc = 1.0 if scale is None else scale
                T.emit("act", lambda e: e.activation(out=out, in_=in_, func=AF.Copy, scale=sc), reads=reads, writes=writes)
            else:
                T.emit("dve", lambda e: e.tensor_copy(out=out, in_=in_), reads=reads, writes=writes)

        def mm(out, lhsT, rhs, start, stop, reads, writes, sig):
            T.emit("pe", lambda e: e.matmul(out, lhsT, rhs, start=start, stop=stop), reads=reads, writes=writes, sig=sig)

        def rstd_from_ss(out, ss_ap, reads, writes):
            T.emit("act", lambda e: e.activation(out=out, in_=ss_ap, func=AF.Sqrt, bias=epsc[:, 0:1], scale=1.0 / D),
                   reads=list(reads) + [CONST], writes=writes)
            T.emit("dve", lambda e: e.reciprocal(out=out, in_=out), reads=writes, writes=writes)

        with ExitStack() as st:
            xin = sb(st, "xin", [128, 2, 4, 1024], F32)
            xTs = sb(st, "xTs", [128, 2, 4, 8, 128], F32)
            XIN = [Buf("xin0"), Buf("xin1")]
            XTS = [Buf("xts0"), Buf("xts1")]
            bi = 0
            for s in range(nseq):
                for t in range(4):
                    b = (s * 4 + t) % 2
                    t0 = s * S + t * 512
                    T.dma("sp", xin[:, b], x_c[t0:t0 + 512, :].rearrange("(j p) d -> p j d", p=128), writes=[XIN[b]])
                    for k in range(8):
                        bk = bi % 4
                        bi += 1
                        for j in range(4):
                            T.emit("pe", lambda e, bk=bk, j=j, k=k, b=b: e.transpose(
                                out=banks[bk][:, j * 128:(j + 1) * 128], in_=xin[:, b, j, k * 128:(k + 1) * 128],
                                identity=identf[:]), reads=[XIN[b], CONST], writes=[BK[bk]], sig=(j == 3))
                        evac_copy(xTs[:, b, :, k, :], banks[bk][:, :].rearrange("p (j t) -> p j t", t=128), [BK[bk]], [XTS[b]])
                    T.dma("sp", xT_d[t0 // 128:t0 // 128 + 4].rearrange("j p k t -> p j k t"), xTs[:, b], reads=[XTS[b]],
                          writes=[XD[s][t]])
            T.barrier()

        def m_phase(l, s):
            tok0 = s * S
            with ExitStack() as pm:
                H = sb(pm, "H", [128, 8, S], BF16)
                qT = sb(pm, "qT", [128, 4, S], BF16)
                kT = sb(pm, "kT", [128, 4, S], BF16)
                V2 = sb(pm, "V2", [128, 31, 4, 192], BF16)
                pT = sb(pm, "pT", [128, 4, S], BF16)
                pw_sb = sb(pm, "pw_sb", [128, 4, 128], BF16)
                HT = [Buf(f"H{i}") for i in range(8)]
                QB = [[Buf(f"q{m}_{t}") for t in range(4)] for m in range(4)]
                KB = [[Buf(f"k{m}_{t}") for t in range(4)] for m in range(4)]
                VB = [Buf(f"v{i}") for i in range(31)]
                VONES = Buf("vones")
                PB = [[Buf(f"p{g}_{t}") for t in range(4)] for g in range(4)]
                PW = Buf("pw")
                with ExitStack() as pb:
                    w_in_sb = sb(pb, "w_in_sb", [128, 8, 2048], BF16)
                    U2 = sb(pb, "U2", [128, 16, 512], BF16)
                    pooledT = sb(pb, "pooledT", [128, 2, 512], BF16)
                    sq = sb(pb, "sq", [128, 2, 8, 128], BF16)
                    xt = sb(pb, "xt", [128, 2, 8, 128], F32)
                    rstd = sb(pb, "rstd", [128, 1, 256], F32)
                    WI = [Buf(f"wi{c}") for c in range(4)]
                    UB = [Buf(f"u{j}") for j in range(16)]
                    PLB = [Buf("pl0"), Buf("pl1")]
                    SQ = Buf("sq")
                    XT = [Buf("xt0"), Buf("xt1")]
                    RS = [Buf("rs0"), Buf("rs1")]
                    for c in range(4):
                        T.dma("sp", w_in_sb[:, :, c * 512:(c + 1) * 512],
                              w_in_b[l][:, c * 512:(c + 1) * 512].rearrange("(k p) f -> p k f", p=128),
                              reads=[WB[("in", l)]], writes=[WI[c]])
                    T.dma("sp", pw_sb[:], pool_w_b[l].rearrange("g c d -> c g d"), reads=[WB[("pw", l)]], writes=[PW])
                    T.emit("dve", lambda e: e.memset(V2[:, :, :, 64:128], 1.0), writes=[VONES])
                    def emit_norm(i):
                        b = 0
                        c0 = i * 256
                        tl = (tok0 + c0) // 128
                        T.dma("sp", xt[:], xT_d[tl:tl + 2].rearrange("j p k t -> p j k t"),
                              reads=[XD[s][i // 2]], writes=[XT[b]])
                        T.emit("act", lambda e: e.activation(out=sq[:], in_=xt[:], func=AF.Square),
                               reads=[XT[b]], writes=[SQ])
                        bk = 6 + b
                        for j in range(2):
                            for k in range(8):
                                mm(banks[bk][:, j * 128:(j + 1) * 128], onesb[:], sq[:, j, k, :], k == 0, k == 7, [SQ, CONST], [BK[bk]],
                                   (k == 7 and j == 1))
                        rstd_from_ss(rstd[:, b, :], banks[bk][:, 0:256], [BK[bk]], [RS[b]])
                        for k in range(8):
                            T.emit("dve", lambda e, b=b, k=k, c0=c0: e.scalar_tensor_tensor(
                                out=H[:, k, c0:c0 + 256].rearrange("p (j t) -> p j t", t=128), in0=xt[:, :, k, :],
                                scalar=gm[:, l * 8 + k:l * 8 + k + 1],
                                in1=rstd[:, b, :].rearrange("p (j t) -> p j t", t=128), op0=ALU.mult, op1=ALU.mult),
                                reads=[XT[b], RS[b], CONST], writes=[HT[i]])
                    bi_ = [0]

                    def emit_qkproj(t):
                        bi = bi_[0]
                        for m in range(8):
                            bk = bi % 4
                            bi += 1
                            for k in range(8):
                                mm(banks[bk][:, :], w_in_sb[:, k, m * 128:(m + 1) * 128], H[:, k, t * 512:(t + 1) * 512],
                                   k == 0, k == 7, [WI[m // 4], HT[2 * t], HT[2 * t + 1]], [BK[bk]], k == 7)
                            if m < 4:
                                evac_copy(qT[:, m, t * 512:(t + 1) * 512], banks[bk][:, :], [BK[bk]], [QB[m][t]], scale=0.125)
                            else:
                                T.emit("dve", lambda e, bk=bk, m=m, t=t: e.tensor_copy(
                                    out=kT[:, m - 4, t * 512:(t + 1) * 512], in_=banks[bk][:, :]),
                                    reads=[BK[bk]], writes=[KB[m - 4][t]])
                        bi_[0] = bi

                    def emit_v(sidx):
                        bk = bi_[0] % 4
                        bi_[0] += 1
                        a0 = 64 * sidx
                        hts = sorted({a0 // 256, (a0 + 127) // 256})
                        for k in range(8):
                            mm(banks[bk][:, :], H[:, k, a0:a0 + 128], w_in_sb[:, k, 1024:1536], k == 0, k == 7,
                               [WI[2]] + [HT[i] for i in hts], [BK[bk]], k == 7)
                        evac_copy(V2[:, sidx, :, :].rearrange("p a (b d) -> p a b d", d=64)[:, :, 0:3:2, :],
                                  banks[bk][:, :].rearrange("p (a b d) -> p a b d", b=2, d=64), [BK[bk]], [VB[sidx]])

                    def emit_u(j):
                        bk = bi_[0] % 4
                        bi_[0] += 1
                        for k in range(8):
                            mm(banks[bk][:, :], H[:, k, j * 128:(j + 1) * 128], w_in_sb[:, k, 1536:2048], k == 0, k == 7,
                               [WI[3], HT[j // 2]], [BK[bk]], k == 7)
                        evac_copy(U2[:, j, :], banks[bk][:, :], [BK[bk]], [UB[j]])

                    def emit_proj(t):
                        emit_qkproj(t)
                        for sidx in range(max(0, 8 * t - 1), min(31, 8 * t + 7)):
                            emit_v(sidx)
                        for j in range(4 * t, 4 * t + 4):
                            emit_u(j)

                    for t in range(4):
                        emit_norm(2 * t)
                        emit_norm(2 * t + 1)
                        if t > 0:
                            emit_proj(t - 1)
                    emit_proj(3)
                    bi = bi_[0]
                    for t in range(4):
                        for g in range(4):
                            pbuf = g % 2
                            bk = bi % 4
                            bi += 1
                            for Tq in range(4):
                                Tt = 4 * t + Tq
                                terms = []
                                if Tt > 0:
                                    terms.append((Tt - 1, 0))
                                if Tt == 0:
                                    terms += [(Tt, 3), (Tt, 4)]
                                elif Tt == 15:
                                    terms += [(Tt, 5), (Tt, 6)]
                                else:
                                    terms.append((Tt, 1))
                                if Tt < 15:
                                    terms.append((Tt + 1, 2))
                                for ti, (tp, var) in enumerate(terms):
                                    mm(banks[bk][:, Tq * 128:(Tq + 1) * 128], U2[:, tp, g * 128:(g + 1) * 128],
                                       A_bf[:, g * 7 + var, :], ti == 0, ti == len(terms) - 1,
                                       [UB[tp], CONST], [BK[bk]], (Tq == 3 and ti == len(terms) - 1))
                            T.emit("act", lambda e, bk=bk, pbuf=pbuf, g=g: e.activation(
                                out=pooledT[:, pbuf, :], in_=banks[bk][:, :], func=AF.Copy),
                                reads=[BK[bk]], writes=[PLB[pbuf]])
                            bk2 = 4 + (g % 2)
                            mm(banks[bk2][:, :], pw_sb[:, g, :], pooledT[:, pbuf, :], True, True, [PW, PLB[pbuf]], [BK[bk2]], True)
                            T.emit("dve", lambda e, bk2=bk2, g=g, t=t: e.tensor_scalar(
                                out=pT[:, g, t * 512:(t + 1) * 512], in0=banks[bk2][:, :],
                                scalar1=psc[:, l * 4 + g:l * 4 + g + 1], scalar2=None, op0=ALU.mult),
                                reads=[BK[bk2], CONST], writes=[PB[g][t]])
                    T.barrier()
                with ExitStack() as pc:
                    bias = sb(pc, "bias", [128, 4, 2, 14, 64], F32)
                    Sb = sb(pc, "Sb", [128, 4, 2, 4, 64], F32)
                    Pm = sb(pc, "Pm", [128, 4, 2, 4, 64], BF16)
                    Rr = sb(pc, "Rr", [128, 2, 4, 64], F32)
                    w_out_sb = sb(pc, "w_out_sb", [128, 8, D], BF16)
                    xt2 = sb(pc, "xt2", [128, 2, 8, 128], F32)
                    BIAS = [Buf(f"bias{hp}") for hp in range(4)]
                    SBB = [Buf(f"sb{i}") for i in range(4)]
                    PMB = [Buf(f"pm{i}") for i in range(4)]
                    RRB = [Buf("rr0"), Buf("rr1")]
                    WO = [Buf("wo0"), Buf("wo1")]
                    XT2 = [Buf("xt20"), Buf("xt21")]
                    for hp in range(4):
                        T.dma("sp", bias[:, hp].rearrange("p a b c -> p (a b c)"), bias_d[l, hp], writes=[BIAS[hp]])
                        T.emit("act", lambda e, hp=hp: e.activation(out=bias[:, hp].rearrange("p a b c -> p (a b c)"),
                                                                    in_=bias[:, hp].rearrange("p a b c -> p (a b c)"), func=AF.Exp),
                               reads=[BIAS[hp]], writes=[BIAS[hp]])
                    for c in range(2):
                        T.dma("sp", w_out_sb[:, :, c * 512:(c + 1) * 512],
                              w_out_b[l][:, c * 512:(c + 1) * 512].rearrange("(k p) f -> p k f", p=128),
                              reads=[WB[("out", l)]], writes=[WO[c]])
                    units = [(r, hp) for r in range(32) for hp in range(4)]

                    def rstart(r):
                        return min(max(r - 4, 0), 24)

                    def emit_qk(ui):
                        r, hp = units[ui]
                        u4 = ui % 4
                        rs_ = rstart(r)
                        i0 = rs_ - r + 7
                        kts = sorted({(rs_ * 64) // 512, (rs_ * 64 + 511) // 512})
                        for half in (0, 1):
                            bk = (u4 // 2) * 2 + half
                            cb = (u4 % 2) * 256
                            p0 = 64 * half
                            for c in range(4):
                                ks = (rs_ + 2 * c) * 64
                                mm(banks[bk][:, cb + c * 64:cb + (c + 1) * 64], kT[p0:p0 + 64, hp, ks:ks + 128],
                                   qT[p0:p0 + 64, hp, r * 64:(r + 1) * 64], True, True,
                                   [KB[hp][t] for t in kts] + [QB[hp][r // 8]], [BK[bk]], c == 3)
                            T.emit("act", lambda e, bk=bk, cb=cb, u4=u4, half=half: e.activation(
                                out=Sb[:, u4, half], in_=banks[bk][:, cb:cb + 256].rearrange("p (c q) -> p c q", q=64),
                                func=AF.Exp), reads=[BK[bk]], writes=[SBB[u4]])
                        T.emit("pool" if (l >= POOL_FROM_LAYER and ui % 2 == 1) else "dve", lambda e, u4=u4, hp=hp, i0=i0: e.tensor_tensor(
                            out=Pm[:, u4], in0=Sb[:, u4], in1=bias[:, hp, :, i0:i0 + 7:2, :], op=ALU.mult),
                            reads=[SBB[u4], BIAS[hp]], writes=[PMB[u4]])

                    def emit_pv(ui):
                        r, hp = units[ui]
                        u2 = ui % 4
                        rs_ = rstart(r)
                        ob = 4 + (r % 2)
                        for half in (0, 1):
                            h = 2 * hp + half
                            for c in range(4):
                                sidx = rs_ + 2 * c
                                lhsT = V2[:, sidx, hp, 64 * half:64 * half + 128]
                                col = (hp * 2 + half) * 64
                                mm(banks[ob][:, col:col + 64], lhsT, Pm[:, u2, half, c, :], c == 0, c == 3,
                                   [VB[sidx], VONES, PMB[u2]], [BK[ob]], c == 3)
                        if hp == 3:
                            rb = r % 2
                            Ov = banks[ob][:, :].rearrange("p (a b q) -> p a b q", b=2, q=64)
                            T.emit("dve", lambda e, rb=rb, ob=ob: e.reciprocal(
                                out=Rr[0:64, rb], in_=banks[ob][64:128, :].rearrange("p (a b q) -> p a b q", b=2, q=64)[:, :, 0, :]),
                                reads=[BK[ob]], writes=[RRB[rb]])
                            T.emit("dve", lambda e, rb=rb, ob=ob: e.reciprocal(
                                out=Rr[64:128, rb], in_=banks[ob][0:64, :].rearrange("p (a b q) -> p a b q", b=2, q=64)[:, :, 1, :]),
                                reads=[BK[ob]], writes=[RRB[rb]])
                            T.emit("dve", lambda e, rb=rb, ob=ob, r=r: e.tensor_tensor(
                                out=H[0:64, 0:4, r * 64:(r + 1) * 64],
                                in0=banks[ob][0:64, :].rearrange("p (a b q) -> p a b q", b=2, q=64)[:, :, 0, :],
                                in1=Rr[0:64, rb], op=ALU.mult), reads=[BK[ob], RRB[rb]], writes=[HT[r // 4]])
                            T.emit("dve", lambda e, rb=rb, ob=ob, r=r: e.tensor_tensor(
                                out=H[64:128, 0:4, r * 64:(r + 1) * 64],
                                in0=banks[ob][64:128, :].rearrange("p (a b q) -> p a b q", b=2, q=64)[:, :, 1, :],
                                in1=Rr[64:128, rb], op=ALU.mult), reads=[BK[ob], RRB[rb]], writes=[HT[r // 4]])

                    def emit_outproj(i):
                        b = i % 2
                        c0 = i * 128
                        T.dma("sp", xt2[:, b], xT_d[(tok0 + c0) // 128],
                              reads=[XD[s][i // 4]], writes=[XT2[b]])
                        for m in range(8):
                            bk = 6 + (m % 2)
                            for k in range(8):
                                rhs = H[:, k, c0:c0 + 128] if k < 4 else pT[:, k - 4, c0:c0 + 128]
                                rd = [HT[i // 2]] if k < 4 else [PB[k - 4][i // 4]]
                                mm(banks[bk][:, 0:128], w_out_sb[:, k, m * 128:(m + 1) * 128], rhs, k == 0, k == 7,
                                   [WO[m // 4]] + rd, [BK[bk]], k == 7)
                            T.emit("dve", lambda e, b=b, m=m, bk=bk: e.tensor_tensor(
                                out=xt2[:, b, m, :], in0=banks[bk][:, 0:128], in1=xt2[:, b, m, :], op=ALU.add),
                                reads=[BK[bk], XT2[b]], writes=[XT2[b]])
                        T.dma("sp", xT_d[(tok0 + c0) // 128], xt2[:, b],
                              reads=[XT2[b]], writes=[XD[s][i // 4]])

                    def pv_and_out(ui):
                        emit_pv(ui)
                        r, hp = units[ui]
                        if INTERLEAVE_OUT and hp == 3 and r % 2 == 1:
                            emit_outproj(r // 2)

                    for ui in range(len(units)):
                        emit_qk(ui)
                        if ui > 1:
                            pv_and_out(ui - 2)
                    pv_and_out(len(units) - 2)
                    pv_and_out(len(units) - 1)
                    if not INTERLEAVE_OUT:
                        for i in range(16):
                            emit_outproj(i)

                    T.barrier()

        def final_out(xT, XB, s):
            tok0 = s * S
            if True:
                if True:
                    with ExitStack() as po:
                        gF = sb(po, "gF", [128, D], F32)
                        ost = sb(po, "ost", [128, 1, D], F32)
                        junk = sb(po, "junk", [128, 512], BF16)
                        ssA = sb(po, "ssA", [128, 2], F32)
                        rsF = sb(po, "rsF", [128, 1], F32)
                        GF, OST, JK, SSA, RSF = Buf("gF"), [Buf("ost0"), Buf("ost1")], Buf("junk"), Buf("ssA"), Buf("rsF")
                        T.dma("sp", gF[:], gF_d, writes=[GF])
                        for j in range(16):
                            ob = 0
                            t = j // 4
                            for hb in range(2):
                                bk = 2 * (j % 2) + hb
                                for kk in range(4):
                                    k = hb * 4 + kk
                                    T.emit("pe", lambda e, bk=bk, kk=kk, k=k, j=j: e.transpose(
                                        out=banks[bk][:, kk * 128:(kk + 1) * 128], in_=xT[:, j, k, :],
                                        identity=identf[:]), reads=[XB[k][t], CONST], writes=[BK[bk]], sig=(kk == 3))
                                T.emit("act", lambda e, bk=bk, hb=hb: e.activation(
                                    out=junk[:], in_=banks[bk][:, :], func=AF.Square, accum_out=ssA[:, hb:hb + 1]),
                                    reads=[BK[bk]], writes=[JK, SSA])
                            T.emit("dve", lambda e: e.tensor_tensor(out=rsF[:], in0=ssA[:, 0:1], in1=ssA[:, 1:2], op=ALU.add),
                                   reads=[SSA], writes=[RSF])
                            rstd_from_ss(rsF[:], rsF[:], [RSF], [RSF])
                            for hb in range(2):
                                bk = 2 * (j % 2) + hb
                                T.emit("dve", lambda e, bk=bk, hb=hb, ob=ob: e.scalar_tensor_tensor(
                                    out=ost[:, ob, hb * 512:(hb + 1) * 512], in0=banks[bk][:, :], scalar=rsF[:, 0:1],
                                    in1=gF[:, hb * 512:(hb + 1) * 512], op0=ALU.mult, op1=ALU.mult),
                                    reads=[BK[bk], RSF, GF], writes=[OST[ob]])
                            T.dma("sp", out_c[tok0 + j * 128:tok0 + (j + 1) * 128, :], ost[:, ob, :], reads=[OST[ob]],
                                  writes=[XD[s][t]])

        def f_phase_sparse(l, s, last):
            U32 = mybir.dt.uint32
            tok0 = s * S
            with ExitStack() as pf:
                xT = sb(pf, "xT", [128, 16, 8, 128], F32)
                ring = sb(pf, "ring", [128, 6, 4096], BF16)
                wr_sb = sb(pf, "wr_sb", [128, 8, 20], F32)
                wrp = sb(pf, "wrp", [128, 8, 20], F32)
                rt = sb(pf, "rt", [128, 16], F32)
                LGs = sb(pf, "LGs", [128, 16, 20], F32)
                r1 = sb(pf, "r1", [128, 16, 16], F32)
                gmax = sb(pf, "gmax", [128, 16], F32)
                goh = sb(pf, "goh", [128, 16, 4], F32)
                gex = sb(pf, "gex", [128, 16, 4], F32)
                gw = sb(pf, "gw", [128, 16], F32)
                esel = sb(pf, "esel", [128, 16, 4], F32)
                em = sb(pf, "em", [128, 16, 4], F32)
                m1 = sb(pf, "m1", [128, 16], F32)
                m2 = sb(pf, "m2", [128, 16], F32)
                oh1 = sb(pf, "oh1", [128, 16, 4], F32)
                oh2 = sb(pf, "oh2", [128, 16, 4], F32)
                w1 = sb(pf, "w1", [128, 16], F32)
                w2 = sb(pf, "w2", [128, 16], F32)
                M1 = sb(pf, "M1", [128, 16, 16], F32)
                M2 = sb(pf, "M2", [128, 16, 16], F32)
                Mb = sb(pf, "Mb", [128, 256], BF16)
                TOT = sb(pf, "TOT", [128, 16, 16], F32)
                JP = sb(pf, "JP", [128, 16, 16], F32)
                SL = sb(pf, "SL", [128, 16, 16], F32)
                ne = sb(pf, "ne", [128, 16], F32)
                ntl = sb(pf, "ntl", [128, 16], F32)
                base = sb(pf, "base", [128, 16], F32)
                bend = sb(pf, "bend", [128, 16], F32)
                b256 = sb(pf, "b256", [128, 16], F32)
                sl1 = sb(pf, "sl1", [128, 16], F32)
                sl2 = sb(pf, "sl2", [128, 16], F32)
                s1u = sb(pf, "s1u", [128, 16], U32)
                s2u = sb(pf, "s2u", [128, 16], U32)
                eidx = sb(pf, "eidx", [128, NTILE], F32)
                widf = sb(pf, "widf", [128, NTILE], F32)
                widx = sb(pf, "widx", [128, NTILE], U32)
                XB = [[Buf(f"X{k}_{t}") for t in range(4)] for k in range(8)]
                RSLOT = [Buf(f"ring{i}") for i in range(6)]
                WR, WRP = Buf("wr"), Buf("wrp")
                ROUT = Buf("router")
                HSB = [Buf(f"hs{i}") for i in range(32)]
                YSB = [Buf(f"ys{i}") for i in range(NTILE)]

                def load_tile_w(i, which=(0, 1, 2)):
                    for j, wp in enumerate((w_gate_p, w_up_p, w_down_p)):
                        if j not in which:
                            continue
                        slot = (3 * i + j) % 6
                        T.idma(ring[:, slot, :], None, wp[l], widx[:, i:i + 1],
                               reads=[ROUT] + [WB[(("g", "u", "d")[j], l, e_)] for e_ in range(NE)], writes=[RSLOT[slot]])

                for t in range(4):
                    tl = tok0 // 128 + 4 * t
                    T.dma("sp", xT[:, 4 * t:4 * t + 4], xT_d[tl:tl + 4].rearrange("j p k t -> p j k t"), reads=[XD[s][t]],
                          writes=[XB[k][t] for k in range(8)])
                T.dma("sp", wr_sb[:], wr_d[:, l], writes=[WR])
                for k in range(8):
                    T.emit("dve", lambda e, k=k: e.tensor_scalar(out=wrp[:, k, :], in0=wr_sb[:, k, :],
                                                                 scalar1=gf[:, l * 8 + k:l * 8 + k + 1], scalar2=None,
                                                                 op0=ALU.mult), reads=[WR, CONST], writes=[WRP])

                def dv(fn, extra_reads=()):
                    T.emit("dve", fn, reads=[ROUT] + list(extra_reads), writes=[ROUT])

                def b3(ap2, n):
                    return ap2.unsqueeze(2).broadcast_to([128, 16, n])

                with ExitStack() as pa:
                    H = sb(pa, "Hf", [128, 8, S], BF16)
                    sq = sb(pa, "sqf", [128, 4, 8, 128], BF16)
                    rstd = sb(pa, "rstdf", [128, 512], F32)
                    hrow = sb(pa, "hrow", [128, 2, D], BF16)
                    HB = [Buf(f"Hf{t}") for t in range(4)]
                    SQ, RS = Buf("sq"), Buf("rs")
                    HROW = [Buf("hrow0"), Buf("hrow1")]
                    for t in range(4):
                        c0 = t * 512
                        T.emit("act", lambda e, t=t: e.activation(out=sq[:], in_=xT[:, 4 * t:4 * t + 4], func=AF.Square),
                               reads=[XB[k][t] for k in range(8)], writes=[SQ])
                        for j in range(4):
                            for k in range(8):
                                mm(banks[6][:, j * 128:(j + 1) * 128], onesb[:], sq[:, j, k, :], k == 0, k == 7, [SQ, CONST], [BK[6]],
                                   (k == 7 and j == 3))
                        rstd_from_ss(rstd[:], banks[6][:, :], [BK[6]], [RS])
                        for k in range(8):
                            T.emit("dve", lambda e, k=k, c0=c0, t=t: e.scalar_tensor_tensor(
                                out=H[:, k, c0:c0 + 512].rearrange("p (j t) -> p j t", t=128), in0=xT[:, 4 * t:4 * t + 4, k, :],
                                scalar=gf[:, l * 8 + k:l * 8 + k + 1],
                                in1=rstd[:].rearrange("p (j t) -> p j t", t=128), op0=ALU.mult, op1=ALU.mult),
                                reads=[XB[k][t], RS, CONST], writes=[HB[t]])
                        for j in range(4):
                            jj = 4 * t + j
                            for k in range(8):
                                mm(banks[7][:, 320 + jj:320 + jj + 1], sq[:, j, k, :], onesb[:, 0:1],
                                   k == 0, k == 7, [SQ, CONST], [BK[7]], False)
                            for k in range(8):
                                mm(banks[7][:, jj * 20:(jj + 1) * 20], xT[:, jj, k, :], wrp[:, k, :],
                                   k == 0, k == 7, [XB[k][t], WRP], [BK[7]], (k == 7))
                    rstd_from_ss(rt[:], banks[7][:, 320:336], [BK[7], ROUT], [ROUT])
                    dv(lambda e: e.tensor_tensor(out=LGs[:], in0=banks[7][:, 0:320].rearrange("p (j e) -> p j e", e=20),
                                                 in1=b3(rt[:], 20), op=ALU.mult), [BK[7]])
                    dv(lambda e: e.tensor_reduce(out=gmax[:], in_=LGs[:, :, 0:4], axis=AX.X, op=ALU.max))
                    dv(lambda e: e.tensor_tensor(out=goh[:], in0=LGs[:, :, 0:4], in1=b3(gmax[:], 4), op=ALU.is_equal))
                    dv(lambda e: e.tensor_tensor(out=gex[:], in0=LGs[:, :, 0:4], in1=b3(gmax[:], 4), op=ALU.subtract))
                    T.emit("act", lambda e: e.activation(out=gex[:], in_=gex[:], func=AF.Exp), reads=[ROUT], writes=[ROUT])
                    dv(lambda e: e.tensor_reduce(out=gw[:], in_=gex[:], axis=AX.X, op=ALU.add))
                    dv(lambda e: e.reciprocal(out=gw[:], in_=gw[:]))
                    dv(lambda e: e.tensor_tensor(out=r1[:].rearrange("p j (g i) -> p j g i", i=4),
                                                 in0=LGs[:, :, 4:20].rearrange("p j (g i) -> p j g i", i=4),
                                                 in1=goh[:].unsqueeze(3).broadcast_to([128, 16, 4, 4]), op=ALU.mult))
                    dv(lambda e: e.tensor_reduce(out=esel[:], in_=r1[:].rearrange("p j (g i) -> p j i g", i=4),
                                                 axis=AX.X, op=ALU.add))
                    dv(lambda e: e.tensor_reduce(out=m1[:], in_=esel[:], axis=AX.X, op=ALU.max))
                    dv(lambda e: e.tensor_tensor(out=oh1[:], in0=esel[:], in1=b3(m1[:], 4), op=ALU.is_equal))
                    dv(lambda e: e.scalar_tensor_tensor(out=em[:], in0=oh1[:], scalar=-1.0e30, in1=esel[:],
                                                        op0=ALU.mult, op1=ALU.add))
                    dv(lambda e: e.tensor_reduce(out=m2[:], in_=em[:], axis=AX.X, op=ALU.max))
                    dv(lambda e: e.tensor_tensor(out=oh2[:], in0=em[:], in1=b3(m2[:], 4), op=ALU.is_equal))
                    dv(lambda e: e.tensor_tensor(out=w2[:], in0=m2[:], in1=m1[:], op=ALU.subtract))
                    T.emit("act", lambda e: e.activation(out=w2[:], in_=w2[:], func=AF.Exp), reads=[ROUT], writes=[ROUT])
                    dv(lambda e: e.tensor_scalar(out=w2[:], in0=w2[:], scalar1=1.0, scalar2=None, op0=ALU.add))
                    dv(lambda e: e.reciprocal(out=w1[:], in_=w2[:]))
                    dv(lambda e: e.tensor_tensor(out=w1[:], in0=w1[:], in1=gw[:], op=ALU.mult))
                    dv(lambda e: e.tensor_tensor(out=w2[:], in0=gw[:], in1=w1[:], op=ALU.subtract))
                    g44 = lambda ap: ap.rearrange("p j (g i) -> p j g i", i=4)
                    dv(lambda e: e.tensor_tensor(out=g44(M1[:]), in0=goh[:].unsqueeze(3).broadcast_to([128, 16, 4, 4]),
                                                 in1=oh1[:].unsqueeze(2).broadcast_to([128, 16, 4, 4]), op=ALU.mult))
                    dv(lambda e: e.tensor_tensor(out=g44(M2[:]), in0=goh[:].unsqueeze(3).broadcast_to([128, 16, 4, 4]),
                                                 in1=oh2[:].unsqueeze(2).broadcast_to([128, 16, 4, 4]), op=ALU.mult))
                    dv(lambda e: e.tensor_tensor(out=Mb[:].rearrange("p (j e) -> p j e", e=16), in0=M1[:], in1=M2[:], op=ALU.add))
                    mm(banks[6][:, 0:256], ustr[:], Mb[:], True, True, [ROUT, CONST], [BK[6]], True)
                    mm(banks[7][:, 0:256], onesb[:], Mb[:], True, True, [ROUT, CONST], [BK[7]], True)
                    dv(lambda e: e.tensor_copy(out=TOT[:].rearrange("p j e -> p (j e)"), in_=banks[7][:, 0:256]), [BK[7]])
                    dv(lambda e: e.memset(JP[:, 0, :], 0.0))
                    for j in range(1, 16):
                        dv(lambda e, j=j: e.tensor_tensor(out=JP[:, j, :], in0=JP[:, j - 1, :], in1=TOT[:, j - 1, :], op=ALU.add))
                    dv(lambda e: e.tensor_tensor(out=ne[:], in0=JP[:, 15, :], in1=TOT[:, 15, :], op=ALU.add))
                    dv(lambda e: e.tensor_scalar(out=ntl[:], in0=ne[:], scalar1=0.0, scalar2=None, op0=ALU.is_gt))
                    for q in range(1, S // TS):
                        dv(lambda e, q=q: e.scalar_tensor_tensor(out=ntl[:], in0=ne[:], scalar=float(TS * q), in1=ntl[:],
                                                                 op0=ALU.is_gt, op1=ALU.add))
                    dv(lambda e: e.memset(base[:, 0:1], 0.0))
                    for e_ in range(1, 16):
                        dv(lambda e, e_=e_: e.tensor_tensor(out=base[:, e_:e_ + 1], in0=base[:, e_ - 1:e_],
                                                            in1=ntl[:, e_ - 1:e_], op=ALU.add))
                    dv(lambda e: e.tensor_tensor(out=bend[:], in0=base[:], in1=ntl[:], op=ALU.add))
                    dv(lambda e: e.tensor_scalar(out=b256[:], in0=base[:], scalar1=float(TS), scalar2=None, op0=ALU.mult))
                    dv(lambda e: e.tensor_tensor(out=SL[:], in0=banks[6][:, 0:256].rearrange("p (j e) -> p j e", e=16),
                                                 in1=JP[:], op=ALU.add), [BK[6]])
                    dv(lambda e: e.tensor_tensor(out=SL[:], in0=SL[:], in1=b256[:].unsqueeze(1).broadcast_to([128, 16, 16]),
                                                 op=ALU.add))
                    dv(lambda e: e.tensor_tensor(out=M1[:], in0=M1[:], in1=SL[:], op=ALU.mult))
                    dv(lambda e: e.tensor_tensor(out=M2[:], in0=M2[:], in1=SL[:], op=ALU.mult))
                    dv(lambda e: e.tensor_reduce(out=sl1[:], in_=M1[:], axis=AX.X, op=ALU.add))
                    dv(lambda e: e.tensor_reduce(out=sl2[:], in_=M2[:], axis=AX.X, op=ALU.add))
                    dv(lambda e: e.tensor_copy(out=s1u[:], in_=sl1[:]))
                    dv(lambda e: e.tensor_copy(out=s2u[:], in_=sl2[:]))
                    dv(lambda e: e.memset(eidx[:], 0.0))
                    for e_ in range(16):
                        dv(lambda e, e_=e_: e.scalar_tensor_tensor(out=eidx[:], in0=iotas[:, 1:1 + NTILE], scalar=bend[:, e_:e_ + 1],
                                                                   in1=eidx[:], op0=ALU.is_ge, op1=ALU.add), [CONST])
                    dv(lambda e: e.tensor_scalar(out=eidx[:], in0=eidx[:], scalar1=15.0, scalar2=None, op0=ALU.min))
                    dv(lambda e: e.tensor_scalar(out=widf[:], in0=eidx[:], scalar1=128.0, scalar2=iotas[:, 0:1],
                                                 op0=ALU.mult, op1=ALU.add), [CONST])
                    dv(lambda e: e.tensor_copy(out=widx[:], in_=widf[:]))
                    load_tile_w(0)
                    load_tile_w(1, which=(0, 1))
                    for j in range(16):
                        hb = j % 2
                        bk = 4 + hb
                        pv = banks[bk][:, :].bitcast(BF16)
                        for k in range(8):
                            T.emit("pe", lambda e, pv=pv, k=k, j=j: e.transpose(
                                out=pv[:, k * 128:(k + 1) * 128], in_=H[:, k, j * 128:(j + 1) * 128], identity=identb[:]),
                                reads=[HB[j // 4], CONST], writes=[BK[bk]], sig=(k == 7))
                        evac_copy(hrow[:, hb, :], pv, [BK[bk]], [HROW[hb]])
                        T.idma(HS_d, s1u[:, j:j + 1], hrow[:, hb, :], None, reads=[HROW[hb], ROUT], writes=[HSB[2 * j]])
                        T.idma(HS_d, s2u[:, j:j + 1], hrow[:, hb, :], None, reads=[HROW[hb], ROUT], writes=[HSB[2 * j + 1]])
                    T.barrier()

                with ExitStack() as pbx:
                    hs_sb = sb(pbx, "hs_sb", [128, 3, 2, D], BF16)
                    hcT = sb(pbx, "hcT", [128, 2, 8, TS], BF16)
                    a_e = sb(pbx, "a_e", [128, 2, 4, TS], BF16)
                    sg = sb(pbx, "sg", [128, 2, TS], F32)
                    ys = sb(pbx, "ys", [128, 2, 2, D], F32)
                    HSS = [Buf("hss0"), Buf("hss1"), Buf("hss2")]
                    HCT = [Buf("hct0"), Buf("hct1")]
                    AE = [[Buf(f"ae{b}_{f}") for f in range(4)] for b in range(2)]
                    SG = [Buf("sg0"), Buf("sg1")]
                    YS = [Buf("ysb0"), Buf("ysb1")]

                    def emit_hs(i):
                        hb3 = i % 3
                        T.dma("sp", hs_sb[:, hb3], HS_d[i * TS:(i + 1) * TS, :].rearrange("(a p) d -> p a d", p=128),
                              reads=HSB, writes=[HSS[hb3]])

                    def emit_tr(i):
                        ab = i % 2
                        hb3 = i % 3
                        for hb in range(2):
                            bk = 6 + hb
                            pv = banks[bk][:, :].bitcast(BF16)
                            for kk in range(4):
                                k = hb * 4 + kk
                                for a in range(2):
                                    T.emit("pe", lambda e, pv=pv, kk=kk, a=a, k=k, hb3=hb3: e.transpose(
                                        out=pv[:, kk * TS + a * 128:kk * TS + (a + 1) * 128],
                                        in_=hs_sb[:, hb3, a, k * 128:(k + 1) * 128], identity=identb[:]),
                                        reads=[HSS[hb3], CONST], writes=[BK[bk]], sig=(kk == 3 and a == 1))
                            evac_copy(hcT[:, ab, hb * 4:(hb + 1) * 4, :].rearrange("p k t -> p (k t)"), pv, [BK[bk]], [HCT[ab]])

                    def emit_gu(i):
                        ab = i % 2
                        sg_, su_ = (3 * i) % 6, (3 * i + 1) % 6
                        for f in range(4):
                            bg, bu = f % 2, 2 + f % 2
                            for k in range(8):
                                mm(banks[bg][:, 0:TS], ring[:, sg_, k * 512 + f * 128:k * 512 + (f + 1) * 128], hcT[:, ab, k, :],
                                   k == 0, k == 7, [RSLOT[sg_], HCT[ab]], [BK[bg]], k == 7)
                            for k in range(8):
                                mm(banks[bu][:, 0:TS], ring[:, su_, k * 512 + f * 128:k * 512 + (f + 1) * 128], hcT[:, ab, k, :],
                                   k == 0, k == 7, [RSLOT[su_], HCT[ab]], [BK[bu]], k == 7)
                            i2 = f % 2
                            T.emit("act", lambda e, bg=bg, i2=i2: e.activation(out=sg[:, i2, :], in_=banks[bg][:, 0:TS], func=AF.Silu),
                                   reads=[BK[bg]], writes=[SG[i2]])
                            T.emit("dve", lambda e, bu=bu, i2=i2, ab=ab, f=f: e.tensor_tensor(
                                out=a_e[:, ab, f, :], in0=banks[bu][:, 0:TS], in1=sg[:, i2, :], op=ALU.mult),
                                reads=[BK[bu], SG[i2]], writes=[AE[ab][f]])

                    def emit_d(i):
                        ab = i % 2
                        sd_ = (3 * i + 2) % 6
                        for a in range(2):
                            for dh in range(2):
                                bk = 4 + dh
                                for f in range(4):
                                    mm(banks[bk][:, :], a_e[:, ab, f, a * 128:(a + 1) * 128],
                                       ring[:, sd_, f * 1024 + dh * 512:f * 1024 + (dh + 1) * 512],
                                       f == 0, f == 3, [RSLOT[sd_], AE[ab][f]], [BK[bk]], f == 3)
                                evac_copy(ys[:, ab, a, dh * 512:(dh + 1) * 512], banks[bk][:, :], [BK[bk]], [YS[ab]])
                        T.dma("sp", YS_d[i * TS:(i + 1) * TS, :].rearrange("(a p) d -> p a d", p=128), ys[:, ab],
                              reads=[YS[ab]], writes=[YSB[i]])

                    emit_hs(0)
                    emit_hs(1)
                    emit_tr(0)
                    for i in range(NTILE):
                        if i + 2 < NTILE:
                            emit_hs(i + 2)
                        if i + 1 < NTILE:
                            emit_tr(i + 1)
                        emit_gu(i)
                        if i + 2 < NTILE:
                            load_tile_w(i + 2, which=(0, 1))
                        if i > 0:
                            emit_d(i - 1)
                        if i + 1 < NTILE:
                            load_tile_w(i + 1, which=(2,))
                    emit_d(NTILE - 1)
                    T.barrier()

                with ExitStack() as pcx:
                    g0 = sb(pcx, "g0", [128, 4, D], F32)
                    g1 = sb(pcx, "g1", [128, 4, D], F32)
                    G0 = [Buf(f"g0{i}") for i in range(4)]
                    G1 = [Buf(f"g1{i}") for i in range(4)]
                    for j in range(16):
                        b = j % 4
                        T.idma(g0[:, b, :], None, YS_d, s1u[:, j:j + 1], reads=YSB + [ROUT], writes=[G0[b]])
                        T.idma(g1[:, b, :], None, YS_d, s2u[:, j:j + 1], reads=YSB + [ROUT], writes=[G1[b]])
                        T.emit("dve", lambda e, b=b, j=j: e.tensor_scalar(out=g0[:, b, :], in0=g0[:, b, :], scalar1=w1[:, j:j + 1],
                                                                          scalar2=None, op0=ALU.mult), reads=[G0[b], ROUT], writes=[G0[b]])
                        T.emit("dve", lambda e, b=b, j=j: e.scalar_tensor_tensor(out=g0[:, b, :], in0=g1[:, b, :], scalar=w2[:, j:j + 1],
                                                                                 in1=g0[:, b, :], op0=ALU.mult, op1=ALU.add),
                               reads=[G0[b], G1[b], ROUT], writes=[G0[b]])
                        for hb in range(2):
                            bk = 2 * (j % 2) + hb
                            for kk in range(4):
                                k = hb * 4 + kk
                                T.emit("pe", lambda e, bk=bk, kk=kk, k=k, b=b: e.transpose(
                                    out=banks[bk][:, kk * 128:(kk + 1) * 128], in_=g0[:, b, k * 128:(k + 1) * 128],
                                    identity=identf[:]), reads=[G0[b], CONST], writes=[BK[bk]], sig=(kk == 3))
                            T.emit("dve", lambda e, bk=bk, hb=hb, j=j: e.tensor_tensor(
                                out=xT[:, j, hb * 4:(hb + 1) * 4, :],
                                in0=banks[bk][:, :].rearrange("p (k t) -> p k t", t=128),
                                in1=xT[:, j, hb * 4:(hb + 1) * 4, :], op=ALU.add),
                                reads=[BK[bk]] + [XB[hb * 4 + kk][j // 4] for kk in range(4)],
                                writes=[XB[hb * 4 + kk][j // 4] for kk in range(4)])

                if not last:
                    for t in range(4):
                        tl = tok0 // 128 + 4 * t
                        T.dma("sp", xT_d[tl:tl + 4].rearrange("j p k t -> p j k t"), xT[:, 4 * t:4 * t + 4],
                              reads=[XB[k][t] for k in range(8)], writes=[XD[s][t]])
                else:
                    T.barrier()
                    final_out(xT, XB, s)
                T.barrier()

        def f_phase(l, s, last):
            tok0 = s * S
            with ExitStack() as pf:
                xT = sb(pf, "xT", [128, 8, S], F32)
                H = sb(pf, "Hf", [128, 8, S], BF16)
                ring = sb(pf, "ring", [128, 6, 4096], BF16)
                sq = sb(pf, "sqf", [128, 8, 512], BF16)
                rstd = sb(pf, "rstdf", [128, 512], F32)
                wr_sb = sb(pf, "wr_sb", [128, 8, 20], F32)
                wrp = sb(pf, "wrp", [128, 8, 20], F32)
                a_e = sb(pf, "a_e", [128, 2, 4, 512], BF16)
                sg = sb(pf, "sg", [128, 2, 512], F32)
                tt = sb(pf, "tt", [128, 2, 512], F32)
                Cb = sb(pf, "Cb", [128, 2, 512], F32)
                rt = sb(pf, "rt", [128, 16], F32)
                LGs = sb(pf, "LGs", [128, 16, 20], F32)
                r1 = sb(pf, "r1", [128, 16, 16], F32)
                r2 = sb(pf, "r2", [128, 16, 16], F32)
                gmax = sb(pf, "gmax", [128, 16], F32)
                goh = sb(pf, "goh", [128, 16, 4], F32)
                gex = sb(pf, "gex", [128, 16, 4], F32)
                gw = sb(pf, "gw", [128, 16], F32)
                esel = sb(pf, "esel", [128, 16, 4], F32)
                em = sb(pf, "em", [128, 16, 4], F32)
                m1 = sb(pf, "m1", [128, 16], F32)
                m2 = sb(pf, "m2", [128, 16], F32)
                oh1 = sb(pf, "oh1", [128, 16, 4], F32)
                oh2 = sb(pf, "oh2", [128, 16, 4], F32)
                w1 = sb(pf, "w1", [128, 16], F32)
                w2 = sb(pf, "w2", [128, 16], F32)
                c4 = sb(pf, "c4", [128, 16, 4], F32)
                Cm = sb(pf, "Cm", [128, 16, 16], F32)
                Chi = sb(pf, "Chi", [128, 16, 16], BF16)
                Clo = sb(pf, "Clo", [128, 16, 16], BF16)
                XB = [[Buf(f"X{k}_{t}") for t in range(4)] for k in range(8)]
                HB = [Buf(f"Hf{t}") for t in range(4)]
                RSLOT = [Buf(f"ring{i}") for i in range(6)]
                SQ, RS, WR, WRP, RT = Buf("sq"), Buf("rs"), Buf("wr"), Buf("wrp"), Buf("rt")
                ROUT = Buf("router")
                AE = [[Buf(f"ae{b}_{f}") for f in range(4)] for b in range(2)]
                SG = [Buf(f"sg{i}") for i in range(2)]
                TT = [Buf(f"tt{i}") for i in range(2)]
                CB = [Buf("cb0"), Buf("cb1")]

                def load_expert(e):
                    e4 = e // 4
                    for j, (nm, wb) in enumerate((("g", w_gate_b), ("u", w_up_b))):
                        slot = (3 * e + j) % 6
                        T.dma("sp", ring[:, slot, :].rearrange("p (k f) -> p k f", f=512),
                              wb[l, e].rearrange("(k p) f -> p k f", p=128), reads=[WB[(nm, l, e4)]], writes=[RSLOT[slot]])
                    slot = (3 * e + 2) % 6
                    T.dma("sp", ring[:, slot, :].rearrange("p (k f) -> p k f", f=1024),
                          w_down_b[l, e].rearrange("(k p) f -> p k f", p=128), reads=[WB[("d", l, e4)]], writes=[RSLOT[slot]])

                for k in range(8):
                    T.dma("sp", xT[:, k, :], xT_d[k, :, tok0:tok0 + S], reads=[XD[s][t] for t in range(4)],
                          writes=[XB[k][t] for t in range(4)])
                T.dma("sp", wr_sb[:], wr_d[:, l], writes=[WR])
                load_expert(0)
                for k in range(8):
                    T.emit("dve", lambda e, k=k: e.tensor_scalar(out=wrp[:, k, :], in0=wr_sb[:, k, :],
                                                                 scalar1=gf[:, l * 8 + k:l * 8 + k + 1], scalar2=None,
                                                                 op0=ALU.mult), reads=[WR, CONST], writes=[WRP])
                for t in range(4):
                    c0 = t * 512
                    T.emit("act", lambda e, c0=c0: e.activation(out=sq[:], in_=xT[:, :, c0:c0 + 512], func=AF.Square),
                           reads=[XB[k][t] for k in range(8)], writes=[SQ])
                    for k in range(8):
                        mm(banks[6][:, :], onesb[:], sq[:, k, :], k == 0, k == 7, [SQ, CONST], [BK[6]], k == 7)
                    rstd_from_ss(rstd[:], banks[6][:, :], [BK[6]], [RS])
                    for k in range(8):
                        T.emit("dve", lambda e, k=k, c0=c0: e.scalar_tensor_tensor(
                            out=H[:, k, c0:c0 + 512], in0=xT[:, k, c0:c0 + 512], scalar=gf[:, l * 8 + k:l * 8 + k + 1],
                            in1=rstd[:], op0=ALU.mult, op1=ALU.mult), reads=[XB[k][t], RS, CONST], writes=[HB[t]])
                    for j in range(4):
                        jj = 4 * t + j
                        for k in range(8):
                            mm(banks[7][:, 320 + jj:320 + jj + 1], sq[:, k, j * 128:(j + 1) * 128], onesb[:, 0:1],
                               k == 0, k == 7, [SQ, CONST], [BK[7]], False)
                        for k in range(8):
                            mm(banks[7][:, jj * 20:(jj + 1) * 20], xT[:, k, c0 + j * 128:c0 + (j + 1) * 128], wrp[:, k, :],
                               k == 0, k == 7, [XB[k][t], WRP], [BK[7]], (k == 7))
                R_ = [ROUT]

                def dv(fn, extra_reads=()):
                    T.emit("dve", fn, reads=[ROUT] + list(extra_reads), writes=[ROUT])

                def b3(ap2, n):
                    return ap2.unsqueeze(2).broadcast_to([128, 16, n])

                rstd_from_ss(rt[:], banks[7][:, 320:336], [BK[7], ROUT], [ROUT])
                dv(lambda e: e.tensor_tensor(out=LGs[:], in0=banks[7][:, 0:320].rearrange("p (j e) -> p j e", e=20),
                                             in1=b3(rt[:], 20), op=ALU.mult), [BK[7]])
                dv(lambda e: e.tensor_reduce(out=gmax[:], in_=LGs[:, :, 0:4], axis=AX.X, op=ALU.max))
                dv(lambda e: e.tensor_tensor(out=goh[:], in0=LGs[:, :, 0:4], in1=b3(gmax[:], 4), op=ALU.is_equal))
                dv(lambda e: e.tensor_tensor(out=gex[:], in0=LGs[:, :, 0:4], in1=b3(gmax[:], 4), op=ALU.subtract))
                T.emit("act", lambda e: e.activation(out=gex[:], in_=gex[:], func=AF.Exp), reads=[ROUT], writes=[ROUT])
                dv(lambda e: e.tensor_reduce(out=gw[:], in_=gex[:], axis=AX.X, op=ALU.add))
                dv(lambda e: e.reciprocal(out=gw[:], in_=gw[:]))
                dv(lambda e: e.tensor_tensor(out=r1[:].rearrange("p j (g i) -> p j g i", i=4),
                                             in0=LGs[:, :, 4:20].rearrange("p j (g i) -> p j g i", i=4),
                                             in1=goh[:].unsqueeze(3).broadcast_to([128, 16, 4, 4]), op=ALU.mult))
                dv(lambda e: e.tensor_reduce(out=esel[:], in_=r1[:].rearrange("p j (g i) -> p j i g", i=4),
                                             axis=AX.X, op=ALU.add))
                dv(lambda e: e.tensor_reduce(out=m1[:], in_=esel[:], axis=AX.X, op=ALU.max))
                dv(lambda e: e.tensor_tensor(out=oh1[:], in0=esel[:], in1=b3(m1[:], 4), op=ALU.is_equal))
                dv(lambda e: e.scalar_tensor_tensor(out=em[:], in0=oh1[:], scalar=-1.0e30, in1=esel[:],
                                                    op0=ALU.mult, op1=ALU.add))
                dv(lambda e: e.tensor_reduce(out=m2[:], in_=em[:], axis=AX.X, op=ALU.max))
                dv(lambda e: e.tensor_tensor(out=oh2[:], in0=em[:], in1=b3(m2[:], 4), op=ALU.is_equal))
                dv(lambda e: e.tensor_tensor(out=w2[:], in0=m2[:], in1=m1[:], op=ALU.subtract))
                T.emit("act", lambda e: e.activation(out=w2[:], in_=w2[:], func=AF.Exp), reads=[ROUT], writes=[ROUT])
                dv(lambda e: e.tensor_scalar(out=w2[:], in0=w2[:], scalar1=1.0, scalar2=None, op0=ALU.add))
                dv(lambda e: e.reciprocal(out=w1[:], in_=w2[:]))
                dv(lambda e: e.tensor_tensor(out=w1[:], in0=w1[:], in1=gw[:], op=ALU.mult))
                dv(lambda e: e.tensor_tensor(out=w2[:], in0=gw[:], in1=w1[:], op=ALU.subtract))
                dv(lambda e: e.tensor_tensor(out=c4[:], in0=oh1[:], in1=b3(w1[:], 4), op=ALU.mult))
                dv(lambda e: e.tensor_tensor(out=oh2[:], in0=oh2[:], in1=b3(w2[:], 4), op=ALU.mult))
                dv(lambda e: e.tensor_tensor(out=c4[:], in0=c4[:], in1=oh2[:], op=ALU.add))
                dv(lambda e: e.tensor_tensor(out=Cm[:].rearrange("p j (g i) -> p j g i", i=4),
                                             in0=goh[:].unsqueeze(3).broadcast_to([128, 16, 4, 4]),
                                             in1=c4[:].unsqueeze(2).broadcast_to([128, 16, 4, 4]), op=ALU.mult))
                dv(lambda e: e.tensor_copy(out=Chi[:], in_=Cm[:]))
                dv(lambda e: e.tensor_tensor(out=r2[:], in0=Cm[:], in1=Chi[:], op=ALU.subtract))
                dv(lambda e: e.tensor_copy(out=Clo[:], in_=r2[:]))

                steps = [(e_, t_) for e_ in range(NE) for t_ in range(4)]

                def emit_gu(idx):
                    e_, t = steps[idx]
                    ab = idx % 2
                    c0 = t * 512
                    sg_, su_, sd_ = (3 * e_) % 6, (3 * e_ + 1) % 6, (3 * e_ + 2) % 6
                    n = 0
                    for j in range(4):
                        for Cx in (Chi, Clo):
                            mm(banks[6][:, j * 128:(j + 1) * 128], Cx[:, 4 * t + j, e_:e_ + 1].broadcast_to([128, 128]),
                               identb[:], Cx is Chi, Cx is Clo, [ROUT, CONST], [BK[6]], (j == 3 and Cx is Clo))
                    T.emit("act", lambda e, ab=ab: e.activation(out=Cb[:, ab, :], in_=banks[6][:, :], func=AF.Copy),
                           reads=[BK[6]], writes=[CB[ab]])
                    for f in range(4):
                        bg, bu = f % 2, 2 + f % 2
                        for k in range(8):
                            mm(banks[bg][:, :], ring[:, sg_, k * 512 + f * 128:k * 512 + (f + 1) * 128], H[:, k, c0:c0 + 512],
                               k == 0, k == 7, [RSLOT[sg_], HB[t]], [BK[bg]], k == 7)
                        for k in range(8):
                            mm(banks[bu][:, :], ring[:, su_, k * 512 + f * 128:k * 512 + (f + 1) * 128], H[:, k, c0:c0 + 512],
                               k == 0, k == 7, [RSLOT[su_], HB[t]], [BK[bu]], k == 7)
                        i3 = f % 2
                        T.emit("act", lambda e, bg=bg, i3=i3: e.activation(out=sg[:, i3, :], in_=banks[bg][:, :], func=AF.Silu),
                               reads=[BK[bg]], writes=[SG[i3]])
                        T.emit("dve", lambda e, bu=bu, i3=i3: e.tensor_tensor(out=tt[:, i3, :], in0=banks[bu][:, :],
                                                                            in1=sg[:, i3, :], op=ALU.mult),
                               reads=[BK[bu], SG[i3]], writes=[TT[i3]])
                        T.emit("dve", lambda e, ab=ab, f=f, i3=i3: e.tensor_tensor(out=a_e[:, ab, f, :], in0=tt[:, i3, :],
                                                                                   in1=Cb[:, ab, :], op=ALU.mult),
                               reads=[TT[i3], CB[ab]], writes=[AE[ab][f]])

                def emit_d(idx):
                    e_, t = steps[idx]
                    ab = idx % 2
                    c0 = t * 512
                    sd_ = (3 * e_ + 2) % 6
                    for m in range(8):
                        bk = 4 + m % 2
                        for f in range(4):
                            mm(banks[bk][:, :], ring[:, sd_, f * 1024 + m * 128:f * 1024 + (m + 1) * 128], a_e[:, ab, f, :],
                               f == 0, f == 3, [RSLOT[sd_], AE[ab][f]], [BK[bk]], f == 3)
                        T.emit("dve", lambda e, m=m, bk=bk, c0=c0: e.tensor_tensor(
                            out=xT[:, m, c0:c0 + 512], in0=banks[bk][:, :], in1=xT[:, m, c0:c0 + 512], op=ALU.add),
                            reads=[BK[bk], XB[m][t]], writes=[XB[m][t]])

                for idx in range(len(steps)):
                    e_, t = steps[idx]
                    emit_gu(idx)
                    if idx > 0:
                        emit_d(idx - 1)
                    if t == 0 and e_ + 1 < NE:
                        load_expert(e_ + 1)
                emit_d(len(steps) - 1)

                if not last:
                    for k in range(8):
                        T.dma("sp", xT_d[k, :, tok0:tok0 + S], xT[:, k, :], reads=[XB[k][t] for t in range(4)],
                              writes=[XD[s][t] for t in range(4)])
                else:
                    final_out(xT, XB, s)
                T.barrier()

        done = False
        for l in range(depth):
            for s in range(nseq):
                m_phase(l, s)
            if stop_after == ("M", l):
                done = True
                break
            for s in range(nseq):
                (f_phase_sparse if SPARSE else f_phase)(l, s, last=(l == depth - 1 and stop_after is None))
            if stop_after == ("F", l):
                done = True
                break
        T.barrier()
        T.flush()
    return nc


def _bias_index_map():
    hp = np.arange(4)[:, None, None, None, None]
    p = np.arange(128)[None, :, None, None, None]
    eo = np.arange(2)[None, None, :, None, None]
    i = np.arange(14)[None, None, None, :, None]
    cq = np.arange(64)[None, None, None, None, :]
    kc = p % 64
    half = p // 64
    h = 2 * hp + eo
    dr = i + half
    dc = np.clip(kc - cq, -15, 15) + 15
    qs = np.clip(cq - 8, 0, 48)
    valid = (kc >= qs) & (kc < qs + 16)
    flat = h * (15 * 31 + 1) + np.where(valid, dr * 31 + dc, 15 * 31)
    return np.broadcast_to(flat, (4, 128, 2, 14, 64)).copy()


def _pool_consts():
    import ml_dtypes
    out = np.zeros((4, 7, 128, 128), np.float32)
    Sr = 512
    t = np.arange(Sr)
    for g, w in enumerate(POOL_W):
        lo = np.clip(t - w // 2, 0, Sr)
        hi = np.clip(t - w // 2 + w, 0, Sr)
        cnt = (hi - lo).astype(np.float64)
        A = np.zeros((Sr, Sr), np.float64)
        for tt_ in range(Sr):
            A[lo[tt_]:hi[tt_], tt_] = 1.0 / cnt[tt_]
        A -= np.eye(Sr)

        def blk(a, b):
            return A[a * 128:(a + 1) * 128, b * 128:(b + 1) * 128]

        def hilo(M):
            hi_ = M.astype(np.float32).astype(ml_dtypes.bfloat16).astype(np.float64)
            lo_ = (M - hi_).astype(np.float32).astype(ml_dtypes.bfloat16).astype(np.float64)
            return hi_, lo_

        out[g, 0] = blk(1, 2)
        out[g, 1] = blk(2, 2)
        out[g, 2] = blk(3, 2)
        out[g, 3], out[g, 4] = hilo(blk(0, 0))
        out[g, 5], out[g, 6] = hilo(blk(3, 3))
    return np.ascontiguousarray(out.reshape(28, 128, 128).transpose(1, 0, 2))


_BIAS_MAP = None
_PROG = {}


def prep_inputs(inp, nseq=NSEQ_FULL, ncores=NCORES):
    global _BIAS_MAP
    f = lambda a: np.ascontiguousarray(np.asarray(a, dtype=np.float32))
    Lw = L_FULL
    if _BIAS_MAP is None:
        _BIAS_MAP = _bias_index_map()
    rpb = f(inp["rpb"])
    pad = np.concatenate([rpb.reshape(Lw, 8, 15 * 31), np.full((Lw, 8, 1), NEG, np.float32)], axis=2).reshape(Lw, -1)
    biasT = pad[:, _BIAS_MAP]
    biasT = np.ascontiguousarray(biasT.reshape(Lw, 4, 128, 2 * 14 * 64))
    shared = {
        "w_in": f(inp["w_in"]), "w_out": f(inp["w_out"]), "pool_w": f(inp["pool_w"]),
        "w_gate": f(inp["w_gate"]), "w_up": f(inp["w_up"]), "w_down": f(inp["w_down"]),
        "gm": np.ascontiguousarray(f(inp["norm_mix_g"]).reshape(Lw, 8, 128).transpose(2, 0, 1).reshape(128, Lw * 8)),
        "gf": np.ascontiguousarray(f(inp["norm_ffn_g"]).reshape(Lw, 8, 128).transpose(2, 0, 1).reshape(128, Lw * 8)),
        "ps": np.ascontiguousarray(f(inp["pool_scale"]).reshape(Lw, 4, 128).transpose(2, 0, 1).reshape(128, Lw * 4)),
        "gF": np.ascontiguousarray(np.broadcast_to(f(inp["final_g"])[None, :], (128, D))),
        "wr": np.ascontiguousarray(np.concatenate([f(inp["w_router_group"]), f(inp["w_router_expert"])], axis=-1)
                                   .reshape(Lw, 8, 128, 20).transpose(2, 0, 1, 3)),
        "biasT": biasT,
        "poolA": _pool_consts(),
        "ident": np.eye(128, dtype=np.float32),
        "ustrict": np.triu(np.ones((128, 128), np.float32), k=1),
        "iotas": np.ascontiguousarray(np.concatenate([np.arange(128, dtype=np.float32)[:, None],
                                                      np.broadcast_to(np.arange(32, dtype=np.float32)[None, :], (128, 32))], axis=1)),
    }
    x = f(inp["x"]).reshape(-1, S, D)
    maps = []
    for c in range(ncores):
        m = dict(shared)
        m["x"] = np.ascontiguousarray(x[c * nseq:(c + 1) * nseq].reshape(nseq * S, D))
        maps.append(m)
    return maps


def kernel(x, norm_mix_g, w_in, rpb, pool_w, pool_scale, w_out, norm_ffn_g, w_router_group, w_router_expert,
           w_gate, w_up, w_down, final_g):
    inp = dict(x=x, norm_mix_g=norm_mix_g, w_in=w_in, rpb=rpb, pool_w=pool_w, pool_scale=pool_scale, w_out=w_out,
               norm_ffn_g=norm_ffn_g, w_router_group=w_router_group, w_router_expert=w_router_expert,
               w_gate=w_gate, w_up=w_up, w_down=w_down, final_g=final_g)
    maps = prep_inputs(inp)
    if "full" not in _PROG:
        _PROG["full"] = build_program()
    nc = _PROG["full"]
    res = run_bass_kernel_spmd(nc, maps, core_ids=list(range(NCORES)))
    outs = [np.asarray(r["out"], dtype=np.float32).reshape(NSEQ_FULL, S, D) for r in res.results]
    return np.concatenate(outs, axis=0)
```

```python
import numpy as np
from contextlib import ExitStack
import concourse.bass as bass
import concourse.mybir as mybir
from concourse.bass_utils import run_bass_kernel_spmd

F32 = mybir.dt.float32
BF16 = mybir.dt.bfloat16
AF = mybir.ActivationFunctionType
ALU = mybir.AluOpType
AX = mybir.AxisListType

D = 1024
S = 2048
L_FULL = 4
NSEQ_FULL = 4
NCORES = 8
NE = 16
DE = 512
EPS = 1e-6
NEG = -30000.0
POOL_W = (2, 4, 8, 16)


class Buf:
    __slots__ = ("name", "w", "r")

    def __init__(self, name):
        self.name = name
        self.w = None
        self.r = {}


class _Eng:
    def __init__(self, name):
        self.name = name
        self.q = []
        self.known = {}
        self.count = 0
        self.psem = None


class Tracker:
    ENGS = ("pe", "act", "dve", "pool", "sp")

    def __init__(self, nc, stack, n_dma_sems=24):
        self.nc = nc
        self.sems = []
        self.eng = {n: _Eng(n) for n in self.ENGS}
        for n in ("pe", "act", "dve", "pool"):
            self.eng[n].psem = self._new_sem(stack, "p_" + n)
        self.dpool = [self._new_sem(stack, f"dq{i}") for i in range(n_dma_sems)]
        self.dcnt = [0] * n_dma_sems
        self.dnext = 0
        self.stack = stack
        self.extra = []

    def _new_sem(self, stack, name):
        h = stack.enter_context(self.nc.semaphore(name))
        self.sems.append(h)
        return len(self.sems) - 1

    def _deps(self, E, reads, writes, extra=()):
        need = {}

        def req(ev):
            if ev is None:
                return
            s, v = ev
            if s == E.psem and E.name == "pe":
                return
            if E.known.get(s, 0) >= v:
                return
            if need.get(s, 0) < v:
                need[s] = v

        for ev in extra:
            req(ev)
        for b in reads:
            req(b.w)
        for b in writes:
            req(b.w)
            for s, v in b.r.items():
                req((s, v))
        for s, v in need.items():
            E.q.append(("wait", s, v))
            E.known[s] = v

    def _mark(self, ev, reads, writes):
        for b in reads:
            if b.r.get(ev[0], 0) < ev[1]:
                b.r[ev[0]] = ev[1]
        for b in writes:
            b.w = ev
            b.r = {}

    def emit(self, eng, fn, reads=(), writes=(), sig=True):
        E = self.eng[eng]
        self._deps(E, reads, writes)
        if sig:
            E.count += 1
            ev = (E.psem, E.count)
        else:
            ev = (E.psem, E.count + 1)
        E.q.append(("op", fn, sig))
        self._mark(ev, reads, writes)
        return ev

    def dma(self, q, out, in_, reads=(), writes=(), own_sem=False):
        E = self.eng[q]
        if own_sem:
            s = self._new_sem(self.stack, f"ds{len(self.sems)}")
            self.extra.append(s)
            prev = 0
            self._deps(E, reads, writes)
            ev = (s, 16)
        else:
            i = self.dnext
            self.dnext = (i + 1) % len(self.dpool)
            s = self.dpool[i]
            prev = self.dcnt[i]
            self._deps(E, reads, writes, extra=[(s, prev)] if prev else ())
            self.dcnt[i] += 16
            ev = (s, self.dcnt[i])
        E.q.append(("dma", out, in_, s))
        self._mark(ev, reads, writes)
        return ev

    def idma(self, out, out_idx, in_, in_idx, reads=(), writes=(), bounds=None):
        E = self.eng["pool"]
        i = self.dnext
        self.dnext = (i + 1) % len(self.dpool)
        s = self.dpool[i]
        prev = self.dcnt[i]
        self._deps(E, reads, writes, extra=[(s, prev)] if prev else ())
        self.dcnt[i] += 16
        ev = (s, self.dcnt[i])
        E.q.append(("idma", out, out_idx, in_, in_idx, s, bounds))
        self._mark(ev, reads, writes)
        return ev

    def barrier(self):
        evs = []
        for n in ("pe", "act", "dve", "pool"):
            e = self.eng[n]
            if e.count:
                evs.append((e.psem, e.count))
        for i, s in enumerate(self.dpool):
            if self.dcnt[i]:
                evs.append((s, self.dcnt[i]))
        for s in self.extra:
            evs.append((s, 16))
        for fn in getattr(self, "extra_ev_fns", []):
            evs.extend(fn())
        for n in self.ENGS:
            E = self.eng[n]
            for s, v in evs:
                if s == E.psem:
                    continue
                if E.known.get(s, 0) < v:
                    E.q.append(("wait", s, v))
                    E.known[s] = v

    def flush(self):
        nc = self.nc
        sems = self.sems

        def run(E, h):
            psem = sems[E.psem] if E.psem is not None else None
            for it in E.q:
                if it[0] == "wait":
                    h.wait_ge(sems[it[1]], it[2])
                elif it[0] == "op":
                    ins = it[1](h)
                    if it[2]:
                        ins.then_inc(psem, 1)
                elif it[0] == "idma":
                    oo = bass.IndirectOffsetOnAxis(ap=it[2], axis=0) if it[2] is not None else None
                    io = bass.IndirectOffsetOnAxis(ap=it[4], axis=0) if it[4] is not None else None
                    if it[6] is None:
                        h.indirect_dma_start(out=it[1], out_offset=oo, in_=it[3], in_offset=io).then_inc(sems[it[5]], 16)
                    else:
                        h.indirect_dma_start(out=it[1], out_offset=oo, in_=it[3], in_offset=io, bounds_check=it[6],
                                             oob_is_err=False).then_inc(sems[it[5]], 16)
                else:
                    h.dma_start(out=it[1], in_=it[2]).then_inc(sems[it[3]], 16)

        with nc.Block() as block:
            @block.tensor
            def _(h):
                run(self.eng["pe"], h)

            @block.scalar
            def _(h):
                run(self.eng["act"], h)

            @block.vector
            def _(h):
                run(self.eng["dve"], h)

            @block.gpsimd
            def _(h):
                run(self.eng["pool"], h)

            @block.sync
            def _(h):
                run(self.eng["sp"], h)


SPARSE = True
INTERLEAVE_OUT = True
POOL_FROM_LAYER = 99
TS = 256
NTILE = 32


def build_program(nseq=NSEQ_FULL, depth=L_FULL, stop_after=None, debug_out=False):
    nc = bass.Bass("TRN2", target_bir_lowering=False)
    NT = nseq * S
    Lw = L_FULL

    def din(name, shape, dt=F32):
        return nc.dram_tensor(name, list(shape), dt, kind="ExternalInput").ap()

    x_c = din("x", [NT, D])
    w_in = din("w_in", [Lw, D, 2048])
    w_out = din("w_out", [Lw, D, D])
    pool_w = din("pool_w", [Lw, 4, 128, 128])
    w_gate = din("w_gate", [Lw, NE, D, DE])
    w_up = din("w_up", [Lw, NE, D, DE])
    w_down = din("w_down", [Lw, NE, DE, D])
    gm_d = din("gm", [128, Lw * 8])
    gf_d = din("gf", [128, Lw * 8])
    ps_d = din("ps", [128, Lw * 4])
    gF_d = din("gF", [128, D])
    wr_d = din("wr", [128, Lw, 8, 20])
    bias_d = din("biasT", [Lw, 4, 128, 2 * 14 * 64])
    poolA_d = din("poolA", [128, 28, 128])
    ident_d = din("ident", [128, 128])
    ustrict_d = din("ustrict", [128, 128])
    iotas_d = din("iotas", [128, 33])
    if stop_after is None:
        out_c = nc.dram_tensor("out", [NT, D], F32, kind="ExternalOutput").ap()
        xT_d = nc.dram_tensor("xT_d", [NT // 128, 128, 8, 128], F32, kind="Internal").ap()
    else:
        xT_d = nc.dram_tensor("xT_out", [NT // 128, 128, 8, 128], F32, kind="ExternalOutput").ap()
        out_c = None
    w_in_b = nc.dram_tensor("w_in_b", [Lw, D, 2048], BF16, kind="Internal").ap()
    w_out_b = nc.dram_tensor("w_out_b", [Lw, D, D], BF16, kind="Internal").ap()
    pool_w_b = nc.dram_tensor("pool_w_b", [Lw, 4, 128, 128], BF16, kind="Internal").ap()
    w_gate_b = nc.dram_tensor("w_gate_b", [Lw, NE, D, DE], BF16, kind="Internal").ap()
    w_up_b = nc.dram_tensor("w_up_b", [Lw, NE, D, DE], BF16, kind="Internal").ap()
    w_down_b = nc.dram_tensor("w_down_b", [Lw, NE, DE, D], BF16, kind="Internal").ap()
    w_gate_p = [nc.dram_tensor(f"w_gate_p{i}", [NE * 128, 4096], BF16, kind="Internal").ap() for i in range(Lw)]
    w_up_p = [nc.dram_tensor(f"w_up_p{i}", [NE * 128, 4096], BF16, kind="Internal").ap() for i in range(Lw)]
    w_down_p = [nc.dram_tensor(f"w_down_p{i}", [NE * 128, 4096], BF16, kind="Internal").ap() for i in range(Lw)]
    HS_d = nc.dram_tensor("HS_d", [NTILE * TS, D], BF16, kind="Internal").ap()
    YS_d = nc.dram_tensor("YS_d", [NTILE * TS, D], F32, kind="Internal").ap()

    top = ExitStack()
    with top:
        T = Tracker(nc, top)

        uid = [0]

        def sb(stack, name, shape, dt):
            uid[0] += 1
            return stack.enter_context(nc.sbuf_tensor(f"{name}_s{uid[0]}", list(shape), dt))

        banks = [top.enter_context(nc.psum_tensor(f"bank{i}", [128, 512], F32)) for i in range(8)]
        BK = [Buf(f"bank{i}") for i in range(8)]

        onesb = sb(top, "onesb", [128, 128], BF16)
        identf = sb(top, "identf", [128, 128], F32)
        identb = sb(top, "identb", [128, 128], BF16)
        A_bf = sb(top, "A_bf", [128, 28, 128], BF16)
        gm = sb(top, "gm", [128, Lw * 8], F32)
        gf = sb(top, "gf", [128, Lw * 8], F32)
        psc = sb(top, "psc", [128, Lw * 4], F32)
        epsc = sb(top, "epsc", [128, 1], F32)
        ustr_f = sb(top, "ustr_f", [128, 128], F32)
        ustr = sb(top, "ustr", [128, 128], BF16)
        iotas = sb(top, "iotas", [128, 33], F32)
        CONST = Buf("const")
        with nc.sbuf_tensor("A_stage", [128, 28, 128], F32) as A_st:
            ASB = Buf("A_stage")
            T.emit("dve", lambda e: e.memset(onesb[:], 1.0), writes=[CONST])
            T.emit("dve", lambda e: e.memset(epsc[:], EPS), writes=[CONST])
            T.dma("sp", identf[:], ident_d, writes=[CONST])
            T.dma("sp", gm[:], gm_d, writes=[CONST])
            T.dma("sp", gf[:], gf_d, writes=[CONST])
            T.dma("sp", psc[:], ps_d, writes=[CONST])
            T.dma("sp", A_st[:], poolA_d, writes=[ASB])
            T.dma("sp", ustr_f[:], ustrict_d, writes=[CONST])
            T.dma("sp", iotas[:], iotas_d, writes=[CONST])
            T.emit("dve", lambda e: e.tensor_copy(out=ustr[:], in_=ustr_f[:]), reads=[CONST], writes=[CONST])
            T.emit("dve", lambda e: e.tensor_copy(out=identb[:], in_=identf[:]), reads=[CONST], writes=[CONST])
            T.emit("dve", lambda e: e.tensor_copy(out=A_bf[:], in_=A_st[:]), reads=[ASB], writes=[CONST])
            T.barrier()

        WB = {}

        cast_sems = [T._new_sem(top, f"cs{i}") for i in range(4)]
        cast_cnt = [0] * 4
        cast_i = [0]

        def cast(key, dst, src, n=0):
            WB[key] = Buf(str(key))
            E = T.eng["pool"]
            i = cast_i[0] % 4
            cast_i[0] += 1
            sm = cast_sems[i]
            if cast_cnt[i] and E.known.get(sm, 0) < cast_cnt[i]:
                E.q.append(("wait", sm, cast_cnt[i]))
                E.known[sm] = cast_cnt[i]
            cast_cnt[i] += 16
            E.q.append(("dma", dst, src, sm))
            WB[key].w = (sm, cast_cnt[i])

        for l in range(depth):
            cast(("in", l), w_in_b[l], w_in[l])
            cast(("pw", l), pool_w_b[l].rearrange("g (a b) d -> (g a) (b d)", b=16),
                 pool_w[l].rearrange("g (a b) d -> (g a) (b d)", b=16))
            cast(("out", l), w_out_b[l].rearrange("(r q) d -> r (q d)", q=2),
                 w_out[l].rearrange("(r q) d -> r (q d)", q=2))
            for e_ in range(NE):
                cast(("g", l, e_), w_gate_p[l][e_ * 128:(e_ + 1) * 128, :].rearrange("p (k f) -> p k f", f=512),
                     w_gate[l, e_].rearrange("(k p) f -> p k f", p=128))
                cast(("u", l, e_), w_up_p[l][e_ * 128:(e_ + 1) * 128, :].rearrange("p (k f) -> p k f", f=512),
                     w_up[l, e_].rearrange("(k p) f -> p k f", p=128))
                cast(("d", l, e_), w_down_p[l][e_ * 128:(e_ + 1) * 128, :].rearrange("p (k f) -> p k f", f=1024),
                     w_down[l, e_].rearrange("(k p) f -> p k f", p=128))

        XD = [[Buf(f"xd{s}_{t}") for t in range(4)] for s in range(nseq)]
        flip = [0]

        def evac_copy(out, in_, reads, writes, scale=None):
            flip[0] ^= 1
            if scale is not None or flip[0]:
                sc = 1.0 if scale is None else scale
                T.emit("act", lambda e: e.activation(out=out, in_=in_, func=AF.Copy, scale=sc), reads=reads, writes=writes)
            else:
                T.emit("dve", lambda e: e.tensor_copy(out=out, in_=in_), reads=reads, writes=writes)

        def mm(out, lhsT, rhs, start, stop, reads, writes, sig):
            T.emit("pe", lambda e: e.matmul(out, lhsT, rhs, start=start, stop=stop), reads=reads, writes=writes, sig=sig)

        def rstd_from_ss(out, ss_ap, reads, writes):
            T.emit("act", lambda e: e.activation(out=out, in_=ss_ap, func=AF.Sqrt, bias=epsc[:, 0:1], scale=1.0 / D),
                   reads=list(reads) + [CONST], writes=writes)
            T.emit("dve", lambda e: e.reciprocal(out=out, in_=out), reads=writes, writes=writes)

        with ExitStack() as st:
            xin = sb(st, "xin", [128, 2, 4, 1024], F32)
            xTs = sb(st, "xTs", [128, 2, 4, 8, 128], F32)
            XIN = [Buf("xin0"), Buf("xin1")]
            XTS = [Buf("xts0"), Buf("xts1")]
            bi = 0
            for s in range(nseq):
                for t in range(4):
                    b = (s * 4 + t) % 2
                    t0 = s * S + t * 512
                    T.dma("sp", xin[:, b], x_c[t0:t0 + 512, :].rearrange("(j p) d -> p j d", p=128), writes=[XIN[b]])
                    for k in range(8):
                        bk = bi % 4
                        bi += 1
                        for j in range(4):
                            T.emit("pe", lambda e, bk=bk, j=j, k=k, b=b: e.transpose(
                                out=banks[bk][:, j * 128:(j + 1) * 128], in_=xin[:, b, j, k * 128:(k + 1) * 128],
                                identity=identf[:]), reads=[XIN[b], CONST], writes=[BK[bk]], sig=(j == 3))
                        evac_copy(xTs[:, b, :, k, :], banks[bk][:, :].rearrange("p (j t) -> p j t", t=128), [BK[bk]], [XTS[b]])
                    T.dma("sp", xT_d[t0 // 128:t0 // 128 + 4].rearrange("j p k t -> p j k t"), xTs[:, b], reads=[XTS[b]],
                          writes=[XD[s][t]])
            T.barrier()

        def m_phase(l, s):
            tok0 = s * S
            with ExitStack() as pm:
                H = sb(pm, "H", [128, 8, S], BF16)
                qT = sb(pm, "qT", [128, 4, S], BF16)
                kT = sb(pm, "kT", [128, 4, S], BF16)
                V2 = sb(pm, "V2", [128, 31, 4, 192], BF16)
                pT = sb(pm, "pT", [128, 4, S], BF16)
                pw_sb = sb(pm, "pw_sb", [128, 4, 128], BF16)
                HT = [Buf(f"H{i}") for i in range(8)]
                QB = [[Buf(f"q{m}_{t}") for t in range(4)] for m in range(4)]
                KB = [[Buf(f"k{m}_{t}") for t in range(4)] for m in range(4)]
                VB = [Buf(f"v{i}") for i in range(31)]
                VONES = Buf("vones")
                PB = [[Buf(f"p{g}_{t}") for t in range(4)] for g in range(4)]
                PW = Buf("pw")
                with ExitStack() as pb:
                    w_in_sb = sb(pb, "w_in_sb", [128, 8, 2048], BF16)
                    U2 = sb(pb, "U2", [128, 16, 512], BF16)
                    pooledT = sb(pb, "pooledT", [128, 2, 512], BF16)
                    sq = sb(pb, "sq", [128, 2, 8, 128], BF16)
                    xt = sb(pb, "xt", [128, 2, 8, 128], F32)
                    rstd = sb(pb, "rstd", [128, 1, 256], F32)
                    WI = [Buf(f"wi{c}") for c in range(4)]
                    UB = [Buf(f"u{j}") for j in range(16)]
                    PLB = [Buf("pl0"), Buf("pl1")]
                    SQ = Buf("sq")
                    XT = [Buf("xt0"), Buf("xt1")]
                    RS = [Buf("rs0"), Buf("rs1")]
                    for c in range(4):
                        T.dma("sp", w_in_sb[:, :, c * 512:(c + 1) * 512],
                              w_in_b[l][:, c * 512:(c + 1) * 512].rearrange("(k p) f -> p k f", p=128),
                              reads=[WB[("in", l)]], writes=[WI[c]])
                    T.dma("sp", pw_sb[:], pool_w_b[l].rearrange("g c d -> c g d"), reads=[WB[("pw", l)]], writes=[PW])
                    T.emit("dve", lambda e: e.memset(V2[:, :, :, 64:128], 1.0), writes=[VONES])
                    def emit_norm(i):
                        b = 0
                        c0 = i * 256
                        tl = (tok0 + c0) // 128
                        T.dma("sp", xt[:], xT_d[tl:tl + 2].rearrange("j p k t -> p j k t"),
                              reads=[XD[s][i // 2]], writes=[XT[b]])
                        T.emit("act", lambda e: e.activation(out=sq[:], in_=xt[:], func=AF.Square),
                               reads=[XT[b]], writes=[SQ])
                        bk = 6 + b
                        for j in range(2):
                            for k in range(8):
                                mm(banks[bk][:, j * 128:(j + 1) * 128], onesb[:], sq[:, j, k, :], k == 0, k == 7, [SQ, CONST], [BK[bk]],
                                   (k == 7 and j == 1))
                        rstd_from_ss(rstd[:, b, :], banks[bk][:, 0:256], [BK[bk]], [RS[b]])
                        for k in range(8):
                            T.emit("dve", lambda e, b=b, k=k, c0=c0: e.scalar_tensor_tensor(
                                out=H[:, k, c0:c0 + 256].rearrange("p (j t) -> p j t", t=128), in0=xt[:, :, k, :],
                                scalar=gm[:, l * 8 + k:l * 8 + k + 1],
                                in1=rstd[:, b, :].rearrange("p (j t) -> p j t", t=128), op0=ALU.mult, op1=ALU.mult),
                                reads=[XT[b], RS[b], CONST], writes=[HT[i]])
                    bi_ = [0]

                    def emit_qkproj(t):
                        bi = bi_[0]
                        for m in range(8):
                            bk = bi % 4
                            bi += 1
                            for k in range(8):
                                mm(banks[bk][:, :], w_in_sb[:, k, m * 128:(m + 1) * 128], H[:, k, t * 512:(t + 1) * 512],
                                   k == 0, k == 7, [WI[m // 4], HT[2 * t], HT[2 * t + 1]], [BK[bk]], k == 7)
                            if m < 4:
                                evac_copy(qT[:, m, t * 512:(t + 1) * 512], banks[bk][:, :], [BK[bk]], [QB[m][t]], scale=0.125)
                            else:
                                T.emit("dve", lambda e, bk=bk, m=m, t=t: e.tensor_copy(
                                    out=kT[:, m - 4, t * 512:(t + 1) * 512], in_=banks[bk][:, :]),
                                    reads=[BK[bk]], writes=[KB[m - 4][t]])
                        bi_[0] = bi

                    def emit_v(sidx):
                        bk = bi_[0] % 4
                        bi_[0] += 1
                        a0 = 64 * sidx
                        hts = sorted({a0 // 256, (a0 + 127) // 256})
                        for k in range(8):
                            mm(banks[bk][:, :], H[:, k, a0:a0 + 128], w_in_sb[:, k, 1024:1536], k == 0, k == 7,
                               [WI[2]] + [HT[i] for i in hts], [BK[bk]], k == 7)
                        evac_copy(V2[:, sidx, :, :].rearrange("p a (b d) -> p a b d", d=64)[:, :, 0:3:2, :],
                                  banks[bk][:, :].rearrange("p (a b d) -> p a b d", b=2, d=64), [BK[bk]], [VB[sidx]])

                    def emit_u(j):
                        bk = bi_[0] % 4
                        bi_[0] += 1
                        for k in range(8):
                            mm(banks[bk][:, :], H[:, k, j * 128:(j + 1) * 128], w_in_sb[:, k, 1536:2048], k == 0, k == 7,
                               [WI[3], HT[j // 2]], [BK[bk]], k == 7)
                        evac_copy(U2[:, j, :], banks[bk][:, :], [BK[bk]], [UB[j]])

                    def emit_proj(t):
                        emit_qkproj(t)
                        for sidx in range(max(0, 8 * t - 1), min(31, 8 * t + 7)):
                            emit_v(sidx)
                        for j in range(4 * t, 4 * t + 4):
                            emit_u(j)

                    for t in range(4):
                        emit_norm(2 * t)
                        emit_norm(2 * t + 1)
                        if t > 0:
                            emit_proj(t - 1)
                    emit_proj(3)
                    bi = bi_[0]
                    for t in range(4):
                        for g in range(4):
                            pbuf = g % 2
                            bk = bi % 4
                            bi += 1
                            for Tq in range(4):
                                Tt = 4 * t + Tq
                                terms = []
                                if Tt > 0:
                                    terms.append((Tt - 1, 0))
                                if Tt == 0:
                                    terms += [(Tt, 3), (Tt, 4)]
                                elif Tt == 15:
                                    terms += [(Tt, 5), (Tt, 6)]
                                else:
                                    terms.append((Tt, 1))
                                if Tt < 15:
                                    terms.append((Tt + 1, 2))
                                for ti, (tp, var) in enumerate(terms):
                                    mm(banks[bk][:, Tq * 128:(Tq + 1) * 128], U2[:, tp, g * 128:(g + 1) * 128],
                                       A_bf[:, g * 7 + var, :], ti == 0, ti == len(terms) - 1,
                                       [UB[tp], CONST], [BK[bk]], (Tq == 3 and ti == len(terms) - 1))
                            T.emit("act", lambda e, bk=bk, pbuf=pbuf, g=g: e.activation(
                                out=pooledT[:, pbuf, :], in_=banks[bk][:, :], func=AF.Copy),
                                reads=[BK[bk]], writes=[PLB[pbuf]])
                            bk2 = 4 + (g % 2)
                            mm(banks[bk2][:, :], pw_sb[:, g, :], pooledT[:, pbuf, :], True, True, [PW, PLB[pbuf]], [BK[bk2]], True)
                            T.emit("dve", lambda e, bk2=bk2, g=g, t=t: e.tensor_scalar(
                                out=pT[:, g, t * 512:(t + 1) * 512], in0=banks[bk2][:, :],
                                scalar1=psc[:, l * 4 + g:l * 4 + g + 1], scalar2=None, op0=ALU.mult),
                                reads=[BK[bk2], CONST], writes=[PB[g][t]])
                    T.barrier()
                with ExitStack() as pc:
                    bias = sb(pc, "bias", [128, 4, 2, 14, 64], F32)
                    Sb = sb(pc, "Sb", [128, 4, 2, 4, 64], F32)
                    Pm = sb(pc, "Pm", [128, 4, 2, 4, 64], BF16)
                    Rr = sb(pc, "Rr", [128, 2, 4, 64], F32)
                    w_out_sb = sb(pc, "w_out_sb", [128, 8, D], BF16)
                    xt2 = sb(pc, "xt2", [128, 2, 8, 128], F32)
                    BIAS = [Buf(f"bias{hp}") for hp in range(4)]
                    SBB = [Buf(f"sb{i}") for i in range(4)]
                    PMB = [Buf(f"pm{i}") for i in range(4)]
                    RRB = [Buf("rr0"), Buf("rr1")]
                    WO = [Buf("wo0"), Buf("wo1")]
                    XT2 = [Buf("xt20"), Buf("xt21")]
                    for hp in range(4):
                        T.dma("sp", bias[:, hp].rearrange("p a b c -> p (a b c)"), bias_d[l, hp], writes=[BIAS[hp]])
                        T.emit("act", lambda e, hp=hp: e.activation(out=bias[:, hp].rearrange("p a b c -> p (a b c)"),
                                                                    in_=bias[:, hp].rearrange("p a b c -> p (a b c)"), func=AF.Exp),
                               reads=[BIAS[hp]], writes=[BIAS[hp]])
                    for c in range(2):
                        T.dma("sp", w_out_sb[:, :, c * 512:(c + 1) * 512],
                              w_out_b[l][:, c * 512:(c + 1) * 512].rearrange("(k p) f -> p k f", p=128),
                              reads=[WB[("out", l)]], writes=[WO[c]])
                    units = [(r, hp) for r in range(32) for hp in range(4)]

                    def rstart(r):
                        return min(max(r - 4, 0), 24)

                    def emit_qk(ui):
                        r, hp = units[ui]
                        u4 = ui % 4
                        rs_ = rstart(r)
                        i0 = rs_ - r + 7
                        kts = sorted({(rs_ * 64) // 512, (rs_ * 64 + 511) // 512})
                        for half in (0, 1):
                            bk = (u4 // 2) * 2 + half
                            cb = (u4 % 2) * 256
                            p0 = 64 * half
                            for c in range(4):
                                ks = (rs_ + 2 * c) * 64
                                mm(banks[bk][:, cb + c * 64:cb + (c + 1) * 64], kT[p0:p0 + 64, hp, ks:ks + 128],
                                   qT[p0:p0 + 64, hp, r * 64:(r + 1) * 64], True, True,
                                   [KB[hp][t] for t in kts] + [QB[hp][r // 8]], [BK[bk]], c == 3)
                            T.emit("act", lambda e, bk=bk, cb=cb, u4=u4, half=half: e.activation(
                                out=Sb[:, u4, half], in_=banks[bk][:, cb:cb + 256].rearrange("p (c q) -> p c q", q=64),
                                func=AF.Exp), reads=[BK[bk]], writes=[SBB[u4]])
                        T.emit("pool" if (l >= POOL_FROM_LAYER and ui % 2 == 1) else "dve", lambda e, u4=u4, hp=hp, i0=i0: e.tensor_tensor(
                            out=Pm[:, u4], in0=Sb[:, u4], in1=bias[:, hp, :, i0:i0 + 7:2, :], op=ALU.mult),
                            reads=[SBB[u4], BIAS[hp]], writes=[PMB[u4]])

                    def emit_pv(ui):
                        r, hp = units[ui]
                        u2 = ui % 4
                        rs_ = rstart(r)
                        ob = 4 + (r % 2)
                        for half in (0, 1):
                            h = 2 * hp + half
                            for c in range(4):
                                sidx = rs_ + 2 * c
                                lhsT = V2[:, sidx, hp, 64 * half:64 * half + 128]
                                col = (hp * 2 + half) * 64
                                mm(banks[ob][:, col:col + 64], lhsT, Pm[:, u2, half, c, :], c == 0, c == 3,
                                   [VB[sidx], VONES, PMB[u2]], [BK[ob]], c == 3)
                        if hp == 3:
                            rb = r % 2
                            Ov = banks[ob][:, :].rearrange("p (a b q) -> p a b q", b=2, q=64)
                            T.emit("dve", lambda e, rb=rb, ob=ob: e.reciprocal(
                                out=Rr[0:64, rb], in_=banks[ob][64:128, :].rearrange("p (a b q) -> p a b q", b=2, q=64)[:, :, 0, :]),
                                reads=[BK[ob]], writes=[RRB[rb]])
                            T.emit("dve", lambda e, rb=rb, ob=ob: e.reciprocal(
                                out=Rr[64:128, rb], in_=banks[ob][0:64, :].rearrange("p (a b q) -> p a b q", b=2, q=64)[:, :, 1, :]),
                                reads=[BK[ob]], writes=[RRB[rb]])
                            T.emit("dve", lambda e, rb=rb, ob=ob, r=r: e.tensor_tensor(
                                out=H[0:64, 0:4, r * 64:(r + 1) * 64],
                                in0=banks[ob][0:64, :].rearrange("p (a b q) -> p a b q", b=2, q=64)[:, :, 0, :],
                                in1=Rr[0:64, rb], op=ALU.mult), reads=[BK[ob], RRB[rb]], writes=[HT[r // 4]])
                            T.emit("dve", lambda e, rb=rb, ob=ob, r=r: e.tensor_tensor(
                                out=H[64:128, 0:4, r * 64:(r + 1) * 64],
                                in0=banks[ob][64:128, :].rearrange("p (a b q) -> p a b q", b=2, q=64)[:, :, 1, :],
                                in1=Rr[64:128, rb], op=ALU.mult), reads=[BK[ob], RRB[rb]], writes=[HT[r // 4]])

                    def emit_outproj(i):
                        b = i % 2
                        c0 = i * 128
                        T.dma("sp", xt2[:, b], xT_d[(tok0 + c0) // 128],
                              reads=[XD[s][i // 4]], writes=[XT2[b]])
                        for m in range(8):
                            bk = 6 + (m % 2)
                            for k in range(8):
                                rhs = H[:, k, c0:c0 + 128] if k < 4 else pT[:, k - 4, c0:c0 + 128]
                                rd = [HT[i // 2]] if k < 4 else [PB[k - 4][i // 4]]
                                mm(banks[bk][:, 0:128], w_out_sb[:, k, m * 128:(m + 1) * 128], rhs, k == 0, k == 7,
                                   [WO[m // 4]] + rd, [BK[bk]], k == 7)
                            T.emit("dve", lambda e, b=b, m=m, bk=bk: e.tensor_tensor(
                                out=xt2[:, b, m, :], in0=banks[bk][:, 0:128], in1=xt2[:, b, m, :], op=ALU.add),
                                reads=[BK[bk], XT2[b]], writes=[XT2[b]])
                        T.dma("sp", xT_d[(tok0 + c0) // 128], xt2[:, b],
                              reads=[XT2[b]], writes=[XD[s][i // 4]])

                    def pv_and_out(ui):
                        emit_pv(ui)
                        r, hp = units[ui]
                        if INTERLEAVE_OUT and hp == 3 and r % 2 == 1:
                            emit_outproj(r // 2)

                    for ui in range(len(units)):
                        emit_qk(ui)
                        if ui > 1:
                            pv_and_out(ui - 2)
                    pv_and_out(len(units) - 2)
                    pv_and_out(len(units) - 1)
                    if not INTERLEAVE_OUT:
                        for i in range(16):
                            emit_outproj(i)

                    T.barrier()

        def final_out(xT, XB, s):
            tok0 = s * S
            if True:
                if True:
                    with ExitStack() as po:
                        gF = sb(po, "gF", [128, D], F32)
                        ost = sb(po, "ost", [128, 1, D], F32)
                        junk = sb(po, "junk", [128, 512], BF16)
                        ssA = sb(po, "ssA", [128, 2], F32)
                        rsF = sb(po, "rsF", [128, 1], F32)
                        GF, OST, JK, SSA, RSF = Buf("gF"), [Buf("ost0"), Buf("ost1")], Buf("junk"), Buf("ssA"), Buf("rsF")
                        T.dma("sp", gF[:], gF_d, writes=[GF])
                        for j in range(16):
                            ob = 0
                            t = j // 4
                            for hb in range(2):
                                bk = 2 * (j % 2) + hb
                                for kk in range(4):
                                    k = hb * 4 + kk
                                    T.emit("pe", lambda e, bk=bk, kk=kk, k=k, j=j: e.transpose(
                                        out=banks[bk][:, kk * 128:(kk + 1) * 128], in_=xT[:, j, k, :],
                                        identity=identf[:]), reads=[XB[k][t], CONST], writes=[BK[bk]], sig=(kk == 3))
                                T.emit("act", lambda e, bk=bk, hb=hb: e.activation(
                                    out=junk[:], in_=banks[bk][:, :], func=AF.Square, accum_out=ssA[:, hb:hb + 1]),
                                    reads=[BK[bk]], writes=[JK, SSA])
                            T.emit("dve", lambda e: e.tensor_tensor(out=rsF[:], in0=ssA[:, 0:1], in1=ssA[:, 1:2], op=ALU.add),
                                   reads=[SSA], writes=[RSF])
                            rstd_from_ss(rsF[:], rsF[:], [RSF], [RSF])
                            for hb in range(2):
                                bk = 2 * (j % 2) + hb
                                T.emit("dve", lambda e, bk=bk, hb=hb, ob=ob: e.scalar_tensor_tensor(
                                    out=ost[:, ob, hb * 512:(hb + 1) * 512], in0=banks[bk][:, :], scalar=rsF[:, 0:1],
                                    in1=gF[:, hb * 512:(hb + 1) * 512], op0=ALU.mult, op1=ALU.mult),
                                    reads=[BK[bk], RSF, GF], writes=[OST[ob]])
                            T.dma("sp", out_c[tok0 + j * 128:tok0 + (j + 1) * 128, :], ost[:, ob, :], reads=[OST[ob]],
                                  writes=[XD[s][t]])

        def f_phase_sparse(l, s, last):
            U32 = mybir.dt.uint32
            tok0 = s * S
            with ExitStack() as pf:
                xT = sb(pf, "xT", [128, 16, 8, 128], F32)
                ring = sb(pf, "ring", [128, 6, 4096], BF16)
                wr_sb = sb(pf, "wr_sb", [128, 8, 20], F32)
                wrp = sb(pf, "wrp", [128, 8, 20], F32)
                rt = sb(pf, "rt", [128, 16], F32)
                LGs = sb(pf, "LGs", [128, 16, 20], F32)
                r1 = sb(pf, "r1", [128, 16, 16], F32)
                gmax = sb(pf, "gmax", [128, 16], F32)
                goh = sb(pf, "goh", [128, 16, 4], F32)
                gex = sb(pf, "gex", [128, 16, 4], F32)
                gw = sb(pf, "gw", [128, 16], F32)
                esel = sb(pf, "esel", [128, 16, 4], F32)
                em = sb(pf, "em", [128, 16, 4], F32)
                m1 = sb(pf, "m1", [128, 16], F32)
                m2 = sb(pf, "m2", [128, 16], F32)
                oh1 = sb(pf, "oh1", [128, 16, 4], F32)
                oh2 = sb(pf, "oh2", [128, 16, 4], F32)
                usum = sb(pf, "usum", [128, 16, 1], F32)
                utmp = sb(pf, "utmp", [128, 16, 1], F32)
                w1 = sb(pf, "w1", [128, 16], F32)
                w2 = sb(pf, "w2", [128, 16], F32)
                M1 = sb(pf, "M1", [128, 16, 16], F32)
                M2 = sb(pf, "M2", [128, 16, 16], F32)
                Mb = sb(pf, "Mb", [128, 256], BF16)
                TOT = sb(pf, "TOT", [128, 16, 16], F32)
                JP = sb(pf, "JP", [128, 16, 16], F32)
                SL = sb(pf, "SL", [128, 16, 16], F32)
                ne = sb(pf, "ne", [128, 16], F32)
                ntl = sb(pf, "ntl", [128, 16], F32)
                base = sb(pf, "base", [128, 16], F32)
                bend = sb(pf, "bend", [128, 16], F32)
                b256 = sb(pf, "b256", [128, 16], F32)
                sl1 = sb(pf, "sl1", [128, 16], F32)
                sl2 = sb(pf, "sl2", [128, 16], F32)
                s1u = sb(pf, "s1u", [128, 16], U32)
                s2u = sb(pf, "s2u", [128, 16], U32)
                eidx = sb(pf, "eidx", [128, NTILE], F32)
                widf = sb(pf, "widf", [128, NTILE], F32)
                widx = sb(pf, "widx", [128, NTILE], U32)
                XB = [[Buf(f"X{k}_{t}") for t in range(4)] for k in range(8)]
                RSLOT = [Buf(f"ring{i}") for i in range(6)]
                WR, WRP = Buf("wr"), Buf("wrp")
                ROUT = Buf("router")
                HSB = [Buf(f"hs{i}") for i in range(32)]
                YSB = [Buf(f"ys{i}") for i in range(NTILE)]

                def load_tile_w(i, which=(0, 1, 2)):
                    for j, wp in enumerate((w_gate_p, w_up_p, w_down_p)):
                        if j not in which:
                            continue
                        slot = (3 * i + j) % 6
                        T.idma(ring[:, slot, :], None, wp[l], widx[:, i:i + 1],
                               reads=[ROUT] + [WB[(("g", "u", "d")[j], l, e_)] for e_ in range(NE)], writes=[RSLOT[slot]])

                for t in range(4):
                    tl = tok0 // 128 + 4 * t
                    T.dma("sp", xT[:, 4 * t:4 * t + 4], xT_d[tl:tl + 4].rearrange("j p k t -> p j k t"), reads=[XD[s][t]],
                          writes=[XB[k][t] for k in range(8)])
                T.dma("sp", wr_sb[:], wr_d[:, l], writes=[WR])
                for k in range(8):
                    T.emit("dve", lambda e, k=k: e.tensor_scalar(out=wrp[:, k, :], in0=wr_sb[:, k, :],
                                                                 scalar1=gf[:, l * 8 + k:l * 8 + k + 1], scalar2=None,
                                                                 op0=ALU.mult), reads=[WR, CONST], writes=[WRP])

                def dv(fn, extra_reads=()):
                    T.emit("dve", fn, reads=[ROUT] + list(extra_reads), writes=[ROUT])

                def b3(ap2, n):
                    return ap2.unsqueeze(2).broadcast_to([128, 16, n])

                def uniq(oh):
                    dv(lambda e: e.tensor_copy(out=usum[:], in_=oh[:, :, 0:1]))
                    for i in range(1, 4):
                        dv(lambda e: e.tensor_scalar(out=utmp[:], in0=usum[:], scalar1=-1.0, scalar2=1.0,
                                                     op0=ALU.mult, op1=ALU.add))
                        dv(lambda e, i=i: e.tensor_tensor(out=oh[:, :, i:i + 1], in0=oh[:, :, i:i + 1], in1=utmp[:], op=ALU.mult))
                        if i < 3:
                            dv(lambda e, i=i: e.tensor_tensor(out=usum[:], in0=usum[:], in1=oh[:, :, i:i + 1], op=ALU.add))

                with ExitStack() as pa:
                    H = sb(pa, "Hf", [128, 8, S], BF16)
                    sq = sb(pa, "sqf", [128, 4, 8, 128], BF16)
                    rstd = sb(pa, "rstdf", [128, 512], F32)
                    hrow = sb(pa, "hrow", [128, 2, D], BF16)
                    HB = [Buf(f"Hf{t}") for t in range(4)]
                    SQ, RS = Buf("sq"), Buf("rs")
                    HROW = [Buf("hrow0"), Buf("hrow1")]
                    for t in range(4):
                        c0 = t * 512
                        T.emit("act", lambda e, t=t: e.activation(out=sq[:], in_=xT[:, 4 * t:4 * t + 4], func=AF.Square),
                               reads=[XB[k][t] for k in range(8)], writes=[SQ])
                        for j in range(4):
                            for k in range(8):
                                mm(banks[6][:, j * 128:(j + 1) * 128], onesb[:], sq[:, j, k, :], k == 0, k == 7, [SQ, CONST], [BK[6]],
                                   (k == 7 and j == 3))
                        rstd_from_ss(rstd[:], banks[6][:, :], [BK[6]], [RS])
                        for k in range(8):
                            T.emit("dve", lambda e, k=k, c0=c0, t=t: e.scalar_tensor_tensor(
                                out=H[:, k, c0:c0 + 512].rearrange("p (j t) -> p j t", t=128), in0=xT[:, 4 * t:4 * t + 4, k, :],
                                scalar=gf[:, l * 8 + k:l * 8 + k + 1],
                                in1=rstd[:].rearrange("p (j t) -> p j t", t=128), op0=ALU.mult, op1=ALU.mult),
                                reads=[XB[k][t], RS, CONST], writes=[HB[t]])
                        for j in range(4):
                            jj = 4 * t + j
                            for k in range(8):
                                mm(banks[7][:, 320 + jj:320 + jj + 1], sq[:, j, k, :], onesb[:, 0:1],
                                   k == 0, k == 7, [SQ, CONST], [BK[7]], False)
                            for k in range(8):
                                mm(banks[7][:, jj * 20:(jj + 1) * 20], xT[:, jj, k, :], wrp[:, k, :],
                                   k == 0, k == 7, [XB[k][t], WRP], [BK[7]], (k == 7))
                    rstd_from_ss(rt[:], banks[7][:, 320:336], [BK[7], ROUT], [ROUT])
                    dv(lambda e: e.tensor_tensor(out=LGs[:], in0=banks[7][:, 0:320].rearrange("p (j e) -> p j e", e=20),
                                                 in1=b3(rt[:], 20), op=ALU.mult), [BK[7]])
                    dv(lambda e: e.tensor_reduce(out=gmax[:], in_=LGs[:, :, 0:4], axis=AX.X, op=ALU.max))
                    dv(lambda e: e.tensor_tensor(out=goh[:], in0=LGs[:, :, 0:4], in1=b3(gmax[:], 4), op=ALU.is_equal))
                    uniq(goh)
                    dv(lambda e: e.tensor_tensor(out=gex[:], in0=LGs[:, :, 0:4], in1=b3(gmax[:], 4), op=ALU.subtract))
                    T.emit("act", lambda e: e.activation(out=gex[:], in_=gex[:], func=AF.Exp), reads=[ROUT], writes=[ROUT])
                    dv(lambda e: e.tensor_reduce(out=gw[:], in_=gex[:], axis=AX.X, op=ALU.add))
                    dv(lambda e: e.reciprocal(out=gw[:], in_=gw[:]))
                    dv(lambda e: e.tensor_tensor(out=r1[:].rearrange("p j (g i) -> p j g i", i=4),
                                                 in0=LGs[:, :, 4:20].rearrange("p j (g i) -> p j g i", i=4),
                                                 in1=goh[:].unsqueeze(3).broadcast_to([128, 16, 4, 4]), op=ALU.mult))
                    dv(lambda e: e.tensor_reduce(out=esel[:], in_=r1[:].rearrange("p j (g i) -> p j i g", i=4),
                                                 axis=AX.X, op=ALU.add))
                    dv(lambda e: e.tensor_reduce(out=m1[:], in_=esel[:], axis=AX.X, op=ALU.max))
                    dv(lambda e: e.tensor_tensor(out=oh1[:], in0=esel[:], in1=b3(m1[:], 4), op=ALU.is_equal))
                    uniq(oh1)
                    dv(lambda e: e.scalar_tensor_tensor(out=em[:], in0=oh1[:], scalar=-1.0e30, in1=esel[:],
                                                        op0=ALU.mult, op1=ALU.add))
                    dv(lambda e: e.tensor_reduce(out=m2[:], in_=em[:], axis=AX.X, op=ALU.max))
                    dv(lambda e: e.tensor_tensor(out=oh2[:], in0=em[:], in1=b3(m2[:], 4), op=ALU.is_equal))
                    uniq(oh2)
                    dv(lambda e: e.tensor_tensor(out=w2[:], in0=m2[:], in1=m1[:], op=ALU.subtract))
                    T.emit("act", lambda e: e.activation(out=w2[:], in_=w2[:], func=AF.Exp), reads=[ROUT], writes=[ROUT])
                    dv(lambda e: e.tensor_scalar(out=w2[:], in0=w2[:], scalar1=1.0, scalar2=None, op0=ALU.add))
                    dv(lambda e: e.reciprocal(out=w1[:], in_=w2[:]))
                    dv(lambda e: e.tensor_tensor(out=w1[:], in0=w1[:], in1=gw[:], op=ALU.mult))
                    dv(lambda e: e.tensor_tensor(out=w2[:], in0=gw[:], in1=w1[:], op=ALU.subtract))
                    g44 = lambda ap: ap.rearrange("p j (g i) -> p j g i", i=4)
                    dv(lambda e: e.tensor_tensor(out=g44(M1[:]), in0=goh[:].unsqueeze(3).broadcast_to([128, 16, 4, 4]),
                                                 in1=oh1[:].unsqueeze(2).broadcast_to([128, 16, 4, 4]), op=ALU.mult))
                    dv(lambda e: e.tensor_tensor(out=g44(M2[:]), in0=goh[:].unsqueeze(3).broadcast_to([128, 16, 4, 4]),
                                                 in1=oh2[:].unsqueeze(2).broadcast_to([128, 16, 4, 4]), op=ALU.mult))
                    dv(lambda e: e.tensor_tensor(out=Mb[:].rearrange("p (j e) -> p j e", e=16), in0=M1[:], in1=M2[:], op=ALU.add))
                    mm(banks[6][:, 0:256], ustr[:], Mb[:], True, True, [ROUT, CONST], [BK[6]], True)
                    mm(banks[7][:, 0:256], onesb[:], Mb[:], True, True, [ROUT, CONST], [BK[7]], True)
                    dv(lambda e: e.tensor_copy(out=TOT[:].rearrange("p j e -> p (j e)"), in_=banks[7][:, 0:256]), [BK[7]])
                    dv(lambda e: e.memset(JP[:, 0, :], 0.0))
                    for j in range(1, 16):
                        dv(lambda e, j=j: e.tensor_tensor(out=JP[:, j, :], in0=JP[:, j - 1, :], in1=TOT[:, j - 1, :], op=ALU.add))
                    dv(lambda e: e.tensor_tensor(out=ne[:], in0=JP[:, 15, :], in1=TOT[:, 15, :], op=ALU.add))
                    dv(lambda e: e.tensor_scalar(out=ntl[:], in0=ne[:], scalar1=0.0, scalar2=None, op0=ALU.is_gt))
                    for q in range(1, S // TS):
                        dv(lambda e, q=q: e.scalar_tensor_tensor(out=ntl[:], in0=ne[:], scalar=float(TS * q), in1=ntl[:],
                                                                 op0=ALU.is_gt, op1=ALU.add))
                    dv(lambda e: e.memset(base[:, 0:1], 0.0))
                    for e_ in range(1, 16):
                        dv(lambda e, e_=e_: e.tensor_tensor(out=base[:, e_:e_ + 1], in0=base[:, e_ - 1:e_],
                                                            in1=ntl[:, e_ - 1:e_], op=ALU.add))
                    dv(lambda e: e.tensor_tensor(out=bend[:], in0=base[:], in1=ntl[:], op=ALU.add))
                    dv(lambda e: e.tensor_scalar(out=b256[:], in0=base[:], scalar1=float(TS), scalar2=None, op0=ALU.mult))
                    dv(lambda e: e.tensor_tensor(out=SL[:], in0=banks[6][:, 0:256].rearrange("p (j e) -> p j e", e=16),
                                                 in1=JP[:], op=ALU.add), [BK[6]])
                    dv(lambda e: e.tensor_tensor(out=SL[:], in0=SL[:], in1=b256[:].unsqueeze(1).broadcast_to([128, 16, 16]),
                                                 op=ALU.add))
                    dv(lambda e: e.tensor_tensor(out=M1[:], in0=M1[:], in1=SL[:], op=ALU.mult))
                    dv(lambda e: e.tensor_tensor(out=M2[:], in0=M2[:], in1=SL[:], op=ALU.mult))
                    dv(lambda e: e.tensor_reduce(out=sl1[:], in_=M1[:], axis=AX.X, op=ALU.add))
                    dv(lambda e: e.tensor_reduce(out=sl2[:], in_=M2[:], axis=AX.X, op=ALU.add))
                    dv(lambda e: e.tensor_copy(out=s1u[:], in_=sl1[:]))
                    dv(lambda e: e.tensor_copy(out=s2u[:], in_=sl2[:]))
                    dv(lambda e: e.memset(eidx[:], 0.0))
                    for e_ in range(16):
                        dv(lambda e, e_=e_: e.scalar_tensor_tensor(out=eidx[:], in0=iotas[:, 1:1 + NTILE], scalar=bend[:, e_:e_ + 1],
                                                                   in1=eidx[:], op0=ALU.is_ge, op1=ALU.add), [CONST])
                    dv(lambda e: e.tensor_scalar(out=eidx[:], in0=eidx[:], scalar1=15.0, scalar2=None, op0=ALU.min))
                    dv(lambda e: e.tensor_scalar(out=widf[:], in0=eidx[:], scalar1=128.0, scalar2=iotas[:, 0:1],
                                                 op0=ALU.mult, op1=ALU.add), [CONST])
                    dv(lambda e: e.tensor_copy(out=widx[:], in_=widf[:]))
                    load_tile_w(0)
                    load_tile_w(1, which=(0, 1))
                    for j in range(16):
                        hb = j % 2
                        bk = 4 + hb
                        pv = banks[bk][:, :].bitcast(BF16)
                        for k in range(8):
                            T.emit("pe", lambda e, pv=pv, k=k, j=j: e.transpose(
                                out=pv[:, k * 128:(k + 1) * 128], in_=H[:, k, j * 128:(j + 1) * 128], identity=identb[:]),
                                reads=[HB[j // 4], CONST], writes=[BK[bk]], sig=(k == 7))
                        evac_copy(hrow[:, hb, :], pv, [BK[bk]], [HROW[hb]])
                        T.idma(HS_d, s1u[:, j:j + 1], hrow[:, hb, :], None, reads=[HROW[hb], ROUT], writes=[HSB[2 * j]])
                        T.idma(HS_d, s2u[:, j:j + 1], hrow[:, hb, :], None, reads=[HROW[hb], ROUT], writes=[HSB[2 * j + 1]])
                    T.barrier()

                with ExitStack() as pbx:
                    hs_sb = sb(pbx, "hs_sb", [128, 3, 2, D], BF16)
                    hcT = sb(pbx, "hcT", [128, 2, 8, TS], BF16)
                    a_e = sb(pbx, "a_e", [128, 2, 4, TS], BF16)
                    sg = sb(pbx, "sg", [128, 2, TS], F32)
                    ys = sb(pbx, "ys", [128, 2, 2, D], F32)
                    HSS = [Buf("hss0"), Buf("hss1"), Buf("hss2")]
                    HCT = [Buf("hct0"), Buf("hct1")]
                    AE = [[Buf(f"ae{b}_{f}") for f in range(4)] for b in range(2)]
                    SG = [Buf("sg0"), Buf("sg1")]
                    YS = [Buf("ysb0"), Buf("ysb1")]

                    def emit_hs(i):
                        hb3 = i % 3
                        T.dma("sp", hs_sb[:, hb3], HS_d[i * TS:(i + 1) * TS, :].rearrange("(a p) d -> p a d", p=128),
                              reads=HSB, writes=[HSS[hb3]])

                    def emit_tr(i):
                        ab = i % 2
                        hb3 = i % 3
                        for hb in range(2):
                            bk = 6 + hb
                            pv = banks[bk][:, :].bitcast(BF16)
                            for kk in range(4):
                                k = hb * 4 + kk
                                for a in range(2):
                                    T.emit("pe", lambda e, pv=pv, kk=kk, a=a, k=k, hb3=hb3: e.transpose(
                                        out=pv[:, kk * TS + a * 128:kk * TS + (a + 1) * 128],
                                        in_=hs_sb[:, hb3, a, k * 128:(k + 1) * 128], identity=identb[:]),
                                        reads=[HSS[hb3], CONST], writes=[BK[bk]], sig=(kk == 3 and a == 1))
                            evac_copy(hcT[:, ab, hb * 4:(hb + 1) * 4, :].rearrange("p k t -> p (k t)"), pv, [BK[bk]], [HCT[ab]])

                    def emit_gu(i):
                        ab = i % 2
                        sg_, su_ = (3 * i) % 6, (3 * i + 1) % 6
                        for f in range(4):
                            bg, bu = f % 2, 2 + f % 2
                            for k in range(8):
                                mm(banks[bg][:, 0:TS], ring[:, sg_, k * 512 + f * 128:k * 512 + (f + 1) * 128], hcT[:, ab, k, :],
                                   k == 0, k == 7, [RSLOT[sg_], HCT[ab]], [BK[bg]], k == 7)
                            for k in range(8):
                                mm(banks[bu][:, 0:TS], ring[:, su_, k * 512 + f * 128:k * 512 + (f + 1) * 128], hcT[:, ab, k, :],
                                   k == 0, k == 7, [RSLOT[su_], HCT[ab]], [BK[bu]], k == 7)
                            i2 = f % 2
                            T.emit("act", lambda e, bg=bg, i2=i2: e.activation(out=sg[:, i2, :], in_=banks[bg][:, 0:TS], func=AF.Silu),
                                   reads=[BK[bg]], writes=[SG[i2]])
                            T.emit("dve", lambda e, bu=bu, i2=i2, ab=ab, f=f: e.tensor_tensor(
                                out=a_e[:, ab, f, :], in0=banks[bu][:, 0:TS], in1=sg[:, i2, :], op=ALU.mult),
                                reads=[BK[bu], SG[i2]], writes=[AE[ab][f]])

                    def emit_d(i):
                        ab = i % 2
                        sd_ = (3 * i + 2) % 6
                        for a in range(2):
                            for dh in range(2):
                                bk = 4 + dh
                                for f in range(4):
                                    mm(banks[bk][:, :], a_e[:, ab, f, a * 128:(a + 1) * 128],
                                       ring[:, sd_, f * 1024 + dh * 512:f * 1024 + (dh + 1) * 512],
                                       f == 0, f == 3, [RSLOT[sd_], AE[ab][f]], [BK[bk]], f == 3)
                                evac_copy(ys[:, ab, a, dh * 512:(dh + 1) * 512], banks[bk][:, :], [BK[bk]], [YS[ab]])
                        T.dma("sp", YS_d[i * TS:(i + 1) * TS, :].rearrange("(a p) d -> p a d", p=128), ys[:, ab],
                              reads=[YS[ab]], writes=[YSB[i]])

                    emit_hs(0)
                    emit_hs(1)
                    emit_tr(0)
                    for i in range(NTILE):
                        if i + 2 < NTILE:
                            emit_hs(i + 2)
                        if i + 1 < NTILE:
                            emit_tr(i + 1)
                        emit_gu(i)
                        if i + 2 < NTILE:
                            load_tile_w(i + 2, which=(0, 1))
                        if i > 0:
                            emit_d(i - 1)
                        if i + 1 < NTILE:
                            load_tile_w(i + 1, which=(2,))
                    emit_d(NTILE - 1)
                    T.barrier()

                with ExitStack() as pcx:
                    g0 = sb(pcx, "g0", [128, 4, D], F32)
                    g1 = sb(pcx, "g1", [128, 4, D], F32)
                    G0 = [Buf(f"g0{i}") for i in range(4)]
                    G1 = [Buf(f"g1{i}") for i in range(4)]
                    for j in range(16):
                        b = j % 4
                        T.idma(g0[:, b, :], None, YS_d, s1u[:, j:j + 1], reads=YSB + [ROUT], writes=[G0[b]])
                        T.idma(g1[:, b, :], None, YS_d, s2u[:, j:j + 1], reads=YSB + [ROUT], writes=[G1[b]])
                        T.emit("dve", lambda e, b=b, j=j: e.tensor_scalar(out=g0[:, b, :], in0=g0[:, b, :], scalar1=w1[:, j:j + 1],
                                                                          scalar2=None, op0=ALU.mult), reads=[G0[b], ROUT], writes=[G0[b]])
                        T.emit("dve", lambda e, b=b, j=j: e.scalar_tensor_tensor(out=g0[:, b, :], in0=g1[:, b, :], scalar=w2[:, j:j + 1],
                                                                                 in1=g0[:, b, :], op0=ALU.mult, op1=ALU.add),
                               reads=[G0[b], G1[b], ROUT], writes=[G0[b]])
                        for hb in range(2):
                            bk = 2 * (j % 2) + hb
                            for kk in range(4):
                                k = hb * 4 + kk
                                T.emit("pe", lambda e, bk=bk, kk=kk, k=k, b=b: e.transpose(
                                    out=banks[bk][:, kk * 128:(kk + 1) * 128], in_=g0[:, b, k * 128:(k + 1) * 128],
                                    identity=identf[:]), reads=[G0[b], CONST], writes=[BK[bk]], sig=(kk == 3))
                            T.emit("dve", lambda e, bk=bk, hb=hb, j=j: e.tensor_tensor(
                                out=xT[:, j, hb * 4:(hb + 1) * 4, :],
                                in0=banks[bk][:, :].rearrange("p (k t) -> p k t", t=128),
                                in1=xT[:, j, hb * 4:(hb + 1) * 4, :], op=ALU.add),
                                reads=[BK[bk]] + [XB[hb * 4 + kk][j // 4] for kk in range(4)],
                                writes=[XB[hb * 4 + kk][j // 4] for kk in range(4)])

                if not last:
                    for t in range(4):
                        tl = tok0 // 128 + 4 * t
                        T.dma("sp", xT_d[tl:tl + 4].rearrange("j p k t -> p j k t"), xT[:, 4 * t:4 * t + 4],
                              reads=[XB[k][t] for k in range(8)], writes=[XD[s][t]])
                else:
                    T.barrier()
                    final_out(xT, XB, s)
                T.barrier()

        def f_phase(l, s, last):
            tok0 = s * S
            with ExitStack() as pf:
                xT = sb(pf, "xT", [128, 8, S], F32)
                H = sb(pf, "Hf", [128, 8, S], BF16)
                ring = sb(pf, "ring", [128, 6, 4096], BF16)
                sq = sb(pf, "sqf", [128, 8, 512], BF16)
                rstd = sb(pf, "rstdf", [128, 512], F32)
                wr_sb = sb(pf, "wr_sb", [128, 8, 20], F32)
                wrp = sb(pf, "wrp", [128, 8, 20], F32)
                a_e = sb(pf, "a_e", [128, 2, 4, 512], BF16)
                sg = sb(pf, "sg", [128, 2, 512], F32)
                tt = sb(pf, "tt", [128, 2, 512], F32)
                Cb = sb(pf, "Cb", [128, 2, 512], F32)
                rt = sb(pf, "rt", [128, 16], F32)
                LGs = sb(pf, "LGs", [128, 16, 20], F32)
                r1 = sb(pf, "r1", [128, 16, 16], F32)
                r2 = sb(pf, "r2", [128, 16, 16], F32)
                gmax = sb(pf, "gmax", [128, 16], F32)
                goh = sb(pf, "goh", [128, 16, 4], F32)
                gex = sb(pf, "gex", [128, 16, 4], F32)
                gw = sb(pf, "gw", [128, 16], F32)
                esel = sb(pf, "esel", [128, 16, 4], F32)
                em = sb(pf, "em", [128, 16, 4], F32)
                m1 = sb(pf, "m1", [128, 16], F32)
                m2 = sb(pf, "m2", [128, 16], F32)
                oh1 = sb(pf, "oh1", [128, 16, 4], F32)
                oh2 = sb(pf, "oh2", [128, 16, 4], F32)
                w1 = sb(pf, "w1", [128, 16], F32)
                w2 = sb(pf, "w2", [128, 16], F32)
                c4 = sb(pf, "c4", [128, 16, 4], F32)
                Cm = sb(pf, "Cm", [128, 16, 16], F32)
                Chi = sb(pf, "Chi", [128, 16, 16], BF16)
                Clo = sb(pf, "Clo", [128, 16, 16], BF16)
                XB = [[Buf(f"X{k}_{t}") for t in range(4)] for k in range(8)]
                HB = [Buf(f"Hf{t}") for t in range(4)]
                RSLOT = [Buf(f"ring{i}") for i in range(6)]
                SQ, RS, WR, WRP, RT = Buf("sq"), Buf("rs"), Buf("wr"), Buf("wrp"), Buf("rt")
                ROUT = Buf("router")
                AE = [[Buf(f"ae{b}_{f}") for f in range(4)] for b in range(2)]
                SG = [Buf(f"sg{i}") for i in range(2)]
                TT = [Buf(f"tt{i}") for i in range(2)]
                CB = [Buf("cb0"), Buf("cb1")]

                def load_expert(e):
                    e4 = e // 4
                    for j, (nm, wb) in enumerate((("g", w_gate_b), ("u", w_up_b))):
                        slot = (3 * e + j) % 6
                        T.dma("sp", ring[:, slot, :].rearrange("p (k f) -> p k f", f=512),
                              wb[l, e].rearrange("(k p) f -> p k f", p=128), reads=[WB[(nm, l, e4)]], writes=[RSLOT[slot]])
                    slot = (3 * e + 2) % 6
                    T.dma("sp", ring[:, slot, :].rearrange("p (k f) -> p k f", f=1024),
                          w_down_b[l, e].rearrange("(k p) f -> p k f", p=128), reads=[WB[("d", l, e4)]], writes=[RSLOT[slot]])

                for k in range(8):
                    T.dma("sp", xT[:, k, :], xT_d[k, :, tok0:tok0 + S], reads=[XD[s][t] for t in range(4)],
                          writes=[XB[k][t] for t in range(4)])
                T.dma("sp", wr_sb[:], wr_d[:, l], writes=[WR])
                load_expert(0)
                for k in range(8):
                    T.emit("dve", lambda e, k=k: e.tensor_scalar(out=wrp[:, k, :], in0=wr_sb[:, k, :],
                                                                 scalar1=gf[:, l * 8 + k:l * 8 + k + 1], scalar2=None,
                                                                 op0=ALU.mult), reads=[WR, CONST], writes=[WRP])
                for t in range(4):
                    c0 = t * 512
                    T.emit("act", lambda e, c0=c0: e.activation(out=sq[:], in_=xT[:, :, c0:c0 + 512], func=AF.Square),
                           reads=[XB[k][t] for k in range(8)], writes=[SQ])
                    for k in range(8):
                        mm(banks[6][:, :], onesb[:], sq[:, k, :], k == 0, k == 7, [SQ, CONST], [BK[6]], k == 7)
                    rstd_from_ss(rstd[:], banks[6][:, :], [BK[6]], [RS])
                    for k in range(8):
                        T.emit("dve", lambda e, k=k, c0=c0: e.scalar_tensor_tensor(
                            out=H[:, k, c0:c0 + 512], in0=xT[:, k, c0:c0 + 512], scalar=gf[:, l * 8 + k:l * 8 + k + 1],
                            in1=rstd[:], op0=ALU.mult, op1=ALU.mult), reads=[XB[k][t], RS, CONST], writes=[HB[t]])
                    for j in range(4):
                        jj = 4 * t + j
                        for k in range(8):
                            mm(banks[7][:, 320 + jj:320 + jj + 1], sq[:, k, j * 128:(j + 1) * 128], onesb[:, 0:1],
                               k == 0, k == 7, [SQ, CONST], [BK[7]], False)
                        for k in range(8):
                            mm(banks[7][:, jj * 20:(jj + 1) * 20], xT[:, k, c0 + j * 128:c0 + (j + 1) * 128], wrp[:, k, :],
                               k == 0, k == 7, [XB[k][t], WRP], [BK[7]], (k == 7))
                R_ = [ROUT]

                def dv(fn, extra_reads=()):
                    T.emit("dve", fn, reads=[ROUT] + list(extra_reads), writes=[ROUT])

                def b3(ap2, n):
                    return ap2.unsqueeze(2).broadcast_to([128, 16, n])

                rstd_from_ss(rt[:], banks[7][:, 320:336], [BK[7], ROUT], [ROUT])
                dv(lambda e: e.tensor_tensor(out=LGs[:], in0=banks[7][:, 0:320].rearrange("p (j e) -> p j e", e=20),
                                             in1=b3(rt[:], 20), op=ALU.mult), [BK[7]])
                dv(lambda e: e.tensor_reduce(out=gmax[:], in_=LGs[:, :, 0:4], axis=AX.X, op=ALU.max))
                dv(lambda e: e.tensor_tensor(out=goh[:], in0=LGs[:, :, 0:4], in1=b3(gmax[:], 4), op=ALU.is_equal))
                dv(lambda e: e.tensor_tensor(out=gex[:], in0=LGs[:, :, 0:4], in1=b3(gmax[:], 4), op=ALU.subtract))
                T.emit("act", lambda e: e.activation(out=gex[:], in_=gex[:], func=AF.Exp), reads=[ROUT], writes=[ROUT])
                dv(lambda e: e.tensor_reduce(out=gw[:], in_=gex[:], axis=AX.X, op=ALU.add))
                dv(lambda e: e.reciprocal(out=gw[:], in_=gw[:]))
                dv(lambda e: e.tensor_tensor(out=r1[:].rearrange("p j (g i) -> p j g i", i=4),
                                             in0=LGs[:, :, 4:20].rearrange("p j (g i) -> p j g i", i=4),
                                             in1=goh[:].unsqueeze(3).broadcast_to([128, 16, 4, 4]), op=ALU.mult))
                dv(lambda e: e.tensor_reduce(out=esel[:], in_=r1[:].rearrange("p j (g i) -> p j i g", i=4),
                                             axis=AX.X, op=ALU.add))
                dv(lambda e: e.tensor_reduce(out=m1[:], in_=esel[:], axis=AX.X, op=ALU.max))
                dv(lambda e: e.tensor_tensor(out=oh1[:], in0=esel[:], in1=b3(m1[:], 4), op=ALU.is_equal))
                dv(lambda e: e.scalar_tensor_tensor(out=em[:], in0=oh1[:], scalar=-1.0e30, in1=esel[:],
                                                    op0=ALU.mult, op1=ALU.add))
                dv(lambda e: e.tensor_reduce(out=m2[:], in_=em[:], axis=AX.X, op=ALU.max))
                dv(lambda e: e.tensor_tensor(out=oh2[:], in0=em[:], in1=b3(m2[:], 4), op=ALU.is_equal))
                dv(lambda e: e.tensor_tensor(out=w2[:], in0=m2[:], in1=m1[:], op=ALU.subtract))
                T.emit("act", lambda e: e.activation(out=w2[:], in_=w2[:], func=AF.Exp), reads=[ROUT], writes=[ROUT])
                dv(lambda e: e.tensor_scalar(out=w2[:], in0=w2[:], scalar1=1.0, scalar2=None, op0=ALU.add))
                dv(lambda e: e.reciprocal(out=w1[:], in_=w2[:]))
                dv(lambda e: e.tensor_tensor(out=w1[:], in0=w1[:], in1=gw[:], op=ALU.mult))
                dv(lambda e: e.tensor_tensor(out=w2[:], in0=gw[:], in1=w1[:], op=ALU.subtract))
                dv(lambda e: e.tensor_tensor(out=c4[:], in0=oh1[:], in1=b3(w1[:], 4), op=ALU.mult))
                dv(lambda e: e.tensor_tensor(out=oh2[:], in0=oh2[:], in1=b3(w2[:], 4), op=ALU.mult))
                dv(lambda e: e.tensor_tensor(out=c4[:], in0=c4[:], in1=oh2[:], op=ALU.add))
                dv(lambda e: e.tensor_tensor(out=Cm[:].rearrange("p j (g i) -> p j g i", i=4),
                                             in0=goh[:].unsqueeze(3).broadcast_to([128, 16, 4, 4]),
                                             in1=c4[:].unsqueeze(2).broadcast_to([128, 16, 4, 4]), op=ALU.mult))
                dv(lambda e: e.tensor_copy(out=Chi[:], in_=Cm[:]))
                dv(lambda e: e.tensor_tensor(out=r2[:], in0=Cm[:], in1=Chi[:], op=ALU.subtract))
                dv(lambda e: e.tensor_copy(out=Clo[:], in_=r2[:]))

                steps = [(e_, t_) for e_ in range(NE) for t_ in range(4)]

                def emit_gu(idx):
                    e_, t = steps[idx]
                    ab = idx % 2
                    c0 = t * 512
                    sg_, su_, sd_ = (3 * e_) % 6, (3 * e_ + 1) % 6, (3 * e_ + 2) % 6
                    n = 0
                    for j in range(4):
                        for Cx in (Chi, Clo):
                            mm(banks[6][:, j * 128:(j + 1) * 128], Cx[:, 4 * t + j, e_:e_ + 1].broadcast_to([128, 128]),
                               identb[:], Cx is Chi, Cx is Clo, [ROUT, CONST], [BK[6]], (j == 3 and Cx is Clo))
                    T.emit("act", lambda e, ab=ab: e.activation(out=Cb[:, ab, :], in_=banks[6][:, :], func=AF.Copy),
                           reads=[BK[6]], writes=[CB[ab]])
                    for f in range(4):
                        bg, bu = f % 2, 2 + f % 2
                        for k in range(8):
                            mm(banks[bg][:, :], ring[:, sg_, k * 512 + f * 128:k * 512 + (f + 1) * 128], H[:, k, c0:c0 + 512],
                               k == 0, k == 7, [RSLOT[sg_], HB[t]], [BK[bg]], k == 7)
                        for k in range(8):
                            mm(banks[bu][:, :], ring[:, su_, k * 512 + f * 128:k * 512 + (f + 1) * 128], H[:, k, c0:c0 + 512],
                               k == 0, k == 7, [RSLOT[su_], HB[t]], [BK[bu]], k == 7)
                        i3 = f % 2
                        T.emit("act", lambda e, bg=bg, i3=i3: e.activation(out=sg[:, i3, :], in_=banks[bg][:, :], func=AF.Silu),
                               reads=[BK[bg]], writes=[SG[i3]])
                        T.emit("dve", lambda e, bu=bu, i3=i3: e.tensor_tensor(out=tt[:, i3, :], in0=banks[bu][:, :],
                                                                            in1=sg[:, i3, :], op=ALU.mult),
                               reads=[BK[bu], SG[i3]], writes=[TT[i3]])
                        T.emit("dve", lambda e, ab=ab, f=f, i3=i3: e.tensor_tensor(out=a_e[:, ab, f, :], in0=tt[:, i3, :],
                                                                                   in1=Cb[:, ab, :], op=ALU.mult),
                               reads=[TT[i3], CB[ab]], writes=[AE[ab][f]])

                def emit_d(idx):
                    e_, t = steps[idx]
                    ab = idx % 2
                    c0 = t * 512
                    sd_ = (3 * e_ + 2) % 6
                    for m in range(8):
                        bk = 4 + m % 2
                        for f in range(4):
                            mm(banks[bk][:, :], ring[:, sd_, f * 1024 + m * 128:f * 1024 + (m + 1) * 128], a_e[:, ab, f, :],
                               f == 0, f == 3, [RSLOT[sd_], AE[ab][f]], [BK[bk]], f == 3)
                        T.emit("dve", lambda e, m=m, bk=bk, c0=c0: e.tensor_tensor(
                            out=xT[:, m, c0:c0 + 512], in0=banks[bk][:, :], in1=xT[:, m, c0:c0 + 512], op=ALU.add),
                            reads=[BK[bk], XB[m][t]], writes=[XB[m][t]])

                for idx in range(len(steps)):
                    e_, t = steps[idx]
                    emit_gu(idx)
                    if idx > 0:
                        emit_d(idx - 1)
                    if t == 0 and e_ + 1 < NE:
                        load_expert(e_ + 1)
                emit_d(len(steps) - 1)

                if not last:
                    for k in range(8):
                        T.dma("sp", xT_d[k, :, tok0:tok0 + S], xT[:, k, :], reads=[XB[k][t] for t in range(4)],
                              writes=[XD[s][t] for t in range(4)])
                else:
                    final_out(xT, XB, s)
                T.barrier()

        done = False
        for l in range(depth):
            for s in range(nseq):
                m_phase(l, s)
            if stop_after == ("M", l):
                done = True
                break
            for s in range(nseq):
                (f_phase_sparse if SPARSE else f_phase)(l, s, last=(l == depth - 1 and stop_after is None))
            if stop_after == ("F", l):
                done = True
                break
        T.barrier()
        T.flush()
    return nc


def _bias_index_map():
    hp = np.arange(4)[:, None, None, None, None]
    p = np.arange(128)[None, :, None, None, None]
    eo = np.arange(2)[None, None, :, None, None]
    i = np.arange(14)[None, None, None, :, None]
    cq = np.arange(64)[None, None, None, None, :]
    kc = p % 64
    half = p // 64
    h = 2 * hp + eo
    dr = i + half
    dc = np.clip(kc - cq, -15, 15) + 15
    qs = np.clip(cq - 8, 0, 48)
    valid = (kc >= qs) & (kc < qs + 16)
    flat = h * (15 * 31 + 1) + np.where(valid, dr * 31 + dc, 15 * 31)
    return np.broadcast_to(flat, (4, 128, 2, 14, 64)).copy()


def _pool_consts():
    import ml_dtypes
    out = np.zeros((4, 7, 128, 128), np.float32)
    Sr = 512
    t = np.arange(Sr)
    for g, w in enumerate(POOL_W):
        lo = np.clip(t - w // 2, 0, Sr)
        hi = np.clip(t - w // 2 + w, 0, Sr)
        cnt = (hi - lo).astype(np.float64)
        A = np.zeros((Sr, Sr), np.float64)
        for tt_ in range(Sr):
            A[lo[tt_]:hi[tt_], tt_] = 1.0 / cnt[tt_]
        A -= np.eye(Sr)

        def blk(a, b):
            return A[a * 128:(a + 1) * 128, b * 128:(b + 1) * 128]

        def hilo(M):
            hi_ = M.astype(np.float32).astype(ml_dtypes.bfloat16).astype(np.float64)
            lo_ = (M - hi_).astype(np.float32).astype(ml_dtypes.bfloat16).astype(np.float64)
            return hi_, lo_

        out[g, 0] = blk(1, 2)
        out[g, 1] = blk(2, 2)
        out[g, 2] = blk(3, 2)
        out[g, 3], out[g, 4] = hilo(blk(0, 0))
        out[g, 5], out[g, 6] = hilo(blk(3, 3))
    return np.ascontiguousarray(out.reshape(28, 128, 128).transpose(1, 0, 2))


_BIAS_MAP = None
_PROG = {}


def prep_inputs(inp, nseq=NSEQ_FULL, ncores=NCORES):
    global _BIAS_MAP
    f = lambda a: np.ascontiguousarray(np.asarray(a, dtype=np.float32))
    Lw = L_FULL
    if _BIAS_MAP is None:
        _BIAS_MAP = _bias_index_map()
    rpb = f(inp["rpb"])
    pad = np.concatenate([rpb.reshape(Lw, 8, 15 * 31), np.full((Lw, 8, 1), NEG, np.float32)], axis=2).reshape(Lw, -1)
    biasT = pad[:, _BIAS_MAP]
    biasT = np.ascontiguousarray(biasT.reshape(Lw, 4, 128, 2 * 14 * 64))
    shared = {
        "w_in": f(inp["w_in"]), "w_out": f(inp["w_out"]), "pool_w": f(inp["pool_w"]),
        "w_gate": f(inp["w_gate"]), "w_up": f(inp["w_up"]), "w_down": f(inp["w_down"]),
        "gm": np.ascontiguousarray(f(inp["norm_mix_g"]).reshape(Lw, 8, 128).transpose(2, 0, 1).reshape(128, Lw * 8)),
        "gf": np.ascontiguousarray(f(inp["norm_ffn_g"]).reshape(Lw, 8, 128).transpose(2, 0, 1).reshape(128, Lw * 8)),
        "ps": np.ascontiguousarray(f(inp["pool_scale"]).reshape(Lw, 4, 128).transpose(2, 0, 1).reshape(128, Lw * 4)),
        "gF": np.ascontiguousarray(np.broadcast_to(f(inp["final_g"])[None, :], (128, D))),
        "wr": np.ascontiguousarray(np.concatenate([f(inp["w_router_group"]), f(inp["w_router_expert"])], axis=-1)
                                   .reshape(Lw, 8, 128, 20).transpose(2, 0, 1, 3)),
        "biasT": biasT,
        "poolA": _pool_consts(),
        "ident": np.eye(128, dtype=np.float32),
        "ustrict": np.triu(np.ones((128, 128), np.float32), k=1),
        "iotas": np.ascontiguousarray(np.concatenate([np.arange(128, dtype=np.float32)[:, None],
                                                      np.broadcast_to(np.arange(32, dtype=np.float32)[None, :], (128, 32))], axis=1)),
    }
    x = f(inp["x"]).reshape(-1, S, D)
    maps = []
    for c in range(ncores):
        m = dict(shared)
        m["x"] = np.ascontiguousarray(x[c * nseq:(c + 1) * nseq].reshape(nseq * S, D))
        maps.append(m)
    return maps


def kernel(x, norm_mix_g, w_in, rpb, pool_w, pool_scale, w_out, norm_ffn_g, w_router_group, w_router_expert,
           w_gate, w_up, w_down, final_g):
    inp = dict(x=x, norm_mix_g=norm_mix_g, w_in=w_in, rpb=rpb, pool_w=pool_w, pool_scale=pool_scale, w_out=w_out,
               norm_ffn_g=norm_ffn_g, w_router_group=w_router_group, w_router_expert=w_router_expert,
               w_gate=w_gate, w_up=w_up, w_down=w_down, final_g=final_g)
    maps = prep_inputs(inp)
    if "full" not in _PROG:
        _PROG["full"] = build_program()
    nc = _PROG["full"]
    res = run_bass_kernel_spmd(nc, maps, core_ids=list(range(NCORES)))
    outs = [np.asarray(r["out"], dtype=np.float32).reshape(NSEQ_FULL, S, D) for r in res.results]
    return np.concatenate(outs, axis=0)
```

```python
import numpy as np
from contextlib import ExitStack
import concourse.bass as bass
import concourse.mybir as mybir
from concourse.bass_utils import run_bass_kernel_spmd

F32 = mybir.dt.float32
BF16 = mybir.dt.bfloat16
AF = mybir.ActivationFunctionType
ALU = mybir.AluOpType
AX = mybir.AxisListType

D = 1024
S = 2048
L_FULL = 4
NSEQ_FULL = 4
NCORES = 8
NE = 16
DE = 512
EPS = 1e-6
NEG = -30000.0
POOL_W = (2, 4, 8, 16)


class Buf:
    __slots__ = ("name", "w", "r")

    def __init__(self, name):
        self.name = name
        self.w = None
        self.r = {}


class _Eng:
    def __init__(self, name):
        self.name = name
        self.q = []
        self.known = {}
        self.count = 0
        self.psem = None


class Tracker:
    ENGS = ("pe", "act", "dve", "pool", "sp")

    def __init__(self, nc, stack, n_dma_sems=24):
        self.nc = nc
        self.sems = []
        self.eng = {n: _Eng(n) for n in self.ENGS}
        for n in ("pe", "act", "dve", "pool"):
            self.eng[n].psem = self._new_sem(stack, "p_" + n)
        self.dpool = [self._new_sem(stack, f"dq{i}") for i in range(n_dma_sems)]
        self.dcnt = [0] * n_dma_sems
        self.dnext = 0
        self.stack = stack
        self.extra = []

    def _new_sem(self, stack, name):
        h = stack.enter_context(self.nc.semaphore(name))
        self.sems.append(h)
        return len(self.sems) - 1

    def _deps(self, E, reads, writes, extra=()):
        need = {}

        def req(ev):
            if ev is None:
                return
            s, v = ev
            if s == E.psem and E.name == "pe":
                return
            if E.known.get(s, 0) >= v:
                return
            if need.get(s, 0) < v:
                need[s] = v

        for ev in extra:
            req(ev)
        for b in reads:
            req(b.w)
        for b in writes:
            req(b.w)
            for s, v in b.r.items():
                req((s, v))
        for s, v in need.items():
            E.q.append(("wait", s, v))
            E.known[s] = v

    def _mark(self, ev, reads, writes):
        for b in reads:
            if b.r.get(ev[0], 0) < ev[1]:
                b.r[ev[0]] = ev[1]
        for b in writes:
            b.w = ev
            b.r = {}

    def emit(self, eng, fn, reads=(), writes=(), sig=True):
        E = self.eng[eng]
        self._deps(E, reads, writes)
        if sig:
            E.count += 1
            ev = (E.psem, E.count)
        else:
            ev = (E.psem, E.count + 1)
        E.q.append(("op", fn, sig))
        self._mark(ev, reads, writes)
        return ev

    def dma(self, q, out, in_, reads=(), writes=(), own_sem=False):
        E = self.eng[q]
        if own_sem:
            s = self._new_sem(self.stack, f"ds{len(self.sems)}")
            self.extra.append(s)
            prev = 0
            self._deps(E, reads, writes)
            ev = (s, 16)
        else:
            i = self.dnext
            self.dnext = (i + 1) % len(self.dpool)
            s = self.dpool[i]
            prev = self.dcnt[i]
            self._deps(E, reads, writes, extra=[(s, prev)] if prev else ())
            self.dcnt[i] += 16
            ev = (s, self.dcnt[i])
        E.q.append(("dma", out, in_, s))
        self._mark(ev, reads, writes)
        return ev

    def idma(self, out, out_idx, in_, in_idx, reads=(), writes=(), bounds=None):
        E = self.eng["pool"]
        i = self.dnext
        self.dnext = (i + 1) % len(self.dpool)
        s = self.dpool[i]
        prev = self.dcnt[i]
        self._deps(E, reads, writes, extra=[(s, prev)] if prev else ())
        self.dcnt[i] += 16
        ev = (s, self.dcnt[i])
        E.q.append(("idma", out, out_idx, in_, in_idx, s, bounds))
        self._mark(ev, reads, writes)
        return ev

    def barrier(self):
        evs = []
        for n in ("pe", "act", "dve", "pool"):
            e = self.eng[n]
            if e.count:
                evs.append((e.psem, e.count))
        for i, s in enumerate(self.dpool):
            if self.dcnt[i]:
                evs.append((s, self.dcnt[i]))
        for s in self.extra:
            evs.append((s, 16))
        for fn in getattr(self, "extra_ev_fns", []):
            evs.extend(fn())
        for n in self.ENGS:
            E = self.eng[n]
            for s, v in evs:
                if s == E.psem:
                    continue
                if E.known.get(s, 0) < v:
                    E.q.append(("wait", s, v))
                    E.known[s] = v

    def flush(self):
        nc = self.nc
        sems = self.sems

        def run(E, h):
            psem = sems[E.psem] if E.psem is not None else None
            for it in E.q:
                if it[0] == "wait":
                    h.wait_ge(sems[it[1]], it[2])
                elif it[0] == "op":
                    ins = it[1](h)
                    if it[2]:
                        ins.then_inc(psem, 1)
                elif it[0] == "idma":
                    oo = bass.IndirectOffsetOnAxis(ap=it[2], axis=0) if it[2] is not None else None
                    io = bass.IndirectOffsetOnAxis(ap=it[4], axis=0) if it[4] is not None else None
                    if it[6] is None:
                        h.indirect_dma_start(out=it[1], out_offset=oo, in_=it[3], in_offset=io).then_inc(sems[it[5]], 16)
                    else:
                        h.indirect_dma_start(out=it[1], out_offset=oo, in_=it[3], in_offset=io, bounds_check=it[6],
                                             oob_is_err=False).then_inc(sems[it[5]], 16)
                else:
                    h.dma_start(out=it[1], in_=it[2]).then_inc(sems[it[3]], 16)

        with nc.Block() as block:
            @block.tensor
            def _(h):
                run(self.eng["pe"], h)

            @block.scalar
            def _(h):
                run(self.eng["act"], h)

            @block.vector
            def _(h):
                run(self.eng["dve"], h)

            @block.gpsimd
            def _(h):
                run(self.eng["pool"], h)

            @block.sync
            def _(h):
                run(self.eng["sp"], h)


SPARSE = True
INTERLEAVE_OUT = True
POOL_FROM_LAYER = 99
TS = 256
NTILE = 32


def build_program(nseq=NSEQ_FULL, depth=L_FULL, stop_after=None, debug_out=False):
    nc = bass.Bass("TRN2", target_bir_lowering=False)
    NT = nseq * S
    Lw = L_FULL

    def din(name, shape, dt=F32):
        return nc.dram_tensor(name, list(shape), dt, kind="ExternalInput").ap()

    x_c = din("x", [NT, D])
    w_in = din("w_in", [Lw, D, 2048])
    w_out = din("w_out", [Lw, D, D])
    pool_w = din("pool_w", [Lw, 4, 128, 128])
    w_gate = din("w_gate", [Lw, NE, D, DE])
    w_up = din("w_up", [Lw, NE, D, DE])
    w_down = din("w_down", [Lw, NE, DE, D])
    gm_d = din("gm", [128, Lw * 8])
    gf_d = din("gf", [128, Lw * 8])
    ps_d = din("ps", [128, Lw * 4])
    gF_d = din("gF", [128, D])
    wr_d = din("wr", [128, Lw, 8, 20])
    bias_d = din("biasT", [Lw, 4, 128, 2 * 14 * 64])
    poolA_d = din("poolA", [128, 28, 128])
    ident_d = din("ident", [128, 128])
    ustrict_d = din("ustrict", [128, 128])
    iotas_d = din("iotas", [128, 33])
    if stop_after is None:
        out_c = nc.dram_tensor("out", [NT, D], F32, kind="ExternalOutput").ap()
        xT_d = nc.dram_tensor("xT_d", [NT // 128, 128, 8, 128], F32, kind="Internal").ap()
    else:
        xT_d = nc.dram_tensor("xT_out", [NT // 128, 128, 8, 128], F32, kind="ExternalOutput").ap()
        out_c = None
    w_in_b = nc.dram_tensor("w_in_b", [Lw, D, 2048], BF16, kind="Internal").ap()
    w_out_b = nc.dram_tensor("w_out_b", [Lw, D, D], BF16, kind="Internal").ap()
    pool_w_b = nc.dram_tensor("pool_w_b", [Lw, 4, 128, 128], BF16, kind="Internal").ap()
    w_gate_b = nc.dram_tensor("w_gate_b", [Lw, NE, D, DE], BF16, kind="Internal").ap()
    w_up_b = nc.dram_tensor("w_up_b", [Lw, NE, D, DE], BF16, kind="Internal").ap()
    w_down_b = nc.dram_tensor("w_down_b", [Lw, NE, DE, D], BF16, kind="Internal").ap()
    w_gate_p = [nc.dram_tensor(f"w_gate_p{i}", [NE * 128, 4096], BF16, kind="Internal").ap() for i in range(Lw)]
    w_up_p = [nc.dram_tensor(f"w_up_p{i}", [NE * 128, 4096], BF16, kind="Internal").ap() for i in range(Lw)]
    w_down_p = [nc.dram_tensor(f"w_down_p{i}", [NE * 128, 4096], BF16, kind="Internal").ap() for i in range(Lw)]
    HS_d = nc.dram_tensor("HS_d", [NTILE * TS, D], BF16, kind="Internal").ap()
    YS_d = nc.dram_tensor("YS_d", [NTILE * TS, D], F32, kind="Internal").ap()

    top = ExitStack()
    with top:
        T = Tracker(nc, top)

        uid = [0]

        def sb(stack, name, shape, dt):
            uid[0] += 1
            return stack.enter_context(nc.sbuf_tensor(f"{name}_s{uid[0]}", list(shape), dt))

        banks = [top.enter_context(nc.psum_tensor(f"bank{i}", [128, 512], F32)) for i in range(8)]
        BK = [Buf(f"bank{i}") for i in range(8)]

        onesb = sb(top, "onesb", [128, 128], BF16)
        identf = sb(top, "identf", [128, 128], F32)
        identb = sb(top, "identb", [128, 128], BF16)
        A_bf = sb(top, "A_bf", [128, 28, 128], BF16)
        gm = sb(top, "gm", [128, Lw * 8], F32)
        gf = sb(top, "gf", [128, Lw * 8], F32)
        psc = sb(top, "psc", [128, Lw * 4], F32)
        epsc = sb(top, "epsc", [128, 1], F32)
        ustr_f = sb(top, "ustr_f", [128, 128], F32)
        ustr = sb(top, "ustr", [128, 128], BF16)
        iotas = sb(top, "iotas", [128, 33], F32)
        CONST = Buf("const")
        with nc.sbuf_tensor("A_stage", [128, 28, 128], F32) as A_st:
            ASB = Buf("A_stage")
            T.emit("dve", lambda e: e.memset(onesb[:], 1.0), writes=[CONST])
            T.emit("dve", lambda e: e.memset(epsc[:], EPS), writes=[CONST])
            T.dma("sp", identf[:], ident_d, writes=[CONST])
            T.dma("sp", gm[:], gm_d, writes=[CONST])
            T.dma("sp", gf[:], gf_d, writes=[CONST])
            T.dma("sp", psc[:], ps_d, writes=[CONST])
            T.dma("sp", A_st[:], poolA_d, writes=[ASB])
            T.dma("sp", ustr_f[:], ustrict_d, writes=[CONST])
            T.dma("sp", iotas[:], iotas_d, writes=[CONST])
            T.emit("dve", lambda e: e.tensor_copy(out=ustr[:], in_=ustr_f[:]), reads=[CONST], writes=[CONST])
            T.emit("dve", lambda e: e.tensor_copy(out=identb[:], in_=identf[:]), reads=[CONST], writes=[CONST])
            T.emit("dve", lambda e: e.tensor_copy(out=A_bf[:], in_=A_st[:]), reads=[ASB], writes=[CONST])
            T.barrier()

        WB = {}

        cast_sems = [T._new_sem(top, f"cs{i}") for i in range(4)]
        cast_cnt = [0] * 4
        cast_i = [0]

        def cast(key, dst, src, n=0):
            WB[key] = Buf(str(key))
            E = T.eng["pool"]
            i = cast_i[0] % 4
            cast_i[0] += 1
            sm = cast_sems[i]
            if cast_cnt[i] and E.known.get(sm, 0) < cast_cnt[i]:
                E.q.append(("wait", sm, cast_cnt[i]))
                E.known[sm] = cast_cnt[i]
            cast_cnt[i] += 16
            E.q.append(("dma", dst, src, sm))
            WB[key].w = (sm, cast_cnt[i])

        for l in range(depth):
            cast(("in", l), w_in_b[l], w_in[l])
            cast(("pw", l), pool_w_b[l].rearrange("g (a b) d -> (g a) (b d)", b=16),
                 pool_w[l].rearrange("g (a b) d -> (g a) (b d)", b=16))
            cast(("out", l), w_out_b[l].rearrange("(r q) d -> r (q d)", q=2),
                 w_out[l].rearrange("(r q) d -> r (q d)", q=2))
            for e_ in range(NE):
                cast(("g", l, e_), w_gate_p[l][e_ * 128:(e_ + 1) * 128, :].rearrange("p (k f) -> p k f", f=512),
                     w_gate[l, e_].rearrange("(k p) f -> p k f", p=128))
                cast(("u", l, e_), w_up_p[l][e_ * 128:(e_ + 1) * 128, :].rearrange("p (k f) -> p k f", f=512),
                     w_up[l, e_].rearrange("(k p) f -> p k f", p=128))
                cast(("d", l, e_), w_down_p[l][e_ * 128:(e_ + 1) * 128, :].rearrange("p (k f) -> p k f", f=1024),
                     w_down[l, e_].rearrange("(k p) f -> p k f", p=128))

        XD = [[Buf(f"xd{s}_{t}") for t in range(4)] for s in range(nseq)]
        flip = [0]

        def evac_copy(out, in_, reads, writes, scale=None):
            flip[0] ^= 1
            if scale is not None or flip[0]:
                sc = 1.0 if scale is None else scale
                T.emit("act", lambda e: e.activation(out=out, in_=in_, func=AF.Copy, scale=sc), reads=reads, writes=writes)
            else:
                T.emit("dve", lambda e: e.tensor_copy(out=out, in_=in_), reads=reads, writes=writes)

        def mm(out, lhsT, rhs, start, stop, reads, writes, sig):
            T.emit("pe", lambda e: e.matmul(out, lhsT, rhs, start=start, stop=stop), reads=reads, writes=writes, sig=sig)

        def rstd_from_ss(out, ss_ap, reads, writes):
            T.emit("act", lambda e: e.activation(out=out, in_=ss_ap, func=AF.Sqrt, bias=epsc[:, 0:1], scale=1.0 / D),
                   reads=list(reads) + [CONST], writes=writes)
            T.emit("dve", lambda e: e.reciprocal(out=out, in_=out), reads=writes, writes=writes)

        with ExitStack() as st:
            xin = sb(st, "xin", [128, 2, 4, 1024], F32)
            xTs = sb(st, "xTs", [128, 2, 4, 8, 128], F32)
            XIN = [Buf("xin0"), Buf("xin1")]
            XTS = [Buf("xts0"), Buf("xts1")]
            bi = 0
            for s in range(nseq):
                for t in range(4):
                    b = (s * 4 + t) % 2
                    t0 = s * S + t * 512
                    T.dma("sp", xin[:, b], x_c[t0:t0 + 512, :].rearrange("(j p) d -> p j d", p=128), writes=[XIN[b]])
                    for k in range(8):
                        bk = bi % 4
                        bi += 1
                        for j in range(4):
                            T.emit("pe", lambda e, bk=bk, j=j, k=k, b=b: e.transpose(
                                out=banks[bk][:, j * 128:(j + 1) * 128], in_=xin[:, b, j, k * 128:(k + 1) * 128],
                                identity=identf[:]), reads=[XIN[b], CONST], writes=[BK[bk]], sig=(j == 3))
                        evac_copy(xTs[:, b, :, k, :], banks[bk][:, :].rearrange("p (j t) -> p j t", t=128), [BK[bk]], [XTS[b]])
                    T.dma("sp", xT_d[t0 // 128:t0 // 128 + 4].rearrange("j p k t -> p j k t"), xTs[:, b], reads=[XTS[b]],
                          writes=[XD[s][t]])
            T.barrier()

        def m_phase(l, s):
            tok0 = s * S
            with ExitStack() as pm:
                H = sb(pm, "H", [128, 8, S], BF16)
                qT = sb(pm, "qT", [128, 4, S], BF16)
                kT = sb(pm, "kT", [128, 4, S], BF16)
                V2 = sb(pm, "V2", [128, 31, 4, 192], BF16)
                pT = sb(pm, "pT", [128, 4, S], BF16)
                pw_sb = sb(pm, "pw_sb", [128, 4, 128], BF16)
                HT = [Buf(f"H{i}") for i in range(8)]
                QB = [[Buf(f"q{m}_{t}") for t in range(4)] for m in range(4)]
                KB = [[Buf(f"k{m}_{t}") for t in range(4)] for m in range(4)]
                VB = [Buf(f"v{i}") for i in range(31)]
                VONES = Buf("vones")
                PB = [[Buf(f"p{g}_{t}") for t in range(4)] for g in range(4)]
                PW = Buf("pw")
                with ExitStack() as pb:
                    w_in_sb = sb(pb, "w_in_sb", [128, 8, 2048], BF16)
                    U2 = sb(pb, "U2", [128, 16, 512], BF16)
                    pooledT = sb(pb, "pooledT", [128, 2, 512], BF16)
                    sq = sb(pb, "sq", [128, 2, 8, 128], BF16)
                    xt = sb(pb, "xt", [128, 2, 8, 128], F32)
                    rstd = sb(pb, "rstd", [128, 1, 256], F32)
                    WI = [Buf(f"wi{c}") for c in range(4)]
                    UB = [Buf(f"u{j}") for j in range(16)]
                    PLB = [Buf("pl0"), Buf("pl1")]
                    SQ = Buf("sq")
                    XT = [Buf("xt0"), Buf("xt1")]
                    RS = [Buf("rs0"), Buf("rs1")]
                    for c in range(4):
                        T.dma("sp", w_in_sb[:, :, c * 512:(c + 1) * 512],
                              w_in_b[l][:, c * 512:(c + 1) * 512].rearrange("(k p) f -> p k f", p=128),
                              reads=[WB[("in", l)]], writes=[WI[c]])
                    T.dma("sp", pw_sb[:], pool_w_b[l].rearrange("g c d -> c g d"), reads=[WB[("pw", l)]], writes=[PW])
                    T.emit("dve", lambda e: e.memset(V2[:, :, :, 64:128], 1.0), writes=[VONES])
                    def emit_norm(i):
                        b = 0
                        c0 = i * 256
                        tl = (tok0 + c0) // 128
                        T.dma("sp", xt[:], xT_d[tl:tl + 2].rearrange("j p k t -> p j k t"),
                              reads=[XD[s][i // 2]], writes=[XT[b]])
                        T.emit("act", lambda e: e.activation(out=sq[:], in_=xt[:], func=AF.Square),
                               reads=[XT[b]], writes=[SQ])
                        bk = 6 + b
                        for j in range(2):
                            for k in range(8):
                                mm(banks[bk][:, j * 128:(j + 1) * 128], onesb[:], sq[:, j, k, :], k == 0, k == 7, [SQ, CONST], [BK[bk]],
                                   (k == 7 and j == 1))
                        rstd_from_ss(rstd[:, b, :], banks[bk][:, 0:256], [BK[bk]], [RS[b]])
                        for k in range(8):
                            T.emit("dve", lambda e, b=b, k=k, c0=c0: e.scalar_tensor_tensor(
                                out=H[:, k, c0:c0 + 256].rearrange("p (j t) -> p j t", t=128), in0=xt[:, :, k, :],
                                scalar=gm[:, l * 8 + k:l * 8 + k + 1],
                                in1=rstd[:, b, :].rearrange("p (j t) -> p j t", t=128), op0=ALU.mult, op1=ALU.mult),
                                reads=[XT[b], RS[b], CONST], writes=[HT[i]])
                    bi_ = [0]

                    def emit_qkproj(t):
                        bi = bi_[0]
                        for m in range(8):
                            bk = bi % 4
                            bi += 1
                            for k in range(8):
                                mm(banks[bk][:, :], w_in_sb[:, k, m * 128:(m + 1) * 128], H[:, k, t * 512:(t + 1) * 512],
                                   k == 0, k == 7, [WI[m // 4], HT[2 * t], HT[2 * t + 1]], [BK[bk]], k == 7)
                            if m < 4:
                                evac_copy(qT[:, m, t * 512:(t + 1) * 512], banks[bk][:, :], [BK[bk]], [QB[m][t]], scale=0.125)
                            else:
                                T.emit("dve", lambda e, bk=bk, m=m, t=t: e.tensor_copy(
                                    out=kT[:, m - 4, t * 512:(t + 1) * 512], in_=banks[bk][:, :]),
                                    reads=[BK[bk]], writes=[KB[m - 4][t]])
                        bi_[0] = bi

                    def emit_v(sidx):
                        bk = bi_[0] % 4
                        bi_[0] += 1
                        a0 = 64 * sidx
                        hts = sorted({a0 // 256, (a0 + 127) // 256})
                        for k in range(8):
                            mm(banks[bk][:, :], H[:, k, a0:a0 + 128], w_in_sb[:, k, 1024:1536], k == 0, k == 7,
                               [WI[2]] + [HT[i] for i in hts], [BK[bk]], k == 7)
                        evac_copy(V2[:, sidx, :, :].rearrange("p a (b d) -> p a b d", d=64)[:, :, 0:3:2, :],
                                  banks[bk][:, :].rearrange("p (a b d) -> p a b d", b=2, d=64), [BK[bk]], [VB[sidx]])

                    def emit_u(j):
                        bk = bi_[0] % 4
                        bi_[0] += 1
                        for k in range(8):
                            mm(banks[bk][:, :], H[:, k, j * 128:(j + 1) * 128], w_in_sb[:, k, 1536:2048], k == 0, k == 7,
                               [WI[3], HT[j // 2]], [BK[bk]], k == 7)
                        evac_copy(U2[:, j, :], banks[bk][:, :], [BK[bk]], [UB[j]])

                    def emit_proj(t):
                        emit_qkproj(t)
                        for sidx in range(max(0, 8 * t - 1), min(31, 8 * t + 7)):
                            emit_v(sidx)
                        for j in range(4 * t, 4 * t + 4):
                            emit_u(j)

                    for t in range(4):
                        emit_norm(2 * t)
                        emit_norm(2 * t + 1)
                        if t > 0:
                            emit_proj(t - 1)
                    emit_proj(3)
                    bi = bi_[0]
                    for t in range(4):
                        for g in range(4):
                            pbuf = g % 2
                            bk = bi % 4
                            bi += 1
                            for Tq in range(4):
                                Tt = 4 * t + Tq
                                terms = []
                                if Tt > 0:
                                    terms.append((Tt - 1, 0))
                                if Tt == 0:
                                    terms += [(Tt, 3), (Tt, 4)]
                                elif Tt == 15:
                                    terms += [(Tt, 5), (Tt, 6)]
                                else:
                                    terms.append((Tt, 1))
                                if Tt < 15:
                                    terms.append((Tt + 1, 2))
                                for ti, (tp, var) in enumerate(terms):
                                    mm(banks[bk][:, Tq * 128:(Tq + 1) * 128], U2[:, tp, g * 128:(g + 1) * 128],
                                       A_bf[:, g * 7 + var, :], ti == 0, ti == len(terms) - 1,
                                       [UB[tp], CONST], [BK[bk]], (Tq == 3 and ti == len(terms) - 1))
                            T.emit("act", lambda e, bk=bk, pbuf=pbuf, g=g: e.activation(
                                out=pooledT[:, pbuf, :], in_=banks[bk][:, :], func=AF.Copy),
                                reads=[BK[bk]], writes=[PLB[pbuf]])
                            bk2 = 4 + (g % 2)
                            mm(banks[bk2][:, :], pw_sb[:, g, :], pooledT[:, pbuf, :], True, True, [PW, PLB[pbuf]], [BK[bk2]], True)
                            T.emit("dve", lambda e, bk2=bk2, g=g, t=t: e.tensor_scalar(
                                out=pT[:, g, t * 512:(t + 1) * 512], in0=banks[bk2][:, :],
                                scalar1=psc[:, l * 4 + g:l * 4 + g + 1], scalar2=None, op0=ALU.mult),
                                reads=[BK[bk2], CONST], writes=[PB[g][t]])
                    T.barrier()
                with ExitStack() as pc:
                    bias = sb(pc, "bias", [128, 4, 2, 14, 64], F32)
                    Sb = sb(pc, "Sb", [128, 4, 2, 4, 64], F32)
                    Pm = sb(pc, "Pm", [128, 4, 2, 4, 64], BF16)
                    Rr = sb(pc, "Rr", [128, 2, 4, 64], F32)
                    w_out_sb = sb(pc, "w_out_sb", [128, 8, D], BF16)
                    xt2 = sb(pc, "xt2", [128, 2, 8, 128], F32)
                    BIAS = [Buf(f"bias{hp}") for hp in range(4)]
                    SBB = [Buf(f"sb{i}") for i in range(4)]
                    PMB = [Buf(f"pm{i}") for i in range(4)]
                    RRB = [Buf("rr0"), Buf("rr1")]
                    WO = [Buf("wo0"), Buf("wo1")]
                    XT2 = [Buf("xt20"), Buf("xt21")]
                    for hp in range(4):
                        T.dma("sp", bias[:, hp].rearrange("p a b c -> p (a b c)"), bias_d[l, hp], writes=[BIAS[hp]])
                        T.emit("act", lambda e, hp=hp: e.activation(out=bias[:, hp].rearrange("p a b c -> p (a b c)"),
                                                                    in_=bias[:, hp].rearrange("p a b c -> p (a b c)"), func=AF.Exp),
                               reads=[BIAS[hp]], writes=[BIAS[hp]])
                    for c in range(2):
                        T.dma("sp", w_out_sb[:, :, c * 512:(c + 1) * 512],
                              w_out_b[l][:, c * 512:(c + 1) * 512].rearrange("(k p) f -> p k f", p=128),
                              reads=[WB[("out", l)]], writes=[WO[c]])
                    units = [(r, hp) for r in range(32) for hp in range(4)]
                    ATB_ = [Buf(f"at{i}") for i in range(16)]

                    def rstart(r):
                        return min(max(r - 4, 0), 24)

                    def emit_qk(ui):
                        r, hp = units[ui]
                        u4 = ui % 4
                        rs_ = rstart(r)
                        i0 = rs_ - r + 7
                        kts = sorted({(rs_ * 64) // 512, (rs_ * 64 + 511) // 512})
                        for half in (0, 1):
                            bk = (u4 // 2) * 2 + half
                            cb = (u4 % 2) * 256
                            p0 = 64 * half
                            for c in range(4):
                                ks = (rs_ + 2 * c) * 64
                                mm(banks[bk][:, cb + c * 64:cb + (c + 1) * 64], kT[p0:p0 + 64, hp, ks:ks + 128],
                                   qT[p0:p0 + 64, hp, r * 64:(r + 1) * 64], True, True,
                                   [KB[hp][t] for t in kts] + [QB[hp][r // 8]], [BK[bk]], c == 3)
                            T.emit("act", lambda e, bk=bk, cb=cb, u4=u4, half=half: e.activation(
                                out=Sb[:, u4, half], in_=banks[bk][:, cb:cb + 256].rearrange("p (c q) -> p c q", q=64),
                                func=AF.Exp), reads=[BK[bk]], writes=[SBB[u4]])
                        T.emit("pool" if (l >= POOL_FROM_LAYER and ui % 2 == 1) else "dve", lambda e, u4=u4, hp=hp, i0=i0: e.tensor_tensor(
                            out=Pm[:, u4], in0=Sb[:, u4], in1=bias[:, hp, :, i0:i0 + 7:2, :], op=ALU.mult),
                            reads=[SBB[u4], BIAS[hp]], writes=[PMB[u4]])

                    def emit_pv(ui):
                        r, hp = units[ui]
                        u2 = ui % 4
                        rs_ = rstart(r)
                        ob = 4 + (r % 2)
                        for half in (0, 1):
                            h = 2 * hp + half
                            for c in range(4):
                                sidx = rs_ + 2 * c
                                lhsT = V2[:, sidx, hp, 64 * half:64 * half + 128]
                                col = (hp * 2 + half) * 64
                                mm(banks[ob][:, col:col + 64], lhsT, Pm[:, u2, half, c, :], c == 0, c == 3,
                                   [VB[sidx], VONES, PMB[u2]], [BK[ob]], c == 3)
                        if hp == 3:
                            rb = r % 2
                            Ov = banks[ob][:, :].rearrange("p (a b q) -> p a b q", b=2, q=64)
                            T.emit("dve", lambda e, rb=rb, ob=ob: e.reciprocal(
                                out=Rr[0:64, rb], in_=banks[ob][64:128, :].rearrange("p (a b q) -> p a b q", b=2, q=64)[:, :, 0, :]),
                                reads=[BK[ob]], writes=[RRB[rb]])
                            T.emit("dve", lambda e, rb=rb, ob=ob: e.reciprocal(
                                out=Rr[64:128, rb], in_=banks[ob][0:64, :].rearrange("p (a b q) -> p a b q", b=2, q=64)[:, :, 1, :]),
                                reads=[BK[ob]], writes=[RRB[rb]])
                            T.emit("dve", lambda e, rb=rb, ob=ob, r=r: e.tensor_tensor(
                                out=H[0:64, 0:4, r * 64:(r + 1) * 64],
                                in0=banks[ob][0:64, :].rearrange("p (a b q) -> p a b q", b=2, q=64)[:, :, 0, :],
                                in1=Rr[0:64, rb], op=ALU.mult), reads=[BK[ob], RRB[rb]], writes=[ATB_[r // 2]])
                            T.emit("dve", lambda e, rb=rb, ob=ob, r=r: e.tensor_tensor(
                                out=H[64:128, 0:4, r * 64:(r + 1) * 64],
                                in0=banks[ob][64:128, :].rearrange("p (a b q) -> p a b q", b=2, q=64)[:, :, 1, :],
                                in1=Rr[64:128, rb], op=ALU.mult), reads=[BK[ob], RRB[rb]], writes=[ATB_[r // 2]])

                    ATB = ATB_
                    pending = []

                    def out_load(i):
                        b = i % 2
                        T.dma("sp", xt2[:, b], xT_d[(tok0 + i * 128) // 128], reads=[XD[s][i // 4]], writes=[XT2[b]])

                    def out_group(i, m):
                        b = i % 2
                        c0 = i * 128
                        bk = 6 + (m % 2)
                        for k in range(8):
                            rhs = H[:, k, c0:c0 + 128] if k < 4 else pT[:, k - 4, c0:c0 + 128]
                            rd = [ATB[i]] if k < 4 else [PB[k - 4][i // 4]]
                            mm(banks[bk][:, 0:128], w_out_sb[:, k, m * 128:(m + 1) * 128], rhs, k == 0, k == 7,
                               [WO[m // 4]] + rd, [BK[bk]], k == 7)
                        T.emit("dve", lambda e, b=b, m=m, bk=bk: e.tensor_tensor(
                            out=xt2[:, b, m, :], in0=banks[bk][:, 0:128], in1=xt2[:, b, m, :], op=ALU.add),
                            reads=[BK[bk], XT2[b]], writes=[XT2[b]])
                        if m == 7:
                            T.dma("sp", xT_d[(tok0 + c0) // 128], xt2[:, b], reads=[XT2[b]], writes=[XD[s][i // 4]])

                    def pump(n):
                        for _ in range(n):
                            if pending:
                                out_group(*pending.pop(0))

                    def pv_and_out(ui):
                        emit_pv(ui)
                        r, hp = units[ui]
                        if hp == 3 and r % 2 == 1:
                            out_load(r // 2)
                            pending.extend((r // 2, m) for m in range(8))
                        pump(1)

                    for ui in range(len(units)):
                        emit_qk(ui)
                        if ui > 1:
                            pv_and_out(ui - 2)
                    pv_and_out(len(units) - 2)
                    pv_and_out(len(units) - 1)
                    pump(len(pending))

                    T.barrier()

        def final_out(xT, XB, s):
            tok0 = s * S
            if True:
                if True:
                    with ExitStack() as po:
                        gF = sb(po, "gF", [128, D], F32)
                        ost = sb(po, "ost", [128, 1, D], F32)
                        junk = sb(po, "junk", [128, 512], BF16)
                        ssA = sb(po, "ssA", [128, 2], F32)
                        rsF = sb(po, "rsF", [128, 1], F32)
                        GF, OST, JK, SSA, RSF = Buf("gF"), [Buf("ost0"), Buf("ost1")], Buf("junk"), Buf("ssA"), Buf("rsF")
                        T.dma("sp", gF[:], gF_d, writes=[GF])
                        for j in range(16):
                            ob = 0
                            t = j // 4
                            for hb in range(2):
                                bk = 2 * (j % 2) + hb
                                for kk in range(4):
                                    k = hb * 4 + kk
                                    T.emit("pe", lambda e, bk=bk, kk=kk, k=k, j=j: e.transpose(
                                        out=banks[bk][:, kk * 128:(kk + 1) * 128], in_=xT[:, j, k, :],
                                        identity=identf[:]), reads=[XB[k][t], CONST], writes=[BK[bk]], sig=(kk == 3))
                                T.emit("act", lambda e, bk=bk, hb=hb: e.activation(
                                    out=junk[:], in_=banks[bk][:, :], func=AF.Square, accum_out=ssA[:, hb:hb + 1]),
                                    reads=[BK[bk]], writes=[JK, SSA])
                            T.emit("dve", lambda e: e.tensor_tensor(out=rsF[:], in0=ssA[:, 0:1], in1=ssA[:, 1:2], op=ALU.add),
                                   reads=[SSA], writes=[RSF])
                            rstd_from_ss(rsF[:], rsF[:], [RSF], [RSF])
                            for hb in range(2):
                                bk = 2 * (j % 2) + hb
                                T.emit("dve", lambda e, bk=bk, hb=hb, ob=ob: e.scalar_tensor_tensor(
                                    out=ost[:, ob, hb * 512:(hb + 1) * 512], in0=banks[bk][:, :], scalar=rsF[:, 0:1],
                                    in1=gF[:, hb * 512:(hb + 1) * 512], op0=ALU.mult, op1=ALU.mult),
                                    reads=[BK[bk], RSF, GF], writes=[OST[ob]])
                            T.dma("sp", out_c[tok0 + j * 128:tok0 + (j + 1) * 128, :], ost[:, ob, :], reads=[OST[ob]],
                                  writes=[XD[s][t]])

        def f_phase_sparse(l, s, last):
            U32 = mybir.dt.uint32
            tok0 = s * S
            with ExitStack() as pf:
                xT = sb(pf, "xT", [128, 16, 8, 128], F32)
                ring = sb(pf, "ring", [128, 6, 4096], BF16)
                wr_sb = sb(pf, "wr_sb", [128, 8, 20], F32)
                wrp = sb(pf, "wrp", [128, 8, 20], F32)
                rt = sb(pf, "rt", [128, 16], F32)
                LGs = sb(pf, "LGs", [128, 16, 20], F32)
                r1 = sb(pf, "r1", [128, 16, 16], F32)
                gmax = sb(pf, "gmax", [128, 16], F32)
                goh = sb(pf, "goh", [128, 16, 4], F32)
                gex = sb(pf, "gex", [128, 16, 4], F32)
                gw = sb(pf, "gw", [128, 16], F32)
                esel = sb(pf, "esel", [128, 16, 4], F32)
                em = sb(pf, "em", [128, 16, 4], F32)
                m1 = sb(pf, "m1", [128, 16], F32)
                m2 = sb(pf, "m2", [128, 16], F32)
                oh1 = sb(pf, "oh1", [128, 16, 4], F32)
                oh2 = sb(pf, "oh2", [128, 16, 4], F32)
                usum = sb(pf, "usum", [128, 16, 1], F32)
                utmp = sb(pf, "utmp", [128, 16, 1], F32)
                w1 = sb(pf, "w1", [128, 16], F32)
                w2 = sb(pf, "w2", [128, 16], F32)
                M1 = sb(pf, "M1", [128, 16, 16], F32)
                M2 = sb(pf, "M2", [128, 16, 16], F32)
                Mb = sb(pf, "Mb", [128, 256], BF16)
                TOT = sb(pf, "TOT", [128, 16, 16], F32)
                JP = sb(pf, "JP", [128, 16, 16], F32)
                SL = sb(pf, "SL", [128, 16, 16], F32)
                ne = sb(pf, "ne", [128, 16], F32)
                ntl = sb(pf, "ntl", [128, 16], F32)
                base = sb(pf, "base", [128, 16], F32)
                bend = sb(pf, "bend", [128, 16], F32)
                b256 = sb(pf, "b256", [128, 16], F32)
                sl1 = sb(pf, "sl1", [128, 16], F32)
                sl2 = sb(pf, "sl2", [128, 16], F32)
                s1u = sb(pf, "s1u", [128, 16], U32)
                s2u = sb(pf, "s2u", [128, 16], U32)
                eidx = sb(pf, "eidx", [128, NTILE], F32)
                widf = sb(pf, "widf", [128, NTILE], F32)
                widx = sb(pf, "widx", [128, NTILE], U32)
                XB = [[Buf(f"X{k}_{t}") for t in range(4)] for k in range(8)]
                RSLOT = [Buf(f"ring{i}") for i in range(6)]
                WR, WRP = Buf("wr"), Buf("wrp")
                ROUT = Buf("router")
                HSB = [Buf(f"hs{i}") for i in range(32)]
                YSB = [Buf(f"ys{i}") for i in range(NTILE)]

                def load_tile_w(i, which=(0, 1, 2)):
                    for j, wp in enumerate((w_gate_p, w_up_p, w_down_p)):
                        if j not in which:
                            continue
                        slot = (3 * i + j) % 6
                        T.idma(ring[:, slot, :], None, wp[l], widx[:, i:i + 1],
                               reads=[ROUT] + [WB[(("g", "u", "d")[j], l, e_)] for e_ in range(NE)], writes=[RSLOT[slot]])

                for t in range(4):
                    tl = tok0 // 128 + 4 * t
                    T.dma("sp", xT[:, 4 * t:4 * t + 4], xT_d[tl:tl + 4].rearrange("j p k t -> p j k t"), reads=[XD[s][t]],
                          writes=[XB[k][t] for k in range(8)])
                T.dma("sp", wr_sb[:], wr_d[:, l], writes=[WR])
                for k in range(8):
                    T.emit("dve", lambda e, k=k: e.tensor_scalar(out=wrp[:, k, :], in0=wr_sb[:, k, :],
                                                                 scalar1=gf[:, l * 8 + k:l * 8 + k + 1], scalar2=None,
                                                                 op0=ALU.mult), reads=[WR, CONST], writes=[WRP])

                def dv(fn, extra_reads=()):
                    T.emit("dve", fn, reads=[ROUT] + list(extra_reads), writes=[ROUT])

                def b3(ap2, n):
                    return ap2.unsqueeze(2).broadcast_to([128, 16, n])

                def uniq(oh):
                    dv(lambda e: e.tensor_copy(out=usum[:], in_=oh[:, :, 0:1]))
                    for i in range(1, 4):
                        dv(lambda e: e.tensor_scalar(out=utmp[:], in0=usum[:], scalar1=-1.0, scalar2=1.0,
                                                     op0=ALU.mult, op1=ALU.add))
                        dv(lambda e, i=i: e.tensor_tensor(out=oh[:, :, i:i + 1], in0=oh[:, :, i:i + 1], in1=utmp[:], op=ALU.mult))
                        if i < 3:
                            dv(lambda e, i=i: e.tensor_tensor(out=usum[:], in0=usum[:], in1=oh[:, :, i:i + 1], op=ALU.add))

                with ExitStack() as pa:
                    H = sb(pa, "Hf", [128, 8, S], BF16)
                    sq = sb(pa, "sqf", [128, 4, 8, 128], BF16)
                    rstd = sb(pa, "rstdf", [128, 512], F32)
                    hrow = sb(pa, "hrow", [128, 2, D], BF16)
                    HB = [Buf(f"Hf{t}") for t in range(4)]
                    SQ, RS = Buf("sq"), Buf("rs")
                    HROW = [Buf("hrow0"), Buf("hrow1")]
                    for t in range(4):
                        c0 = t * 512
                        T.emit("act", lambda e, t=t: e.activation(out=sq[:], in_=xT[:, 4 * t:4 * t + 4], func=AF.Square),
                               reads=[XB[k][t] for k in range(8)], writes=[SQ])
                        for j in range(4):
                            for k in range(8):
                                mm(banks[6][:, j * 128:(j + 1) * 128], onesb[:], sq[:, j, k, :], k == 0, k == 7, [SQ, CONST], [BK[6]],
                                   (k == 7 and j == 3))
                        rstd_from_ss(rstd[:], banks[6][:, :], [BK[6]], [RS])
                        for k in range(8):
                            T.emit("dve", lambda e, k=k, c0=c0, t=t: e.scalar_tensor_tensor(
                                out=H[:, k, c0:c0 + 512].rearrange("p (j t) -> p j t", t=128), in0=xT[:, 4 * t:4 * t + 4, k, :],
                                scalar=gf[:, l * 8 + k:l * 8 + k + 1],
                                in1=rstd[:].rearrange("p (j t) -> p j t", t=128), op0=ALU.mult, op1=ALU.mult),
                                reads=[XB[k][t], RS, CONST], writes=[HB[t]])
                        for j in range(4):
                            jj = 4 * t + j
                            for k in range(8):
                                mm(banks[7][:, 320 + jj:320 + jj + 1], sq[:, j, k, :], onesb[:, 0:1],
                                   k == 0, k == 7, [SQ, CONST], [BK[7]], False)
                            for k in range(8):
                                mm(banks[7][:, jj * 20:(jj + 1) * 20], xT[:, jj, k, :], wrp[:, k, :],
                                   k == 0, k == 7, [XB[k][t], WRP], [BK[7]], (k == 7))
                    rstd_from_ss(rt[:], banks[7][:, 320:336], [BK[7], ROUT], [ROUT])
                    dv(lambda e: e.tensor_tensor(out=LGs[:], in0=banks[7][:, 0:320].rearrange("p (j e) -> p j e", e=20),
                                                 in1=b3(rt[:], 20), op=ALU.mult), [BK[7]])
                    dv(lambda e: e.tensor_reduce(out=gmax[:], in_=LGs[:, :, 0:4], axis=AX.X, op=ALU.max))
                    dv(lambda e: e.tensor_tensor(out=goh[:], in0=LGs[:, :, 0:4], in1=b3(gmax[:], 4), op=ALU.is_equal))
                    uniq(goh)
                    dv(lambda e: e.tensor_tensor(out=gex[:], in0=LGs[:, :, 0:4], in1=b3(gmax[:], 4), op=ALU.subtract))
                    T.emit("act", lambda e: e.activation(out=gex[:], in_=gex[:], func=AF.Exp), reads=[ROUT], writes=[ROUT])
                    dv(lambda e: e.tensor_reduce(out=gw[:], in_=gex[:], axis=AX.X, op=ALU.add))
                    dv(lambda e: e.reciprocal(out=gw[:], in_=gw[:]))
                    dv(lambda e: e.tensor_tensor(out=r1[:].rearrange("p j (g i) -> p j g i", i=4),
                                                 in0=LGs[:, :, 4:20].rearrange("p j (g i) -> p j g i", i=4),
                                                 in1=goh[:].unsqueeze(3).broadcast_to([128, 16, 4, 4]), op=ALU.mult))
                    dv(lambda e: e.tensor_reduce(out=esel[:], in_=r1[:].rearrange("p j (g i) -> p j i g", i=4),
                                                 axis=AX.X, op=ALU.add))
                    dv(lambda e: e.tensor_reduce(out=m1[:], in_=esel[:], axis=AX.X, op=ALU.max))
                    dv(lambda e: e.tensor_tensor(out=oh1[:], in0=esel[:], in1=b3(m1[:], 4), op=ALU.is_equal))
                    uniq(oh1)
                    dv(lambda e: e.scalar_tensor_tensor(out=em[:], in0=oh1[:], scalar=-1.0e30, in1=esel[:],
                                                        op0=ALU.mult, op1=ALU.add))
                    dv(lambda e: e.tensor_reduce(out=m2[:], in_=em[:], axis=AX.X, op=ALU.max))
                    dv(lambda e: e.tensor_tensor(out=oh2[:], in0=em[:], in1=b3(m2[:], 4), op=ALU.is_equal))
                    uniq(oh2)
                    dv(lambda e: e.tensor_tensor(out=w2[:], in0=m2[:], in1=m1[:], op=ALU.subtract))
                    T.emit("act", lambda e: e.activation(out=w2[:], in_=w2[:], func=AF.Exp), reads=[ROUT], writes=[ROUT])
                    dv(lambda e: e.tensor_scalar(out=w2[:], in0=w2[:], scalar1=1.0, scalar2=None, op0=ALU.add))
                    dv(lambda e: e.reciprocal(out=w1[:], in_=w2[:]))
                    dv(lambda e: e.tensor_tensor(out=w1[:], in0=w1[:], in1=gw[:], op=ALU.mult))
                    dv(lambda e: e.tensor_tensor(out=w2[:], in0=gw[:], in1=w1[:], op=ALU.subtract))
                    g44 = lambda ap: ap.rearrange("p j (g i) -> p j g i", i=4)
                    dv(lambda e: e.tensor_tensor(out=g44(M1[:]), in0=goh[:].unsqueeze(3).broadcast_to([128, 16, 4, 4]),
                                                 in1=oh1[:].unsqueeze(2).broadcast_to([128, 16, 4, 4]), op=ALU.mult))
                    dv(lambda e: e.tensor_tensor(out=g44(M2[:]), in0=goh[:].unsqueeze(3).broadcast_to([128, 16, 4, 4]),
                                                 in1=oh2[:].unsqueeze(2).broadcast_to([128, 16, 4, 4]), op=ALU.mult))
                    dv(lambda e: e.tensor_tensor(out=Mb[:].rearrange("p (j e) -> p j e", e=16), in0=M1[:], in1=M2[:], op=ALU.add))
                    mm(banks[6][:, 0:256], ustr[:], Mb[:], True, True, [ROUT, CONST], [BK[6]], True)
                    mm(banks[7][:, 0:256], onesb[:], Mb[:], True, True, [ROUT, CONST], [BK[7]], True)
                    dv(lambda e: e.tensor_copy(out=TOT[:].rearrange("p j e -> p (j e)"), in_=banks[7][:, 0:256]), [BK[7]])
                    dv(lambda e: e.memset(JP[:, 0, :], 0.0))
                    for j in range(1, 16):
                        dv(lambda e, j=j: e.tensor_tensor(out=JP[:, j, :], in0=JP[:, j - 1, :], in1=TOT[:, j - 1, :], op=ALU.add))
                    dv(lambda e: e.tensor_tensor(out=ne[:], in0=JP[:, 15, :], in1=TOT[:, 15, :], op=ALU.add))
                    dv(lambda e: e.tensor_scalar(out=ntl[:], in0=ne[:], scalar1=0.0, scalar2=None, op0=ALU.is_gt))
                    for q in range(1, S // TS):
                        dv(lambda e, q=q: e.scalar_tensor_tensor(out=ntl[:], in0=ne[:], scalar=float(TS * q), in1=ntl[:],
                                                                 op0=ALU.is_gt, op1=ALU.add))
                    dv(lambda e: e.memset(base[:, 0:1], 0.0))
                    for e_ in range(1, 16):
                        dv(lambda e, e_=e_: e.tensor_tensor(out=base[:, e_:e_ + 1], in0=base[:, e_ - 1:e_],
                                                            in1=ntl[:, e_ - 1:e_], op=ALU.add))
                    dv(lambda e: e.tensor_tensor(out=bend[:], in0=base[:], in1=ntl[:], op=ALU.add))
                    dv(lambda e: e.tensor_scalar(out=b256[:], in0=base[:], scalar1=float(TS), scalar2=None, op0=ALU.mult))
                    dv(lambda e: e.tensor_tensor(out=SL[:], in0=banks[6][:, 0:256].rearrange("p (j e) -> p j e", e=16),
                                                 in1=JP[:], op=ALU.add), [BK[6]])
                    dv(lambda e: e.tensor_tensor(out=SL[:], in0=SL[:], in1=b256[:].unsqueeze(1).broadcast_to([128, 16, 16]),
                                                 op=ALU.add))
                    dv(lambda e: e.tensor_tensor(out=M1[:], in0=M1[:], in1=SL[:], op=ALU.mult))
                    dv(lambda e: e.tensor_tensor(out=M2[:], in0=M2[:], in1=SL[:], op=ALU.mult))
                    dv(lambda e: e.tensor_reduce(out=sl1[:], in_=M1[:], axis=AX.X, op=ALU.add))
                    dv(lambda e: e.tensor_reduce(out=sl2[:], in_=M2[:], axis=AX.X, op=ALU.add))
                    dv(lambda e: e.tensor_copy(out=s1u[:], in_=sl1[:]))
                    dv(lambda e: e.tensor_copy(out=s2u[:], in_=sl2[:]))
                    dv(lambda e: e.memset(eidx[:], 0.0))
                    for e_ in range(16):
                        dv(lambda e, e_=e_: e.scalar_tensor_tensor(out=eidx[:], in0=iotas[:, 1:1 + NTILE], scalar=bend[:, e_:e_ + 1],
                                                                   in1=eidx[:], op0=ALU.is_ge, op1=ALU.add), [CONST])
                    dv(lambda e: e.tensor_scalar(out=eidx[:], in0=eidx[:], scalar1=15.0, scalar2=None, op0=ALU.min))
                    dv(lambda e: e.tensor_scalar(out=widf[:], in0=eidx[:], scalar1=128.0, scalar2=iotas[:, 0:1],
                                                 op0=ALU.mult, op1=ALU.add), [CONST])
                    dv(lambda e: e.tensor_copy(out=widx[:], in_=widf[:]))
                    load_tile_w(0)
                    load_tile_w(1, which=(0, 1))
                    for j in range(16):
                        hb = j % 2
                        bk = 4 + hb
                        pv = banks[bk][:, :].bitcast(BF16)
                        for k in range(8):
                            T.emit("pe", lambda e, pv=pv, k=k, j=j: e.transpose(
                                out=pv[:, k * 128:(k + 1) * 128], in_=H[:, k, j * 128:(j + 1) * 128], identity=identb[:]),
                                reads=[HB[j // 4], CONST], writes=[BK[bk]], sig=(k == 7))
                        evac_copy(hrow[:, hb, :], pv, [BK[bk]], [HROW[hb]])
                        T.idma(HS_d, s1u[:, j:j + 1], hrow[:, hb, :], None, reads=[HROW[hb], ROUT], writes=[HSB[2 * j]])
                        T.idma(HS_d, s2u[:, j:j + 1], hrow[:, hb, :], None, reads=[HROW[hb], ROUT], writes=[HSB[2 * j + 1]])
                    T.barrier()

                with ExitStack() as pbx:
                    hs_sb = sb(pbx, "hs_sb", [128, 3, 2, D], BF16)
                    hcT = sb(pbx, "hcT", [128, 2, 8, TS], BF16)
                    a_e = sb(pbx, "a_e", [128, 2, 4, TS], BF16)
                    sg = sb(pbx, "sg", [128, 2, TS], F32)
                    ys = sb(pbx, "ys", [128, 2, 2, D], F32)
                    HSS = [Buf("hss0"), Buf("hss1"), Buf("hss2")]
                    HCT = [Buf("hct0"), Buf("hct1")]
                    AE = [[Buf(f"ae{b}_{f}") for f in range(4)] for b in range(2)]
                    SG = [Buf("sg0"), Buf("sg1")]
                    YS = [Buf("ysb0"), Buf("ysb1")]

                    def emit_hs(i):
                        hb3 = i % 3
                        T.dma("sp", hs_sb[:, hb3], HS_d[i * TS:(i + 1) * TS, :].rearrange("(a p) d -> p a d", p=128),
                              reads=HSB, writes=[HSS[hb3]])

                    def emit_tr(i):
                        ab = i % 2
                        hb3 = i % 3
                        for hb in range(2):
                            bk = 6 + hb
                            pv = banks[bk][:, :].bitcast(BF16)
                            for kk in range(4):
                                k = hb * 4 + kk
                                for a in range(2):
                                    T.emit("pe", lambda e, pv=pv, kk=kk, a=a, k=k, hb3=hb3: e.transpose(
                                        out=pv[:, kk * TS + a * 128:kk * TS + (a + 1) * 128],
                                        in_=hs_sb[:, hb3, a, k * 128:(k + 1) * 128], identity=identb[:]),
                                        reads=[HSS[hb3], CONST], writes=[BK[bk]], sig=(kk == 3 and a == 1))
                            evac_copy(hcT[:, ab, hb * 4:(hb + 1) * 4, :].rearrange("p k t -> p (k t)"), pv, [BK[bk]], [HCT[ab]])

                    def emit_gu(i):
                        ab = i % 2
                        sg_, su_ = (3 * i) % 6, (3 * i + 1) % 6
                        for f in range(4):
                            bg, bu = f % 2, 2 + f % 2
                            for k in range(8):
                                mm(banks[bg][:, 0:TS], ring[:, sg_, k * 512 + f * 128:k * 512 + (f + 1) * 128], hcT[:, ab, k, :],
                                   k == 0, k == 7, [RSLOT[sg_], HCT[ab]], [BK[bg]], k == 7)
                            for k in range(8):
                                mm(banks[bu][:, 0:TS], ring[:, su_, k * 512 + f * 128:k * 512 + (f + 1) * 128], hcT[:, ab, k, :],
                                   k == 0, k == 7, [RSLOT[su_], HCT[ab]], [BK[bu]], k == 7)
                            i2 = f % 2
                            T.emit("act", lambda e, bg=bg, i2=i2: e.activation(out=sg[:, i2, :], in_=banks[bg][:, 0:TS], func=AF.Silu),
                                   reads=[BK[bg]], writes=[SG[i2]])
                            T.emit("dve", lambda e, bu=bu, i2=i2, ab=ab, f=f: e.tensor_tensor(
                                out=a_e[:, ab, f, :], in0=banks[bu][:, 0:TS], in1=sg[:, i2, :], op=ALU.mult),
                                reads=[BK[bu], SG[i2]], writes=[AE[ab][f]])

                    def emit_d(i):
                        ab = i % 2
                        sd_ = (3 * i + 2) % 6
                        for a in range(2):
                            for dh in range(2):
                                bk = 4 + dh
                                for f in range(4):
                                    mm(banks[bk][:, :], a_e[:, ab, f, a * 128:(a + 1) * 128],
                                       ring[:, sd_, f * 1024 + dh * 512:f * 1024 + (dh + 1) * 512],
                                       f == 0, f == 3, [RSLOT[sd_], AE[ab][f]], [BK[bk]], f == 3)
                                evac_copy(ys[:, ab, a, dh * 512:(dh + 1) * 512], banks[bk][:, :], [BK[bk]], [YS[ab]])
                        T.dma("sp", YS_d[i * TS:(i + 1) * TS, :].rearrange("(a p) d -> p a d", p=128), ys[:, ab],
                              reads=[YS[ab]], writes=[YSB[i]])

                    emit_hs(0)
                    emit_hs(1)
                    emit_tr(0)
                    for i in range(NTILE):
                        if i + 2 < NTILE:
                            emit_hs(i + 2)
                        if i + 1 < NTILE:
                            emit_tr(i + 1)
                        emit_gu(i)
                        if i + 2 < NTILE:
                            load_tile_w(i + 2, which=(0, 1))
                        if i > 0:
                            emit_d(i - 1)
                        if i + 1 < NTILE:
                            load_tile_w(i + 1, which=(2,))
                    emit_d(NTILE - 1)
                    T.barrier()

                with ExitStack() as pcx:
                    g0 = sb(pcx, "g0", [128, 4, D], F32)
                    g1 = sb(pcx, "g1", [128, 4, D], F32)
                    G0 = [Buf(f"g0{i}") for i in range(4)]
                    G1 = [Buf(f"g1{i}") for i in range(4)]
                    for j in range(16):
                        b = j % 4
                        T.idma(g0[:, b, :], None, YS_d, s1u[:, j:j + 1], reads=YSB + [ROUT], writes=[G0[b]])
                        T.idma(g1[:, b, :], None, YS_d, s2u[:, j:j + 1], reads=YSB + [ROUT], writes=[G1[b]])
                        T.emit("dve", lambda e, b=b, j=j: e.tensor_scalar(out=g0[:, b, :], in0=g0[:, b, :], scalar1=w1[:, j:j + 1],
                                                                          scalar2=None, op0=ALU.mult), reads=[G0[b], ROUT], writes=[G0[b]])
                        T.emit("dve", lambda e, b=b, j=j: e.scalar_tensor_tensor(out=g0[:, b, :], in0=g1[:, b, :], scalar=w2[:, j:j + 1],
                                                                                 in1=g0[:, b, :], op0=ALU.mult, op1=ALU.add),
                               reads=[G0[b], G1[b], ROUT], writes=[G0[b]])
                        for hb in range(2):
                            bk = 2 * (j % 2) + hb
                            for kk in range(4):
                                k = hb * 4 + kk
                                T.emit("pe", lambda e, bk=bk, kk=kk, k=k, b=b: e.transpose(
                                    out=banks[bk][:, kk * 128:(kk + 1) * 128], in_=g0[:, b, k * 128:(k + 1) * 128],
                                    identity=identf[:]), reads=[G0[b], CONST], writes=[BK[bk]], sig=(kk == 3))
                            T.emit("dve", lambda e, bk=bk, hb=hb, j=j: e.tensor_tensor(
                                out=xT[:, j, hb * 4:(hb + 1) * 4, :],
                                in0=banks[bk][:, :].rearrange("p (k t) -> p k t", t=128),
                                in1=xT[:, j, hb * 4:(hb + 1) * 4, :], op=ALU.add),
                                reads=[BK[bk]] + [XB[hb * 4 + kk][j // 4] for kk in range(4)],
                                writes=[XB[hb * 4 + kk][j // 4] for kk in range(4)])

                if not last:
                    for t in range(4):
                        tl = tok0 // 128 + 4 * t
                        T.dma("sp", xT_d[tl:tl + 4].rearrange("j p k t -> p j k t"), xT[:, 4 * t:4 * t + 4],
                              reads=[XB[k][t] for k in range(8)], writes=[XD[s][t]])
                else:
                    T.barrier()
                    final_out(xT, XB, s)
                T.barrier()

        def f_phase(l, s, last):
            tok0 = s * S
            with ExitStack() as pf:
                xT = sb(pf, "xT", [128, 8, S], F32)
                H = sb(pf, "Hf", [128, 8, S], BF16)
                ring = sb(pf, "ring", [128, 6, 4096], BF16)
                sq = sb(pf, "sqf", [128, 8, 512], BF16)
                rstd = sb(pf, "rstdf", [128, 512], F32)
                wr_sb = sb(pf, "wr_sb", [128, 8, 20], F32)
                wrp = sb(pf, "wrp", [128, 8, 20], F32)
                a_e = sb(pf, "a_e", [128, 2, 4, 512], BF16)
                sg = sb(pf, "sg", [128, 2, 512], F32)
                tt = sb(pf, "tt", [128, 2, 512], F32)
                Cb = sb(pf, "Cb", [128, 2, 512], F32)
                rt = sb(pf, "rt", [128, 16], F32)
                LGs = sb(pf, "LGs", [128, 16, 20], F32)
                r1 = sb(pf, "r1", [128, 16, 16], F32)
                r2 = sb(pf, "r2", [128, 16, 16], F32)
                gmax = sb(pf, "gmax", [128, 16], F32)
                goh = sb(pf, "goh", [128, 16, 4], F32)
                gex = sb(pf, "gex", [128, 16, 4], F32)
                gw = sb(pf, "gw", [128, 16], F32)
                esel = sb(pf, "esel", [128, 16, 4], F32)
                em = sb(pf, "em", [128, 16, 4], F32)
                m1 = sb(pf, "m1", [128, 16], F32)
                m2 = sb(pf, "m2", [128, 16], F32)
                oh1 = sb(pf, "oh1", [128, 16, 4], F32)
                oh2 = sb(pf, "oh2", [128, 16, 4], F32)
                w1 = sb(pf, "w1", [128, 16], F32)
                w2 = sb(pf, "w2", [128, 16], F32)
                c4 = sb(pf, "c4", [128, 16, 4], F32)
                Cm = sb(pf, "Cm", [128, 16, 16], F32)
                Chi = sb(pf, "Chi", [128, 16, 16], BF16)
                Clo = sb(pf, "Clo", [128, 16, 16], BF16)
                XB = [[Buf(f"X{k}_{t}") for t in range(4)] for k in range(8)]
                HB = [Buf(f"Hf{t}") for t in range(4)]
                RSLOT = [Buf(f"ring{i}") for i in range(6)]
                SQ, RS, WR, WRP, RT = Buf("sq"), Buf("rs"), Buf("wr"), Buf("wrp"), Buf("rt")
                ROUT = Buf("router")
                AE = [[Buf(f"ae{b}_{f}") for f in range(4)] for b in range(2)]
                SG = [Buf(f"sg{i}") for i in range(2)]
                TT = [Buf(f"tt{i}") for i in range(2)]
                CB = [Buf("cb0"), Buf("cb1")]

                def load_expert(e):
                    e4 = e // 4
                    for j, (nm, wb) in enumerate((("g", w_gate_b), ("u", w_up_b))):
                        slot = (3 * e + j) % 6
                        T.dma("sp", ring[:, slot, :].rearrange("p (k f) -> p k f", f=512),
                              wb[l, e].rearrange("(k p) f -> p k f", p=128), reads=[WB[(nm, l, e4)]], writes=[RSLOT[slot]])
                    slot = (3 * e + 2) % 6
                    T.dma("sp", ring[:, slot, :].rearrange("p (k f) -> p k f", f=1024),
                          w_down_b[l, e].rearrange("(k p) f -> p k f", p=128), reads=[WB[("d", l, e4)]], writes=[RSLOT[slot]])

                for k in range(8):
                    T.dma("sp", xT[:, k, :], xT_d[k, :, tok0:tok0 + S], reads=[XD[s][t] for t in range(4)],
                          writes=[XB[k][t] for t in range(4)])
                T.dma("sp", wr_sb[:], wr_d[:, l], writes=[WR])
                load_expert(0)
                for k in range(8):
                    T.emit("dve", lambda e, k=k: e.tensor_scalar(out=wrp[:, k, :], in0=wr_sb[:, k, :],
                                                                 scalar1=gf[:, l * 8 + k:l * 8 + k + 1], scalar2=None,
                                                                 op0=ALU.mult), reads=[WR, CONST], writes=[WRP])
                for t in range(4):
                    c0 = t * 512
                    T.emit("act", lambda e, c0=c0: e.activation(out=sq[:], in_=xT[:, :, c0:c0 + 512], func=AF.Square),
                           reads=[XB[k][t] for k in range(8)], writes=[SQ])
                    for k in range(8):
                        mm(banks[6][:, :], onesb[:], sq[:, k, :], k == 0, k == 7, [SQ, CONST], [BK[6]], k == 7)
                    rstd_from_ss(rstd[:], banks[6][:, :], [BK[6]], [RS])
                    for k in range(8):
                        T.emit("dve", lambda e, k=k, c0=c0: e.scalar_tensor_tensor(
                            out=H[:, k, c0:c0 + 512], in0=xT[:, k, c0:c0 + 512], scalar=gf[:, l * 8 + k:l * 8 + k + 1],
                            in1=rstd[:], op0=ALU.mult, op1=ALU.mult), reads=[XB[k][t], RS, CONST], writes=[HB[t]])
                    for j in range(4):
                        jj = 4 * t + j
                        for k in range(8):
                            mm(banks[7][:, 320 + jj:320 + jj + 1], sq[:, k, j * 128:(j + 1) * 128], onesb[:, 0:1],
                               k == 0, k == 7, [SQ, CONST], [BK[7]], False)
                        for k in range(8):
                            mm(banks[7][:, jj * 20:(jj + 1) * 20], xT[:, k, c0 + j * 128:c0 + (j + 1) * 128], wrp[:, k, :],
                               k == 0, k == 7, [XB[k][t], WRP], [BK[7]], (k == 7))
                R_ = [ROUT]

                def dv(fn, extra_reads=()):
                    T.emit("dve", fn, reads=[ROUT] + list(extra_reads), writes=[ROUT])

                def b3(ap2, n):
                    return ap2.unsqueeze(2).broadcast_to([128, 16, n])

                rstd_from_ss(rt[:], banks[7][:, 320:336], [BK[7], ROUT], [ROUT])
                dv(lambda e: e.tensor_tensor(out=LGs[:], in0=banks[7][:, 0:320].rearrange("p (j e) -> p j e", e=20),
                                             in1=b3(rt[:], 20), op=ALU.mult), [BK[7]])
                dv(lambda e: e.tensor_reduce(out=gmax[:], in_=LGs[:, :, 0:4], axis=AX.X, op=ALU.max))
                dv(lambda e: e.tensor_tensor(out=goh[:], in0=LGs[:, :, 0:4], in1=b3(gmax[:], 4), op=ALU.is_equal))
                dv(lambda e: e.tensor_tensor(out=gex[:], in0=LGs[:, :, 0:4], in1=b3(gmax[:], 4), op=ALU.subtract))
                T.emit("act", lambda e: e.activation(out=gex[:], in_=gex[:], func=AF.Exp), reads=[ROUT], writes=[ROUT])
                dv(lambda e: e.tensor_reduce(out=gw[:], in_=gex[:], axis=AX.X, op=ALU.add))
                dv(lambda e: e.reciprocal(out=gw[:], in_=gw[:]))
                dv(lambda e: e.tensor_tensor(out=r1[:].rearrange("p j (g i) -> p j g i", i=4),
                                             in0=LGs[:, :, 4:20].rearrange("p j (g i) -> p j g i", i=4),
                                             in1=goh[:].unsqueeze(3).broadcast_to([128, 16, 4, 4]), op=ALU.mult))
                dv(lambda e: e.tensor_reduce(out=esel[:], in_=r1[:].rearrange("p j (g i) -> p j i g", i=4),
                                             axis=AX.X, op=ALU.add))
                dv(lambda e: e.tensor_reduce(out=m1[:], in_=esel[:], axis=AX.X, op=ALU.max))
                dv(lambda e: e.tensor_tensor(out=oh1[:], in0=esel[:], in1=b3(m1[:], 4), op=ALU.is_equal))
                dv(lambda e: e.scalar_tensor_tensor(out=em[:], in0=oh1[:], scalar=-1.0e30, in1=esel[:],
                                                    op0=ALU.mult, op1=ALU.add))
                dv(lambda e: e.tensor_reduce(out=m2[:], in_=em[:], axis=AX.X, op=ALU.max))
                dv(lambda e: e.tensor_tensor(out=oh2[:], in0=em[:], in1=b3(m2[:], 4), op=ALU.is_equal))
                dv(lambda e: e.tensor_tensor(out=w2[:], in0=m2[:], in1=m1[:], op=ALU.subtract))
                T.emit("act", lambda e: e.activation(out=w2[:], in_=w2[:], func=AF.Exp), reads=[ROUT], writes=[ROUT])
                dv(lambda e: e.tensor_scalar(out=w2[:], in0=w2[:], scalar1=1.0, scalar2=None, op0=ALU.add))
                dv(lambda e: e.reciprocal(out=w1[:], in_=w2[:]))
                dv(lambda e: e.tensor_tensor(out=w1[:], in0=w1[:], in1=gw[:], op=ALU.mult))
                dv(lambda e: e.tensor_tensor(out=w2[:], in0=gw[:], in1=w1[:], op=ALU.subtract))
                dv(lambda e: e.tensor_tensor(out=c4[:], in0=oh1[:], in1=b3(w1[:], 4), op=ALU.mult))
                dv(lambda e: e.tensor_tensor(out=oh2[:], in0=oh2[:], in1=b3(w2[:], 4), op=ALU.mult))
                dv(lambda e: e.tensor_tensor(out=c4[:], in0=c4[:], in1=oh2[:], op=ALU.add))
                dv(lambda e: e.tensor_tensor(out=Cm[:].rearrange("p j (g i) -> p j g i", i=4),
                                             in0=goh[:].unsqueeze(3).broadcast_to([128, 16, 4, 4]),
                                             in1=c4[:].unsqueeze(2).broadcast_to([128, 16, 4, 4]), op=ALU.mult))
                dv(lambda e: e.tensor_copy(out=Chi[:], in_=Cm[:]))
                dv(lambda e: e.tensor_tensor(out=r2[:], in0=Cm[:], in1=Chi[:], op=ALU.subtract))
                dv(lambda e: e.tensor_copy(out=Clo[:], in_=r2[:]))

                steps = [(e_, t_) for e_ in range(NE) for t_ in range(4)]

                def emit_gu(idx):
                    e_, t = steps[idx]
                    ab = idx % 2
                    c0 = t * 512
                    sg_, su_, sd_ = (3 * e_) % 6, (3 * e_ + 1) % 6, (3 * e_ + 2) % 6
                    n = 0
                    for j in range(4):
                        for Cx in (Chi, Clo):
                            mm(banks[6][:, j * 128:(j + 1) * 128], Cx[:, 4 * t + j, e_:e_ + 1].broadcast_to([128, 128]),
                               identb[:], Cx is Chi, Cx is Clo, [ROUT, CONST], [BK[6]], (j == 3 and Cx is Clo))
                    T.emit("act", lambda e, ab=ab: e.activation(out=Cb[:, ab, :], in_=banks[6][:, :], func=AF.Copy),
                           reads=[BK[6]], writes=[CB[ab]])
                    for f in range(4):
                        bg, bu = f % 2, 2 + f % 2
                        for k in range(8):
                            mm(banks[bg][:, :], ring[:, sg_, k * 512 + f * 128:k * 512 + (f + 1) * 128], H[:, k, c0:c0 + 512],
                               k == 0, k == 7, [RSLOT[sg_], HB[t]], [BK[bg]], k == 7)
                        for k in range(8):
                            mm(banks[bu][:, :], ring[:, su_, k * 512 + f * 128:k * 512 + (f + 1) * 128], H[:, k, c0:c0 + 512],
                               k == 0, k == 7, [RSLOT[su_], HB[t]], [BK[bu]], k == 7)
                        i3 = f % 2
                        T.emit("act", lambda e, bg=bg, i3=i3: e.activation(out=sg[:, i3, :], in_=banks[bg][:, :], func=AF.Silu),
                               reads=[BK[bg]], writes=[SG[i3]])
                        T.emit("dve", lambda e, bu=bu, i3=i3: e.tensor_tensor(out=tt[:, i3, :], in0=banks[bu][:, :],
                                                                            in1=sg[:, i3, :], op=ALU.mult),
                               reads=[BK[bu], SG[i3]], writes=[TT[i3]])
                        T.emit("dve", lambda e, ab=ab, f=f, i3=i3: e.tensor_tensor(out=a_e[:, ab, f, :], in0=tt[:, i3, :],
                                                                                   in1=Cb[:, ab, :], op=ALU.mult),
                               reads=[TT[i3], CB[ab]], writes=[AE[ab][f]])

                def emit_d(idx):
                    e_, t = steps[idx]
                    ab = idx % 2
                    c0 = t * 512
                    sd_ = (3 * e_ + 2) % 6
                    for m in range(8):
                        bk = 4 + m % 2
                        for f in range(4):
                            mm(banks[bk][:, :], ring[:, sd_, f * 1024 + m * 128:f * 1024 + (m + 1) * 128], a_e[:, ab, f, :],
                               f == 0, f == 3, [RSLOT[sd_], AE[ab][f]], [BK[bk]], f == 3)
                        T.emit("dve", lambda e, m=m, bk=bk, c0=c0: e.tensor_tensor(
                            out=xT[:, m, c0:c0 + 512], in0=banks[bk][:, :], in1=xT[:, m, c0:c0 + 512], op=ALU.add),
                            reads=[BK[bk], XB[m][t]], writes=[XB[m][t]])

                for idx in range(len(steps)):
                    e_, t = steps[idx]
                    emit_gu(idx)
                    if idx > 0:
                        emit_d(idx - 1)
                    if t == 0 and e_ + 1 < NE:
                        load_expert(e_ + 1)
                emit_d(len(steps) - 1)

                if not last:
                    for k in range(8):
                        T.dma("sp", xT_d[k, :, tok0:tok0 + S], xT[:, k, :], reads=[XB[k][t] for t in range(4)],
                              writes=[XD[s][t] for t in range(4)])
                else:
                    final_out(xT, XB, s)
                T.barrier()

        done = False
        for l in range(depth):
            for s in range(nseq):
                m_phase(l, s)
            if stop_after == ("M", l):
                done = True
                break
            for s in range(nseq):
                (f_phase_sparse if SPARSE else f_phase)(l, s, last=(l == depth - 1 and stop_after is None))
            if stop_after == ("F", l):
                done = True
                break
        T.barrier()
        T.flush()
    return nc


def _bias_index_map():
    hp = np.arange(4)[:, None, None, None, None]
    p = np.arange(128)[None, :, None, None, None]
    eo = np.arange(2)[None, None, :, None, None]
    i = np.arange(14)[None, None, None, :, None]
    cq = np.arange(64)[None, None, None, None, :]
    kc = p % 64
    half = p // 64
    h = 2 * hp + eo
    dr = i + half
    dc = np.clip(kc - cq, -15, 15) + 15
    qs = np.clip(cq - 8, 0, 48)
    valid = (kc >= qs) & (kc < qs + 16)
    flat = h * (15 * 31 + 1) + np.where(valid, dr * 31 + dc, 15 * 31)
    return np.broadcast_to(flat, (4, 128, 2, 14, 64)).copy()


def _pool_consts():
    import ml_dtypes
    out = np.zeros((4, 7, 128, 128), np.float32)
    Sr = 512
    t = np.arange(Sr)
    for g, w in enumerate(POOL_W):
        lo = np.clip(t - w // 2, 0, Sr)
        hi = np.clip(t - w // 2 + w, 0, Sr)
        cnt = (hi - lo).astype(np.float64)
        A = np.zeros((Sr, Sr), np.float64)
        for tt_ in range(Sr):
            A[lo[tt_]:hi[tt_], tt_] = 1.0 / cnt[tt_]
        A -= np.eye(Sr)

        def blk(a, b):
            return A[a * 128:(a + 1) * 128, b * 128:(b + 1) * 128]

        def hilo(M):
            hi_ = M.astype(np.float32).astype(ml_dtypes.bfloat16).astype(np.float64)
            lo_ = (M - hi_).astype(np.float32).astype(ml_dtypes.bfloat16).astype(np.float64)
            return hi_, lo_

        out[g, 0] = blk(1, 2)
        out[g, 1] = blk(2, 2)
        out[g, 2] = blk(3, 2)
        out[g, 3], out[g, 4] = hilo(blk(0, 0))
        out[g, 5], out[g, 6] = hilo(blk(3, 3))
    return np.ascontiguousarray(out.reshape(28, 128, 128).transpose(1, 0, 2))


_BIAS_MAP = None
_PROG = {}


def prep_inputs(inp, nseq=NSEQ_FULL, ncores=NCORES):
    global _BIAS_MAP
    f = lambda a: np.ascontiguousarray(np.asarray(a, dtype=np.float32))
    Lw = L_FULL
    if _BIAS_MAP is None:
        _BIAS_MAP = _bias_index_map()
    rpb = f(inp["rpb"])
    pad = np.concatenate([rpb.reshape(Lw, 8, 15 * 31), np.full((Lw, 8, 1), NEG, np.float32)], axis=2).reshape(Lw, -1)
    biasT = pad[:, _BIAS_MAP]
    biasT = np.ascontiguousarray(biasT.reshape(Lw, 4, 128, 2 * 14 * 64))
    shared = {
        "w_in": f(inp["w_in"]), "w_out": f(inp["w_out"]), "pool_w": f(inp["pool_w"]),
        "w_gate": f(inp["w_gate"]), "w_up": f(inp["w_up"]), "w_down": f(inp["w_down"]),
        "gm": np.ascontiguousarray(f(inp["norm_mix_g"]).reshape(Lw, 8, 128).transpose(2, 0, 1).reshape(128, Lw * 8)),
        "gf": np.ascontiguousarray(f(inp["norm_ffn_g"]).reshape(Lw, 8, 128).transpose(2, 0, 1).reshape(128, Lw * 8)),
        "ps": np.ascontiguousarray(f(inp["pool_scale"]).reshape(Lw, 4, 128).transpose(2, 0, 1).reshape(128, Lw * 4)),
        "gF": np.ascontiguousarray(np.broadcast_to(f(inp["final_g"])[None, :], (128, D))),
        "wr": np.ascontiguousarray(np.concatenate([f(inp["w_router_group"]), f(inp["w_router_expert"])], axis=-1)
                                   .reshape(Lw, 8, 128, 20).transpose(2, 0, 1, 3)),
        "biasT": biasT,
        "poolA": _pool_consts(),
        "ident": np.eye(128, dtype=np.float32),
        "ustrict": np.triu(np.ones((128, 128), np.float32), k=1),
        "iotas": np.ascontiguousarray(np.concatenate([np.arange(128, dtype=np.float32)[:, None],
                                                      np.broadcast_to(np.arange(32, dtype=np.float32)[None, :], (128, 32))], axis=1)),
    }
    x = f(inp["x"]).reshape(-1, S, D)
    maps = []
    for c in range(ncores):
        m = dict(shared)
        m["x"] = np.ascontiguousarray(x[c * nseq:(c + 1) * nseq].reshape(nseq * S, D))
        maps.append(m)
    return maps


def kernel(x, norm_mix_g, w_in, rpb, pool_w, pool_scale, w_out, norm_ffn_g, w_router_group, w_router_expert,
           w_gate, w_up, w_down, final_g):
    inp = dict(x=x, norm_mix_g=norm_mix_g, w_in=w_in, rpb=rpb, pool_w=pool_w, pool_scale=pool_scale, w_out=w_out,
               norm_ffn_g=norm_ffn_g, w_router_group=w_router_group, w_router_expert=w_router_expert,
               w_gate=w_gate, w_up=w_up, w_down=w_down, final_g=final_g)
    maps = prep_inputs(inp)
    if "full" not in _PROG:
        _PROG["full"] = build_program()
    nc = _PROG["full"]
    res = run_bass_kernel_spmd(nc, maps, core_ids=list(range(NCORES)))
    outs = [np.asarray(r["out"], dtype=np.float32).reshape(NSEQ_FULL, S, D) for r in res.results]
    return np.concatenate(outs, axis=0)
```
